# Optimizing a Trainium2 kernel written in Bass

```python
import math
import jax, jax.numpy as jnp
from jax import lax
import numpy as np

D_MODEL = 1024
BATCH = 16
SEQ = 2048
DEPTH = 1

CHUNK = 64
N_META = 16
D_FF = ((8 * D_MODEL // 3) + 127) // 128 * 128
RMS_EPS = 1e-6
SSM_WIDTH = D_MODEL // 2
SSM_GROUP = 16
SSM_GROUPS = SSM_WIDTH // SSM_GROUP
SSM_STATE = 64
HEAD_DIM = 64
ATT_WIDTH = D_MODEL // 2
N_HEADS = ATT_WIDTH // HEAD_DIM
KV_DIM = HEAD_DIM
IDX_HEADS = 8
IDX_DIM = 64
TOPK_MAX = 256
Q_BLOCK = 128
REL_BUCKETS = 32
REL_MAX_DIST = 128
N_BRANCH = 2
IN_SPLITS = (SSM_WIDTH, IDX_HEADS * IDX_DIM, IDX_DIM, IDX_HEADS, ATT_WIDTH, KV_DIM, KV_DIM, N_BRANCH * D_MODEL)
IN_WIDTH = sum(IN_SPLITS)

kernel_name = "hybrid_s5_dsa_gated_encoder_layer"


def split_columns(t, sizes):
    out, start = [], 0
    for s in sizes:
        out.append(t[..., start:start + s])
        start += s
    return out


def rms_norm(x, g):
    xf = x.astype(jnp.float32)
    y = xf * lax.rsqrt(jnp.mean(xf * xf, axis=-1, keepdims=True) + RMS_EPS)
    return (y * g.astype(jnp.float32)).astype(x.dtype)


def swiglu(x, w_gate, w_up, w_down):
    return (jax.nn.silu(x @ w_gate) * (x @ w_up)) @ w_down


def chunk_ids(pos):
    return jnp.where(pos < N_META, 0, 1 + (pos - N_META) // CHUNK)


def rel_bucket(rel):
    half = REL_BUCKETS // 2
    max_exact = half // 2
    base = jnp.where(rel > 0, half, 0)
    n = jnp.abs(rel)
    nf = jnp.maximum(n, 1).astype(jnp.float32)
    large = max_exact + (jnp.log(nf / max_exact) / math.log(REL_MAX_DIST / max_exact)
                         * (half - max_exact)).astype(jnp.int32)
    large = jnp.minimum(large, half - 1)
    return base + jnp.where(n < max_exact, n, large)


def s5_mixer(u, lambda_re, lambda_im, log_dt, b_re, b_im, c_re, c_im, d_skip, w_glu):
    f32 = jnp.float32
    bsz, L, _ = u.shape
    uf = u.astype(f32).reshape(bsz, L, SSM_GROUPS, SSM_GROUP)
    lr = lambda_re.astype(f32)
    li = lambda_im.astype(f32)
    dt = jnp.exp(log_dt.astype(f32))[:, None]
    mag = jnp.exp(lr * dt)
    a_re = mag * jnp.cos(li * dt)
    a_im = mag * jnp.sin(li * dt)
    den = lr * lr + li * li
    num_re = a_re - 1.0
    num_im = a_im
    coef_re = (num_re * lr + num_im * li) / den
    coef_im = (num_im * lr - num_re * li) / den
    br = b_re.astype(f32)
    bi = b_im.astype(f32)
    bbar_re = coef_re[..., None] * br - coef_im[..., None] * bi
    bbar_im = coef_re[..., None] * bi + coef_im[..., None] * br
    bu_re = jnp.einsum('blgm,gpm->blgp', uf, bbar_re)
    bu_im = jnp.einsum('blgm,gpm->blgp', uf, bbar_im)
    a_re_t = jnp.broadcast_to(a_re[None, None], (1, L, SSM_GROUPS, SSM_STATE))
    a_im_t = jnp.broadcast_to(a_im[None, None], (1, L, SSM_GROUPS, SSM_STATE))

    def combine(e1, e2):
        a1r, a1i, b1r, b1i = e1
        a2r, a2i, b2r, b2i = e2
        return (a2r * a1r - a2i * a1i,
                a2r * a1i + a2i * a1r,
                a2r * b1r - a2i * b1i + b2r,
                a2r * b1i + a2i * b1r + b2i)

    _, _, x_re, x_im = lax.associative_scan(combine, (a_re_t, a_im_t, bu_re, bu_im), axis=1)
    y = (jnp.einsum('blgp,gmp->blgm', x_re, c_re.astype(f32))
         - jnp.einsum('blgp,gmp->blgm', x_im, c_im.astype(f32))
         + d_skip.astype(f32) * uf)
    y = jax.nn.gelu(y.reshape(bsz, L, SSM_WIDTH)).astype(u.dtype)
    return y * jax.nn.sigmoid(y @ w_glu)


def dsa_mixer(q_idx, k_idx, w_idx, q, k, v, rel_bias, top_k):
    f32 = jnp.float32
    bsz, L, _ = q.shape
    Lp = -(-L // Q_BLOCK) * Q_BLOCK
    nblk = Lp // Q_BLOCK
    pad = lambda t: jnp.pad(t, ((0, 0), (0, Lp - L), (0, 0)))
    q_idx = pad(q_idx).reshape(bsz, Lp, IDX_HEADS, IDX_DIM)
    w_idx = pad(w_idx)
    q = pad(q).reshape(bsz, Lp, N_HEADS, HEAD_DIM)
    kidx_f = pad(k_idx).astype(f32)
    k = pad(k)
    v = pad(v)
    pos = jnp.arange(Lp, dtype=jnp.int32)
    cid = chunk_ids(pos)
    rb = rel_bias.astype(f32)

    def to_blocks(t):
        return jnp.moveaxis(t.reshape(bsz, nblk, Q_BLOCK, *t.shape[2:]), 1, 0)

    def block(args):
        qi, wi, qb, qpos = args
        s = jax.nn.relu(jnp.einsum('bqhd,bsd->bqhs', qi.astype(f32), kidx_f) * IDX_DIM ** -0.5)
        score = jnp.einsum('bqhs,bqh->bqs', s, wi.astype(f32)) * IDX_HEADS ** -0.5
        qcid = chunk_ids(qpos)
        adm = cid[None, :] <= qcid[:, None]
        score = jnp.where(adm[None], score, -1e30)
        _, sel = lax.top_k(score, top_k)
        valid = cid[sel] <= qcid[None, :, None]
        k_sel = jax.vmap(lambda kb, ib: kb[ib])(k, sel)
        v_sel = jax.vmap(lambda vb, ib: vb[ib])(v, sel)
        logits = jnp.einsum('bqhd,bqkd->bqhk', qb.astype(f32), k_sel.astype(f32)) * HEAD_DIM ** -0.5
        bias = rb[rel_bucket(sel - qpos[None, :, None])]
        logits = logits + jnp.moveaxis(bias, -1, 2)
        logits = jnp.where(valid[:, :, None, :], logits, -1e30)
        p = jax.nn.softmax(logits, axis=-1)
        return jnp.einsum('bqhk,bqkd->bqhd', p.astype(v.dtype), v_sel)

    outs = lax.map(block, (to_blocks(q_idx), to_blocks(w_idx), to_blocks(q), pos.reshape(nblk, Q_BLOCK)))
    out = jnp.moveaxis(outs, 0, 1).reshape(bsz, Lp, ATT_WIDTH)
    return out[:, :L]


def setup_inputs(seed: int = 0) -> dict:
    key = jax.random.key(seed)
    keys = jax.random.split(key, 32)
    f32 = jnp.float32

    def normal(i, shape, scale):
        return jax.random.normal(keys[i], shape, f32) * scale

    def gain(i):
        return 1.0 + 0.02 * jax.random.normal(keys[i], (DEPTH, D_MODEL), f32)

    G, P, M = SSM_GROUPS, SSM_STATE, SSM_GROUP
    n = jnp.arange(P, dtype=f32)
    return {
        "x": normal(0, (BATCH, SEQ, D_MODEL), 1.0),
        "meta_tokens": normal(1, (N_META, D_MODEL), 1.0),
        "ff1_norm_pre": gain(2),
        "ff1_norm_post": gain(3),
        "mix_norm_pre": gain(4),
        "mix_norm_post": gain(5),
        "ff2_norm_pre": gain(6),
        "ff2_norm_post": gain(7),
        "ff1_w_gate": normal(8, (DEPTH, D_MODEL, D_FF), D_MODEL ** -0.5),
        "ff1_w_up": normal(9, (DEPTH, D_MODEL, D_FF), D_MODEL ** -0.5),
        "ff1_w_down": normal(10, (DEPTH, D_FF, D_MODEL), D_FF ** -0.5),
        "ff2_w_gate": normal(11, (DEPTH, D_MODEL, D_FF), D_MODEL ** -0.5),
        "ff2_w_up": normal(12, (DEPTH, D_MODEL, D_FF), D_MODEL ** -0.5),
        "ff2_w_down": normal(13, (DEPTH, D_FF, D_MODEL), D_FF ** -0.5),
        "w_in": normal(14, (DEPTH, D_MODEL, IN_WIDTH), D_MODEL ** -0.5),
        "ssm_lambda_re": -0.5 + normal(15, (DEPTH, G, P), 0.01),
        "ssm_lambda_im": math.pi * n + normal(16, (DEPTH, G, P), 0.01),
        "ssm_log_dt": jax.random.uniform(keys[17], (DEPTH, G), f32, math.log(1e-3), math.log(1e-1)),
        "ssm_b_re": normal(18, (DEPTH, G, P, M), (2 * M) ** -0.5),
        "ssm_b_im": normal(19, (DEPTH, G, P, M), (2 * M) ** -0.5),
        "ssm_c_re": normal(20, (DEPTH, G, M, P), P ** -0.5),
        "ssm_c_im": normal(21, (DEPTH, G, M, P), P ** -0.5),
        "ssm_d": normal(22, (DEPTH, G, M), 1.0),
        "ssm_w_glu": normal(23, (DEPTH, SSM_WIDTH, SSM_WIDTH), SSM_WIDTH ** -0.5),
        "w_branch_a": normal(24, (DEPTH, SSM_WIDTH, D_MODEL), SSM_WIDTH ** -0.5),
        "rel_bias": normal(25, (REL_BUCKETS, N_HEADS), 0.2),
        "w_branch_b": normal(26, (DEPTH, ATT_WIDTH, D_MODEL), ATT_WIDTH ** -0.5),
        "w_out": normal(27, (DEPTH, D_MODEL, D_MODEL), D_MODEL ** -0.5),
    }


def reference(x, meta_tokens, ff1_norm_pre, ff1_norm_post, mix_norm_pre, mix_norm_post,
              ff2_norm_pre, ff2_norm_post, ff1_w_gate, ff1_w_up, ff1_w_down,
              ff2_w_gate, ff2_w_up, ff2_w_down, w_in, ssm_lambda_re, ssm_lambda_im, ssm_log_dt,
              ssm_b_re, ssm_b_im, ssm_c_re, ssm_c_im, ssm_d, ssm_w_glu, w_branch_a, rel_bias,
              w_branch_b, w_out):
    bsz = x.shape[0]
    meta = jnp.broadcast_to(meta_tokens[None].astype(x.dtype), (bsz, N_META, D_MODEL))
    h = jnp.concatenate([meta, x], axis=1)
    L = h.shape[1]
    top_k = min(TOPK_MAX, SEQ // 4)
    for l in range(DEPTH):
        hn = rms_norm(h, ff1_norm_pre[l])
        h = h + 0.5 * rms_norm(swiglu(hn, ff1_w_gate[l], ff1_w_up[l], ff1_w_down[l]), ff1_norm_post[l])
        hn = rms_norm(h, mix_norm_pre[l])
        proj = hn @ w_in[l]
        u_a, q_idx, k_idx, w_idx, q, k, v, gates = split_columns(proj, IN_SPLITS)
        y_a = s5_mixer(u_a, ssm_lambda_re[l], ssm_lambda_im[l], ssm_log_dt[l], ssm_b_re[l],
                       ssm_b_im[l], ssm_c_re[l], ssm_c_im[l], ssm_d[l], ssm_w_glu[l])
        y_b = dsa_mixer(q_idx, k_idx, w_idx, q, k, v, rel_bias, top_k)
        g = jax.nn.sigmoid(gates).reshape(bsz, L, N_BRANCH, D_MODEL)
        merged = g[:, :, 0] * (y_a @ w_branch_a[l]) + g[:, :, 1] * (y_b @ w_branch_b[l])
        h = h + rms_norm(merged @ w_out[l], mix_norm_post[l])
        hn = rms_norm(h, ff2_norm_pre[l])
        h = h + 0.5 * rms_norm(swiglu(hn, ff2_w_gate[l], ff2_w_up[l], ff2_w_down[l]), ff2_norm_post[l])
    return h[:, N_META:]
```

```python
import math
from contextlib import ExitStack
import numpy as np
import ml_dtypes
import concourse.bass as bass
import concourse.mybir as mybir
from concourse.bass_utils import run_bass_kernel_spmd

F32 = mybir.dt.float32
BF16 = mybir.dt.bfloat16
AF = mybir.ActivationFunctionType
ALU = mybir.AluOpType
AX = mybir.AxisListType

NCORES = 8
D = 1024
DC = 8
SEQ = 2048
NSEQ = 2
NMETA = 16
DFF = 2816
FC = 22
EPS = 1e-6
TT = 512
NT = SEQ // TT
FT = 256
NFT = SEQ // FT


class Sync:
    def __init__(self, nc, es):
        self.nc = nc
        self.eng = {"pe": nc.tensor, "act": nc.scalar, "dve": nc.vector, "pool": nc.gpsimd, "sp": nc.sync}
        self.sem = {k: es.enter_context(nc.semaphore("s_" + k)) for k in self.eng}
        self.cnt = {k: 0 for k in self.eng}
        self.dsem = {}
        self.dcnt = {}
        self.es = es
        self.seen = {k: {} for k in self.eng}
        self.lastw = {}
        self.readers = {}
        self.ninst = 0

    NPOOL = {"sp": 12, "pool": 12, "act": 8}

    def dma_sem(self, q):
        if q not in self.dsem:
            self.dsem[q] = [self.es.enter_context(self.nc.semaphore("d_%s%d" % (q, i))) for i in range(self.NPOOL[q])]
            self.dcnt[q] = [0] * self.NPOOL[q]
            self.drr = getattr(self, "drr", {})
            self.drr[q] = 0
        i = self.drr[q]
        self.drr[q] = (i + 1) % self.NPOOL[q]
        return i

    def _wait(self, e, reads, writes):
        need = {}
        for k in reads:
            lw = self.lastw.get(k)
            if lw is not None:
                need[lw[0]] = max(need.get(lw[0], (0, None))[0], lw[1]), lw[2]
        for k in writes:
            lw = self.lastw.get(k)
            if lw is not None:
                need[lw[0]] = max(need.get(lw[0], (0, None))[0], lw[1]), lw[2]
            for r in self.readers.get(k, ()):
                need[r[0]] = max(need.get(r[0], (0, None))[0], r[1]), r[2]
        E = self.eng[e]
        for semid, (val, semobj) in need.items():
            if semid == "e_" + e and e == "pe":
                continue
            if self.seen[e].get(semid, 0) >= val:
                continue
            E.wait_ge(semobj, val)
            self.seen[e][semid] = val

    def _record(self, rec, reads, writes):
        for k in reads:
            self.readers.setdefault(k, []).append(rec)
        for k in writes:
            self.lastw[k] = rec
            self.readers[k] = []

    def op(self, e, fn, reads=(), writes=()):
        self._wait(e, reads, writes)
        inst = fn(self.eng[e])
        self.cnt[e] += 1
        inst.then_inc(self.sem[e], 1)
        self.ninst += 1
        self._record(("e_" + e, self.cnt[e], self.sem[e]), reads, writes)

    def dma(self, q, semname, out, in_, reads=(), writes=(), **kw):
        self._wait(q, reads, writes)
        i = self.dma_sem(q)
        sem = self.dsem[q][i]
        semid = "d_%s%d" % (q, i)
        if self.dcnt[q][i] > 0 and self.seen[q].get(semid, 0) < self.dcnt[q][i]:
            self.eng[q].wait_ge(sem, self.dcnt[q][i])
            self.seen[q][semid] = self.dcnt[q][i]
        inst = self.eng[q].dma_start(out=out, in_=in_, **kw)
        self.dcnt[q][i] += 16
        inst.then_inc(sem, 16)
        self.ninst += 1
        self._record((semid, self.dcnt[q][i], sem), reads, writes)

    def drain(self, e):
        E = self.eng[e]
        for k in self.eng:
            if k != e and self.cnt[k] > 0:
                E.wait_ge(self.sem[k], self.cnt[k])
        for q, sems in self.dsem.items():
            for i, sm in enumerate(sems):
                if self.dcnt[q][i] > 0:
                    E.wait_ge(sm, self.dcnt[q][i])


def build(debug=None):
    nc = bass.Bass("TRN2", target_bir_lowering=False)
    es = ExitStack()
    with es:
        S = Sync(nc, es)

        def din(name, shape, dt=F32):
            return nc.dram_tensor(name, list(shape), dt, kind="ExternalInput").ap()

        def dscr(name, shape, dt=F32):
            return nc.dram_tensor(name, list(shape), dt, kind="Internal").ap()

        xT = din("xT", [NSEQ, 128, DC, SEQ])
        metaT = din("metaT", [128, DC, NMETA])
        gains = din("gains", [128, 48])
        ff1_wg = din("ff1_wg", [128, DC, DFF])
        ff1_wu = din("ff1_wu", [128, DC, DFF])
        ff1_wd = din("ff1_wd", [128, FC, D])
        outT = nc.dram_tensor("outT", [NSEQ, 128, DC, SEQ], F32, kind="ExternalOutput").ap()
        h1T = dscr("h1T", [NSEQ, 128, DC, SEQ])
        h1m = dscr("h1m", [128, DC, NMETA])

        def sb(stack, name, shape, dt=F32):
            return stack.enter_context(nc.sbuf_tensor(name, list(shape), dt))

        def ps(stack, name, shape, dt=F32):
            return stack.enter_context(nc.psum_tensor(name, list(shape), dt))

        gains_sb = sb(es, "gains_sb", [128, 48])
        ones_f = sb(es, "ones_f", [128, 128])
        S.dma("sp", "c", gains_sb[:, :], gains[:, :], writes=["gains"])
        S.op("dve", lambda e: e.memset(ones_f[:, :], 1.0), writes=["ones_f"])

        def rstd_from_sumsq(pss, rs, n, key_pss, key_rs, half=False):
            S.op("act", lambda e: e.activation(out=rs[:, :n], in_=pss[:, :n], func=AF.Sqrt,
                                               bias=eps_sb[:, (1 if half else 0):(2 if half else 1)],
                                               scale=(4.0 if half else 1.0) / D),
                 reads=[key_pss, "eps"], writes=[key_rs])
            S.op("dve", lambda e: e.reciprocal(out=rs[:, :n], in_=rs[:, :n]), reads=[key_rs], writes=[key_rs])

        eps_sb = sb(es, "eps_sb", [128, 2])
        S.op("dve", lambda e: e.memset(eps_sb[:, 0:1], EPS), writes=["eps"])
        S.op("dve", lambda e: e.memset(eps_sb[:, 1:2], 4.0 * EPS), writes=["eps"])

        def ffn_phase(tag, wg, wu, wd, gpre_col, gpost_col, tiles):
            with ExitStack() as st:
                wg_sb = sb(st, tag + "wg", [128, DC, DFF], BF16)
                wu_sb = sb(st, tag + "wu", [128, DC, DFF], BF16)
                wd_sb = sb(st, tag + "wd", [128, FC, D], BF16)
                xt = [sb(st, tag + "xt%d" % i, [128, DC, FT]) for i in range(2)]
                sq = sb(st, tag + "sq", [128, 2, FT])
                hn = sb(st, tag + "hn", [128, DC, FT], BF16)
                act = sb(st, tag + "act", [128, FC, FT], BF16)
                sg = [sb(st, tag + "sg%d" % i, [128, FT]) for i in range(2)]
                ysb = sb(st, tag + "y", [128, DC, FT])
                rs = sb(st, tag + "rs", [128, FT])
                psg = [ps(st, tag + "psg%d" % i, [128, FT]) for i in range(2)]
                psu = [ps(st, tag + "psu%d" % i, [128, FT]) for i in range(2)]
                psy = [ps(st, tag + "psy%d" % i, [128, FT]) for i in range(2)]
                pss = ps(st, tag + "pss", [128, FT])
                for k in range(DC):
                    S.dma("pool", "w", wg_sb[:, k, :], wg[:, k, :], writes=[(tag, "wg", k)], max_dma_last_dim=5632)
                    S.dma("pool", "w", wu_sb[:, k, :], wu[:, k, :], writes=[(tag, "wu", k)], max_dma_last_dim=5632)
                for j in range(FC):
                    S.dma("pool", "w", wd_sb[:, j, :], wd[:, j, :], writes=[(tag, "wd", j)], max_dma_last_dim=4096)

                def load(i):
                    src, dst, n, sr, dw = tiles[i]
                    S.dma("sp", "x", xt[i % 2][:, :, :n], src, reads=sr, writes=[(tag, "xt", i % 2)])

                load(0)
                for i, (src, dst, n, sr, dw) in enumerate(tiles):
                    if i + 1 < len(tiles):
                        load(i + 1)
                    x = xt[i % 2]
                    kx = (tag, "xt", i % 2)
                    for c in range(DC):
                        S.op("act", lambda e, c=c: e.activation(out=sq[:, c % 2, :n], in_=x[:, c, :n], func=AF.Square),
                             reads=[kx], writes=[(tag, "sq", c % 2)])
                        S.op("pe", lambda e, c=c: e.matmul(pss[:, :n], lhsT=ones_f[:, :], rhs=sq[:, c % 2, :n],
                                                          start=(c == 0), stop=(c == DC - 1)),
                             reads=[(tag, "sq", c % 2), "ones_f"], writes=[(tag, "pss")])
                    rstd_from_sumsq(pss, rs, n, (tag, "pss"), (tag, "rs"))
                    for c in range(DC):
                        S.op("dve", lambda e, c=c: e.scalar_tensor_tensor(
                            out=hn[:, c, :n], in0=x[:, c, :n], scalar=gains_sb[:, gpre_col + c:gpre_col + c + 1],
                            in1=rs[:, :n], op0=ALU.mult, op1=ALU.mult),
                            reads=[kx, (tag, "rs"), "gains"], writes=[(tag, "hn", c)])
                    for j in range(FC):
                        b = j % 2
                        for k in range(DC):
                            S.op("pe", lambda e, k=k, j=j, b=b: e.matmul(
                                psg[b][:, :n], lhsT=wg_sb[:, k, j * 128:(j + 1) * 128], rhs=hn[:, k, :n],
                                start=(k == 0), stop=(k == DC - 1)),
                                reads=[(tag, "wg", k), (tag, "hn", k)], writes=[(tag, "psg", b)])
                        for k in range(DC):
                            S.op("pe", lambda e, k=k, j=j, b=b: e.matmul(
                                psu[b][:, :n], lhsT=wu_sb[:, k, j * 128:(j + 1) * 128], rhs=hn[:, k, :n],
                                start=(k == 0), stop=(k == DC - 1)),
                                reads=[(tag, "wu", k), (tag, "hn", k)], writes=[(tag, "psu", b)])
                        S.op("act", lambda e, b=b: e.activation(out=sg[b][:, :n], in_=psg[b][:, :n], func=AF.Silu),
                             reads=[(tag, "psg", b)], writes=[(tag, "sg", b)])
                        S.op("dve", lambda e, b=b, j=j: e.tensor_tensor(out=act[:, j, :n], in0=sg[b][:, :n],
                                                                        in1=psu[b][:, :n], op=ALU.mult),
                             reads=[(tag, "sg", b), (tag, "psu", b)], writes=[(tag, "act", j)])
                    for c in range(DC):
                        b = c % 2
                        for j in range(FC):
                            S.op("pe", lambda e, c=c, j=j, b=b: e.matmul(
                                psy[b][:, :n], lhsT=wd_sb[:, j, c * 128:(c + 1) * 128], rhs=act[:, j, :n],
                                start=(j == 0), stop=(j == FC - 1)),
                                reads=[(tag, "wd", j), (tag, "act", j)], writes=[(tag, "psy", b)])
                        S.op("act", lambda e, c=c, b=b: e.activation(out=ysb[:, c, :n], in_=psy[b][:, :n], func=AF.Copy),
                             reads=[(tag, "psy", b)], writes=[(tag, "y", c)])
                        S.op("act", lambda e, c=c, b=b: e.activation(out=sq[:, c % 2, :n], in_=psy[b][:, :n], func=AF.Square),
                             reads=[(tag, "psy", b)], writes=[(tag, "sq", c % 2)])
                        S.op("pe", lambda e, c=c: e.matmul(pss[:, :n], lhsT=ones_f[:, :], rhs=sq[:, c % 2, :n],
                                                          start=(c == 0), stop=(c == DC - 1)),
                             reads=[(tag, "sq", c % 2), "ones_f"], writes=[(tag, "pss")])
                    rstd_from_sumsq(pss, rs, n, (tag, "pss"), (tag, "rs"), half=True)
                    for c in range(DC):
                        S.op("dve", lambda e, c=c: e.scalar_tensor_tensor(
                            out=ysb[:, c, :n], in0=ysb[:, c, :n], scalar=gains_sb[:, gpost_col + c:gpost_col + c + 1],
                            in1=rs[:, :n], op0=ALU.mult, op1=ALU.mult),
                            reads=[(tag, "y", c), (tag, "rs"), "gains"], writes=[(tag, "y", c)])
                        S.op("pool", lambda e, c=c: e.tensor_tensor(
                            out=ysb[:, c, :n], in0=ysb[:, c, :n], in1=x[:, c, :n], op=ALU.add),
                            reads=[(tag, "y", c), kx], writes=[(tag, "y", c)])
                    S.dma("sp", "o", dst, ysb[:, :, :n], reads=[(tag, "y", c) for c in range(DC)], writes=dw)

        def barrier():
            for e in S.eng:
                S.drain(e)

        S.barrier = barrier

        def mms(out, pairs, reads, wkey):
            n_ = len(pairs)
            for idx, (l, r) in enumerate(pairs):
                S.op("pe", lambda e, l=l, r=r, idx=idx: e.matmul(out, lhsT=l, rhs=r, start=(idx == 0),
                                                                 stop=(idx == n_ - 1)),
                     reads=reads, writes=[wkey])

        def prenorm(tag, x, kx, n, gcol, hn, sq, pss, rs):
            for c in range(DC):
                S.op("act", lambda e, c=c: e.activation(out=sq[:, c % 2, :n], in_=x[:, c, :n], func=AF.Square),
                     reads=[kx], writes=[(tag, "sq", c % 2)])
                S.op("pe", lambda e, c=c: e.matmul(pss[:, :n], lhsT=ones_f[:, :], rhs=sq[:, c % 2, :n],
                                                  start=(c == 0), stop=(c == DC - 1)),
                     reads=[(tag, "sq", c % 2), "ones_f"], writes=[(tag, "pss")])
            rstd_from_sumsq(pss, rs, n, (tag, "pss"), (tag, "rs"))
            for c in range(DC):
                S.op("dve", lambda e, c=c: e.scalar_tensor_tensor(
                    out=hn[:, c, :n], in0=x[:, c, :n], scalar=gains_sb[:, gcol + c:gcol + c + 1],
                    in1=rs[:, :n], op0=ALU.mult, op1=ALU.mult),
                    reads=[kx, (tag, "rs"), "gains"], writes=[(tag, "hn")])

        tilesA = [(metaT[:, :, :], h1m[:, :, :], NMETA, [], ["h1m"])]
        for s in range(NSEQ):
            for t in range(NFT):
                tilesA.append((xT[s, :, :, t * FT:(t + 1) * FT], h1T[s, :, :, t * FT:(t + 1) * FT], FT, [],
                               [("h1T", s, t * FT // TT)]))
        if debug == "A":
            tilesA = tilesA[:2]
        ffn_phase("A", ff1_wg, ff1_wu, ff1_wd, 0, 8, tilesA)
        barrier()

        LP = NMETA + SEQ
        w_inA = din("w_inA", [128, DC, 2048])
        w_inB = din("w_inB", [128, DC, 72])
        uT = dscr("uT", [NSEQ, 128, 6, LP], BF16)
        kiT = dscr("kiT", [NSEQ, 128, LP], BF16)
        kT = dscr("kT", [NSEQ, 128, LP], BF16)
        qiT = dscr("qiT", [NSEQ, 128, 4, SEQ], BF16)
        qT = dscr("qT", [NSEQ, 128, 4, SEQ], BF16)
        vwS = dscr("vwS", [NSEQ, LP, 72], F32)
        hnT = dscr("hnT", [NSEQ, 128, DC, SEQ], BF16)

        def phase_B():
            tag = "B"
            with ExitStack() as st:
                wA = sb(st, "BwA", [128, DC, 2048], BF16)
                wB = sb(st, "BwB", [128, DC, 72], BF16)
                for k in range(DC):
                    S.dma("pool", "w", wA[:, k, :], w_inA[:, k, :], writes=[("B", "wA")], max_dma_last_dim=4096)
                S.dma("pool", "w", wB[:, :, :], w_inB[:, :, :], writes=[("B", "wB")])
                xt = [sb(st, "Bxt%d" % i, [128, DC, TT]) for i in range(2)]
                hn = sb(st, "Bhn", [128, DC, TT], BF16)
                sq = sb(st, "Bsq", [128, 2, TT])
                rs = sb(st, "Brs", [128, TT])
                stage = sb(st, "Bstage", [128, 16, TT], BF16)
                vw = sb(st, "Bvw", [128, 4, 72])
                pss = ps(st, "Bpss", [128, TT])
                pp = [ps(st, "Bpp%d" % i, [128, TT]) for i in range(2)]
                pv = [ps(st, "Bpv%d" % i, [128, 72]) for i in range(2)]
                tiles = [("m", 0)] + [(s, t) for s in range(NSEQ) for t in range(NT)]

                def load(i):
                    s_, t_ = tiles[i]
                    if s_ == "m":
                        S.dma("sp", "x", xt[i % 2][:, :, :NMETA], h1m[:, :, :], reads=["h1m"], writes=[("B", "xt", i % 2)])
                    else:
                        S.dma("sp", "x", xt[i % 2][:, :, :], h1T[s_, :, :, t_ * TT:(t_ + 1) * TT],
                              reads=[("h1T", s_, t_)], writes=[("B", "xt", i % 2)])

                load(0)
                for i, (s_, t_) in enumerate(tiles):
                    if i + 1 < len(tiles):
                        load(i + 1)
                    n = NMETA if s_ == "m" else TT
                    x = xt[i % 2]
                    kx = ("B", "xt", i % 2)
                    prenorm("B", x, kx, n, 16, hn, sq, pss, rs)
                    if s_ != "m":
                        S.dma("act", "o", hnT[s_, :, :, t_ * TT:(t_ + 1) * TT], hn[:, :, :], reads=[("B", "hn")],
                              writes=[("hnT", s_, t_)])
                    for cc in range(16):
                        b_ = cc % 2
                        mms(pp[b_][:, :n], [(wA[:, k, cc * 128:(cc + 1) * 128], hn[:, k, :n]) for k in range(DC)],
                            [("B", "wA"), ("B", "hn")], ("B", "pp", b_))
                        sc_ = 0.125 if 10 <= cc < 14 else 1.0
                        S.op("act", lambda e, cc=cc, b_=b_, sc_=sc_: e.activation(
                            out=stage[:, cc, :n], in_=pp[b_][:, :n], func=AF.Copy, scale=sc_),
                            reads=[("B", "pp", b_)], writes=[("B", "stage")])
                    if s_ == "m":
                        for s2 in range(NSEQ):
                            S.dma("sp", "o", uT[s2, :, :, 0:NMETA], stage[:, 0:6, :NMETA], reads=[("B", "stage")],
                                  writes=[("uT", s2, "m")])
                            S.dma("sp", "o", kiT[s2, :, 0:NMETA], stage[:, 14, :NMETA], reads=[("B", "stage")],
                                  writes=[("kiT", s2, "m")])
                            S.dma("sp", "o", kT[s2, :, 0:NMETA], stage[:, 15, :NMETA], reads=[("B", "stage")],
                                  writes=[("kT", s2, "m")])
                    else:
                        t0 = t_ * TT
                        S.dma("sp", "o", uT[s_, :, :, NMETA + t0:NMETA + t0 + TT], stage[:, 0:6, :],
                              reads=[("B", "stage")], writes=[("uT", s_, t_)])
                        S.dma("sp", "o", qiT[s_, :, :, t0:t0 + TT], stage[:, 6:10, :], reads=[("B", "stage")],
                              writes=[("qiT", s_, t_)])
                        S.dma("sp", "o", qT[s_, :, :, t0:t0 + TT], stage[:, 10:14, :], reads=[("B", "stage")],
                              writes=[("qT", s_, t_)])
                        S.dma("sp", "o", kiT[s_, :, NMETA + t0:NMETA + t0 + TT], stage[:, 14, :],
                              reads=[("B", "stage")], writes=[("kiT", s_, t_)])
                        S.dma("sp", "o", kT[s_, :, NMETA + t0:NMETA + t0 + TT], stage[:, 15, :],
                              reads=[("B", "stage")], writes=[("kT", s_, t_)])
                    nb = max(1, n // 128)
                    rows = min(n, 128)
                    for blk in range(nb):
                        b_ = blk % 2
                        mms(pv[b_][:rows, :], [(hn[:, k, blk * 128:blk * 128 + rows], wB[:, k, :]) for k in range(DC)],
                            [("B", "wB"), ("B", "hn")], ("B", "pv", b_))
                        S.op("dve", lambda e, blk=blk, b_=b_: e.tensor_copy(out=vw[:rows, blk, :], in_=pv[b_][:rows, :]),
                             reads=[("B", "pv", b_)], writes=[("B", "vw")])
                    if s_ == "m":
                        for s2 in range(NSEQ):
                            S.dma("sp", "o", vwS[s2, 0:NMETA, :], vw[:NMETA, 0, :], reads=[("B", "vw")],
                                  writes=[("vwS", s2, "m")])
                    else:
                        S.dma("sp", "o", vwS[s_, NMETA + t0:NMETA + t0 + TT, :].rearrange("(b p) c -> p b c", p=128),
                              vw[:, :, :], reads=[("B", "vw")], writes=[("vwS", s_, t_)])

        if debug != "A":
            phase_B()
            barrier()

        ALLT = ["m"] + list(range(NT))

        if debug == "B":
            with ExitStack() as st:
                tmp = sb(st, "dbgtmp", [128, 6, LP], BF16)
                tmp2 = sb(st, "dbgtmp2", [128, 6, LP], F32)
                S.dma("sp", "x", tmp[:, :, :], uT[0, :, :, :], reads=[("uT", 0, t) for t in ALLT], writes=["dbgtmp"])
                S.op("dve", lambda e: e.tensor_copy(out=tmp2[:, :, :], in_=tmp[:, :, :]), reads=["dbgtmp"], writes=["dbgtmp2"])
                S.dma("sp", "o", outT[0, :, 0:6, 0:SEQ], tmp2[:, :, NMETA:LP], reads=["dbgtmp2"], writes=["out"])
                S.dma("sp", "x", tmp2[:, 0, 0:72 * 16].rearrange("p (b c) -> p b c", c=72),
                      vwS[0, NMETA:NMETA + 2048, :].rearrange("(b p) c -> p b c", p=128),
                      reads=[("vwS", 0, t) for t in ALLT] + ["out"], writes=["dbgtmp2"])
                S.dma("sp", "o", outT[1, :, 0, 0:72 * 16], tmp2[:, 0, 0:72 * 16], reads=["dbgtmp2"], writes=["out"])


        s5_pcm = din("s5_pcm", [128, 6, 3, 128])
        s5_bcm = din("s5_bcm", [128, 6, 2, 128])
        s5_psm = din("s5_psm", [128, 3, 16])
        s5_bsm = din("s5_bsm", [128, 16, 2, 32])
        s5_csm = din("s5_csm", [128, 16, 2, 32])
        s5_d = din("s5_d", [128, 6])
        ident_d = din("ident", [128, 128])
        yaT = dscr("yaT", [NSEQ, 128, 6, SEQ], BF16)
        NCH = LP // 16
        ident_f = sb(es, "ident_f", [128, 128])
        ident_b = sb(es, "ident_b", [128, 128], BF16)
        S.dma("sp", "c", ident_f[:, :], ident_d[:, :], writes=["ident_f"])
        S.op("dve", lambda e: e.tensor_copy(out=ident_b[:, :], in_=ident_f[:, :]), reads=["ident_f"], writes=["ident_b"])

        def phase_C():
            K_ = "Cprep"
            R_, W_ = [K_], [K_]

            def tt(eng, out, a, b_, op):
                S.op(eng, lambda e: e.tensor_tensor(out=out, in0=a, in1=b_, op=op), reads=R_, writes=W_)

            def tsc(eng, out, a, s1, op0, s2=None, op1=None):
                if op1 is None:
                    S.op(eng, lambda e: e.tensor_scalar(out=out, in0=a, scalar1=s1, scalar2=None, op0=op0), reads=R_, writes=W_)
                else:
                    S.op(eng, lambda e: e.tensor_scalar(out=out, in0=a, scalar1=s1, scalar2=s2, op0=op0, op1=op1),
                         reads=R_, writes=W_)

            def stt(out, a, sc_, b_, op0, op1):
                S.op("dve", lambda e: e.scalar_tensor_tensor(out=out, in0=a, scalar=sc_, in1=b_, op0=op0, op1=op1),
                     reads=R_, writes=W_)

            def actf(out, a, func, scale=1.0):
                S.op("act", lambda e: e.activation(out=out, in_=a, func=func, scale=scale), reads=R_, writes=W_)

            def cparams(st, nm, lr, li, ldt_, shp):
                T = lambda n_: sb(st, "C%s_%s" % (nm, n_), shp)
                dt, mag, th, sh, c, s_, t1, t2, t3 = [T(n_) for n_ in ("dt", "mag", "th", "sh", "c", "s", "t1", "t2", "t3")]
                are, aim, cre_, cim_ = [T(n_) for n_ in ("are", "aim", "cre", "cim")]
                A = lambda t_: t_[tuple(slice(None) for _ in shp)]
                actf(A(dt), ldt_, AF.Exp)
                tt("dve", A(t1), lr, A(dt), ALU.mult)
                actf(A(mag), A(t1), AF.Exp)
                tt("dve", A(th), li, A(dt), ALU.mult)
                actf(A(sh), A(th), AF.Sin, scale=1.0 / 32)
                actf(A(s_), A(th), AF.Sin, scale=1.0 / 16)
                tt("dve", A(t1), A(sh), A(sh), ALU.mult)
                tsc("dve", A(c), A(t1), -2.0, ALU.mult, 1.0, ALU.add)
                for _ in range(4):
                    tt("dve", A(t1), A(c), A(c), ALU.mult)
                    tt("dve", A(t2), A(s_), A(s_), ALU.mult)
                    tt("dve", A(t3), A(c), A(s_), ALU.mult)
                    tt("dve", A(c), A(t1), A(t2), ALU.subtract)
                    tsc("dve", A(s_), A(t3), 2.0, ALU.mult)
                tt("dve", A(are), A(mag), A(c), ALU.mult)
                tt("dve", A(aim), A(mag), A(s_), ALU.mult)
                tt("dve", A(t1), lr, lr, ALU.mult)
                tt("dve", A(t2), li, li, ALU.mult)
                tt("dve", A(t1), A(t1), A(t2), ALU.add)
                S.op("dve", lambda e: e.reciprocal(out=A(t1), in_=A(t1)), reads=R_, writes=W_)
                tsc("dve", A(t2), A(are), -1.0, ALU.add)
                tt("dve", A(t3), A(t2), lr, ALU.mult)
                tt("dve", A(c), A(aim), li, ALU.mult)
                tt("dve", A(t3), A(t3), A(c), ALU.add)
                tt("dve", A(cre_), A(t3), A(t1), ALU.mult)
                tt("dve", A(t3), A(aim), lr, ALU.mult)
                tt("dve", A(c), A(t2), li, ALU.mult)
                tt("dve", A(t3), A(t3), A(c), ALU.subtract)
                tt("dve", A(cim_), A(t3), A(t1), ALU.mult)
                return are, aim, cre_, cim_

            with ExitStack() as st:
                WS = sb(st, "C_WS", [128, 16, 6, 2, 128], BF16)
                WO = sb(st, "C_WO", [128, 16, 16, 2, 32], BF16)
                WK = sb(st, "C_WK", [128, 16, 6, 128], BF16)
                a16 = sb(st, "C_a16", [128, 2, 16])
                with ExitStack() as st2:
                    pc = sb(st2, "C_pc", [128, 6, 3, 128])
                    bc = sb(st2, "C_bc", [128, 6, 2, 128])
                    S.dma("sp", "c", pc[:, :, :, :], s5_pcm[:, :, :, :], writes=W_)
                    S.dma("sp", "c", bc[:, :, :, :], s5_bcm[:, :, :, :], writes=W_)
                    are, aim, cre_, cim_ = cparams(st2, "cm", pc[:, :, 0, :], pc[:, :, 1, :], pc[:, :, 2, :], [128, 6, 128])
                    wr = sb(st2, "C_wr", [128, 6, 128])
                    wi = sb(st2, "C_wi", [128, 6, 128])
                    u1 = sb(st2, "C_u1", [128, 6, 128])
                    u2 = sb(st2, "C_u2", [128, 6, 128])
                    F3 = (slice(None),) * 3

                    def cmul(orr, oi, xr, xi, yr, yi):
                        tt("dve", u1[F3], xr, yr, ALU.mult)
                        tt("pool", u2[F3], xi, yi, ALU.mult)
                        tt("dve", u1[F3], u1[F3], u2[F3], ALU.subtract)
                        tt("pool", u2[F3], xr, yi, ALU.mult)
                        tt("dve", oi, xi, yr, ALU.mult)
                        tt("dve", oi, oi, u2[F3], ALU.add)
                        S.op("dve", lambda e: e.tensor_copy(out=orr, in_=u1[F3]), reads=R_, writes=W_)

                    cmul(wr[F3], wi[F3], cre_[F3], cim_[F3], bc[:, :, 0, :], bc[:, :, 1, :])
                    for lag in range(16):
                        S.op("act", lambda e, lag=lag: e.activation(out=WS[:, lag, :, 0, :], in_=wr[F3], func=AF.Copy),
                             reads=R_, writes=W_)
                        S.op("act", lambda e, lag=lag: e.activation(out=WS[:, lag, :, 1, :], in_=wi[F3], func=AF.Copy),
                             reads=R_, writes=W_)
                        if lag < 15:
                            cmul(wr[F3], wi[F3], wr[F3], wi[F3], are[F3], aim[F3])
                    pm = sb(st2, "C_pm", [128, 3, 16])
                    bs = sb(st2, "C_bs", [128, 16, 2, 32])
                    cs = sb(st2, "C_cs", [128, 16, 2, 32])
                    dsb = sb(st2, "C_d", [128, 6])
                    S.dma("sp", "c", pm[:, :, :], s5_psm[:, :, :], writes=W_)
                    S.dma("sp", "c", bs[:, :, :, :], s5_bsm[:, :, :, :], writes=W_)
                    S.dma("sp", "c", cs[:, :, :, :], s5_csm[:, :, :, :], writes=W_)
                    S.dma("sp", "c", dsb[:, :], s5_d[:, :], writes=W_)
                    sre, sim, scr, sci = cparams(st2, "sm", pm[:, 0, :], pm[:, 1, :], pm[:, 2, :], [128, 16])
                    apr = sb(st2, "C_apr", [128, 17, 16])
                    api = sb(st2, "C_api", [128, 17, 16])
                    napi = sb(st2, "C_napi", [128, 17, 16])
                    v1 = sb(st2, "C_v1", [128, 16])
                    v2 = sb(st2, "C_v2", [128, 16])
                    S.op("dve", lambda e: e.memset(apr[:, 0, :], 1.0), reads=R_, writes=W_)
                    S.op("dve", lambda e: e.memset(api[:, 0, :], 0.0), reads=R_, writes=W_)
                    for k in range(1, 17):
                        tt("dve", v1[:, :], apr[:, k - 1, :], sre[:, :], ALU.mult)
                        tt("dve", v2[:, :], api[:, k - 1, :], sim[:, :], ALU.mult)
                        tt("dve", apr[:, k, :], v1[:, :], v2[:, :], ALU.subtract)
                        tt("dve", v1[:, :], apr[:, k - 1, :], sim[:, :], ALU.mult)
                        tt("dve", v2[:, :], api[:, k - 1, :], sre[:, :], ALU.mult)
                        tt("dve", api[:, k, :], v1[:, :], v2[:, :], ALU.add)
                    tsc("dve", napi[:, :, :], api[:, :, :], -1.0, ALU.mult)
                    napr = sb(st2, "C_napr", [128, 17, 16])
                    tsc("dve", napr[:, :, :], apr[:, :, :], -1.0, ALU.mult)
                    S.op("dve", lambda e: e.tensor_copy(out=a16[:, 0, :], in_=apr[:, 16, :]), reads=R_, writes=W_)
                    S.op("dve", lambda e: e.tensor_copy(out=a16[:, 1, :], in_=api[:, 16, :]), reads=R_, writes=W_)
                    nsci = sb(st2, "C_nsci", [128, 16])
                    tsc("dve", nsci[:, :], sci[:, :], -1.0, ALU.mult)
                    bb = sb(st2, "C_bb", [128, 16, 2, 32])
                    x1 = sb(st2, "C_x1", [128, 32])
                    for i in range(16):
                        tsc("dve", x1[:, :], bs[:, i, 0, :], scr[:, i:i + 1], ALU.mult)
                        stt(bb[:, i, 0, :], bs[:, i, 1, :], nsci[:, i:i + 1], x1[:, :], ALU.mult, ALU.add)
                        tsc("dve", x1[:, :], bs[:, i, 1, :], scr[:, i:i + 1], ALU.mult)
                        stt(bb[:, i, 1, :], bs[:, i, 0, :], sci[:, i:i + 1], x1[:, :], ALU.mult, ALU.add)
                    AB = sb(st2, "C_AB", [128, 16, 2, 32], BF16)
                    crb = sb(st2, "C_crb", [128, 16, 2, 32], BF16)
                    S.op("dve", lambda e: e.tensor_copy(out=crb[:, :, 0, :], in_=cs[:, :, 0, :]), reads=R_, writes=W_)
                    tsc("dve", crb[:, :, 1, :], cs[:, :, 1, :], -1.0, ALU.mult)
                    S.op("pool", lambda e: e.memset(WK[:, :, :, :], 0.0), reads=R_, writes=W_)
                    psK = ps(st2, "C_psK", [128, 192])
                    for lag in range(16):
                        for i in range(16):
                            tsc("dve", x1[:, :], bb[:, i, 0, :], apr[:, lag, i:i + 1], ALU.mult)
                            stt(AB[:, i, 0, :], bb[:, i, 1, :], napi[:, lag, i:i + 1], x1[:, :], ALU.mult, ALU.add)
                            tsc("dve", x1[:, :], bb[:, i, 1, :], apr[:, lag, i:i + 1], ALU.mult)
                            stt(AB[:, i, 1, :], bb[:, i, 0, :], api[:, lag, i:i + 1], x1[:, :], ALU.mult, ALU.add)
                            tsc("dve", x1[:, :], cs[:, i, 0, :], apr[:, lag + 1, i:i + 1], ALU.mult)
                            stt(WO[:, lag, i, 0, :], cs[:, i, 1, :], napi[:, lag + 1, i:i + 1], x1[:, :], ALU.mult, ALU.add)
                            tsc("dve", x1[:, :], cs[:, i, 0, :], napi[:, lag + 1, i:i + 1], ALU.mult)
                            stt(WO[:, lag, i, 1, :], cs[:, i, 1, :], napr[:, lag + 1, i:i + 1], x1[:, :], ALU.mult, ALU.add)
                        for i in range(16):
                            r0 = 32 * (i % 3)
                            cc_ = i // 3
                            S.op("pe", lambda e, i=i, r0=r0, cc_=cc_: e.matmul(
                                psK[r0:r0 + 32, cc_ * 32:(cc_ + 1) * 32], lhsT=AB[:, i, 0, :], rhs=crb[:, i, 0, :],
                                start=True, stop=False), reads=R_, writes=W_)
                            S.op("pe", lambda e, i=i, r0=r0, cc_=cc_: e.matmul(
                                psK[r0:r0 + 32, cc_ * 32:(cc_ + 1) * 32], lhsT=AB[:, i, 1, :], rhs=crb[:, i, 1, :],
                                start=False, stop=True), reads=R_, writes=W_)
                            S.op("act", lambda e, i=i, r0=r0, cc_=cc_, lag=lag: e.activation(
                                out=WK[r0:r0 + 32, lag, cc_, r0:r0 + 32], in_=psK[r0:r0 + 32, cc_ * 32:(cc_ + 1) * 32],
                                func=AF.Copy), reads=R_, writes=W_)
                    for cc_ in range(6):
                        stt(WK[:, 0, cc_, :], ident_f[:, :], dsb[:, cc_:cc_ + 1], WK[:, 0, cc_, :], ALU.mult, ALU.add)
                barrier()
                usb = sb(st, "C_u", [128, 6, LP], BF16)
                Ssb = sb(st, "C_S", [128, 16, 2, NCH])
                Xbf = sb(st, "C_Xbf", [128, 16, 2, NCH], BF16)
                ypre = sb(st, "C_ypre", [128, SEQ])
                g1 = sb(st, "C_g1", [128, SEQ])
                ya = sb(st, "C_ya", [128, 6, SEQ], BF16)
                w1 = sb(st, "C_w1", [128, 16])
                w2 = sb(st, "C_w2", [128, 16])
                w3 = sb(st, "C_w3", [128, 16])
                w4 = sb(st, "C_w4", [128, 16])
                psS = [ps(st, "C_psS%d" % i, [128, NCH]) for i in range(2)]
                psY = [ps(st, "C_psY%d" % i, [128, NCH]) for i in range(2)]
                for s_ in range(NSEQ):
                    S.dma("sp", "x", usb[:, :, :], uT[s_, :, :, :], reads=[("uT", s_, t) for t in ALLT], writes=["C_u"])
                    n_ = 0
                    for i in range(16):
                        r0 = 32 * (i % 3)
                        cc_ = i // 3
                        for ri in range(2):
                            b_ = n_ % 2
                            n_ += 1
                            mms(psS[b_][:, :], [(WS[r0:r0 + 32, 15 - j, cc_, ri, :], usb[r0:r0 + 32, cc_, j:LP:16])
                                                for j in range(16)], ["C_u", K_], ("C_psS", b_))
                            S.op("act", lambda e, i=i, ri=ri, b_=b_: e.activation(out=Ssb[:, i, ri, :], in_=psS[b_][:, :],
                                                                               func=AF.Copy),
                                 reads=[("C_psS", b_)], writes=["C_S"])
                    for c in range(1, NCH - 1):
                        S.op("dve", lambda e, c=c: e.tensor_tensor(out=w1[:, :], in0=Ssb[:, :, 0, c - 1], in1=a16[:, 0, :], op=ALU.mult),
                             reads=["C_S", K_], writes=["C_w1"])
                        S.op("dve", lambda e, c=c: e.tensor_tensor(out=w2[:, :], in0=Ssb[:, :, 1, c - 1], in1=a16[:, 1, :], op=ALU.mult),
                             reads=["C_S"], writes=["C_w2"])
                        S.op("pool", lambda e, c=c: e.tensor_tensor(out=w3[:, :], in0=Ssb[:, :, 1, c - 1], in1=a16[:, 0, :], op=ALU.mult),
                             reads=["C_S", K_], writes=["C_w3"])
                        S.op("pool", lambda e, c=c: e.tensor_tensor(out=w4[:, :], in0=Ssb[:, :, 0, c - 1], in1=a16[:, 1, :], op=ALU.mult),
                             reads=["C_S"], writes=["C_w4"])
                        S.op("dve", lambda e: e.tensor_tensor(out=w1[:, :], in0=w1[:, :], in1=w2[:, :], op=ALU.subtract),
                             reads=["C_w1", "C_w2"], writes=["C_w1"])
                        S.op("pool", lambda e: e.tensor_tensor(out=w3[:, :], in0=w3[:, :], in1=w4[:, :], op=ALU.add),
                             reads=["C_w3", "C_w4"], writes=["C_w3"])
                        S.op("dve", lambda e, c=c: e.tensor_tensor(out=Ssb[:, :, 0, c], in0=w1[:, :], in1=Ssb[:, :, 0, c], op=ALU.add),
                             reads=["C_w1", "C_w3", "C_S"], writes=["C_Sa"])
                        S.op("pool", lambda e, c=c: e.tensor_tensor(out=Ssb[:, :, 1, c], in0=w3[:, :], in1=Ssb[:, :, 1, c], op=ALU.add),
                             reads=["C_w3", "C_Sa", "C_S"], writes=["C_S"])
                    S.op("dve", lambda e: e.memset(Xbf[:, :, :, 0], 0.0), reads=["C_Xbf"], writes=["C_Xbf"])
                    S.op("dve", lambda e: e.tensor_copy(out=Xbf[:, :, :, 1:NCH], in_=Ssb[:, :, :, 0:NCH - 1]), reads=["C_S", "C_Xbf"], writes=["C_Xbf"])
                    n_ = 0
                    for cc_ in range(6):
                        tiles_cc = [i for i in range(3 * cc_, min(3 * cc_ + 3, 16))]
                        for tau in range(16):
                            b_ = n_ % 2
                            n_ += 1
                            pairs = [(WK[:, tau - j, cc_, :], usb[:, cc_, j:LP:16]) for j in range(tau + 1)]
                            mms_out = psY[b_]
                            for idx, (l, r_) in enumerate(pairs):
                                S.op("pe", lambda e, l=l, r_=r_, idx=idx: e.matmul(mms_out[:, :], lhsT=l, rhs=r_, start=(idx == 0), stop=False),
                                     reads=["C_u", K_], writes=[("C_psY", b_)])
                            for i in tiles_cc:
                                r0 = 32 * (i % 3)
                                for ri in range(2):
                                    last = (i == tiles_cc[-1] and ri == 1)
                                    S.op("pe", lambda e, i=i, ri=ri, r0=r0, last=last, tau=tau: e.matmul(
                                        mms_out[r0:r0 + 32, :], lhsT=WO[:, tau, i, ri, :], rhs=Xbf[:, i, ri, :], start=False, stop=last),
                                        reads=["C_Xbf", K_], writes=[("C_psY", b_)])
                            S.op("act", lambda e, tau=tau, b_=b_: e.activation(
                                out=ypre[:, tau:SEQ:16], in_=psY[b_][:, 1:NCH], func=AF.Copy),
                                reads=[("C_psY", b_)], writes=["C_ypre"])
                        S.op("act", lambda e: e.activation(out=g1[:, :], in_=ypre[:, :], func=AF.Square), reads=["C_ypre"], writes=["C_g1"])
                        S.op("dve", lambda e: e.tensor_scalar(out=g1[:, :], in0=g1[:, :], scalar1=0.0713548162726, scalar2=1.5957691216,
                                                              op0=ALU.mult, op1=ALU.add), reads=["C_g1"], writes=["C_g1"])
                        S.op("dve", lambda e: e.tensor_tensor(out=g1[:, :], in0=g1[:, :], in1=ypre[:, :], op=ALU.mult), reads=["C_g1", "C_ypre"], writes=["C_g1"])
                        S.op("act", lambda e: e.activation(out=g1[:, :], in_=g1[:, :], func=AF.Sigmoid), reads=["C_g1"], writes=["C_g1"])
                        S.op("dve", lambda e, cc_=cc_: e.tensor_tensor(out=ya[:, cc_, :], in0=g1[:, :], in1=ypre[:, :], op=ALU.mult),
                             reads=["C_g1", "C_ypre"], writes=["C_ya"])
                    S.dma("sp", "o", yaT[s_, :, :, :], ya[:, :, :], reads=["C_ya"], writes=[("yaT", s_)])

        if debug not in ("A", "B"):
            phase_C()
            barrier()

        if debug == "C":
            with ExitStack() as st:
                tmp = sb(st, "dbgtmp", [128, 6, SEQ], BF16)
                tmp2 = sb(st, "dbgtmp2", [128, 6, SEQ], F32)
                S.dma("sp", "x", tmp[:, :, :], yaT[0, :, :, :], reads=[("yaT", 0)], writes=["dbgtmp"])
                S.op("dve", lambda e: e.tensor_copy(out=tmp2[:, :, :], in_=tmp[:, :, :]), reads=["dbgtmp"], writes=["dbgtmp2"])
                S.dma("sp", "o", outT[0, :, 0:6, 0:SEQ], tmp2[:, :, :], reads=["dbgtmp2"], writes=["out"])

        biasG_d = din("biasG", [128, 8, 1024])
        biasM_d = din("biasM", [16, 8, 512])
        cvec_d = din("cvec", [128, 8])
        ybT = dscr("ybT", [NSEQ, 128, 4, SEQ], BF16)
        ones_b = sb(es, "ones_b", [128, 128], BF16)
        S.op("dve", lambda e: e.memset(ones_b[:, :], 1.0), writes=["ones_b"])
        NIT = 22
        TOPK = 256.0

        def phase_D():
            with ExitStack() as st:
                Gb = sb(st, "D_Gb", [128, 8, 1024], BF16)
                Mb = sb(st, "D_Mb", [16, 8, 512], BF16)
                cv = sb(st, "D_cv", [128, 8])
                S.dma("sp", "c", cv[:, :], cvec_d[:, :], writes=["D_cv"])
                with ExitStack() as st2:
                    Gf = sb(st2, "D_Gf", [128, 8, 1024])
                    Mf = sb(st2, "D_Mf", [16, 8, 512])
                    S.dma("sp", "c", Gf[:, :, :], biasG_d[:, :, :], writes=["D_Gf"])
                    S.dma("sp", "c", Mf[:, :, :], biasM_d[:, :, :], writes=["D_Mf"])
                    for h in range(8):
                        S.op("dve", lambda e, h=h: e.tensor_scalar(out=Gb[:, h, :], in0=Gf[:, h, :], scalar1=cv[:, h:h + 1],
                                                                   scalar2=None, op0=ALU.subtract),
                             reads=["D_Gf", "D_cv"], writes=["D_Gb"])
                        S.op("dve", lambda e, h=h: e.tensor_scalar(out=Mb[:, h, :], in0=Mf[:, h, :], scalar1=cv[:16, h:h + 1],
                                                                   scalar2=None, op0=ALU.subtract),
                             reads=["D_Mf", "D_cv"], writes=["D_Mb"])
                    barrier()
                qi = sb(st, "D_qi", [128, 4, SEQ], BF16)
                ki = sb(st, "D_ki", [128, LP], BF16)
                qq = sb(st, "D_q", [128, 4, SEQ], BF16)
                kk = sb(st, "D_k", [128, LP], BF16)
                vd = sb(st, "D_vd", [128, 17, 128], BF16)
                wq = sb(st, "D_wq", [128, 16, 8])
                sc = sb(st, "D_sc", [128, 4, LP])
                MA = sb(st, "D_MA", [128, 4, LP], BF16)
                junk = sb(st, "D_junk", [128, LP], BF16)
                Rb = [sb(st, "D_Rb%d" % i, [128, 512], BF16) for i in range(2)]
                dg = sb(st, "D_dg", [128, 8, 128], BF16)
                maskT = sb(st, "D_maskT", [128, 17, 512], BF16)
                Pt = [sb(st, "D_Pt%d" % i, [128, 512], BF16) for i in range(3)]
                rd = sb(st, "D_rd", [128, 512])
                yb = sb(st, "D_yb", [128, 4, 512], BF16)
                lo = sb(st, "D_lo", [128, 4])
                hi = sb(st, "D_hi", [128, 4])
                W0 = sb(st, "D_W0", [128, 4])
                Wk = sb(st, "D_Wk", [128, 4])
                mid = sb(st, "D_mid", [128, 4])
                cnt = sb(st, "D_cnt", [128, 4])
                stp = sb(st, "D_stp", [128, 4])
                pq = [ps(st, "D_pq%d" % i, [128, 512]) for i in range(2)]
                psc = ps(st, "D_psc", [128, 512])
                pT = ps(st, "D_pT", [128, 512], BF16)
                pL = [ps(st, "D_pL%d" % i, [128, 512]) for i in range(2)]
                pO = ps(st, "D_pO", [128, 512])
                pDen = ps(st, "D_pDen", [128, 512])
                for s_ in range(NSEQ):
                    S.dma("sp", "x", qi[:, :, :], qiT[s_, :, :, :], reads=[("qiT", s_, t) for t in range(NT)], writes=["D_qi"])
                    S.dma("sp", "x", ki[:, :], kiT[s_, :, :], reads=[("kiT", s_, t) for t in ALLT], writes=["D_ki"])
                    S.dma("sp", "x", qq[:, :, :], qT[s_, :, :, :], reads=[("qT", s_, t) for t in range(NT)], writes=["D_q"])
                    S.dma("sp", "x", kk[:, :], kT[s_, :, :], reads=[("kT", s_, t) for t in ALLT], writes=["D_k"])
                    vr = [("vwS", s_, t) for t in ALLT]
                    for half in range(2):
                        S.dma("pool", "x", vd[:, 1:17, 64 * half:64 * half + 64],
                              vwS[s_, NMETA:LP, 0:64].rearrange("(b p) c -> p b c", p=128), reads=vr, writes=["D_vd"])
                        S.dma("pool", "x", vd[:NMETA, 0, 64 * half:64 * half + 64], vwS[s_, 0:NMETA, 0:64], reads=vr, writes=["D_vd"])
                    S.dma("sp", "x", wq[:, :, :], vwS[s_, NMETA:LP, 64:72].rearrange("(b p) c -> p b c", p=128), reads=vr, writes=["D_wq"])
                    for Q in range(4):
                        for jl in range(4):
                            j = 4 * Q + jl
                            Nj = NMETA + 128 * (j + 1)
                            for h in range(8):
                                S.op("dve", lambda e, h=h, j=j: e.tensor_scalar(out=dg[:, h, :], in0=ident_f[:, :], scalar1=wq[:, j, h:h + 1],
                                                                              scalar2=None, op0=ALU.mult),
                                     reads=["ident_f", "D_wq"], writes=["D_dg"])
                            for c0 in range(0, Nj, 512):
                                cw = min(512, Nj - c0)
                                for h in range(8):
                                    hh, hp, b_ = h % 2, h // 2, h % 2
                                    S.op("pe", lambda e, hh=hh, hp=hp, b_=b_, j=j, c0=c0, cw=cw: e.matmul(
                                        pq[b_][:, :cw], lhsT=qi[64 * hh:64 * hh + 64, hp, 128 * j:128 * j + 128],
                                        rhs=ki[64 * hh:64 * hh + 64, c0:c0 + cw], start=True, stop=True),
                                        reads=["D_qi", "D_ki"], writes=[("D_pq", b_)])
                                    if h % 2 == 0:
                                        S.op("act", lambda e, b_=b_, cw=cw: e.activation(out=Rb[b_][:, :cw], in_=pq[b_][:, :cw], func=AF.Relu),
                                             reads=[("D_pq", b_)], writes=[("D_Rb", b_)])
                                    else:
                                        S.op("dve", lambda e, b_=b_, cw=cw: e.tensor_scalar(out=Rb[b_][:, :cw], in0=pq[b_][:, :cw], scalar1=0.0,
                                                                                          scalar2=None, op0=ALU.max),
                                             reads=[("D_pq", b_)], writes=[("D_Rb", b_)])
                                    S.op("pe", lambda e, h=h, b_=b_, cw=cw: e.matmul(psc[:, :cw], lhsT=dg[:, h, :], rhs=Rb[b_][:, :cw],
                                                                                   start=(h == 0), stop=(h == 7)),
                                         reads=[("D_Rb", b_), "D_dg"], writes=["D_psc"])
                                S.op("act", lambda e, jl=jl, c0=c0, cw=cw: e.activation(out=sc[:, jl, c0:c0 + cw], in_=psc[:, :cw], func=AF.Copy),
                                     reads=["D_psc"], writes=[("D_sc", jl)])
                            S.op("dve", lambda e, jl=jl, Nj=Nj: e.tensor_reduce(out=lo[:, jl:jl + 1], in_=sc[:, jl, :Nj], axis=AX.X, op=ALU.min),
                                 reads=[("D_sc", jl)], writes=["D_lo"])
                            S.op("dve", lambda e, jl=jl, Nj=Nj: e.tensor_reduce(out=hi[:, jl:jl + 1], in_=sc[:, jl, :Nj], axis=AX.X, op=ALU.max),
                                 reads=[("D_sc", jl)], writes=["D_hi"])
                            S.op("dve", lambda e, jl=jl, Nj=Nj: e.memset(sc[0:64, jl, Nj - 64:Nj], -1e30),
                                 reads=[("D_sc", jl), "D_lo", "D_hi"], writes=[("D_sc", jl)])
                        S.op("dve", lambda e: e.tensor_tensor(out=W0[:, :], in0=hi[:, :], in1=lo[:, :], op=ALU.subtract),
                             reads=["D_lo", "D_hi"], writes=["D_W0"])
                        for it in range(NIT):
                            S.op("dve", lambda e, it=it: e.tensor_scalar(out=Wk[:, :], in0=W0[:, :], scalar1=2.0 ** (-(it + 1)), scalar2=None,
                                                                        op0=ALU.mult), reads=["D_W0", "D_stp"], writes=["D_Wk"])
                            S.op("dve", lambda e: e.tensor_tensor(out=mid[:, :], in0=lo[:, :], in1=Wk[:, :], op=ALU.add),
                                 reads=["D_lo", "D_Wk"], writes=["D_mid"])
                            for jl in range(4):
                                Nj = NMETA + 128 * (4 * Q + jl + 1)
                                S.op("dve", lambda e, jl=jl, Nj=Nj: e.tensor_scalar(
                                    out=junk[:, :Nj], in0=sc[:, jl, :Nj], scalar1=mid[:, jl:jl + 1], scalar2=0.0,
                                    op0=ALU.is_ge, op1=ALU.add, accum_out=cnt[:, jl:jl + 1]),
                                    reads=[("D_sc", jl), "D_mid"], writes=["D_junk", "D_cnt"])
                            S.op("dve", lambda e: e.tensor_scalar(out=stp[:, :], in0=cnt[:, :], scalar1=TOPK, scalar2=None, op0=ALU.is_ge),
                                 reads=["D_cnt"], writes=["D_stp"])
                            S.op("dve", lambda e: e.tensor_tensor(out=stp[:, :], in0=stp[:, :], in1=Wk[:, :], op=ALU.mult),
                                 reads=["D_stp", "D_Wk"], writes=["D_stp"])
                            S.op("dve", lambda e: e.tensor_tensor(out=lo[:, :], in0=lo[:, :], in1=stp[:, :], op=ALU.add),
                                 reads=["D_lo", "D_stp"], writes=["D_lo"])
                        for jl in range(4):
                            Nj = NMETA + 128 * (4 * Q + jl + 1)
                            S.op("dve", lambda e, jl=jl, Nj=Nj: e.tensor_scalar(out=MA[:, jl, :Nj], in0=sc[:, jl, :Nj], scalar1=lo[:, jl:jl + 1],
                                                                              scalar2=None, op0=ALU.is_ge),
                                 reads=[("D_sc", jl), "D_lo"], writes=["D_MA"])
                        nblk = 4 * Q + 5
                        geo = []
                        for b in range(nblk):
                            w = NMETA if b == 0 else 128
                            pc0 = 0 if b == 0 else NMETA + 128 * (b - 1)
                            jl0 = max(0, b - 1 - 4 * Q)
                            geo.append((w, pc0, jl0))
                            for jl in range(jl0, 4):
                                S.op("pe", lambda e, w=w, pc0=pc0, jl=jl: e.transpose(pT[:w, jl * 128:(jl + 1) * 128],
                                                                                     MA[:, jl, pc0:pc0 + w], ident_b[:, :]),
                                     reads=["D_MA", "ident_b"], writes=["D_pT"])
                            if b % 2 == 0:
                                S.op("act", lambda e, w=w, b=b, jl0=jl0: e.activation(out=maskT[:w, b, jl0 * 128:512], in_=pT[:w, jl0 * 128:512],
                                                                                    func=AF.Copy), reads=["D_pT"], writes=[("D_maskT", b)])
                            else:
                                S.op("dve", lambda e, w=w, b=b, jl0=jl0: e.tensor_copy(out=maskT[:w, b, jl0 * 128:512], in_=pT[:w, jl0 * 128:512]),
                                     reads=["D_pT"], writes=[("D_maskT", b)])
                        n_ = 0
                        for h in range(8):
                            hh, hp = h % 2, h // 2
                            for b in range(nblk):
                                w, pc0, jl0 = geo[b]
                                c0 = jl0 * 128
                                near = (b == 0 and Q == 0) or (b >= 1 and b - 1 >= 4 * Q - 1)
                                lb = n_ % 2
                                pb_ = n_ % 3
                                n_ += 1
                                S.op("pe", lambda e, w=w, pc0=pc0, c0=c0, hh=hh, hp=hp, lb=lb, near=near, Q=Q: e.matmul(
                                    pL[lb][:w, c0:512], lhsT=kk[64 * hh:64 * hh + 64, pc0:pc0 + w],
                                    rhs=qq[64 * hh:64 * hh + 64, hp, 512 * Q + c0:512 * Q + 512], start=True, stop=(not near)),
                                    reads=["D_k", "D_q"], writes=[("D_pL", lb)])
                                if near:
                                    if b == 0:
                                        S.op("pe", lambda e, h=h, c0=c0, lb=lb: e.matmul(pL[lb][:NMETA, c0:512], lhsT=ident_b[:NMETA, :NMETA],
                                                                                       rhs=Mb[:NMETA, h, c0:512], start=False, stop=True),
                                             reads=["D_Mb", "ident_b"], writes=[("D_pL", lb)])
                                    else:
                                        z0 = 512 * Q + c0 - 128 * (b - 1) + 384
                                        S.op("pe", lambda e, h=h, c0=c0, lb=lb, z0=z0: e.matmul(pL[lb][:, c0:512], lhsT=ident_b[:, :],
                                                                                              rhs=Gb[:, h, z0:z0 + 512 - c0], start=False, stop=True),
                                             reads=["D_Gb", "ident_b"], writes=[("D_pL", lb)])
                                S.op("act", lambda e, w=w, c0=c0, lb=lb, pb_=pb_: e.activation(out=Pt[pb_][:w, c0:512], in_=pL[lb][:w, c0:512], func=AF.Exp),
                                     reads=[("D_pL", lb)], writes=[("D_Pt", pb_)])
                                S.op("pool", lambda e, w=w, c0=c0, b=b, pb_=pb_: e.tensor_tensor(out=Pt[pb_][:w, c0:512], in0=Pt[pb_][:w, c0:512],
                                                                                              in1=maskT[:w, b, c0:512], op=ALU.mult),
                                     reads=[("D_Pt", pb_), ("D_maskT", b)], writes=[("D_Pt", pb_)])
                                S.op("pe", lambda e, w=w, c0=c0, b=b, pb_=pb_, nblk=nblk: e.matmul(pO[:, c0:512], lhsT=vd[:w, b, :], rhs=Pt[pb_][:w, c0:512],
                                                                                                start=(b == 0), stop=(b == nblk - 1)),
                                     reads=[("D_Pt", pb_), "D_vd"], writes=["D_pO"])
                                S.op("pe", lambda e, w=w, c0=c0, b=b, pb_=pb_, nblk=nblk: e.matmul(pDen[:, c0:512], lhsT=ones_b[:w, :], rhs=Pt[pb_][:w, c0:512],
                                                                                                start=(b == 0), stop=(b == nblk - 1)),
                                     reads=[("D_Pt", pb_), "ones_b"], writes=["D_pDen"])
                            S.op("dve", lambda e, hh=hh: e.reciprocal(out=rd[64 * hh:64 * hh + 64, :], in_=pDen[64 * hh:64 * hh + 64, :]),
                                 reads=["D_pDen"], writes=["D_rd"])
                            S.op("dve", lambda e, hh=hh, hp=hp: e.tensor_tensor(out=yb[64 * hh:64 * hh + 64, hp, :], in0=rd[64 * hh:64 * hh + 64, :],
                                                                              in1=pO[64 * hh:64 * hh + 64, :], op=ALU.mult),
                                 reads=["D_rd", "D_pO", "D_pDen"], writes=["D_yb"])
                        S.dma("sp", "o", ybT[s_, :, :, 512 * Q:512 * Q + 512], yb[:, :, :], reads=["D_yb"], writes=[("ybT", s_, Q)])

        if debug not in ("A", "B", "C"):
            phase_D()
            barrier()

        if debug == "D":
            with ExitStack() as st:
                tmp = sb(st, "dbgtmp", [128, 4, SEQ], BF16)
                tmp2 = sb(st, "dbgtmp2", [128, 4, SEQ], F32)
                S.dma("sp", "x", tmp[:, :, :], ybT[0, :, :, :], reads=[("ybT", 0, t) for t in range(NT)], writes=["dbgtmp"])
                S.op("dve", lambda e: e.tensor_copy(out=tmp2[:, :, :], in_=tmp[:, :, :]), reads=["dbgtmp"], writes=["dbgtmp2"])
                S.dma("sp", "o", outT[0, :, 0:4, 0:SEQ], tmp2[:, :, :], reads=["dbgtmp2"], writes=["out"])

        w_glu_d = din("w_glu", [128, 6, 768])
        w_a_d = din("w_a", [128, 6, D])
        w_b_d = din("w_b", [128, 4, D])
        w_o_d = din("w_o", [128, DC, D])
        w_g_d = din("w_g", [128, DC, 2 * D])
        h2T = dscr("h2T", [NSEQ, 128, DC, SEQ])

        def phase_E():
            with ExitStack() as st:
                wglu = sb(st, "E_wglu", [128, 6, 768], BF16)
                wa = sb(st, "E_wa", [128, 6, D], BF16)
                wb = sb(st, "E_wb", [128, 4, D], BF16)
                wo = sb(st, "E_wo", [128, DC, D], BF16)
                wgt = sb(st, "E_wg", [128, DC, 2 * D], BF16)
                S.dma("pool", "w", wglu[:, :, :], w_glu_d[:, :, :], writes=["E_w"], max_dma_last_dim=3072)
                for k in range(6):
                    S.dma("pool", "w", wa[:, k, :], w_a_d[:, k, :], writes=["E_w"])
                for k in range(4):
                    S.dma("pool", "w", wb[:, k, :], w_b_d[:, k, :], writes=["E_w"])
                for k in range(DC):
                    S.dma("pool", "w", wo[:, k, :], w_o_d[:, k, :], writes=["E_w"])
                    S.dma("pool", "w", wgt[:, k, :], w_g_d[:, k, :], writes=["E_w"], max_dma_last_dim=4096)
                hn = sb(st, "E_hn", [128, DC, TT], BF16)
                ya = sb(st, "E_ya", [128, 6, TT], BF16)
                yg = sb(st, "E_yg", [128, 6, TT], BF16)
                ybt = sb(st, "E_yb", [128, 4, TT], BF16)
                h1t = sb(st, "E_h1", [128, DC, TT])
                sgl = sb(st, "E_sgl", [128, TT])
                ga = sb(st, "E_ga", [128, TT])
                gb = sb(st, "E_gb", [128, TT])
                t1 = sb(st, "E_t1", [128, TT])
                t2 = sb(st, "E_t2", [128, TT])
                mg = sb(st, "E_mg", [128, DC, TT], BF16)
                ysb = sb(st, "E_y", [128, DC, TT])
                sq = sb(st, "E_sq", [128, 2, TT])
                rs = sb(st, "E_rs", [128, TT])
                pga = ps(st, "E_pga", [128, TT])
                pgb = ps(st, "E_pgb", [128, TT])
                pa = ps(st, "E_pa", [128, TT])
                pb = ps(st, "E_pb", [128, TT])
                py = [ps(st, "E_py%d" % i, [128, TT]) for i in range(2)]
                pss = ps(st, "E_pss", [128, TT])
                for s_ in range(NSEQ):
                    for t_ in range(NT):
                        tsl = slice(t_ * TT, (t_ + 1) * TT)
                        S.dma("sp", "x", hn[:, :, :], hnT[s_, :, :, tsl], reads=[("hnT", s_, t_)], writes=["E_hn"])
                        S.dma("sp", "x", ya[:, :, :], yaT[s_, :, :, tsl], reads=[("yaT", s_)], writes=["E_ya"])
                        S.dma("sp", "x", ybt[:, :, :], ybT[s_, :, :, tsl], reads=[("ybT", s_, t_)], writes=["E_yb"])
                        S.dma("sp", "x", h1t[:, :, :], h1T[s_, :, :, tsl], reads=[("h1T", s_, t_)], writes=["E_h1"])
                        for oc in range(6):
                            b_ = oc % 2
                            mms(py[b_][:, :], [(wglu[:, k, oc * 128:(oc + 1) * 128], ya[:, k, :]) for k in range(6)],
                                ["E_w", "E_ya"], ("E_py", b_))
                            S.op("act", lambda e, b_=b_: e.activation(out=sgl[:, :], in_=py[b_][:, :], func=AF.Sigmoid),
                                 reads=[("E_py", b_)], writes=["E_sgl"])
                            S.op("dve", lambda e, oc=oc: e.tensor_tensor(out=yg[:, oc, :], in0=sgl[:, :], in1=ya[:, oc, :], op=ALU.mult),
                                 reads=["E_sgl", "E_ya"], writes=["E_yg"])
                        for dc in range(DC):
                            mms(pga[:, :], [(wgt[:, k, dc * 128:(dc + 1) * 128], hn[:, k, :]) for k in range(DC)], ["E_w", "E_hn"], "E_pga")
                            mms(pgb[:, :], [(wgt[:, k, D + dc * 128:D + (dc + 1) * 128], hn[:, k, :]) for k in range(DC)], ["E_w", "E_hn"], "E_pgb")
                            mms(pa[:, :], [(wa[:, k, dc * 128:(dc + 1) * 128], yg[:, k, :]) for k in range(6)], ["E_w", "E_yg"], "E_pa")
                            mms(pb[:, :], [(wb[:, k, dc * 128:(dc + 1) * 128], ybt[:, k, :]) for k in range(4)], ["E_w", "E_yb"], "E_pb")
                            S.op("act", lambda e: e.activation(out=ga[:, :], in_=pga[:, :], func=AF.Sigmoid), reads=["E_pga"], writes=["E_ga"])
                            S.op("act", lambda e: e.activation(out=gb[:, :], in_=pgb[:, :], func=AF.Sigmoid), reads=["E_pgb"], writes=["E_gb"])
                            S.op("dve", lambda e: e.tensor_tensor(out=t1[:, :], in0=ga[:, :], in1=pa[:, :], op=ALU.mult), reads=["E_ga", "E_pa"], writes=["E_t1"])
                            S.op("dve", lambda e: e.tensor_tensor(out=t2[:, :], in0=gb[:, :], in1=pb[:, :], op=ALU.mult), reads=["E_gb", "E_pb"], writes=["E_t2"])
                            S.op("pool", lambda e, dc=dc: e.tensor_tensor(out=mg[:, dc, :], in0=t1[:, :], in1=t2[:, :], op=ALU.add),
                                 reads=["E_t1", "E_t2"], writes=["E_mg"])
                        for c in range(DC):
                            b_ = c % 2
                            mms(py[b_][:, :], [(wo[:, k, c * 128:(c + 1) * 128], mg[:, k, :]) for k in range(DC)], ["E_w", "E_mg"], ("E_py", b_))
                            S.op("act", lambda e, c=c, b_=b_: e.activation(out=ysb[:, c, :], in_=py[b_][:, :], func=AF.Copy),
                                 reads=[("E_py", b_)], writes=[("E_y", c)])
                            S.op("act", lambda e, c=c, b_=b_: e.activation(out=sq[:, c % 2, :], in_=py[b_][:, :], func=AF.Square),
                                 reads=[("E_py", b_)], writes=[("E_sq", c % 2)])
                            S.op("pe", lambda e, c=c: e.matmul(pss[:, :], lhsT=ones_f[:, :], rhs=sq[:, c % 2, :], start=(c == 0), stop=(c == DC - 1)),
                                 reads=[("E_sq", c % 2), "ones_f"], writes=["E_pss"])
                        rstd_from_sumsq(pss, rs, TT, "E_pss", "E_rs")
                        for c in range(DC):
                            S.op("dve", lambda e, c=c: e.scalar_tensor_tensor(
                                out=ysb[:, c, :], in0=ysb[:, c, :], scalar=gains_sb[:, 24 + c:24 + c + 1], in1=rs[:, :],
                                op0=ALU.mult, op1=ALU.mult), reads=[("E_y", c), "E_rs", "gains"], writes=[("E_y", c)])
                            S.op("pool", lambda e, c=c: e.tensor_tensor(out=ysb[:, c, :], in0=ysb[:, c, :], in1=h1t[:, c, :], op=ALU.add),
                                 reads=[("E_y", c), "E_h1"], writes=[("E_y", c)])
                        S.dma("sp", "o", h2T[s_, :, :, tsl], ysb[:, :, :], reads=[("E_y", c) for c in range(DC)], writes=[("h2T", s_, t_)])

        if debug not in ("A", "B", "C", "D"):
            phase_E()
            barrier()
            ff2_wg = din("ff2_wg", [128, DC, DFF])
            ff2_wu = din("ff2_wu", [128, DC, DFF])
            ff2_wd = din("ff2_wd", [128, FC, D])
            tilesF = []
            for s in range(NSEQ):
                for t in range(NFT):
                    tilesF.append((h2T[s, :, :, t * FT:(t + 1) * FT], outT[s, :, :, t * FT:(t + 1) * FT], FT,
                                   [("h2T", s, t * FT // TT)], [("out", s, t)]))
            ffn_phase("F", ff2_wg, ff2_wu, ff2_wd, 32, 40, tilesF)

        if debug == "A":
            with ExitStack() as st:
                tmp = sb(st, "dbgtmp", [128, DC, FT])
                S.dma("sp", "x", tmp[:, :, :], h1T[0, :, :, 0:FT], reads=[("h1T", 0, 0)], writes=["dbgtmp"])
                S.dma("sp", "o", outT[0, :, :, 0:FT], tmp[:, :, :], reads=["dbgtmp"], writes=["out"])
                S.dma("sp", "x", tmp[:, :, :NMETA], h1m[:, :, :], reads=["h1m", "out"], writes=["dbgtmp"])
                S.dma("sp", "o", outT[1, :, :, 0:NMETA], tmp[:, :, :NMETA], reads=["dbgtmp"], writes=["out"])

        S.drain("sp")
        print("instructions:", S.ninst)
    return nc


def _rel_bucket(rel):
    half, me = 16, 8
    base = np.where(rel > 0, half, 0)
    n = np.abs(rel)
    nf = np.maximum(n, 1).astype(np.float32)
    large = me + (np.log(nf / me) / math.log(128 / me) * (half - me)).astype(np.int32)
    large = np.minimum(large, half - 1)
    return base + np.where(n < me, n, large)


def prep_inputs(inp):
    f = lambda a: np.ascontiguousarray(np.asarray(a, dtype=np.float32))
    x = f(inp["x"])
    B = x.shape[0]
    xT = np.ascontiguousarray(x.reshape(B, SEQ, DC, 128).transpose(0, 3, 2, 1))
    metaT = np.ascontiguousarray(f(inp["meta_tokens"]).reshape(NMETA, DC, 128).transpose(2, 1, 0))
    gl = [inp[k] for k in ("ff1_norm_pre", "ff1_norm_post", "mix_norm_pre", "mix_norm_post", "ff2_norm_pre",
                           "ff2_norm_post")]
    gains = np.ascontiguousarray(np.concatenate([f(g)[0].reshape(DC, 128).T for g in gl], axis=1))

    def wk(w, kc):
        w = f(w)
        return np.ascontiguousarray(w.reshape(kc, 128, w.shape[-1]).transpose(1, 0, 2))

    shared = {
        "metaT": metaT, "gains": gains,
        "ff1_wg": wk(inp["ff1_w_gate"][0], DC), "ff1_wu": wk(inp["ff1_w_up"][0], DC),
        "ff1_wd": wk(inp["ff1_w_down"][0], FC),
    }
    win = f(inp["w_in"][0])
    upad = np.zeros((D, 6, 128), np.float32)
    for c6 in range(6):
        w_ = min(96, 512 - 96 * c6)
        upad[:, c6, :w_] = win[:, 96 * c6:96 * c6 + w_]
    winA = np.concatenate([upad.reshape(D, 768), win[:, 512:1024], win[:, 1096:1608], win[:, 1024:1088], win[:, 1024:1088],
                           win[:, 1608:1672], win[:, 1608:1672]], axis=1)
    winB = np.concatenate([win[:, 1672:1736], win[:, 1088:1096]], axis=1)
    shared["w_inA"] = wk(winA, DC)
    shared["w_inB"] = wk(winB, DC)
    lre, lim, ldt = f(inp["ssm_lambda_re"][0]), f(inp["ssm_lambda_im"][0]), f(inp["ssm_log_dt"][0])
    bre, bim = f(inp["ssm_b_re"][0]), f(inp["ssm_b_im"][0])
    cre, cim = f(inp["ssm_c_re"][0]), f(inp["ssm_c_im"][0])
    r = np.arange(128)
    sidx = np.arange(128)
    cc = np.arange(6)
    i_rc = 3 * cc[None, :] + (r[:, None] // 32)
    val_rc = (r[:, None] < 96) & (i_rc < 16)
    i_rc = np.where(val_rc, i_rc, 0)
    g_rcs = 2 * i_rc[:, :, None] + (sidx[None, None, :] // 64)
    p_s = sidx % 64
    pcm = np.stack([lre[g_rcs, p_s[None, None, :]], lim[g_rcs, p_s[None, None, :]], ldt[g_rcs]], axis=2)
    glr = (r % 32) // 16
    m_r = r % 16
    msk = (glr[:, None, None] == (sidx[None, None, :] // 64)) & val_rc[:, :, None]
    bcm = np.stack([np.where(msk, bre[g_rcs, p_s[None, None, :], m_r[:, None, None]], 0.0),
                    np.where(msk, bim[g_rcs, p_s[None, None, :], m_r[:, None, None]], 0.0)], axis=2)
    ii = np.arange(16)
    g_si = 2 * ii[None, :] + (sidx[:, None] // 64)
    psm = np.stack([lre[g_si, p_s[:, None]], lim[g_si, p_s[:, None]], ldt[g_si]], axis=1)
    q = np.arange(32)
    mq = q % 16
    mskq = ((q[None, None, :] // 16) == (sidx[:, None, None] // 64))
    bsm = np.stack([np.where(mskq, bre[g_si[:, :, None], p_s[:, None, None], mq[None, None, :]], 0.0),
                    np.where(mskq, bim[g_si[:, :, None], p_s[:, None, None], mq[None, None, :]], 0.0)], axis=2)
    csm = np.stack([np.where(mskq, cre[g_si[:, :, None], mq[None, None, :], p_s[:, None, None]], 0.0),
                    np.where(mskq, cim[g_si[:, :, None], mq[None, None, :], p_s[:, None, None]], 0.0)], axis=2)
    dflat = f(inp["ssm_d"][0]).reshape(512)
    ch_rc = 96 * cc[None, :] + r[:, None]
    vch = (r[:, None] < 96) & (ch_rc < 512)
    dsk = np.where(vch, dflat[np.where(vch, ch_rc, 0)], 0.0)
    shared["s5_pcm"] = f(pcm)
    shared["s5_bcm"] = f(bcm)
    shared["s5_psm"] = f(psm)
    shared["s5_bsm"] = f(bsm)
    shared["s5_csm"] = f(csm)
    shared["s5_d"] = f(dsk)
    shared["ident"] = np.eye(128, dtype=np.float32)
    rb = f(inp["rel_bias"])
    sl = np.arange(128)[:, None]
    zi = np.arange(1024)[None, :]
    shared["biasG"] = f(rb[_rel_bucket(sl - (zi - 384))].transpose(0, 2, 1))
    mm_ = np.arange(16)[:, None]
    tq = np.arange(512)[None, :]
    shared["biasM"] = f(rb[_rel_bucket(mm_ - 16 - tq)].transpose(0, 2, 1))
    shared["cvec"] = f(np.broadcast_to(rb[15][None, :], (128, 8)))
    def pad6rows(w):
        o = np.zeros((128, 6, w.shape[1]), np.float32)
        for c6 in range(6):
            w_ = min(96, 512 - 96 * c6)
            o[:w_, c6, :] = w[96 * c6:96 * c6 + w_]
        return o
    wg_ = f(inp["ssm_w_glu"][0])
    wgp = np.zeros((512, 6, 128), np.float32)
    for c6 in range(6):
        w_ = min(96, 512 - 96 * c6)
        wgp[:, c6, :w_] = wg_[:, 96 * c6:96 * c6 + w_]
    shared["w_glu"] = pad6rows(wgp.reshape(512, 768))
    shared["w_a"] = pad6rows(f(inp["w_branch_a"][0]))
    shared["w_b"] = wk(inp["w_branch_b"][0], 4)
    shared["w_o"] = wk(inp["w_out"][0], DC)
    shared["w_g"] = wk(win[:, 1736:3784], DC)
    shared["ff2_wg"] = wk(inp["ff2_w_gate"][0], DC)
    shared["ff2_wu"] = wk(inp["ff2_w_up"][0], DC)
    shared["ff2_wd"] = wk(inp["ff2_w_down"][0], FC)
    maps = []
    for c in range(NCORES):
        m = dict(shared)
        m["xT"] = xT[c * NSEQ:(c + 1) * NSEQ]
        maps.append(m)
    return maps


def kernel(**inputs):
    maps = prep_inputs(inputs)
    nc = build()
    res = run_bass_kernel_spmd(nc, maps, core_ids=list(range(NCORES)))
    outs = [r["outT"] for r in res.results]
    o = np.concatenate(outs, axis=0)
    out = o.transpose(0, 3, 2, 1).reshape(o.shape[0], SEQ, D)
    return np.ascontiguousarray(out.astype(np.float32))
```

```python
import math
from contextlib import ExitStack
import numpy as np
import ml_dtypes
import concourse.bass as bass
import concourse.mybir as mybir
from concourse.bass_utils import run_bass_kernel_spmd

F32 = mybir.dt.float32
BF16 = mybir.dt.bfloat16
AF = mybir.ActivationFunctionType
ALU = mybir.AluOpType
AX = mybir.AxisListType

NCORES = 8
D = 1024
DC = 8
SEQ = 2048
NSEQ = 2
NMETA = 16
DFF = 2816
FC = 22
EPS = 1e-6
TT = 512
NT = SEQ // TT
FT = 256
NFT = SEQ // FT


class Sync:
    def __init__(self, nc, es):
        self.nc = nc
        self.eng = {"pe": nc.tensor, "act": nc.scalar, "dve": nc.vector, "pool": nc.gpsimd, "sp": nc.sync}
        self.sem = {k: es.enter_context(nc.semaphore("s_" + k)) for k in self.eng}
        self.cnt = {k: 0 for k in self.eng}
        self.dsem = {}
        self.dcnt = {}
        self.es = es
        self.seen = {k: {} for k in self.eng}
        self.lastw = {}
        self.readers = {}
        self.ninst = 0

    NPOOL = {"sp": 12, "pool": 12, "act": 8}

    def dma_sem(self, q):
        if q not in self.dsem:
            self.dsem[q] = [self.es.enter_context(self.nc.semaphore("d_%s%d" % (q, i))) for i in range(self.NPOOL[q])]
            self.dcnt[q] = [0] * self.NPOOL[q]
            self.drr = getattr(self, "drr", {})
            self.drr[q] = 0
        i = self.drr[q]
        self.drr[q] = (i + 1) % self.NPOOL[q]
        return i

    def _wait(self, e, reads, writes):
        need = {}
        for k in reads:
            lw = self.lastw.get(k)
            if lw is not None:
                need[lw[0]] = max(need.get(lw[0], (0, None))[0], lw[1]), lw[2]
        for k in writes:
            lw = self.lastw.get(k)
            if lw is not None:
                need[lw[0]] = max(need.get(lw[0], (0, None))[0], lw[1]), lw[2]
            for r in self.readers.get(k, ()):
                need[r[0]] = max(need.get(r[0], (0, None))[0], r[1]), r[2]
        E = self.eng[e]
        for semid, (val, semobj) in need.items():
            if semid == "e_" + e and e == "pe":
                continue
            if self.seen[e].get(semid, 0) >= val:
                continue
            E.wait_ge(semobj, val)
            self.seen[e][semid] = val

    def _record(self, rec, reads, writes):
        for k in reads:
            self.readers.setdefault(k, []).append(rec)
        for k in writes:
            self.lastw[k] = rec
            self.readers[k] = []

    def op(self, e, fn, reads=(), writes=()):
        self._wait(e, reads, writes)
        inst = fn(self.eng[e])
        self.cnt[e] += 1
        inst.then_inc(self.sem[e], 1)
        self.ninst += 1
        self._record(("e_" + e, self.cnt[e], self.sem[e]), reads, writes)

    def dma(self, q, semname, out, in_, reads=(), writes=(), **kw):
        self._wait(q, reads, writes)
        i = self.dma_sem(q)
        sem = self.dsem[q][i]
        semid = "d_%s%d" % (q, i)
        if self.dcnt[q][i] > 0 and self.seen[q].get(semid, 0) < self.dcnt[q][i]:
            self.eng[q].wait_ge(sem, self.dcnt[q][i])
            self.seen[q][semid] = self.dcnt[q][i]
        inst = self.eng[q].dma_start(out=out, in_=in_, **kw)
        self.dcnt[q][i] += 16
        inst.then_inc(sem, 16)
        self.ninst += 1
        self._record((semid, self.dcnt[q][i], sem), reads, writes)

    def drain(self, e):
        E = self.eng[e]
        for k in self.eng:
            if k != e and self.cnt[k] > 0:
                E.wait_ge(self.sem[k], self.cnt[k])
        for q, sems in self.dsem.items():
            for i, sm in enumerate(sems):
                if self.dcnt[q][i] > 0:
                    E.wait_ge(sm, self.dcnt[q][i])


def build(debug=None):
    nc = bass.Bass("TRN2", target_bir_lowering=False)
    es = ExitStack()
    with es:
        S = Sync(nc, es)

        def din(name, shape, dt=F32):
            return nc.dram_tensor(name, list(shape), dt, kind="ExternalInput").ap()

        def dscr(name, shape, dt=F32):
            return nc.dram_tensor(name, list(shape), dt, kind="Internal").ap()

        xT = din("xT", [NSEQ, 128, DC, SEQ])
        metaT = din("metaT", [128, DC, NMETA])
        gains = din("gains", [128, 48])
        ff1_wg = din("ff1_wg", [128, DC, DFF])
        ff1_wu = din("ff1_wu", [128, DC, DFF])
        ff1_wd = din("ff1_wd", [128, FC, D])
        outT = nc.dram_tensor("outT", [NSEQ, 128, DC, SEQ], F32, kind="ExternalOutput").ap()
        h1T = dscr("h1T", [NSEQ, 128, DC, SEQ])
        h1m = dscr("h1m", [128, DC, NMETA])

        def sb(stack, name, shape, dt=F32):
            return stack.enter_context(nc.sbuf_tensor(name, list(shape), dt))

        def ps(stack, name, shape, dt=F32):
            return stack.enter_context(nc.psum_tensor(name, list(shape), dt))

        gains_sb = sb(es, "gains_sb", [128, 48])
        ones_f = sb(es, "ones_f", [128, 128])
        S.dma("sp", "c", gains_sb[:, :], gains[:, :], writes=["gains"])
        S.op("dve", lambda e: e.memset(ones_f[:, :], 1.0), writes=["ones_f"])

        def rstd_from_sumsq(pss, rs, n, key_pss, key_rs, half=False):
            S.op("act", lambda e: e.activation(out=rs[:, :n], in_=pss[:, :n], func=AF.Sqrt,
                                               bias=eps_sb[:, (1 if half else 0):(2 if half else 1)],
                                               scale=(4.0 if half else 1.0) / D),
                 reads=[key_pss, "eps"], writes=[key_rs])
            S.op("dve", lambda e: e.reciprocal(out=rs[:, :n], in_=rs[:, :n]), reads=[key_rs], writes=[key_rs])

        eps_sb = sb(es, "eps_sb", [128, 2])
        S.op("dve", lambda e: e.memset(eps_sb[:, 0:1], EPS), writes=["eps"])
        S.op("dve", lambda e: e.memset(eps_sb[:, 1:2], 4.0 * EPS), writes=["eps"])

        def ffn_phase(tag, wg, wu, wd, gpre_col, gpost_col, tiles):
            with ExitStack() as st:
                wg_sb = sb(st, tag + "wg", [128, DC, DFF], BF16)
                wu_sb = sb(st, tag + "wu", [128, DC, DFF], BF16)
                wd_sb = sb(st, tag + "wd", [128, FC, D], BF16)
                xt = [sb(st, tag + "xt%d" % i, [128, DC, FT]) for i in range(2)]
                sq = sb(st, tag + "sq", [128, 2, FT])
                hn = sb(st, tag + "hn", [128, DC, FT], BF16)
                act = sb(st, tag + "act", [128, FC, FT], BF16)
                sg = [sb(st, tag + "sg%d" % i, [128, FT]) for i in range(2)]
                ysb = sb(st, tag + "y", [128, DC, FT])
                rs = sb(st, tag + "rs", [128, FT])
                psg = [ps(st, tag + "psg%d" % i, [128, FT]) for i in range(2)]
                psu = [ps(st, tag + "psu%d" % i, [128, FT]) for i in range(2)]
                psy = [ps(st, tag + "psy%d" % i, [128, FT]) for i in range(2)]
                pss = ps(st, tag + "pss", [128, FT])
                for k in range(DC):
                    S.dma("pool", "w", wg_sb[:, k, :], wg[:, k, :], writes=[(tag, "wg", k)], max_dma_last_dim=5632)
                    S.dma("pool", "w", wu_sb[:, k, :], wu[:, k, :], writes=[(tag, "wu", k)], max_dma_last_dim=5632)
                for j in range(FC):
                    S.dma("pool", "w", wd_sb[:, j, :], wd[:, j, :], writes=[(tag, "wd", j)], max_dma_last_dim=4096)

                def load(i):
                    src, dst, n, sr, dw = tiles[i]
                    S.dma("sp", "x", xt[i % 2][:, :, :n], src, reads=sr, writes=[(tag, "xt", i % 2)])

                load(0)
                for i, (src, dst, n, sr, dw) in enumerate(tiles):
                    if i + 1 < len(tiles):
                        load(i + 1)
                    x = xt[i % 2]
                    kx = (tag, "xt", i % 2)
                    for c in range(DC):
                        S.op("act", lambda e, c=c: e.activation(out=sq[:, c % 2, :n], in_=x[:, c, :n], func=AF.Square),
                             reads=[kx], writes=[(tag, "sq", c % 2)])
                        S.op("pe", lambda e, c=c: e.matmul(pss[:, :n], lhsT=ones_f[:, :], rhs=sq[:, c % 2, :n],
                                                          start=(c == 0), stop=(c == DC - 1)),
                             reads=[(tag, "sq", c % 2), "ones_f"], writes=[(tag, "pss")])
                    rstd_from_sumsq(pss, rs, n, (tag, "pss"), (tag, "rs"))
                    for c in range(DC):
                        S.op("dve", lambda e, c=c: e.scalar_tensor_tensor(
                            out=hn[:, c, :n], in0=x[:, c, :n], scalar=gains_sb[:, gpre_col + c:gpre_col + c + 1],
                            in1=rs[:, :n], op0=ALU.mult, op1=ALU.mult),
                            reads=[kx, (tag, "rs"), "gains"], writes=[(tag, "hn", c)])
                    for j in range(FC):
                        b = j % 2
                        for k in range(DC):
                            S.op("pe", lambda e, k=k, j=j, b=b: e.matmul(
                                psg[b][:, :n], lhsT=wg_sb[:, k, j * 128:(j + 1) * 128], rhs=hn[:, k, :n],
                                start=(k == 0), stop=(k == DC - 1)),
                                reads=[(tag, "wg", k), (tag, "hn", k)], writes=[(tag, "psg", b)])
                        for k in range(DC):
                            S.op("pe", lambda e, k=k, j=j, b=b: e.matmul(
                                psu[b][:, :n], lhsT=wu_sb[:, k, j * 128:(j + 1) * 128], rhs=hn[:, k, :n],
                                start=(k == 0), stop=(k == DC - 1)),
                                reads=[(tag, "wu", k), (tag, "hn", k)], writes=[(tag, "psu", b)])
                        S.op("act", lambda e, b=b: e.activation(out=sg[b][:, :n], in_=psg[b][:, :n], func=AF.Silu),
                             reads=[(tag, "psg", b)], writes=[(tag, "sg", b)])
                        S.op("dve", lambda e, b=b, j=j: e.tensor_tensor(out=act[:, j, :n], in0=sg[b][:, :n],
                                                                        in1=psu[b][:, :n], op=ALU.mult),
                             reads=[(tag, "sg", b), (tag, "psu", b)], writes=[(tag, "act", j)])
                    for c in range(DC):
                        b = c % 2
                        for j in range(FC):
                            S.op("pe", lambda e, c=c, j=j, b=b: e.matmul(
                                psy[b][:, :n], lhsT=wd_sb[:, j, c * 128:(c + 1) * 128], rhs=act[:, j, :n],
                                start=(j == 0), stop=(j == FC - 1)),
                                reads=[(tag, "wd", j), (tag, "act", j)], writes=[(tag, "psy", b)])
                        S.op("act", lambda e, c=c, b=b: e.activation(out=ysb[:, c, :n], in_=psy[b][:, :n], func=AF.Copy),
                             reads=[(tag, "psy", b)], writes=[(tag, "y", c)])
                        S.op("act", lambda e, c=c, b=b: e.activation(out=sq[:, c % 2, :n], in_=psy[b][:, :n], func=AF.Square),
                             reads=[(tag, "psy", b)], writes=[(tag, "sq", c % 2)])
                        S.op("pe", lambda e, c=c: e.matmul(pss[:, :n], lhsT=ones_f[:, :], rhs=sq[:, c % 2, :n],
                                                          start=(c == 0), stop=(c == DC - 1)),
                             reads=[(tag, "sq", c % 2), "ones_f"], writes=[(tag, "pss")])
                    rstd_from_sumsq(pss, rs, n, (tag, "pss"), (tag, "rs"), half=True)
                    for c in range(DC):
                        S.op("dve", lambda e, c=c: e.scalar_tensor_tensor(
                            out=ysb[:, c, :n], in0=ysb[:, c, :n], scalar=gains_sb[:, gpost_col + c:gpost_col + c + 1],
                            in1=rs[:, :n], op0=ALU.mult, op1=ALU.mult),
                            reads=[(tag, "y", c), (tag, "rs"), "gains"], writes=[(tag, "y", c)])
                        S.op("pool", lambda e, c=c: e.tensor_tensor(
                            out=ysb[:, c, :n], in0=ysb[:, c, :n], in1=x[:, c, :n], op=ALU.add),
                            reads=[(tag, "y", c), kx], writes=[(tag, "y", c)])
                    S.dma("sp", "o", dst, ysb[:, :, :n], reads=[(tag, "y", c) for c in range(DC)], writes=dw)

        def barrier():
            for e in S.eng:
                S.drain(e)

        S.barrier = barrier

        def mms(out, pairs, reads, wkey):
            n_ = len(pairs)
            for idx, (l, r) in enumerate(pairs):
                S.op("pe", lambda e, l=l, r=r, idx=idx: e.matmul(out, lhsT=l, rhs=r, start=(idx == 0),
                                                                 stop=(idx == n_ - 1)),
                     reads=reads, writes=[wkey])

        def prenorm(tag, x, kx, n, gcol, hn, sq, pss, rs):
            for c in range(DC):
                S.op("act", lambda e, c=c: e.activation(out=sq[:, c % 2, :n], in_=x[:, c, :n], func=AF.Square),
                     reads=[kx], writes=[(tag, "sq", c % 2)])
                S.op("pe", lambda e, c=c: e.matmul(pss[:, :n], lhsT=ones_f[:, :], rhs=sq[:, c % 2, :n],
                                                  start=(c == 0), stop=(c == DC - 1)),
                     reads=[(tag, "sq", c % 2), "ones_f"], writes=[(tag, "pss")])
            rstd_from_sumsq(pss, rs, n, (tag, "pss"), (tag, "rs"))
            for c in range(DC):
                S.op("dve", lambda e, c=c: e.scalar_tensor_tensor(
                    out=hn[:, c, :n], in0=x[:, c, :n], scalar=gains_sb[:, gcol + c:gcol + c + 1],
                    in1=rs[:, :n], op0=ALU.mult, op1=ALU.mult),
                    reads=[kx, (tag, "rs"), "gains"], writes=[(tag, "hn")])

        tilesA = [(metaT[:, :, :], h1m[:, :, :], NMETA, [], ["h1m"])]
        for s in range(NSEQ):
            for t in range(NFT):
                tilesA.append((xT[s, :, :, t * FT:(t + 1) * FT], h1T[s, :, :, t * FT:(t + 1) * FT], FT, [],
                               [("h1T", s, t * FT // TT)]))
        if debug == "A":
            tilesA = tilesA[:2]
        ffn_phase("A", ff1_wg, ff1_wu, ff1_wd, 0, 8, tilesA)
        barrier()

        LP = NMETA + SEQ
        w_inA = din("w_inA", [128, DC, 2048])
        w_inB = din("w_inB", [128, DC, 72])
        uT = dscr("uT", [NSEQ, 128, 6, LP], BF16)
        kiT = dscr("kiT", [NSEQ, 128, LP], BF16)
        kT = dscr("kT", [NSEQ, 128, LP], BF16)
        qiT = dscr("qiT", [NSEQ, 128, 4, SEQ], BF16)
        qT = dscr("qT", [NSEQ, 128, 4, SEQ], BF16)
        vwS = dscr("vwS", [NSEQ, LP, 72], F32)
        hnT = dscr("hnT", [NSEQ, 128, DC, SEQ], BF16)

        def phase_B():
            tag = "B"
            with ExitStack() as st:
                wA = sb(st, "BwA", [128, DC, 2048], BF16)
                wB = sb(st, "BwB", [128, DC, 72], BF16)
                for k in range(DC):
                    S.dma("pool", "w", wA[:, k, :], w_inA[:, k, :], writes=[("B", "wA")], max_dma_last_dim=4096)
                S.dma("pool", "w", wB[:, :, :], w_inB[:, :, :], writes=[("B", "wB")])
                xt = [sb(st, "Bxt%d" % i, [128, DC, TT]) for i in range(2)]
                hn = sb(st, "Bhn", [128, DC, TT], BF16)
                sq = sb(st, "Bsq", [128, 2, TT])
                rs = sb(st, "Brs", [128, TT])
                stage = sb(st, "Bstage", [128, 16, TT], BF16)
                vw = sb(st, "Bvw", [128, 4, 72])
                pss = ps(st, "Bpss", [128, TT])
                pp = [ps(st, "Bpp%d" % i, [128, TT]) for i in range(2)]
                pv = [ps(st, "Bpv%d" % i, [128, 72]) for i in range(2)]
                tiles = [("m", 0)] + [(s, t) for s in range(NSEQ) for t in range(NT)]

                def load(i):
                    s_, t_ = tiles[i]
                    if s_ == "m":
                        S.dma("sp", "x", xt[i % 2][:, :, :NMETA], h1m[:, :, :], reads=["h1m"], writes=[("B", "xt", i % 2)])
                    else:
                        S.dma("sp", "x", xt[i % 2][:, :, :], h1T[s_, :, :, t_ * TT:(t_ + 1) * TT],
                              reads=[("h1T", s_, t_)], writes=[("B", "xt", i % 2)])

                load(0)
                for i, (s_, t_) in enumerate(tiles):
                    if i + 1 < len(tiles):
                        load(i + 1)
                    n = NMETA if s_ == "m" else TT
                    x = xt[i % 2]
                    kx = ("B", "xt", i % 2)
                    prenorm("B", x, kx, n, 16, hn, sq, pss, rs)
                    if s_ != "m":
                        S.dma("act", "o", hnT[s_, :, :, t_ * TT:(t_ + 1) * TT], hn[:, :, :], reads=[("B", "hn")],
                              writes=[("hnT", s_, t_)])
                    for cc in range(16):
                        b_ = cc % 2
                        mms(pp[b_][:, :n], [(wA[:, k, cc * 128:(cc + 1) * 128], hn[:, k, :n]) for k in range(DC)],
                            [("B", "wA"), ("B", "hn")], ("B", "pp", b_))
                        sc_ = 0.125 if 10 <= cc < 14 else 1.0
                        S.op("act", lambda e, cc=cc, b_=b_, sc_=sc_: e.activation(
                            out=stage[:, cc, :n], in_=pp[b_][:, :n], func=AF.Copy, scale=sc_),
                            reads=[("B", "pp", b_)], writes=[("B", "stage")])
                    if s_ == "m":
                        for s2 in range(NSEQ):
                            S.dma("sp", "o", uT[s2, :, :, 0:NMETA], stage[:, 0:6, :NMETA], reads=[("B", "stage")],
                                  writes=[("uT", s2, "m")])
                            S.dma("sp", "o", kiT[s2, :, 0:NMETA], stage[:, 14, :NMETA], reads=[("B", "stage")],
                                  writes=[("kiT", s2, "m")])
                            S.dma("sp", "o", kT[s2, :, 0:NMETA], stage[:, 15, :NMETA], reads=[("B", "stage")],
                                  writes=[("kT", s2, "m")])
                    else:
                        t0 = t_ * TT
                        S.dma("sp", "o", uT[s_, :, :, NMETA + t0:NMETA + t0 + TT], stage[:, 0:6, :],
                              reads=[("B", "stage")], writes=[("uT", s_, t_)])
                        S.dma("sp", "o", qiT[s_, :, :, t0:t0 + TT], stage[:, 6:10, :], reads=[("B", "stage")],
                              writes=[("qiT", s_, t_)])
                        S.dma("sp", "o", qT[s_, :, :, t0:t0 + TT], stage[:, 10:14, :], reads=[("B", "stage")],
                              writes=[("qT", s_, t_)])
                        S.dma("sp", "o", kiT[s_, :, NMETA + t0:NMETA + t0 + TT], stage[:, 14, :],
                              reads=[("B", "stage")], writes=[("kiT", s_, t_)])
                        S.dma("sp", "o", kT[s_, :, NMETA + t0:NMETA + t0 + TT], stage[:, 15, :],
                              reads=[("B", "stage")], writes=[("kT", s_, t_)])
                    nb = max(1, n // 128)
                    rows = min(n, 128)
                    for blk in range(nb):
                        b_ = blk % 2
                        mms(pv[b_][:rows, :], [(hn[:, k, blk * 128:blk * 128 + rows], wB[:, k, :]) for k in range(DC)],
                            [("B", "wB"), ("B", "hn")], ("B", "pv", b_))
                        S.op("dve", lambda e, blk=blk, b_=b_: e.tensor_copy(out=vw[:rows, blk, :], in_=pv[b_][:rows, :]),
                             reads=[("B", "pv", b_)], writes=[("B", "vw")])
                    if s_ == "m":
                        for s2 in range(NSEQ):
                            S.dma("sp", "o", vwS[s2, 0:NMETA, :], vw[:NMETA, 0, :], reads=[("B", "vw")],
                                  writes=[("vwS", s2, "m")])
                    else:
                        S.dma("sp", "o", vwS[s_, NMETA + t0:NMETA + t0 + TT, :].rearrange("(b p) c -> p b c", p=128),
                              vw[:, :, :], reads=[("B", "vw")], writes=[("vwS", s_, t_)])

        if debug != "A":
            phase_B()
            barrier()

        ALLT = ["m"] + list(range(NT))

        if debug == "B":
            with ExitStack() as st:
                tmp = sb(st, "dbgtmp", [128, 6, LP], BF16)
                tmp2 = sb(st, "dbgtmp2", [128, 6, LP], F32)
                S.dma("sp", "x", tmp[:, :, :], uT[0, :, :, :], reads=[("uT", 0, t) for t in ALLT], writes=["dbgtmp"])
                S.op("dve", lambda e: e.tensor_copy(out=tmp2[:, :, :], in_=tmp[:, :, :]), reads=["dbgtmp"], writes=["dbgtmp2"])
                S.dma("sp", "o", outT[0, :, 0:6, 0:SEQ], tmp2[:, :, NMETA:LP], reads=["dbgtmp2"], writes=["out"])
                S.dma("sp", "x", tmp2[:, 0, 0:72 * 16].rearrange("p (b c) -> p b c", c=72),
                      vwS[0, NMETA:NMETA + 2048, :].rearrange("(b p) c -> p b c", p=128),
                      reads=[("vwS", 0, t) for t in ALLT] + ["out"], writes=["dbgtmp2"])
                S.dma("sp", "o", outT[1, :, 0, 0:72 * 16], tmp2[:, 0, 0:72 * 16], reads=["dbgtmp2"], writes=["out"])


        s5_pcm = din("s5_pcm", [128, 6, 3, 128])
        s5_bcm = din("s5_bcm", [128, 6, 2, 128])
        s5_psm = din("s5_psm", [128, 3, 16])
        s5_bsm = din("s5_bsm", [128, 16, 2, 32])
        s5_csm = din("s5_csm", [128, 16, 2, 32])
        s5_d = din("s5_d", [128, 6])
        ident_d = din("ident", [128, 128])
        yaT = dscr("yaT", [NSEQ, 128, 6, SEQ], BF16)
        NCH = LP // 16
        ident_f = sb(es, "ident_f", [128, 128])
        ident_b = sb(es, "ident_b", [128, 128], BF16)
        S.dma("sp", "c", ident_f[:, :], ident_d[:, :], writes=["ident_f"])
        S.op("dve", lambda e: e.tensor_copy(out=ident_b[:, :], in_=ident_f[:, :]), reads=["ident_f"], writes=["ident_b"])

        def phase_C():
            K_ = "Cprep"
            R_, W_ = [K_], [K_]

            def tt(eng, out, a, b_, op):
                S.op(eng, lambda e: e.tensor_tensor(out=out, in0=a, in1=b_, op=op), reads=R_, writes=W_)

            def tsc(eng, out, a, s1, op0, s2=None, op1=None):
                if op1 is None:
                    S.op(eng, lambda e: e.tensor_scalar(out=out, in0=a, scalar1=s1, scalar2=None, op0=op0), reads=R_, writes=W_)
                else:
                    S.op(eng, lambda e: e.tensor_scalar(out=out, in0=a, scalar1=s1, scalar2=s2, op0=op0, op1=op1),
                         reads=R_, writes=W_)

            def stt(out, a, sc_, b_, op0, op1):
                S.op("dve", lambda e: e.scalar_tensor_tensor(out=out, in0=a, scalar=sc_, in1=b_, op0=op0, op1=op1),
                     reads=R_, writes=W_)

            def actf(out, a, func, scale=1.0):
                S.op("act", lambda e: e.activation(out=out, in_=a, func=func, scale=scale), reads=R_, writes=W_)

            def cparams(st, nm, lr, li, ldt_, shp):
                T = lambda n_: sb(st, "C%s_%s" % (nm, n_), shp)
                dt, mag, th, sh, c, s_, t1, t2, t3 = [T(n_) for n_ in ("dt", "mag", "th", "sh", "c", "s", "t1", "t2", "t3")]
                are, aim, cre_, cim_ = [T(n_) for n_ in ("are", "aim", "cre", "cim")]
                A = lambda t_: t_[tuple(slice(None) for _ in shp)]
                actf(A(dt), ldt_, AF.Exp)
                tt("dve", A(t1), lr, A(dt), ALU.mult)
                actf(A(mag), A(t1), AF.Exp)
                tt("dve", A(th), li, A(dt), ALU.mult)
                actf(A(sh), A(th), AF.Sin, scale=1.0 / 32)
                actf(A(s_), A(th), AF.Sin, scale=1.0 / 16)
                tt("dve", A(t1), A(sh), A(sh), ALU.mult)
                tsc("dve", A(c), A(t1), -2.0, ALU.mult, 1.0, ALU.add)
                for _ in range(4):
                    tt("dve", A(t1), A(c), A(c), ALU.mult)
                    tt("dve", A(t2), A(s_), A(s_), ALU.mult)
                    tt("dve", A(t3), A(c), A(s_), ALU.mult)
                    tt("dve", A(c), A(t1), A(t2), ALU.subtract)
                    tsc("dve", A(s_), A(t3), 2.0, ALU.mult)
                tt("dve", A(are), A(mag), A(c), ALU.mult)
                tt("dve", A(aim), A(mag), A(s_), ALU.mult)
                tt("dve", A(t1), lr, lr, ALU.mult)
                tt("dve", A(t2), li, li, ALU.mult)
                tt("dve", A(t1), A(t1), A(t2), ALU.add)
                S.op("dve", lambda e: e.reciprocal(out=A(t1), in_=A(t1)), reads=R_, writes=W_)
                tsc("dve", A(t2), A(are), -1.0, ALU.add)
                tt("dve", A(t3), A(t2), lr, ALU.mult)
                tt("dve", A(c), A(aim), li, ALU.mult)
                tt("dve", A(t3), A(t3), A(c), ALU.add)
                tt("dve", A(cre_), A(t3), A(t1), ALU.mult)
                tt("dve", A(t3), A(aim), lr, ALU.mult)
                tt("dve", A(c), A(t2), li, ALU.mult)
                tt("dve", A(t3), A(t3), A(c), ALU.subtract)
                tt("dve", A(cim_), A(t3), A(t1), ALU.mult)
                return are, aim, cre_, cim_

            with ExitStack() as st:
                WS = sb(st, "C_WS", [128, 16, 6, 2, 128], BF16)
                WO = sb(st, "C_WO", [128, 16, 16, 2, 32], BF16)
                WK = sb(st, "C_WK", [128, 16, 6, 128], BF16)
                a16 = sb(st, "C_a16", [128, 2, 16])
                with ExitStack() as st2:
                    pc = sb(st2, "C_pc", [128, 6, 3, 128])
                    bc = sb(st2, "C_bc", [128, 6, 2, 128])
                    S.dma("sp", "c", pc[:, :, :, :], s5_pcm[:, :, :, :], writes=W_)
                    S.dma("sp", "c", bc[:, :, :, :], s5_bcm[:, :, :, :], writes=W_)
                    are, aim, cre_, cim_ = cparams(st2, "cm", pc[:, :, 0, :], pc[:, :, 1, :], pc[:, :, 2, :], [128, 6, 128])
                    wr = sb(st2, "C_wr", [128, 6, 128])
                    wi = sb(st2, "C_wi", [128, 6, 128])
                    u1 = sb(st2, "C_u1", [128, 6, 128])
                    u2 = sb(st2, "C_u2", [128, 6, 128])
                    F3 = (slice(None),) * 3

                    def cmul(orr, oi, xr, xi, yr, yi):
                        tt("dve", u1[F3], xr, yr, ALU.mult)
                        tt("pool", u2[F3], xi, yi, ALU.mult)
                        tt("dve", u1[F3], u1[F3], u2[F3], ALU.subtract)
                        tt("pool", u2[F3], xr, yi, ALU.mult)
                        tt("dve", oi, xi, yr, ALU.mult)
                        tt("dve", oi, oi, u2[F3], ALU.add)
                        S.op("dve", lambda e: e.tensor_copy(out=orr, in_=u1[F3]), reads=R_, writes=W_)

                    cmul(wr[F3], wi[F3], cre_[F3], cim_[F3], bc[:, :, 0, :], bc[:, :, 1, :])
                    for lag in range(16):
                        S.op("act", lambda e, lag=lag: e.activation(out=WS[:, lag, :, 0, :], in_=wr[F3], func=AF.Copy),
                             reads=R_, writes=W_)
                        S.op("act", lambda e, lag=lag: e.activation(out=WS[:, lag, :, 1, :], in_=wi[F3], func=AF.Copy),
                             reads=R_, writes=W_)
                        if lag < 15:
                            cmul(wr[F3], wi[F3], wr[F3], wi[F3], are[F3], aim[F3])
                    pm = sb(st2, "C_pm", [128, 3, 16])
                    bs = sb(st2, "C_bs", [128, 16, 2, 32])
                    cs = sb(st2, "C_cs", [128, 16, 2, 32])
                    dsb = sb(st2, "C_d", [128, 6])
                    S.dma("sp", "c", pm[:, :, :], s5_psm[:, :, :], writes=W_)
                    S.dma("sp", "c", bs[:, :, :, :], s5_bsm[:, :, :, :], writes=W_)
                    S.dma("sp", "c", cs[:, :, :, :], s5_csm[:, :, :, :], writes=W_)
                    S.dma("sp", "c", dsb[:, :], s5_d[:, :], writes=W_)
                    sre, sim, scr, sci = cparams(st2, "sm", pm[:, 0, :], pm[:, 1, :], pm[:, 2, :], [128, 16])
                    apr = sb(st2, "C_apr", [128, 17, 16])
                    api = sb(st2, "C_api", [128, 17, 16])
                    napi = sb(st2, "C_napi", [128, 17, 16])
                    v1 = sb(st2, "C_v1", [128, 16])
                    v2 = sb(st2, "C_v2", [128, 16])
                    S.op("dve", lambda e: e.memset(apr[:, 0, :], 1.0), reads=R_, writes=W_)
                    S.op("dve", lambda e: e.memset(api[:, 0, :], 0.0), reads=R_, writes=W_)
                    for k in range(1, 17):
                        tt("dve", v1[:, :], apr[:, k - 1, :], sre[:, :], ALU.mult)
                        tt("dve", v2[:, :], api[:, k - 1, :], sim[:, :], ALU.mult)
                        tt("dve", apr[:, k, :], v1[:, :], v2[:, :], ALU.subtract)
                        tt("dve", v1[:, :], apr[:, k - 1, :], sim[:, :], ALU.mult)
                        tt("dve", v2[:, :], api[:, k - 1, :], sre[:, :], ALU.mult)
                        tt("dve", api[:, k, :], v1[:, :], v2[:, :], ALU.add)
                    tsc("dve", napi[:, :, :], api[:, :, :], -1.0, ALU.mult)
                    napr = sb(st2, "C_napr", [128, 17, 16])
                    tsc("dve", napr[:, :, :], apr[:, :, :], -1.0, ALU.mult)
                    S.op("dve", lambda e: e.tensor_copy(out=a16[:, 0, :], in_=apr[:, 16, :]), reads=R_, writes=W_)
                    S.op("dve", lambda e: e.tensor_copy(out=a16[:, 1, :], in_=api[:, 16, :]), reads=R_, writes=W_)
                    nsci = sb(st2, "C_nsci", [128, 16])
                    tsc("dve", nsci[:, :], sci[:, :], -1.0, ALU.mult)
                    bb = sb(st2, "C_bb", [128, 16, 2, 32])
                    x1 = sb(st2, "C_x1", [128, 32])
                    for i in range(16):
                        tsc("dve", x1[:, :], bs[:, i, 0, :], scr[:, i:i + 1], ALU.mult)
                        stt(bb[:, i, 0, :], bs[:, i, 1, :], nsci[:, i:i + 1], x1[:, :], ALU.mult, ALU.add)
                        tsc("dve", x1[:, :], bs[:, i, 1, :], scr[:, i:i + 1], ALU.mult)
                        stt(bb[:, i, 1, :], bs[:, i, 0, :], sci[:, i:i + 1], x1[:, :], ALU.mult, ALU.add)
                    AB = sb(st2, "C_AB", [128, 16, 2, 32], BF16)
                    crb = sb(st2, "C_crb", [128, 16, 2, 32], BF16)
                    S.op("dve", lambda e: e.tensor_copy(out=crb[:, :, 0, :], in_=cs[:, :, 0, :]), reads=R_, writes=W_)
                    tsc("dve", crb[:, :, 1, :], cs[:, :, 1, :], -1.0, ALU.mult)
                    S.op("pool", lambda e: e.memset(WK[:, :, :, :], 0.0), reads=R_, writes=W_)
                    psK = ps(st2, "C_psK", [128, 192])
                    for lag in range(16):
                        for i in range(16):
                            tsc("dve", x1[:, :], bb[:, i, 0, :], apr[:, lag, i:i + 1], ALU.mult)
                            stt(AB[:, i, 0, :], bb[:, i, 1, :], napi[:, lag, i:i + 1], x1[:, :], ALU.mult, ALU.add)
                            tsc("dve", x1[:, :], bb[:, i, 1, :], apr[:, lag, i:i + 1], ALU.mult)
                            stt(AB[:, i, 1, :], bb[:, i, 0, :], api[:, lag, i:i + 1], x1[:, :], ALU.mult, ALU.add)
                            tsc("dve", x1[:, :], cs[:, i, 0, :], apr[:, lag + 1, i:i + 1], ALU.mult)
                            stt(WO[:, lag, i, 0, :], cs[:, i, 1, :], napi[:, lag + 1, i:i + 1], x1[:, :], ALU.mult, ALU.add)
                            tsc("dve", x1[:, :], cs[:, i, 0, :], napi[:, lag + 1, i:i + 1], ALU.mult)
                            stt(WO[:, lag, i, 1, :], cs[:, i, 1, :], napr[:, lag + 1, i:i + 1], x1[:, :], ALU.mult, ALU.add)
                        for i in range(16):
                            r0 = 32 * (i % 3)
                            cc_ = i // 3
                            S.op("pe", lambda e, i=i, r0=r0, cc_=cc_: e.matmul(
                                psK[r0:r0 + 32, cc_ * 32:(cc_ + 1) * 32], lhsT=AB[:, i, 0, :], rhs=crb[:, i, 0, :],
                                start=True, stop=False), reads=R_, writes=W_)
                            S.op("pe", lambda e, i=i, r0=r0, cc_=cc_: e.matmul(
                                psK[r0:r0 + 32, cc_ * 32:(cc_ + 1) * 32], lhsT=AB[:, i, 1, :], rhs=crb[:, i, 1, :],
                                start=False, stop=True), reads=R_, writes=W_)
                            S.op("act", lambda e, i=i, r0=r0, cc_=cc_, lag=lag: e.activation(
                                out=WK[r0:r0 + 32, lag, cc_, r0:r0 + 32], in_=psK[r0:r0 + 32, cc_ * 32:(cc_ + 1) * 32],
                                func=AF.Copy), reads=R_, writes=W_)
                    for cc_ in range(6):
                        stt(WK[:, 0, cc_, :], ident_f[:, :], dsb[:, cc_:cc_ + 1], WK[:, 0, cc_, :], ALU.mult, ALU.add)
                barrier()
                usb = sb(st, "C_u", [128, 6, LP], BF16)
                Ssb = sb(st, "C_S", [128, 16, 2, NCH])
                Xbf = sb(st, "C_Xbf", [128, 16, 2, NCH], BF16)
                ypre = sb(st, "C_ypre", [128, SEQ])
                g1 = sb(st, "C_g1", [128, SEQ])
                ya = sb(st, "C_ya", [128, 6, SEQ], BF16)
                w1 = sb(st, "C_w1", [128, 16])
                w2 = sb(st, "C_w2", [128, 16])
                w3 = sb(st, "C_w3", [128, 16])
                w4 = sb(st, "C_w4", [128, 16])
                psS = [ps(st, "C_psS%d" % i, [128, NCH]) for i in range(2)]
                psY = [ps(st, "C_psY%d" % i, [128, NCH]) for i in range(2)]
                for s_ in range(NSEQ):
                    S.dma("sp", "x", usb[:, :, :], uT[s_, :, :, :], reads=[("uT", s_, t) for t in ALLT], writes=["C_u"])
                    n_ = 0
                    for i in range(16):
                        r0 = 32 * (i % 3)
                        cc_ = i // 3
                        for ri in range(2):
                            b_ = n_ % 2
                            n_ += 1
                            mms(psS[b_][:, :], [(WS[r0:r0 + 32, 15 - j, cc_, ri, :], usb[r0:r0 + 32, cc_, j:LP:16])
                                                for j in range(16)], ["C_u", K_], ("C_psS", b_))
                            S.op("act", lambda e, i=i, ri=ri, b_=b_: e.activation(out=Ssb[:, i, ri, :], in_=psS[b_][:, :],
                                                                               func=AF.Copy),
                                 reads=[("C_psS", b_)], writes=["C_S"])
                    for c in range(1, NCH - 1):
                        S.op("dve", lambda e, c=c: e.tensor_tensor(out=w1[:, :], in0=Ssb[:, :, 0, c - 1], in1=a16[:, 0, :], op=ALU.mult),
                             reads=["C_S", K_], writes=["C_w1"])
                        S.op("dve", lambda e, c=c: e.tensor_tensor(out=w2[:, :], in0=Ssb[:, :, 1, c - 1], in1=a16[:, 1, :], op=ALU.mult),
                             reads=["C_S"], writes=["C_w2"])
                        S.op("pool", lambda e, c=c: e.tensor_tensor(out=w3[:, :], in0=Ssb[:, :, 1, c - 1], in1=a16[:, 0, :], op=ALU.mult),
                             reads=["C_S", K_], writes=["C_w3"])
                        S.op("pool", lambda e, c=c: e.tensor_tensor(out=w4[:, :], in0=Ssb[:, :, 0, c - 1], in1=a16[:, 1, :], op=ALU.mult),
                             reads=["C_S"], writes=["C_w4"])
                        S.op("dve", lambda e: e.tensor_tensor(out=w1[:, :], in0=w1[:, :], in1=w2[:, :], op=ALU.subtract),
                             reads=["C_w1", "C_w2"], writes=["C_w1"])
                        S.op("pool", lambda e: e.tensor_tensor(out=w3[:, :], in0=w3[:, :], in1=w4[:, :], op=ALU.add),
                             reads=["C_w3", "C_w4"], writes=["C_w3"])
                        S.op("dve", lambda e, c=c: e.tensor_tensor(out=Ssb[:, :, 0, c], in0=w1[:, :], in1=Ssb[:, :, 0, c], op=ALU.add),
                             reads=["C_w1", "C_w3", "C_S"], writes=["C_Sa"])
                        S.op("pool", lambda e, c=c: e.tensor_tensor(out=Ssb[:, :, 1, c], in0=w3[:, :], in1=Ssb[:, :, 1, c], op=ALU.add),
                             reads=["C_w3", "C_Sa", "C_S"], writes=["C_S"])
                    S.op("dve", lambda e: e.memset(Xbf[:, :, :, 0], 0.0), reads=["C_Xbf"], writes=["C_Xbf"])
                    S.op("dve", lambda e: e.tensor_copy(out=Xbf[:, :, :, 1:NCH], in_=Ssb[:, :, :, 0:NCH - 1]), reads=["C_S", "C_Xbf"], writes=["C_Xbf"])
                    n_ = 0
                    for cc_ in range(6):
                        tiles_cc = [i for i in range(3 * cc_, min(3 * cc_ + 3, 16))]
                        for tau in range(16):
                            b_ = n_ % 2
                            n_ += 1
                            pairs = [(WK[:, tau - j, cc_, :], usb[:, cc_, j:LP:16]) for j in range(tau + 1)]
                            mms_out = psY[b_]
                            for idx, (l, r_) in enumerate(pairs):
                                S.op("pe", lambda e, l=l, r_=r_, idx=idx: e.matmul(mms_out[:, :], lhsT=l, rhs=r_, start=(idx == 0), stop=False),
                                     reads=["C_u", K_], writes=[("C_psY", b_)])
                            for i in tiles_cc:
                                r0 = 32 * (i % 3)
                                for ri in range(2):
                                    last = (i == tiles_cc[-1] and ri == 1)
                                    S.op("pe", lambda e, i=i, ri=ri, r0=r0, last=last, tau=tau: e.matmul(
                                        mms_out[r0:r0 + 32, :], lhsT=WO[:, tau, i, ri, :], rhs=Xbf[:, i, ri, :], start=False, stop=last),
                                        reads=["C_Xbf", K_], writes=[("C_psY", b_)])
                            S.op("act", lambda e, tau=tau, b_=b_: e.activation(
                                out=ypre[:, tau:SEQ:16], in_=psY[b_][:, 1:NCH], func=AF.Copy),
                                reads=[("C_psY", b_)], writes=["C_ypre"])
                        S.op("act", lambda e: e.activation(out=g1[:, :], in_=ypre[:, :], func=AF.Square), reads=["C_ypre"], writes=["C_g1"])
                        S.op("dve", lambda e: e.tensor_scalar(out=g1[:, :], in0=g1[:, :], scalar1=0.0713548162726, scalar2=1.5957691216,
                                                              op0=ALU.mult, op1=ALU.add), reads=["C_g1"], writes=["C_g1"])
                        S.op("dve", lambda e: e.tensor_tensor(out=g1[:, :], in0=g1[:, :], in1=ypre[:, :], op=ALU.mult), reads=["C_g1", "C_ypre"], writes=["C_g1"])
                        S.op("act", lambda e: e.activation(out=g1[:, :], in_=g1[:, :], func=AF.Sigmoid), reads=["C_g1"], writes=["C_g1"])
                        S.op("dve", lambda e, cc_=cc_: e.tensor_tensor(out=ya[:, cc_, :], in0=g1[:, :], in1=ypre[:, :], op=ALU.mult),
                             reads=["C_g1", "C_ypre"], writes=["C_ya"])
                    S.dma("sp", "o", yaT[s_, :, :, :], ya[:, :, :], reads=["C_ya"], writes=[("yaT", s_)])

        if debug not in ("A", "B"):
            phase_C()
            barrier()

        if debug == "C":
            with ExitStack() as st:
                tmp = sb(st, "dbgtmp", [128, 6, SEQ], BF16)
                tmp2 = sb(st, "dbgtmp2", [128, 6, SEQ], F32)
                S.dma("sp", "x", tmp[:, :, :], yaT[0, :, :, :], reads=[("yaT", 0)], writes=["dbgtmp"])
                S.op("dve", lambda e: e.tensor_copy(out=tmp2[:, :, :], in_=tmp[:, :, :]), reads=["dbgtmp"], writes=["dbgtmp2"])
                S.dma("sp", "o", outT[0, :, 0:6, 0:SEQ], tmp2[:, :, :], reads=["dbgtmp2"], writes=["out"])

        biasG_d = din("biasG", [128, 8, 1024])
        biasM_d = din("biasM", [16, 8, 512])
        cvec_d = din("cvec", [128, 8])
        ybT = dscr("ybT", [NSEQ, 128, 4, SEQ], BF16)
        ones_b = sb(es, "ones_b", [128, 128], BF16)
        S.op("dve", lambda e: e.memset(ones_b[:, :], 1.0), writes=["ones_b"])
        NIT = 22
        TOPK = 256.0

        def phase_D():
            with ExitStack() as st:
                Gb = sb(st, "D_Gb", [128, 8, 1024], BF16)
                Mb = sb(st, "D_Mb", [16, 8, 512], BF16)
                cv = sb(st, "D_cv", [128, 8])
                S.dma("sp", "c", cv[:, :], cvec_d[:, :], writes=["D_cv"])
                with ExitStack() as st2:
                    Gf = sb(st2, "D_Gf", [128, 8, 1024])
                    Mf = sb(st2, "D_Mf", [16, 8, 512])
                    S.dma("sp", "c", Gf[:, :, :], biasG_d[:, :, :], writes=["D_Gf"])
                    S.dma("sp", "c", Mf[:, :, :], biasM_d[:, :, :], writes=["D_Mf"])
                    for h in range(8):
                        S.op("dve", lambda e, h=h: e.tensor_scalar(out=Gb[:, h, :], in0=Gf[:, h, :], scalar1=cv[:, h:h + 1],
                                                                   scalar2=None, op0=ALU.subtract),
                             reads=["D_Gf", "D_cv"], writes=["D_Gb"])
                        S.op("dve", lambda e, h=h: e.tensor_scalar(out=Mb[:, h, :], in0=Mf[:, h, :], scalar1=cv[:16, h:h + 1],
                                                                   scalar2=None, op0=ALU.subtract),
                             reads=["D_Mf", "D_cv"], writes=["D_Mb"])
                    barrier()
                qi = sb(st, "D_qi", [128, 4, SEQ], BF16)
                ki = sb(st, "D_ki", [128, LP], BF16)
                qq = sb(st, "D_q", [128, 4, SEQ], BF16)
                kk = sb(st, "D_k", [128, LP], BF16)
                vd = sb(st, "D_vd", [128, 17, 2, 128], BF16)
                wq = sb(st, "D_wq", [128, 16, 8])
                sc = [sb(st, "D_sc%d" % i, [128, 4, LP]) for i in range(2)]
                MA = [sb(st, "D_MA%d" % i, [128, 4, LP], BF16) for i in range(2)]
                junk = sb(st, "D_junk", [128, LP], BF16)
                Rb = [sb(st, "D_Rb%d" % i, [128, 512], BF16) for i in range(2)]
                dg = sb(st, "D_dg", [128, 8, 128], BF16)
                Pt = [sb(st, "D_Pt%d" % i, [128, 512], BF16) for i in range(3)]
                rd = sb(st, "D_rd", [128, 512])
                rds = sb(st, "D_rds", [128, 512])
                yb = sb(st, "D_yb", [128, 4, 512], BF16)
                lo = [sb(st, "D_lo%d" % i, [128, 4]) for i in range(2)]
                hi = sb(st, "D_hi", [128, 4])
                W0 = sb(st, "D_W0", [128, 4])
                Wk = sb(st, "D_Wk", [128, 4])
                mid = sb(st, "D_mid", [128, 4])
                cnt = sb(st, "D_cnt", [128, 4])
                stp = sb(st, "D_stp", [128, 4])
                pq = [ps(st, "D_pq%d" % i, [128, 512]) for i in range(2)]
                psc = ps(st, "D_psc", [128, 512])
                pL = [ps(st, "D_pL%d" % i, [128, 512]) for i in range(2)]
                pOD = [ps(st, "D_pOD%d" % i, [128, 512]) for i in range(2)]
                pSh = ps(st, "D_pSh", [128, 512])
                S.op("dve", lambda e: e.memset(vd[:, :, 0, 64:128], 1.0), writes=["D_vd1"])
                S.op("dve", lambda e: e.memset(vd[:, :, 1, 0:64], 1.0), writes=["D_vd1"])
                S.op("dve", lambda e: e.memset(rd[:, :], 1.0), writes=["D_rd"])

                def indexer(s_, Q):
                    u = Q % 2
                    for jl in range(4):
                        j = 4 * Q + jl
                        Nj = NMETA + 128 * (j + 1)
                        for h in range(8):
                            S.op("dve", lambda e, h=h, j=j: e.tensor_scalar(out=dg[:, h, :], in0=ident_f[:, :], scalar1=wq[:, j, h:h + 1],
                                                                          scalar2=None, op0=ALU.mult),
                                 reads=["ident_f", "D_wq"], writes=["D_dg"])
                        for c0 in range(0, Nj, 512):
                            cw = min(512, Nj - c0)

                            def qk(h):
                                hh, hp, b_ = h % 2, h // 2, h % 2
                                S.op("pe", lambda e: e.matmul(
                                    pq[b_][:, :cw], lhsT=qi[64 * hh:64 * hh + 64, hp, 128 * j:128 * j + 128],
                                    rhs=ki[64 * hh:64 * hh + 64, c0:c0 + cw], start=True, stop=True),
                                    reads=["D_qi", "D_ki"], writes=[("D_pq", b_)])
                                if h % 2 == 0:
                                    S.op("act", lambda e: e.activation(out=Rb[b_][:, :cw], in_=pq[b_][:, :cw], func=AF.Relu),
                                         reads=[("D_pq", b_)], writes=[("D_Rb", b_)])
                                else:
                                    S.op("dve", lambda e: e.tensor_scalar(out=Rb[b_][:, :cw], in0=pq[b_][:, :cw], scalar1=0.0,
                                                                          scalar2=None, op0=ALU.max),
                                         reads=[("D_pq", b_)], writes=[("D_Rb", b_)])

                            def dgm(h):
                                b_ = h % 2
                                S.op("pe", lambda e: e.matmul(psc[:, :cw], lhsT=dg[:, h, :], rhs=Rb[b_][:, :cw],
                                                              start=(h == 0), stop=(h == 7)),
                                     reads=[("D_Rb", b_), "D_dg"], writes=["D_psc"])

                            qk(0)
                            qk(1)
                            for h in range(8):
                                dgm(h)
                                if h + 2 < 8:
                                    qk(h + 2)
                            S.op("act", lambda e, jl=jl, c0=c0, cw=cw: e.activation(out=sc[u][:, jl, c0:c0 + cw], in_=psc[:, :cw], func=AF.Copy),
                                 reads=["D_psc"], writes=[("D_sc", u, jl)])
                        S.op("dve", lambda e, jl=jl, Nj=Nj: e.tensor_reduce(out=lo[u][:, jl:jl + 1], in_=sc[u][:, jl, :Nj], axis=AX.X, op=ALU.min),
                             reads=[("D_sc", u, jl)], writes=[("D_lo", u)])
                        S.op("dve", lambda e, jl=jl, Nj=Nj: e.tensor_reduce(out=hi[:, jl:jl + 1], in_=sc[u][:, jl, :Nj], axis=AX.X, op=ALU.max),
                             reads=[("D_sc", u, jl)], writes=["D_hi"])
                        S.op("dve", lambda e, jl=jl, Nj=Nj: e.memset(sc[u][0:64, jl, Nj - 64:Nj], -1e30),
                             reads=[("D_sc", u, jl), ("D_lo", u), "D_hi"], writes=[("D_sc", u, jl)])

                def bisect(Q):
                    u = Q % 2
                    L_ = lo[u]
                    S.op("dve", lambda e: e.tensor_tensor(out=W0[:, :], in0=hi[:, :], in1=L_[:, :], op=ALU.subtract),
                         reads=[("D_lo", u), "D_hi"], writes=["D_W0"])
                    for it in range(NIT):
                        S.op("dve", lambda e, it=it: e.tensor_scalar(out=Wk[:, :], in0=W0[:, :], scalar1=2.0 ** (-(it + 1)), scalar2=None,
                                                                    op0=ALU.mult), reads=["D_W0", "D_stp"], writes=["D_Wk"])
                        S.op("dve", lambda e: e.tensor_tensor(out=mid[:, :], in0=L_[:, :], in1=Wk[:, :], op=ALU.add),
                             reads=[("D_lo", u), "D_Wk"], writes=["D_mid"])
                        for jl in range(4):
                            Nj = NMETA + 128 * (4 * Q + jl + 1)
                            S.op("dve", lambda e, jl=jl, Nj=Nj: e.tensor_scalar(
                                out=junk[:, :Nj], in0=sc[u][:, jl, :Nj], scalar1=mid[:, jl:jl + 1], scalar2=0.0,
                                op0=ALU.is_ge, op1=ALU.add, accum_out=cnt[:, jl:jl + 1]),
                                reads=[("D_sc", u, jl), "D_mid"], writes=["D_junk", "D_cnt"])
                        S.op("dve", lambda e: e.tensor_scalar(out=stp[:, :], in0=cnt[:, :], scalar1=TOPK, scalar2=None, op0=ALU.is_ge),
                             reads=["D_cnt"], writes=["D_stp"])
                        S.op("dve", lambda e: e.tensor_tensor(out=stp[:, :], in0=stp[:, :], in1=Wk[:, :], op=ALU.mult),
                             reads=["D_stp", "D_Wk"], writes=["D_stp"])
                        S.op("dve", lambda e: e.tensor_tensor(out=L_[:, :], in0=L_[:, :], in1=stp[:, :], op=ALU.add),
                             reads=[("D_lo", u), "D_stp"], writes=[("D_lo", u)])
                    for jl in range(4):
                        Nj = NMETA + 128 * (4 * Q + jl + 1)
                        S.op("dve", lambda e, jl=jl, Nj=Nj: e.tensor_scalar(out=MA[u][:, jl, :Nj], in0=sc[u][:, jl, :Nj], scalar1=L_[:, jl:jl + 1],
                                                                          scalar2=-30000.0, op0=ALU.is_lt, op1=ALU.mult),
                             reads=[("D_sc", u, jl), ("D_lo", u)], writes=[("D_MA", u)])

                def attention(s_, Q):
                    u = Q % 2
                    nblk = 4 * Q + 5
                    tiles = []
                    for h in range(8):
                        for b in range(nblk):
                            tiles.append((h, b))
                    NTL = len(tiles)

                    def geo(b):
                        w = NMETA if b == 0 else 128
                        pc0 = 0 if b == 0 else NMETA + 128 * (b - 1)
                        jl0 = max(0, b - 1 - 4 * Q)
                        return w, pc0, jl0

                    def stageA(n):
                        h, b = tiles[n]
                        hh, hp = h % 2, h // 2
                        w, pc0, jl0 = geo(b)
                        c0 = jl0 * 128
                        near = (b == 0 and Q == 0) or (b >= 1 and b - 1 >= 4 * Q - 1)
                        lb, pb_ = n % 2, n % 3
                        S.op("pe", lambda e: e.matmul(
                            pL[lb][:w, c0:512], lhsT=kk[64 * hh:64 * hh + 64, pc0:pc0 + w],
                            rhs=qq[64 * hh:64 * hh + 64, hp, 512 * Q + c0:512 * Q + 512], start=True, stop=False),
                            reads=["D_k", "D_q"], writes=[("D_pL", lb)])
                        if near:
                            if b == 0:
                                S.op("pe", lambda e: e.matmul(pL[lb][:NMETA, c0:512], lhsT=ident_b[:NMETA, :NMETA],
                                                              rhs=Mb[:NMETA, h, c0:512], start=False, stop=False),
                                     reads=["D_Mb", "ident_b"], writes=[("D_pL", lb)])
                            else:
                                z0 = 512 * Q + c0 - 128 * (b - 1) + 384
                                S.op("pe", lambda e: e.matmul(pL[lb][:, c0:512], lhsT=ident_b[:, :],
                                                              rhs=Gb[:, h, z0:z0 + 512 - c0], start=False, stop=False),
                                     reads=["D_Gb", "ident_b"], writes=[("D_pL", lb)])
                        for jl in range(jl0, 4):
                            S.op("pe", lambda e, jl=jl: e.matmul(pL[lb][:w, jl * 128:(jl + 1) * 128], lhsT=MA[u][:, jl, pc0:pc0 + w],
                                                                 rhs=ident_b[:, :], start=False, stop=(jl == 3)),
                                 reads=[("D_MA", u), "ident_b"], writes=[("D_pL", lb)])
                        S.op("act", lambda e: e.activation(out=Pt[pb_][:w, c0:512], in_=pL[lb][:w, c0:512], func=AF.Exp),
                             reads=[("D_pL", lb)], writes=[("D_Pt", pb_)])

                    def stageB(n):
                        h, b = tiles[n]
                        hh, hp = h % 2, h // 2
                        w, pc0, jl0 = geo(b)
                        c0 = jl0 * 128
                        pb_ = n % 3
                        ob = h % 2
                        S.op("pe", lambda e: e.matmul(pOD[ob][:, c0:512], lhsT=vd[:w, b, hh, :], rhs=Pt[pb_][:w, c0:512],
                                                      start=(b == 0), stop=(b == nblk - 1)),
                             reads=[("D_Pt", pb_), "D_vd", "D_vd1"], writes=[("D_pOD", ob)])
                        if b == nblk - 1:
                            orow = slice(64 * hh, 64 * hh + 64)
                            drow = slice(64 * (1 - hh), 64 * (1 - hh) + 64)
                            S.op("dve", lambda e: e.reciprocal(out=rd[drow, :], in_=pOD[ob][drow, :]),
                                 reads=[("D_pOD", ob)], writes=["D_rd"])
                            S.op("pe", lambda e: e.matmul(pSh[orow, :], lhsT=ident_f[drow, drow], rhs=rd[drow, :], start=True, stop=True),
                                 reads=["D_rd", "ident_f"], writes=["D_pSh"])
                            S.op("act", lambda e: e.activation(out=rds[orow, :], in_=pSh[orow, :], func=AF.Copy),
                                 reads=["D_pSh"], writes=["D_rds"])
                            S.op("dve", lambda e: e.tensor_tensor(out=yb[orow, hp, :], in0=rds[orow, :], in1=pOD[ob][orow, :], op=ALU.mult),
                                 reads=["D_rds", ("D_pOD", ob)], writes=["D_yb"])

                    stageA(0)
                    stageA(1)
                    for n in range(NTL):
                        stageB(n)
                        if n + 2 < NTL:
                            stageA(n + 2)
                    S.dma("sp", "o", ybT[s_, :, :, 512 * Q:512 * Q + 512], yb[:, :, :], reads=["D_yb"], writes=[("ybT", s_, Q)])

                for s_ in range(NSEQ):
                    S.dma("sp", "x", qi[:, :, :], qiT[s_, :, :, :], reads=[("qiT", s_, t) for t in range(NT)], writes=["D_qi"])
                    S.dma("sp", "x", ki[:, :], kiT[s_, :, :], reads=[("kiT", s_, t) for t in ALLT], writes=["D_ki"])
                    S.dma("sp", "x", qq[:, :, :], qT[s_, :, :, :], reads=[("qT", s_, t) for t in range(NT)], writes=["D_q"])
                    S.dma("sp", "x", kk[:, :], kT[s_, :, :], reads=[("kT", s_, t) for t in ALLT], writes=["D_k"])
                    vr = [("vwS", s_, t) for t in ALLT]
                    for half in range(2):
                        S.dma("pool", "x", vd[:, 1:17, half, 64 * half:64 * half + 64],
                              vwS[s_, NMETA:LP, 0:64].rearrange("(b p) c -> p b c", p=128), reads=vr, writes=["D_vd"])
                        S.dma("pool", "x", vd[:NMETA, 0, half, 64 * half:64 * half + 64], vwS[s_, 0:NMETA, 0:64], reads=vr, writes=["D_vd"])
                    S.dma("sp", "x", wq[:, :, :], vwS[s_, NMETA:LP, 64:72].rearrange("(b p) c -> p b c", p=128), reads=vr, writes=["D_wq"])
                    indexer(s_, 0)
                    bisect(0)
                    for Q in range(4):
                        if Q + 1 < 4:
                            indexer(s_, Q + 1)
                            bisect(Q + 1)
                        attention(s_, Q)

        if debug not in ("A", "B", "C"):
            phase_D()
            barrier()

        if debug == "D":
            with ExitStack() as st:
                tmp = sb(st, "dbgtmp", [128, 4, SEQ], BF16)
                tmp2 = sb(st, "dbgtmp2", [128, 4, SEQ], F32)
                S.dma("sp", "x", tmp[:, :, :], ybT[0, :, :, :], reads=[("ybT", 0, t) for t in range(NT)], writes=["dbgtmp"])
                S.op("dve", lambda e: e.tensor_copy(out=tmp2[:, :, :], in_=tmp[:, :, :]), reads=["dbgtmp"], writes=["dbgtmp2"])
                S.dma("sp", "o", outT[0, :, 0:4, 0:SEQ], tmp2[:, :, :], reads=["dbgtmp2"], writes=["out"])

        w_glu_d = din("w_glu", [128, 6, 768])
        w_a_d = din("w_a", [128, 6, D])
        w_b_d = din("w_b", [128, 4, D])
        w_o_d = din("w_o", [128, DC, D])
        w_g_d = din("w_g", [128, DC, 2 * D])
        h2T = dscr("h2T", [NSEQ, 128, DC, SEQ])

        def phase_E():
            with ExitStack() as st:
                wglu = sb(st, "E_wglu", [128, 6, 768], BF16)
                wa = sb(st, "E_wa", [128, 6, D], BF16)
                wb = sb(st, "E_wb", [128, 4, D], BF16)
                wo = sb(st, "E_wo", [128, DC, D], BF16)
                wgt = sb(st, "E_wg", [128, DC, 2 * D], BF16)
                S.dma("pool", "w", wglu[:, :, :], w_glu_d[:, :, :], writes=["E_w"], max_dma_last_dim=3072)
                for k in range(6):
                    S.dma("pool", "w", wa[:, k, :], w_a_d[:, k, :], writes=["E_w"])
                for k in range(4):
                    S.dma("pool", "w", wb[:, k, :], w_b_d[:, k, :], writes=["E_w"])
                for k in range(DC):
                    S.dma("pool", "w", wo[:, k, :], w_o_d[:, k, :], writes=["E_w"])
                    S.dma("pool", "w", wgt[:, k, :], w_g_d[:, k, :], writes=["E_w"], max_dma_last_dim=4096)
                hn = sb(st, "E_hn", [128, DC, TT], BF16)
                ya = sb(st, "E_ya", [128, 6, TT], BF16)
                yg = sb(st, "E_yg", [128, 6, TT], BF16)
                ybt = sb(st, "E_yb", [128, 4, TT], BF16)
                h1t = sb(st, "E_h1", [128, DC, TT])
                sgl = sb(st, "E_sgl", [128, TT])
                ga = sb(st, "E_ga", [128, TT])
                gb = sb(st, "E_gb", [128, TT])
                t1 = sb(st, "E_t1", [128, TT])
                t2 = sb(st, "E_t2", [128, TT])
                mg = sb(st, "E_mg", [128, DC, TT], BF16)
                ysb = sb(st, "E_y", [128, DC, TT])
                sq = sb(st, "E_sq", [128, 2, TT])
                rs = sb(st, "E_rs", [128, TT])
                pga = ps(st, "E_pga", [128, TT])
                pgb = ps(st, "E_pgb", [128, TT])
                pa = ps(st, "E_pa", [128, TT])
                pb = ps(st, "E_pb", [128, TT])
                py = [ps(st, "E_py%d" % i, [128, TT]) for i in range(2)]
                pss = ps(st, "E_pss", [128, TT])
                for s_ in range(NSEQ):
                    for t_ in range(NT):
                        tsl = slice(t_ * TT, (t_ + 1) * TT)
                        S.dma("sp", "x", hn[:, :, :], hnT[s_, :, :, tsl], reads=[("hnT", s_, t_)], writes=["E_hn"])
                        S.dma("sp", "x", ya[:, :, :], yaT[s_, :, :, tsl], reads=[("yaT", s_)], writes=["E_ya"])
                        S.dma("sp", "x", ybt[:, :, :], ybT[s_, :, :, tsl], reads=[("ybT", s_, t_)], writes=["E_yb"])
                        S.dma("sp", "x", h1t[:, :, :], h1T[s_, :, :, tsl], reads=[("h1T", s_, t_)], writes=["E_h1"])
                        for oc in range(6):
                            b_ = oc % 2
                            mms(py[b_][:, :], [(wglu[:, k, oc * 128:(oc + 1) * 128], ya[:, k, :]) for k in range(6)],
                                ["E_w", "E_ya"], ("E_py", b_))
                            S.op("act", lambda e, b_=b_: e.activation(out=sgl[:, :], in_=py[b_][:, :], func=AF.Sigmoid),
                                 reads=[("E_py", b_)], writes=["E_sgl"])
                            S.op("dve", lambda e, oc=oc: e.tensor_tensor(out=yg[:, oc, :], in0=sgl[:, :], in1=ya[:, oc, :], op=ALU.mult),
                                 reads=["E_sgl", "E_ya"], writes=["E_yg"])
                        for dc in range(DC):
                            mms(pga[:, :], [(wgt[:, k, dc * 128:(dc + 1) * 128], hn[:, k, :]) for k in range(DC)], ["E_w", "E_hn"], "E_pga")
                            mms(pgb[:, :], [(wgt[:, k, D + dc * 128:D + (dc + 1) * 128], hn[:, k, :]) for k in range(DC)], ["E_w", "E_hn"], "E_pgb")
                            mms(pa[:, :], [(wa[:, k, dc * 128:(dc + 1) * 128], yg[:, k, :]) for k in range(6)], ["E_w", "E_yg"], "E_pa")
                            mms(pb[:, :], [(wb[:, k, dc * 128:(dc + 1) * 128], ybt[:, k, :]) for k in range(4)], ["E_w", "E_yb"], "E_pb")
                            S.op("act", lambda e: e.activation(out=ga[:, :], in_=pga[:, :], func=AF.Sigmoid), reads=["E_pga"], writes=["E_ga"])
                            S.op("act", lambda e: e.activation(out=gb[:, :], in_=pgb[:, :], func=AF.Sigmoid), reads=["E_pgb"], writes=["E_gb"])
                            S.op("dve", lambda e: e.tensor_tensor(out=t1[:, :], in0=ga[:, :], in1=pa[:, :], op=ALU.mult), reads=["E_ga", "E_pa"], writes=["E_t1"])
                            S.op("dve", lambda e: e.tensor_tensor(out=t2[:, :], in0=gb[:, :], in1=pb[:, :], op=ALU.mult), reads=["E_gb", "E_pb"], writes=["E_t2"])
                            S.op("pool", lambda e, dc=dc: e.tensor_tensor(out=mg[:, dc, :], in0=t1[:, :], in1=t2[:, :], op=ALU.add),
                                 reads=["E_t1", "E_t2"], writes=["E_mg"])
                        for c in range(DC):
                            b_ = c % 2
                            mms(py[b_][:, :], [(wo[:, k, c * 128:(c + 1) * 128], mg[:, k, :]) for k in range(DC)], ["E_w", "E_mg"], ("E_py", b_))
                            S.op("act", lambda e, c=c, b_=b_: e.activation(out=ysb[:, c, :], in_=py[b_][:, :], func=AF.Copy),
                                 reads=[("E_py", b_)], writes=[("E_y", c)])
                            S.op("act", lambda e, c=c, b_=b_: e.activation(out=sq[:, c % 2, :], in_=py[b_][:, :], func=AF.Square),
                                 reads=[("E_py", b_)], writes=[("E_sq", c % 2)])
                            S.op("pe", lambda e, c=c: e.matmul(pss[:, :], lhsT=ones_f[:, :], rhs=sq[:, c % 2, :], start=(c == 0), stop=(c == DC - 1)),
                                 reads=[("E_sq", c % 2), "ones_f"], writes=["E_pss"])
                        rstd_from_sumsq(pss, rs, TT, "E_pss", "E_rs")
                        for c in range(DC):
                            S.op("dve", lambda e, c=c: e.scalar_tensor_tensor(
                                out=ysb[:, c, :], in0=ysb[:, c, :], scalar=gains_sb[:, 24 + c:24 + c + 1], in1=rs[:, :],
                                op0=ALU.mult, op1=ALU.mult), reads=[("E_y", c), "E_rs", "gains"], writes=[("E_y", c)])
                            S.op("pool", lambda e, c=c: e.tensor_tensor(out=ysb[:, c, :], in0=ysb[:, c, :], in1=h1t[:, c, :], op=ALU.add),
                                 reads=[("E_y", c), "E_h1"], writes=[("E_y", c)])
                        S.dma("sp", "o", h2T[s_, :, :, tsl], ysb[:, :, :], reads=[("E_y", c) for c in range(DC)], writes=[("h2T", s_, t_)])

        if debug not in ("A", "B", "C", "D"):
            phase_E()
            barrier()
            ff2_wg = din("ff2_wg", [128, DC, DFF])
            ff2_wu = din("ff2_wu", [128, DC, DFF])
            ff2_wd = din("ff2_wd", [128, FC, D])
            tilesF = []
            for s in range(NSEQ):
                for t in range(NFT):
                    tilesF.append((h2T[s, :, :, t * FT:(t + 1) * FT], outT[s, :, :, t * FT:(t + 1) * FT], FT,
                                   [("h2T", s, t * FT // TT)], [("out", s, t)]))
            ffn_phase("F", ff2_wg, ff2_wu, ff2_wd, 32, 40, tilesF)

        if debug == "A":
            with ExitStack() as st:
                tmp = sb(st, "dbgtmp", [128, DC, FT])
                S.dma("sp", "x", tmp[:, :, :], h1T[0, :, :, 0:FT], reads=[("h1T", 0, 0)], writes=["dbgtmp"])
                S.dma("sp", "o", outT[0, :, :, 0:FT], tmp[:, :, :], reads=["dbgtmp"], writes=["out"])
                S.dma("sp", "x", tmp[:, :, :NMETA], h1m[:, :, :], reads=["h1m", "out"], writes=["dbgtmp"])
                S.dma("sp", "o", outT[1, :, :, 0:NMETA], tmp[:, :, :NMETA], reads=["dbgtmp"], writes=["out"])

        S.drain("sp")
        print("instructions:", S.ninst)
    return nc


def _rel_bucket(rel):
    half, me = 16, 8
    base = np.where(rel > 0, half, 0)
    n = np.abs(rel)
    nf = np.maximum(n, 1).astype(np.float32)
    large = me + (np.log(nf / me) / math.log(128 / me) * (half - me)).astype(np.int32)
    large = np.minimum(large, half - 1)
    return base + np.where(n < me, n, large)


def prep_inputs(inp):
    f = lambda a: np.ascontiguousarray(np.asarray(a, dtype=np.float32))
    x = f(inp["x"])
    B = x.shape[0]
    xT = np.ascontiguousarray(x.reshape(B, SEQ, DC, 128).transpose(0, 3, 2, 1))
    metaT = np.ascontiguousarray(f(inp["meta_tokens"]).reshape(NMETA, DC, 128).transpose(2, 1, 0))
    gl = [inp[k] for k in ("ff1_norm_pre", "ff1_norm_post", "mix_norm_pre", "mix_norm_post", "ff2_norm_pre",
                           "ff2_norm_post")]
    gains = np.ascontiguousarray(np.concatenate([f(g)[0].reshape(DC, 128).T for g in gl], axis=1))

    def wk(w, kc):
        w = f(w)
        return np.ascontiguousarray(w.reshape(kc, 128, w.shape[-1]).transpose(1, 0, 2))

    shared = {
        "metaT": metaT, "gains": gains,
        "ff1_wg": wk(inp["ff1_w_gate"][0], DC), "ff1_wu": wk(inp["ff1_w_up"][0], DC),
        "ff1_wd": wk(inp["ff1_w_down"][0], FC),
    }
    win = f(inp["w_in"][0])
    upad = np.zeros((D, 6, 128), np.float32)
    for c6 in range(6):
        w_ = min(96, 512 - 96 * c6)
        upad[:, c6, :w_] = win[:, 96 * c6:96 * c6 + w_]
    winA = np.concatenate([upad.reshape(D, 768), win[:, 512:1024], win[:, 1096:1608], win[:, 1024:1088], win[:, 1024:1088],
                           win[:, 1608:1672], win[:, 1608:1672]], axis=1)
    winB = np.concatenate([win[:, 1672:1736], win[:, 1088:1096]], axis=1)
    shared["w_inA"] = wk(winA, DC)
    shared["w_inB"] = wk(winB, DC)
    lre, lim, ldt = f(inp["ssm_lambda_re"][0]), f(inp["ssm_lambda_im"][0]), f(inp["ssm_log_dt"][0])
    bre, bim = f(inp["ssm_b_re"][0]), f(inp["ssm_b_im"][0])
    cre, cim = f(inp["ssm_c_re"][0]), f(inp["ssm_c_im"][0])
    r = np.arange(128)
    sidx = np.arange(128)
    cc = np.arange(6)
    i_rc = 3 * cc[None, :] + (r[:, None] // 32)
    val_rc = (r[:, None] < 96) & (i_rc < 16)
    i_rc = np.where(val_rc, i_rc, 0)
    g_rcs = 2 * i_rc[:, :, None] + (sidx[None, None, :] // 64)
    p_s = sidx % 64
    pcm = np.stack([lre[g_rcs, p_s[None, None, :]], lim[g_rcs, p_s[None, None, :]], ldt[g_rcs]], axis=2)
    glr = (r % 32) // 16
    m_r = r % 16
    msk = (glr[:, None, None] == (sidx[None, None, :] // 64)) & val_rc[:, :, None]
    bcm = np.stack([np.where(msk, bre[g_rcs, p_s[None, None, :], m_r[:, None, None]], 0.0),
                    np.where(msk, bim[g_rcs, p_s[None, None, :], m_r[:, None, None]], 0.0)], axis=2)
    ii = np.arange(16)
    g_si = 2 * ii[None, :] + (sidx[:, None] // 64)
    psm = np.stack([lre[g_si, p_s[:, None]], lim[g_si, p_s[:, None]], ldt[g_si]], axis=1)
    q = np.arange(32)
    mq = q % 16
    mskq = ((q[None, None, :] // 16) == (sidx[:, None, None] // 64))
    bsm = np.stack([np.where(mskq, bre[g_si[:, :, None], p_s[:, None, None], mq[None, None, :]], 0.0),
                    np.where(mskq, bim[g_si[:, :, None], p_s[:, None, None], mq[None, None, :]], 0.0)], axis=2)
    csm = np.stack([np.where(mskq, cre[g_si[:, :, None], mq[None, None, :], p_s[:, None, None]], 0.0),
                    np.where(mskq, cim[g_si[:, :, None], mq[None, None, :], p_s[:, None, None]], 0.0)], axis=2)
    dflat = f(inp["ssm_d"][0]).reshape(512)
    ch_rc = 96 * cc[None, :] + r[:, None]
    vch = (r[:, None] < 96) & (ch_rc < 512)
    dsk = np.where(vch, dflat[np.where(vch, ch_rc, 0)], 0.0)
    shared["s5_pcm"] = f(pcm)
    shared["s5_bcm"] = f(bcm)
    shared["s5_psm"] = f(psm)
    shared["s5_bsm"] = f(bsm)
    shared["s5_csm"] = f(csm)
    shared["s5_d"] = f(dsk)
    shared["ident"] = np.eye(128, dtype=np.float32)
    rb = f(inp["rel_bias"])
    sl = np.arange(128)[:, None]
    zi = np.arange(1024)[None, :]
    shared["biasG"] = f(rb[_rel_bucket(sl - (zi - 384))].transpose(0, 2, 1))
    mm_ = np.arange(16)[:, None]
    tq = np.arange(512)[None, :]
    shared["biasM"] = f(rb[_rel_bucket(mm_ - 16 - tq)].transpose(0, 2, 1))
    shared["cvec"] = f(np.broadcast_to(rb[15][None, :], (128, 8)))
    def pad6rows(w):
        o = np.zeros((128, 6, w.shape[1]), np.float32)
        for c6 in range(6):
            w_ = min(96, 512 - 96 * c6)
            o[:w_, c6, :] = w[96 * c6:96 * c6 + w_]
        return o
    wg_ = f(inp["ssm_w_glu"][0])
    wgp = np.zeros((512, 6, 128), np.float32)
    for c6 in range(6):
        w_ = min(96, 512 - 96 * c6)
        wgp[:, c6, :w_] = wg_[:, 96 * c6:96 * c6 + w_]
    shared["w_glu"] = pad6rows(wgp.reshape(512, 768))
    shared["w_a"] = pad6rows(f(inp["w_branch_a"][0]))
    shared["w_b"] = wk(inp["w_branch_b"][0], 4)
    shared["w_o"] = wk(inp["w_out"][0], DC)
    shared["w_g"] = wk(win[:, 1736:3784], DC)
    shared["ff2_wg"] = wk(inp["ff2_w_gate"][0], DC)
    shared["ff2_wu"] = wk(inp["ff2_w_up"][0], DC)
    shared["ff2_wd"] = wk(inp["ff2_w_down"][0], FC)
    maps = []
    for c in range(NCORES):
        m = dict(shared)
        m["xT"] = xT[c * NSEQ:(c + 1) * NSEQ]
        maps.append(m)
    return maps


def kernel(**inputs):
    maps = prep_inputs(inputs)
    nc = build()
    res = run_bass_kernel_spmd(nc, maps, core_ids=list(range(NCORES)))
    outs = [r["outT"] for r in res.results]
    o = np.concatenate(outs, axis=0)
    out = o.transpose(0, 3, 2, 1).reshape(o.shape[0], SEQ, D)
    return np.ascontiguousarray(out.astype(np.float32))
```

```python
import math
from contextlib import ExitStack
import numpy as np
import ml_dtypes
import concourse.bass as bass
import concourse.mybir as mybir
from concourse.bass_utils import run_bass_kernel_spmd

F32 = mybir.dt.float32
BF16 = mybir.dt.bfloat16
AF = mybir.ActivationFunctionType
ALU = mybir.AluOpType
AX = mybir.AxisListType

NCORES = 8
D = 1024
DC = 8
SEQ = 2048
NSEQ = 2
NMETA = 16
DFF = 2816
FC = 22
EPS = 1e-6
TT = 512
NT = SEQ // TT
FT = 256
NFT = SEQ // FT


class Sync:
    def __init__(self, nc, es):
        self.nc = nc
        self.eng = {"pe": nc.tensor, "act": nc.scalar, "dve": nc.vector, "pool": nc.gpsimd, "sp": nc.sync}
        self.sem = {k: es.enter_context(nc.semaphore("s_" + k)) for k in self.eng}
        self.cnt = {k: 0 for k in self.eng}
        self.dsem = {}
        self.dcnt = {}
        self.es = es
        self.seen = {k: {} for k in self.eng}
        self.lastw = {}
        self.readers = {}
        self.ninst = 0

    NPOOL = {"sp": 12, "pool": 12, "act": 8}

    def dma_sem(self, q):
        if q not in self.dsem:
            self.dsem[q] = [self.es.enter_context(self.nc.semaphore("d_%s%d" % (q, i))) for i in range(self.NPOOL[q])]
            self.dcnt[q] = [0] * self.NPOOL[q]
            self.drr = getattr(self, "drr", {})
            self.drr[q] = 0
        i = self.drr[q]
        self.drr[q] = (i + 1) % self.NPOOL[q]
        return i

    def _wait(self, e, reads, writes):
        need = {}
        for k in reads:
            lw = self.lastw.get(k)
            if lw is not None:
                need[lw[0]] = max(need.get(lw[0], (0, None))[0], lw[1]), lw[2]
        for k in writes:
            lw = self.lastw.get(k)
            if lw is not None:
                need[lw[0]] = max(need.get(lw[0], (0, None))[0], lw[1]), lw[2]
            for r in self.readers.get(k, ()):
                need[r[0]] = max(need.get(r[0], (0, None))[0], r[1]), r[2]
        E = self.eng[e]
        for semid, (val, semobj) in need.items():
            if semid == "e_" + e and e == "pe":
                continue
            if self.seen[e].get(semid, 0) >= val:
                continue
            E.wait_ge(semobj, val)
            self.seen[e][semid] = val

    def _record(self, rec, reads, writes):
        for k in reads:
            self.readers.setdefault(k, []).append(rec)
        for k in writes:
            self.lastw[k] = rec
            self.readers[k] = []

    def op(self, e, fn, reads=(), writes=()):
        self._wait(e, reads, writes)
        inst = fn(self.eng[e])
        self.cnt[e] += 1
        inst.then_inc(self.sem[e], 1)
        self.ninst += 1
        self._record(("e_" + e, self.cnt[e], self.sem[e]), reads, writes)

    def dma(self, q, semname, out, in_, reads=(), writes=(), **kw):
        self._wait(q, reads, writes)
        i = self.dma_sem(q)
        sem = self.dsem[q][i]
        semid = "d_%s%d" % (q, i)
        if self.dcnt[q][i] > 0 and self.seen[q].get(semid, 0) < self.dcnt[q][i]:
            self.eng[q].wait_ge(sem, self.dcnt[q][i])
            self.seen[q][semid] = self.dcnt[q][i]
        inst = self.eng[q].dma_start(out=out, in_=in_, **kw)
        self.dcnt[q][i] += 16
        inst.then_inc(sem, 16)
        self.ninst += 1
        self._record((semid, self.dcnt[q][i], sem), reads, writes)

    def drain(self, e):
        E = self.eng[e]
        for k in self.eng:
            if k != e and self.cnt[k] > 0:
                E.wait_ge(self.sem[k], self.cnt[k])
        for q, sems in self.dsem.items():
            for i, sm in enumerate(sems):
                if self.dcnt[q][i] > 0:
                    E.wait_ge(sm, self.dcnt[q][i])


def build(debug=None):
    nc = bass.Bass("TRN2", target_bir_lowering=False)
    es = ExitStack()
    with es:
        S = Sync(nc, es)

        def din(name, shape, dt=F32):
            return nc.dram_tensor(name, list(shape), dt, kind="ExternalInput").ap()

        def dscr(name, shape, dt=F32):
            return nc.dram_tensor(name, list(shape), dt, kind="Internal").ap()

        xT = din("xT", [NSEQ, 128, DC, SEQ])
        metaT = din("metaT", [128, DC, NMETA])
        gains = din("gains", [128, 48])
        ff1_wg = din("ff1_wg", [128, DC, DFF])
        ff1_wu = din("ff1_wu", [128, DC, DFF])
        ff1_wd = din("ff1_wd", [128, FC, D])
        outT = nc.dram_tensor("outT", [NSEQ, 128, DC, SEQ], F32, kind="ExternalOutput").ap()
        h1T = dscr("h1T", [NSEQ, 128, DC, SEQ])
        h1m = dscr("h1m", [128, DC, NMETA])

        def sb(stack, name, shape, dt=F32):
            return stack.enter_context(nc.sbuf_tensor(name, list(shape), dt))

        def ps(stack, name, shape, dt=F32):
            return stack.enter_context(nc.psum_tensor(name, list(shape), dt))

        gains_sb = sb(es, "gains_sb", [128, 48])
        ones_f = sb(es, "ones_f", [128, 128])
        S.dma("sp", "c", gains_sb[:, :], gains[:, :], writes=["gains"])
        S.op("dve", lambda e: e.memset(ones_f[:, :], 1.0), writes=["ones_f"])
        zeros_b = sb(es, "zeros_b", [128, 128], BF16)
        S.op("dve", lambda e: e.memset(zeros_b[:, :], 0.0), writes=["zeros_b"])

        def rstd_from_sumsq(pss, rs, n, key_pss, key_rs, half=False):
            S.op("act", lambda e: e.activation(out=rs[:, :n], in_=pss[:, :n], func=AF.Sqrt,
                                               bias=eps_sb[:, (1 if half else 0):(2 if half else 1)],
                                               scale=(4.0 if half else 1.0) / D),
                 reads=[key_pss, "eps"], writes=[key_rs])
            S.op("dve", lambda e: e.reciprocal(out=rs[:, :n], in_=rs[:, :n]), reads=[key_rs], writes=[key_rs])

        eps_sb = sb(es, "eps_sb", [128, 2])
        S.op("dve", lambda e: e.memset(eps_sb[:, 0:1], EPS), writes=["eps"])
        S.op("dve", lambda e: e.memset(eps_sb[:, 1:2], 4.0 * EPS), writes=["eps"])

        def ffn_phase(tag, wg, wu, wd, gpre_col, gpost_col, tiles):
            with ExitStack() as st:
                wg_sb = sb(st, tag + "wg", [128, DC, DFF], BF16)
                wu_sb = sb(st, tag + "wu", [128, DC, DFF], BF16)
                wd_sb = sb(st, tag + "wd", [128, FC, D], BF16)
                xt = [sb(st, tag + "xt%d" % i, [128, DC, FT]) for i in range(2)]
                sq = sb(st, tag + "sq", [128, 2, FT])
                hn = sb(st, tag + "hn", [128, DC, FT], BF16)
                act = sb(st, tag + "act", [128, FC, FT], BF16)
                sg = [sb(st, tag + "sg%d" % i, [128, FT]) for i in range(2)]
                ysb = sb(st, tag + "y", [128, DC, FT])
                rs = sb(st, tag + "rs", [128, FT])
                psg = [ps(st, tag + "psg%d" % i, [128, FT]) for i in range(2)]
                psu = [ps(st, tag + "psu%d" % i, [128, FT]) for i in range(2)]
                psy = [ps(st, tag + "psy%d" % i, [128, FT]) for i in range(2)]
                pss = ps(st, tag + "pss", [128, FT])
                for k in range(DC):
                    S.dma("pool", "w", wg_sb[:, k, :], wg[:, k, :], writes=[(tag, "wg", k)], max_dma_last_dim=5632)
                    S.dma("pool", "w", wu_sb[:, k, :], wu[:, k, :], writes=[(tag, "wu", k)], max_dma_last_dim=5632)
                for j in range(FC):
                    S.dma("pool", "w", wd_sb[:, j, :], wd[:, j, :], writes=[(tag, "wd", j)], max_dma_last_dim=4096)

                def load(i):
                    src, dst, n, sr, dw = tiles[i]
                    S.dma("sp", "x", xt[i % 2][:, :, :n], src, reads=sr, writes=[(tag, "xt", i % 2)])

                load(0)
                for i, (src, dst, n, sr, dw) in enumerate(tiles):
                    if i + 1 < len(tiles):
                        load(i + 1)
                    x = xt[i % 2]
                    kx = (tag, "xt", i % 2)
                    for c in range(DC):
                        S.op("act", lambda e, c=c: e.activation(out=sq[:, c % 2, :n], in_=x[:, c, :n], func=AF.Square),
                             reads=[kx], writes=[(tag, "sq", c % 2)])
                        S.op("pe", lambda e, c=c: e.matmul(pss[:, :n], lhsT=ones_f[:, :], rhs=sq[:, c % 2, :n],
                                                          start=(c == 0), stop=(c == DC - 1)),
                             reads=[(tag, "sq", c % 2), "ones_f"], writes=[(tag, "pss")])
                    rstd_from_sumsq(pss, rs, n, (tag, "pss"), (tag, "rs"))
                    for c in range(DC):
                        S.op("dve", lambda e, c=c: e.scalar_tensor_tensor(
                            out=hn[:, c, :n], in0=x[:, c, :n], scalar=gains_sb[:, gpre_col + c:gpre_col + c + 1],
                            in1=rs[:, :n], op0=ALU.mult, op1=ALU.mult),
                            reads=[kx, (tag, "rs"), "gains"], writes=[(tag, "hn", c)])
                    for j in range(FC):
                        b = j % 2
                        for k in range(DC):
                            S.op("pe", lambda e, k=k, j=j, b=b: e.matmul(
                                psg[b][:, :n], lhsT=wg_sb[:, k, j * 128:(j + 1) * 128], rhs=hn[:, k, :n],
                                start=(k == 0), stop=(k == DC - 1)),
                                reads=[(tag, "wg", k), (tag, "hn", k)], writes=[(tag, "psg", b)])
                        for k in range(DC):
                            S.op("pe", lambda e, k=k, j=j, b=b: e.matmul(
                                psu[b][:, :n], lhsT=wu_sb[:, k, j * 128:(j + 1) * 128], rhs=hn[:, k, :n],
                                start=(k == 0), stop=(k == DC - 1)),
                                reads=[(tag, "wu", k), (tag, "hn", k)], writes=[(tag, "psu", b)])
                        S.op("act", lambda e, b=b: e.activation(out=sg[b][:, :n], in_=psg[b][:, :n], func=AF.Silu),
                             reads=[(tag, "psg", b)], writes=[(tag, "sg", b)])
                        S.op("dve", lambda e, b=b, j=j: e.tensor_tensor(out=act[:, j, :n], in0=sg[b][:, :n],
                                                                        in1=psu[b][:, :n], op=ALU.mult),
                             reads=[(tag, "sg", b), (tag, "psu", b)], writes=[(tag, "act", j)])
                    for c in range(DC):
                        b = c % 2
                        for j in range(FC):
                            S.op("pe", lambda e, c=c, j=j, b=b: e.matmul(
                                psy[b][:, :n], lhsT=wd_sb[:, j, c * 128:(c + 1) * 128], rhs=act[:, j, :n],
                                start=(j == 0), stop=(j == FC - 1)),
                                reads=[(tag, "wd", j), (tag, "act", j)], writes=[(tag, "psy", b)])
                        S.op("act", lambda e, c=c, b=b: e.activation(out=ysb[:, c, :n], in_=psy[b][:, :n], func=AF.Copy),
                             reads=[(tag, "psy", b)], writes=[(tag, "y", c)])
                        S.op("act", lambda e, c=c, b=b: e.activation(out=sq[:, c % 2, :n], in_=psy[b][:, :n], func=AF.Square),
                             reads=[(tag, "psy", b)], writes=[(tag, "sq", c % 2)])
                        S.op("pe", lambda e, c=c: e.matmul(pss[:, :n], lhsT=ones_f[:, :], rhs=sq[:, c % 2, :n],
                                                          start=(c == 0), stop=(c == DC - 1)),
                             reads=[(tag, "sq", c % 2), "ones_f"], writes=[(tag, "pss")])
                    rstd_from_sumsq(pss, rs, n, (tag, "pss"), (tag, "rs"), half=True)
                    for c in range(DC):
                        S.op("dve", lambda e, c=c: e.scalar_tensor_tensor(
                            out=ysb[:, c, :n], in0=ysb[:, c, :n], scalar=gains_sb[:, gpost_col + c:gpost_col + c + 1],
                            in1=rs[:, :n], op0=ALU.mult, op1=ALU.mult),
                            reads=[(tag, "y", c), (tag, "rs"), "gains"], writes=[(tag, "y", c)])
                        S.op("pool", lambda e, c=c: e.tensor_tensor(
                            out=ysb[:, c, :n], in0=ysb[:, c, :n], in1=x[:, c, :n], op=ALU.add),
                            reads=[(tag, "y", c), kx], writes=[(tag, "y", c)])
                    S.dma("sp", "o", dst, ysb[:, :, :n], reads=[(tag, "y", c) for c in range(DC)], writes=dw)

        def barrier():
            for e in S.eng:
                S.drain(e)

        S.barrier = barrier

        def mms(out, pairs, reads, wkey):
            n_ = len(pairs)
            for idx, (l, r) in enumerate(pairs):
                S.op("pe", lambda e, l=l, r=r, idx=idx: e.matmul(out, lhsT=l, rhs=r, start=(idx == 0),
                                                                 stop=(idx == n_ - 1)),
                     reads=reads, writes=[wkey])

        def prenorm(tag, x, kx, n, gcol, hn, sq, pss, rs):
            for c in range(DC):
                S.op("act", lambda e, c=c: e.activation(out=sq[:, c % 2, :n], in_=x[:, c, :n], func=AF.Square),
                     reads=[kx], writes=[(tag, "sq", c % 2)])
                S.op("pe", lambda e, c=c: e.matmul(pss[:, :n], lhsT=ones_f[:, :], rhs=sq[:, c % 2, :n],
                                                  start=(c == 0), stop=(c == DC - 1)),
                     reads=[(tag, "sq", c % 2), "ones_f"], writes=[(tag, "pss")])
            rstd_from_sumsq(pss, rs, n, (tag, "pss"), (tag, "rs"))
            for c in range(DC):
                S.op("dve", lambda e, c=c: e.scalar_tensor_tensor(
                    out=hn[:, c, :n], in0=x[:, c, :n], scalar=gains_sb[:, gcol + c:gcol + c + 1],
                    in1=rs[:, :n], op0=ALU.mult, op1=ALU.mult),
                    reads=[kx, (tag, "rs"), "gains"], writes=[(tag, "hn")])

        tilesA = [(metaT[:, :, :], h1m[:, :, :], NMETA, [], ["h1m"])]
        for s in range(NSEQ):
            for t in range(NFT):
                tilesA.append((xT[s, :, :, t * FT:(t + 1) * FT], h1T[s, :, :, t * FT:(t + 1) * FT], FT, [],
                               [("h1T", s, t * FT // TT)]))
        if debug == "A":
            tilesA = tilesA[:2]
        ffn_phase("A", ff1_wg, ff1_wu, ff1_wd, 0, 8, tilesA)
        barrier()

        LP = NMETA + SEQ
        w_inA = din("w_inA", [128, DC, 2048])
        w_inB = din("w_inB", [128, DC, 72])
        uT = dscr("uT", [NSEQ, 128, 6, LP], BF16)
        kiT = dscr("kiT", [NSEQ, 128, LP], BF16)
        kT = dscr("kT", [NSEQ, 128, LP], BF16)
        qiT = dscr("qiT", [NSEQ, 128, 4, SEQ], BF16)
        qT = dscr("qT", [NSEQ, 128, 4, SEQ], BF16)
        vwS = dscr("vwS", [NSEQ, LP, 72], F32)
        hnT = dscr("hnT", [NSEQ, 128, DC, SEQ], BF16)

        def phase_B():
            tag = "B"
            with ExitStack() as st:
                wA = sb(st, "BwA", [128, DC, 2048], BF16)
                wB = sb(st, "BwB", [128, DC, 72], BF16)
                for k in range(DC):
                    S.dma("pool", "w", wA[:, k, :], w_inA[:, k, :], writes=[("B", "wA")], max_dma_last_dim=4096)
                S.dma("pool", "w", wB[:, :, :], w_inB[:, :, :], writes=[("B", "wB")])
                xt = [sb(st, "Bxt%d" % i, [128, DC, TT]) for i in range(2)]
                hn = sb(st, "Bhn", [128, DC, TT], BF16)
                sq = sb(st, "Bsq", [128, 2, TT])
                rs = sb(st, "Brs", [128, TT])
                stage = sb(st, "Bstage", [128, 16, TT], BF16)
                vw = sb(st, "Bvw", [128, 4, 72])
                pss = ps(st, "Bpss", [128, TT])
                pp = [ps(st, "Bpp%d" % i, [128, TT]) for i in range(2)]
                pv = [ps(st, "Bpv%d" % i, [128, 72]) for i in range(2)]
                tiles = [("m", 0)] + [(s, t) for s in range(NSEQ) for t in range(NT)]

                def load(i):
                    s_, t_ = tiles[i]
                    if s_ == "m":
                        S.dma("sp", "x", xt[i % 2][:, :, :NMETA], h1m[:, :, :], reads=["h1m"], writes=[("B", "xt", i % 2)])
                    else:
                        S.dma("sp", "x", xt[i % 2][:, :, :], h1T[s_, :, :, t_ * TT:(t_ + 1) * TT],
                              reads=[("h1T", s_, t_)], writes=[("B", "xt", i % 2)])

                load(0)
                for i, (s_, t_) in enumerate(tiles):
                    if i + 1 < len(tiles):
                        load(i + 1)
                    n = NMETA if s_ == "m" else TT
                    x = xt[i % 2]
                    kx = ("B", "xt", i % 2)
                    prenorm("B", x, kx, n, 16, hn, sq, pss, rs)
                    if s_ != "m":
                        S.dma("act", "o", hnT[s_, :, :, t_ * TT:(t_ + 1) * TT], hn[:, :, :], reads=[("B", "hn")],
                              writes=[("hnT", s_, t_)])
                    for cc in range(16):
                        b_ = cc % 2
                        mms(pp[b_][:, :n], [(wA[:, k, cc * 128:(cc + 1) * 128], hn[:, k, :n]) for k in range(DC)],
                            [("B", "wA"), ("B", "hn")], ("B", "pp", b_))
                        sc_ = 0.125 if 10 <= cc < 14 else 1.0
                        S.op("act", lambda e, cc=cc, b_=b_, sc_=sc_: e.activation(
                            out=stage[:, cc, :n], in_=pp[b_][:, :n], func=AF.Copy, scale=sc_),
                            reads=[("B", "pp", b_)], writes=[("B", "stage")])
                    if s_ == "m":
                        for s2 in range(NSEQ):
                            S.dma("sp", "o", uT[s2, :, :, 0:NMETA], stage[:, 0:6, :NMETA], reads=[("B", "stage")],
                                  writes=[("uT", s2, "m")])
                            S.dma("sp", "o", kiT[s2, :, 0:NMETA], stage[:, 14, :NMETA], reads=[("B", "stage")],
                                  writes=[("kiT", s2, "m")])
                            S.dma("sp", "o", kT[s2, :, 0:NMETA], stage[:, 15, :NMETA], reads=[("B", "stage")],
                                  writes=[("kT", s2, "m")])
                    else:
                        t0 = t_ * TT
                        S.dma("sp", "o", uT[s_, :, :, NMETA + t0:NMETA + t0 + TT], stage[:, 0:6, :],
                              reads=[("B", "stage")], writes=[("uT", s_, t_)])
                        S.dma("sp", "o", qiT[s_, :, :, t0:t0 + TT], stage[:, 6:10, :], reads=[("B", "stage")],
                              writes=[("qiT", s_, t_)])
                        S.dma("sp", "o", qT[s_, :, :, t0:t0 + TT], stage[:, 10:14, :], reads=[("B", "stage")],
                              writes=[("qT", s_, t_)])
                        S.dma("sp", "o", kiT[s_, :, NMETA + t0:NMETA + t0 + TT], stage[:, 14, :],
                              reads=[("B", "stage")], writes=[("kiT", s_, t_)])
                        S.dma("sp", "o", kT[s_, :, NMETA + t0:NMETA + t0 + TT], stage[:, 15, :],
                              reads=[("B", "stage")], writes=[("kT", s_, t_)])
                    nb = max(1, n // 128)
                    rows = min(n, 128)
                    for blk in range(nb):
                        b_ = blk % 2
                        mms(pv[b_][:rows, :], [(hn[:, k, blk * 128:blk * 128 + rows], wB[:, k, :]) for k in range(DC)],
                            [("B", "wB"), ("B", "hn")], ("B", "pv", b_))
                        S.op("dve", lambda e, blk=blk, b_=b_: e.tensor_copy(out=vw[:rows, blk, :], in_=pv[b_][:rows, :]),
                             reads=[("B", "pv", b_)], writes=[("B", "vw")])
                    if s_ == "m":
                        for s2 in range(NSEQ):
                            S.dma("sp", "o", vwS[s2, 0:NMETA, :], vw[:NMETA, 0, :], reads=[("B", "vw")],
                                  writes=[("vwS", s2, "m")])
                    else:
                        S.dma("sp", "o", vwS[s_, NMETA + t0:NMETA + t0 + TT, :].rearrange("(b p) c -> p b c", p=128),
                              vw[:, :, :], reads=[("B", "vw")], writes=[("vwS", s_, t_)])

        if debug != "A":
            phase_B()
            barrier()

        ALLT = ["m"] + list(range(NT))

        if debug == "B":
            with ExitStack() as st:
                tmp = sb(st, "dbgtmp", [128, 6, LP], BF16)
                tmp2 = sb(st, "dbgtmp2", [128, 6, LP], F32)
                S.dma("sp", "x", tmp[:, :, :], uT[0, :, :, :], reads=[("uT", 0, t) for t in ALLT], writes=["dbgtmp"])
                S.op("dve", lambda e: e.tensor_copy(out=tmp2[:, :, :], in_=tmp[:, :, :]), reads=["dbgtmp"], writes=["dbgtmp2"])
                S.dma("sp", "o", outT[0, :, 0:6, 0:SEQ], tmp2[:, :, NMETA:LP], reads=["dbgtmp2"], writes=["out"])
                S.dma("sp", "x", tmp2[:, 0, 0:72 * 16].rearrange("p (b c) -> p b c", c=72),
                      vwS[0, NMETA:NMETA + 2048, :].rearrange("(b p) c -> p b c", p=128),
                      reads=[("vwS", 0, t) for t in ALLT] + ["out"], writes=["dbgtmp2"])
                S.dma("sp", "o", outT[1, :, 0, 0:72 * 16], tmp2[:, 0, 0:72 * 16], reads=["dbgtmp2"], writes=["out"])


        s5_pcm = din("s5_pcm", [128, 6, 3, 128])
        s5_bcm = din("s5_bcm", [128, 6, 2, 128])
        s5_psm = din("s5_psm", [128, 3, 16])
        s5_bsm = din("s5_bsm", [128, 16, 2, 32])
        s5_csm = din("s5_csm", [128, 16, 2, 32])
        s5_d = din("s5_d", [128, 6])
        ident_d = din("ident", [128, 128])
        yaT = dscr("yaT", [NSEQ, 128, 6, SEQ], BF16)
        NCH = LP // 16
        ident_f = sb(es, "ident_f", [128, 128])
        ident_b = sb(es, "ident_b", [128, 128], BF16)
        S.dma("sp", "c", ident_f[:, :], ident_d[:, :], writes=["ident_f"])
        S.op("dve", lambda e: e.tensor_copy(out=ident_b[:, :], in_=ident_f[:, :]), reads=["ident_f"], writes=["ident_b"])

        def phase_C():
            K_ = "Cprep"
            R_, W_ = [K_], [K_]

            def tt(eng, out, a, b_, op):
                S.op(eng, lambda e: e.tensor_tensor(out=out, in0=a, in1=b_, op=op), reads=R_, writes=W_)

            def tsc(eng, out, a, s1, op0, s2=None, op1=None):
                if op1 is None:
                    S.op(eng, lambda e: e.tensor_scalar(out=out, in0=a, scalar1=s1, scalar2=None, op0=op0), reads=R_, writes=W_)
                else:
                    S.op(eng, lambda e: e.tensor_scalar(out=out, in0=a, scalar1=s1, scalar2=s2, op0=op0, op1=op1),
                         reads=R_, writes=W_)

            def stt(out, a, sc_, b_, op0, op1):
                S.op("dve", lambda e: e.scalar_tensor_tensor(out=out, in0=a, scalar=sc_, in1=b_, op0=op0, op1=op1),
                     reads=R_, writes=W_)

            def actf(out, a, func, scale=1.0):
                S.op("act", lambda e: e.activation(out=out, in_=a, func=func, scale=scale), reads=R_, writes=W_)

            def cparams(st, nm, lr, li, ldt_, shp):
                T = lambda n_: sb(st, "C%s_%s" % (nm, n_), shp)
                dt, mag, th, sh, c, s_, t1, t2, t3 = [T(n_) for n_ in ("dt", "mag", "th", "sh", "c", "s", "t1", "t2", "t3")]
                are, aim, cre_, cim_ = [T(n_) for n_ in ("are", "aim", "cre", "cim")]
                A = lambda t_: t_[tuple(slice(None) for _ in shp)]
                actf(A(dt), ldt_, AF.Exp)
                tt("dve", A(t1), lr, A(dt), ALU.mult)
                actf(A(mag), A(t1), AF.Exp)
                tt("dve", A(th), li, A(dt), ALU.mult)
                actf(A(sh), A(th), AF.Sin, scale=1.0 / 32)
                actf(A(s_), A(th), AF.Sin, scale=1.0 / 16)
                tt("dve", A(t1), A(sh), A(sh), ALU.mult)
                tsc("dve", A(c), A(t1), -2.0, ALU.mult, 1.0, ALU.add)
                for _ in range(4):
                    tt("dve", A(t1), A(c), A(c), ALU.mult)
                    tt("dve", A(t2), A(s_), A(s_), ALU.mult)
                    tt("dve", A(t3), A(c), A(s_), ALU.mult)
                    tt("dve", A(c), A(t1), A(t2), ALU.subtract)
                    tsc("dve", A(s_), A(t3), 2.0, ALU.mult)
                tt("dve", A(are), A(mag), A(c), ALU.mult)
                tt("dve", A(aim), A(mag), A(s_), ALU.mult)
                tt("dve", A(t1), lr, lr, ALU.mult)
                tt("dve", A(t2), li, li, ALU.mult)
                tt("dve", A(t1), A(t1), A(t2), ALU.add)
                S.op("dve", lambda e: e.reciprocal(out=A(t1), in_=A(t1)), reads=R_, writes=W_)
                tsc("dve", A(t2), A(are), -1.0, ALU.add)
                tt("dve", A(t3), A(t2), lr, ALU.mult)
                tt("dve", A(c), A(aim), li, ALU.mult)
                tt("dve", A(t3), A(t3), A(c), ALU.add)
                tt("dve", A(cre_), A(t3), A(t1), ALU.mult)
                tt("dve", A(t3), A(aim), lr, ALU.mult)
                tt("dve", A(c), A(t2), li, ALU.mult)
                tt("dve", A(t3), A(t3), A(c), ALU.subtract)
                tt("dve", A(cim_), A(t3), A(t1), ALU.mult)
                return are, aim, cre_, cim_

            with ExitStack() as st:
                WS = sb(st, "C_WS", [128, 16, 6, 2, 128], BF16)
                WO = sb(st, "C_WO", [128, 16, 16, 2, 32], BF16)
                WK = sb(st, "C_WK", [128, 16, 6, 128], BF16)
                a16 = sb(st, "C_a16", [128, 2, 16])
                with ExitStack() as st2:
                    pc = sb(st2, "C_pc", [128, 6, 3, 128])
                    bc = sb(st2, "C_bc", [128, 6, 2, 128])
                    S.dma("sp", "c", pc[:, :, :, :], s5_pcm[:, :, :, :], writes=W_)
                    S.dma("sp", "c", bc[:, :, :, :], s5_bcm[:, :, :, :], writes=W_)
                    are, aim, cre_, cim_ = cparams(st2, "cm", pc[:, :, 0, :], pc[:, :, 1, :], pc[:, :, 2, :], [128, 6, 128])
                    wr = sb(st2, "C_wr", [128, 6, 128])
                    wi = sb(st2, "C_wi", [128, 6, 128])
                    u1 = sb(st2, "C_u1", [128, 6, 128])
                    u2 = sb(st2, "C_u2", [128, 6, 128])
                    F3 = (slice(None),) * 3

                    def cmul(orr, oi, xr, xi, yr, yi):
                        tt("dve", u1[F3], xr, yr, ALU.mult)
                        tt("pool", u2[F3], xi, yi, ALU.mult)
                        tt("dve", u1[F3], u1[F3], u2[F3], ALU.subtract)
                        tt("pool", u2[F3], xr, yi, ALU.mult)
                        tt("dve", oi, xi, yr, ALU.mult)
                        tt("dve", oi, oi, u2[F3], ALU.add)
                        S.op("dve", lambda e: e.tensor_copy(out=orr, in_=u1[F3]), reads=R_, writes=W_)

                    cmul(wr[F3], wi[F3], cre_[F3], cim_[F3], bc[:, :, 0, :], bc[:, :, 1, :])
                    for lag in range(16):
                        S.op("act", lambda e, lag=lag: e.activation(out=WS[:, lag, :, 0, :], in_=wr[F3], func=AF.Copy),
                             reads=R_, writes=W_)
                        S.op("act", lambda e, lag=lag: e.activation(out=WS[:, lag, :, 1, :], in_=wi[F3], func=AF.Copy),
                             reads=R_, writes=W_)
                        if lag < 15:
                            cmul(wr[F3], wi[F3], wr[F3], wi[F3], are[F3], aim[F3])
                    pm = sb(st2, "C_pm", [128, 3, 16])
                    bs = sb(st2, "C_bs", [128, 16, 2, 32])
                    cs = sb(st2, "C_cs", [128, 16, 2, 32])
                    dsb = sb(st2, "C_d", [128, 6])
                    S.dma("sp", "c", pm[:, :, :], s5_psm[:, :, :], writes=W_)
                    S.dma("sp", "c", bs[:, :, :, :], s5_bsm[:, :, :, :], writes=W_)
                    S.dma("sp", "c", cs[:, :, :, :], s5_csm[:, :, :, :], writes=W_)
                    S.dma("sp", "c", dsb[:, :], s5_d[:, :], writes=W_)
                    sre, sim, scr, sci = cparams(st2, "sm", pm[:, 0, :], pm[:, 1, :], pm[:, 2, :], [128, 16])
                    apr = sb(st2, "C_apr", [128, 17, 16])
                    api = sb(st2, "C_api", [128, 17, 16])
                    napi = sb(st2, "C_napi", [128, 17, 16])
                    v1 = sb(st2, "C_v1", [128, 16])
                    v2 = sb(st2, "C_v2", [128, 16])
                    S.op("dve", lambda e: e.memset(apr[:, 0, :], 1.0), reads=R_, writes=W_)
                    S.op("dve", lambda e: e.memset(api[:, 0, :], 0.0), reads=R_, writes=W_)
                    for k in range(1, 17):
                        tt("dve", v1[:, :], apr[:, k - 1, :], sre[:, :], ALU.mult)
                        tt("dve", v2[:, :], api[:, k - 1, :], sim[:, :], ALU.mult)
                        tt("dve", apr[:, k, :], v1[:, :], v2[:, :], ALU.subtract)
                        tt("dve", v1[:, :], apr[:, k - 1, :], sim[:, :], ALU.mult)
                        tt("dve", v2[:, :], api[:, k - 1, :], sre[:, :], ALU.mult)
                        tt("dve", api[:, k, :], v1[:, :], v2[:, :], ALU.add)
                    tsc("dve", napi[:, :, :], api[:, :, :], -1.0, ALU.mult)
                    napr = sb(st2, "C_napr", [128, 17, 16])
                    tsc("dve", napr[:, :, :], apr[:, :, :], -1.0, ALU.mult)
                    S.op("dve", lambda e: e.tensor_copy(out=a16[:, 0, :], in_=apr[:, 16, :]), reads=R_, writes=W_)
                    S.op("dve", lambda e: e.tensor_copy(out=a16[:, 1, :], in_=api[:, 16, :]), reads=R_, writes=W_)
                    nsci = sb(st2, "C_nsci", [128, 16])
                    tsc("dve", nsci[:, :], sci[:, :], -1.0, ALU.mult)
                    bb = sb(st2, "C_bb", [128, 16, 2, 32])
                    x1 = sb(st2, "C_x1", [128, 32])
                    for i in range(16):
                        tsc("dve", x1[:, :], bs[:, i, 0, :], scr[:, i:i + 1], ALU.mult)
                        stt(bb[:, i, 0, :], bs[:, i, 1, :], nsci[:, i:i + 1], x1[:, :], ALU.mult, ALU.add)
                        tsc("dve", x1[:, :], bs[:, i, 1, :], scr[:, i:i + 1], ALU.mult)
                        stt(bb[:, i, 1, :], bs[:, i, 0, :], sci[:, i:i + 1], x1[:, :], ALU.mult, ALU.add)
                    AB = sb(st2, "C_AB", [128, 16, 2, 32], BF16)
                    crb = sb(st2, "C_crb", [128, 16, 2, 32], BF16)
                    S.op("dve", lambda e: e.tensor_copy(out=crb[:, :, 0, :], in_=cs[:, :, 0, :]), reads=R_, writes=W_)
                    tsc("dve", crb[:, :, 1, :], cs[:, :, 1, :], -1.0, ALU.mult)
                    S.op("pool", lambda e: e.memset(WK[:, :, :, :], 0.0), reads=R_, writes=W_)
                    psK = ps(st2, "C_psK", [128, 192])
                    for lag in range(16):
                        for i in range(16):
                            tsc("dve", x1[:, :], bb[:, i, 0, :], apr[:, lag, i:i + 1], ALU.mult)
                            stt(AB[:, i, 0, :], bb[:, i, 1, :], napi[:, lag, i:i + 1], x1[:, :], ALU.mult, ALU.add)
                            tsc("dve", x1[:, :], bb[:, i, 1, :], apr[:, lag, i:i + 1], ALU.mult)
                            stt(AB[:, i, 1, :], bb[:, i, 0, :], api[:, lag, i:i + 1], x1[:, :], ALU.mult, ALU.add)
                            tsc("dve", x1[:, :], cs[:, i, 0, :], apr[:, lag + 1, i:i + 1], ALU.mult)
                            stt(WO[:, lag, i, 0, :], cs[:, i, 1, :], napi[:, lag + 1, i:i + 1], x1[:, :], ALU.mult, ALU.add)
                            tsc("dve", x1[:, :], cs[:, i, 0, :], napi[:, lag + 1, i:i + 1], ALU.mult)
                            stt(WO[:, lag, i, 1, :], cs[:, i, 1, :], napr[:, lag + 1, i:i + 1], x1[:, :], ALU.mult, ALU.add)
                        for i in range(16):
                            r0 = 32 * (i % 3)
                            cc_ = i // 3
                            S.op("pe", lambda e, i=i, r0=r0, cc_=cc_: e.matmul(
                                psK[r0:r0 + 32, cc_ * 32:(cc_ + 1) * 32], lhsT=AB[:, i, 0, :], rhs=crb[:, i, 0, :],
                                start=True, stop=False), reads=R_, writes=W_)
                            S.op("pe", lambda e, i=i, r0=r0, cc_=cc_: e.matmul(
                                psK[r0:r0 + 32, cc_ * 32:(cc_ + 1) * 32], lhsT=AB[:, i, 1, :], rhs=crb[:, i, 1, :],
                                start=False, stop=True), reads=R_, writes=W_)
                            S.op("act", lambda e, i=i, r0=r0, cc_=cc_, lag=lag: e.activation(
                                out=WK[r0:r0 + 32, lag, cc_, r0:r0 + 32], in_=psK[r0:r0 + 32, cc_ * 32:(cc_ + 1) * 32],
                                func=AF.Copy), reads=R_, writes=W_)
                    for cc_ in range(6):
                        stt(WK[:, 0, cc_, :], ident_f[:, :], dsb[:, cc_:cc_ + 1], WK[:, 0, cc_, :], ALU.mult, ALU.add)
                barrier()
                usb = sb(st, "C_u", [128, 6, LP], BF16)
                Ssb = sb(st, "C_S", [128, 16, 2, NCH])
                Xbf = sb(st, "C_Xbf", [128, 16, 2, NCH], BF16)
                ypre = sb(st, "C_ypre", [128, SEQ])
                g1 = sb(st, "C_g1", [128, SEQ])
                ya = sb(st, "C_ya", [128, 6, SEQ], BF16)
                w1 = sb(st, "C_w1", [128, 16])
                w2 = sb(st, "C_w2", [128, 16])
                w3 = sb(st, "C_w3", [128, 16])
                w4 = sb(st, "C_w4", [128, 16])
                psS = [ps(st, "C_psS%d" % i, [128, NCH]) for i in range(2)]
                psY = [ps(st, "C_psY%d" % i, [128, NCH]) for i in range(2)]
                for s_ in range(NSEQ):
                    S.dma("sp", "x", usb[:, :, :], uT[s_, :, :, :], reads=[("uT", s_, t) for t in ALLT], writes=["C_u"])
                    n_ = 0
                    for i in range(16):
                        r0 = 32 * (i % 3)
                        cc_ = i // 3
                        for ri in range(2):
                            b_ = n_ % 2
                            n_ += 1
                            mms(psS[b_][:, :], [(WS[r0:r0 + 32, 15 - j, cc_, ri, :], usb[r0:r0 + 32, cc_, j:LP:16])
                                                for j in range(16)], ["C_u", K_], ("C_psS", b_))
                            S.op("act", lambda e, i=i, ri=ri, b_=b_: e.activation(out=Ssb[:, i, ri, :], in_=psS[b_][:, :],
                                                                               func=AF.Copy),
                                 reads=[("C_psS", b_)], writes=["C_S"])
                    for c in range(1, NCH - 1):
                        S.op("dve", lambda e, c=c: e.tensor_tensor(out=w1[:, :], in0=Ssb[:, :, 0, c - 1], in1=a16[:, 0, :], op=ALU.mult),
                             reads=["C_S", K_], writes=["C_w1"])
                        S.op("dve", lambda e, c=c: e.tensor_tensor(out=w2[:, :], in0=Ssb[:, :, 1, c - 1], in1=a16[:, 1, :], op=ALU.mult),
                             reads=["C_S"], writes=["C_w2"])
                        S.op("pool", lambda e, c=c: e.tensor_tensor(out=w3[:, :], in0=Ssb[:, :, 1, c - 1], in1=a16[:, 0, :], op=ALU.mult),
                             reads=["C_S", K_], writes=["C_w3"])
                        S.op("pool", lambda e, c=c: e.tensor_tensor(out=w4[:, :], in0=Ssb[:, :, 0, c - 1], in1=a16[:, 1, :], op=ALU.mult),
                             reads=["C_S"], writes=["C_w4"])
                        S.op("dve", lambda e: e.tensor_tensor(out=w1[:, :], in0=w1[:, :], in1=w2[:, :], op=ALU.subtract),
                             reads=["C_w1", "C_w2"], writes=["C_w1"])
                        S.op("pool", lambda e: e.tensor_tensor(out=w3[:, :], in0=w3[:, :], in1=w4[:, :], op=ALU.add),
                             reads=["C_w3", "C_w4"], writes=["C_w3"])
                        S.op("dve", lambda e, c=c: e.tensor_tensor(out=Ssb[:, :, 0, c], in0=w1[:, :], in1=Ssb[:, :, 0, c], op=ALU.add),
                             reads=["C_w1", "C_w3", "C_S"], writes=["C_Sa"])
                        S.op("pool", lambda e, c=c: e.tensor_tensor(out=Ssb[:, :, 1, c], in0=w3[:, :], in1=Ssb[:, :, 1, c], op=ALU.add),
                             reads=["C_w3", "C_Sa", "C_S"], writes=["C_S"])
                    S.op("dve", lambda e: e.memset(Xbf[:, :, :, 0], 0.0), reads=["C_Xbf"], writes=["C_Xbf"])
                    S.op("dve", lambda e: e.tensor_copy(out=Xbf[:, :, :, 1:NCH], in_=Ssb[:, :, :, 0:NCH - 1]), reads=["C_S", "C_Xbf"], writes=["C_Xbf"])
                    n_ = 0
                    for cc_ in range(6):
                        tiles_cc = [i for i in range(3 * cc_, min(3 * cc_ + 3, 16))]
                        for tau in range(16):
                            b_ = n_ % 2
                            n_ += 1
                            pairs = [(WK[:, tau - j, cc_, :], usb[:, cc_, j:LP:16]) for j in range(tau + 1)]
                            if tau == 0:
                                pairs.append((zeros_b[:, :], usb[:, cc_, 0:LP:16]))
                            mms_out = psY[b_]
                            np_ = len(pairs)

                            def intra(idx):
                                l, r_ = pairs[idx]
                                S.op("pe", lambda e: e.matmul(mms_out[:, :], lhsT=l, rhs=r_, start=(idx == 0), stop=(idx == np_ - 1)),
                                     reads=["C_u", K_, "zeros_b"], writes=[("C_psY", b_)])
                            intra(0)
                            for i in tiles_cc:
                                r0 = 32 * (i % 3)
                                for ri in range(2):
                                    S.op("pe", lambda e, i=i, ri=ri, r0=r0, tau=tau: e.matmul(
                                        mms_out[r0:r0 + 32, :], lhsT=WO[:, tau, i, ri, :], rhs=Xbf[:, i, ri, :], start=False, stop=False),
                                        reads=["C_Xbf", K_], writes=[("C_psY", b_)])
                            for idx in range(1, np_):
                                intra(idx)
                            S.op("act", lambda e, tau=tau, b_=b_: e.activation(
                                out=ypre[:, tau:SEQ:16], in_=psY[b_][:, 1:NCH], func=AF.Copy),
                                reads=[("C_psY", b_)], writes=["C_ypre"])
                        S.op("act", lambda e: e.activation(out=g1[:, :], in_=ypre[:, :], func=AF.Square), reads=["C_ypre"], writes=["C_g1"])
                        S.op("dve", lambda e: e.tensor_scalar(out=g1[:, :], in0=g1[:, :], scalar1=0.0713548162726, scalar2=1.5957691216,
                                                              op0=ALU.mult, op1=ALU.add), reads=["C_g1"], writes=["C_g1"])
                        S.op("dve", lambda e: e.tensor_tensor(out=g1[:, :], in0=g1[:, :], in1=ypre[:, :], op=ALU.mult), reads=["C_g1", "C_ypre"], writes=["C_g1"])
                        S.op("act", lambda e: e.activation(out=g1[:, :], in_=g1[:, :], func=AF.Sigmoid), reads=["C_g1"], writes=["C_g1"])
                        S.op("dve", lambda e, cc_=cc_: e.tensor_tensor(out=ya[:, cc_, :], in0=g1[:, :], in1=ypre[:, :], op=ALU.mult),
                             reads=["C_g1", "C_ypre"], writes=["C_ya"])
                    S.dma("sp", "o", yaT[s_, :, :, :], ya[:, :, :], reads=["C_ya"], writes=[("yaT", s_)])

        if debug not in ("A", "B"):
            phase_C()
            barrier()

        if debug == "C":
            with ExitStack() as st:
                tmp = sb(st, "dbgtmp", [128, 6, SEQ], BF16)
                tmp2 = sb(st, "dbgtmp2", [128, 6, SEQ], F32)
                S.dma("sp", "x", tmp[:, :, :], yaT[0, :, :, :], reads=[("yaT", 0)], writes=["dbgtmp"])
                S.op("dve", lambda e: e.tensor_copy(out=tmp2[:, :, :], in_=tmp[:, :, :]), reads=["dbgtmp"], writes=["dbgtmp2"])
                S.dma("sp", "o", outT[0, :, 0:6, 0:SEQ], tmp2[:, :, :], reads=["dbgtmp2"], writes=["out"])

        biasG_d = din("biasG", [128, 8, 1024])
        biasM_d = din("biasM", [16, 8, 512])
        cvec_d = din("cvec", [128, 8])
        ybT = dscr("ybT", [NSEQ, 128, 4, SEQ], BF16)
        ones_b = sb(es, "ones_b", [128, 128], BF16)
        S.op("dve", lambda e: e.memset(ones_b[:, :], 1.0), writes=["ones_b"])
        NIT = 22
        TOPK = 256.0

        def phase_D():
            with ExitStack() as st:
                Gb = sb(st, "D_Gb", [128, 8, 1024], BF16)
                Mb = sb(st, "D_Mb", [16, 8, 512], BF16)
                cv = sb(st, "D_cv", [128, 8])
                S.dma("sp", "c", cv[:, :], cvec_d[:, :], writes=["D_cv"])
                with ExitStack() as st2:
                    Gf = sb(st2, "D_Gf", [128, 8, 1024])
                    Mf = sb(st2, "D_Mf", [16, 8, 512])
                    S.dma("sp", "c", Gf[:, :, :], biasG_d[:, :, :], writes=["D_Gf"])
                    S.dma("sp", "c", Mf[:, :, :], biasM_d[:, :, :], writes=["D_Mf"])
                    for h in range(8):
                        S.op("dve", lambda e, h=h: e.tensor_scalar(out=Gb[:, h, :], in0=Gf[:, h, :], scalar1=cv[:, h:h + 1],
                                                                   scalar2=None, op0=ALU.subtract),
                             reads=["D_Gf", "D_cv"], writes=["D_Gb"])
                        S.op("dve", lambda e, h=h: e.tensor_scalar(out=Mb[:, h, :], in0=Mf[:, h, :], scalar1=cv[:16, h:h + 1],
                                                                   scalar2=None, op0=ALU.subtract),
                             reads=["D_Mf", "D_cv"], writes=["D_Mb"])
                    barrier()
                qi = sb(st, "D_qi", [128, 4, SEQ], BF16)
                ki = sb(st, "D_ki", [128, LP], BF16)
                qq = sb(st, "D_q", [128, 4, SEQ], BF16)
                kk = sb(st, "D_k", [128, LP], BF16)
                vd = sb(st, "D_vd", [128, 17, 2, 128], BF16)
                wq = sb(st, "D_wq", [128, 16, 8])
                sc = [sb(st, "D_sc%d" % i, [128, 4, LP]) for i in range(2)]
                MA = [sb(st, "D_MA%d" % i, [128, 4, LP], BF16) for i in range(2)]
                junk = sb(st, "D_junk", [128, LP], BF16)
                Rb = [sb(st, "D_Rb%d" % i, [128, 512], BF16) for i in range(2)]
                dg = sb(st, "D_dg", [128, 8, 128], BF16)
                Pt = [sb(st, "D_Pt%d" % i, [128, 512], BF16) for i in range(3)]
                rd = sb(st, "D_rd", [128, 512])
                rds = sb(st, "D_rds", [128, 512])
                yb = sb(st, "D_yb", [128, 4, 512], BF16)
                lo = [sb(st, "D_lo%d" % i, [128, 4]) for i in range(2)]
                hi = sb(st, "D_hi", [128, 4])
                W0 = sb(st, "D_W0", [128, 4])
                Wk = sb(st, "D_Wk", [128, 4])
                mid = sb(st, "D_mid", [128, 4])
                cnt = sb(st, "D_cnt", [128, 4])
                stp = sb(st, "D_stp", [128, 4])
                pq = [ps(st, "D_pq%d" % i, [128, 512]) for i in range(2)]
                psc = ps(st, "D_psc", [128, 512])
                pL = [ps(st, "D_pL%d" % i, [128, 512]) for i in range(2)]
                pOD = [ps(st, "D_pOD%d" % i, [128, 512]) for i in range(2)]
                pSh = ps(st, "D_pSh", [128, 512])
                S.op("dve", lambda e: e.memset(vd[:, :, 0, 64:128], 1.0), writes=["D_vd1"])
                S.op("dve", lambda e: e.memset(vd[:, :, 1, 0:64], 1.0), writes=["D_vd1"])
                S.op("dve", lambda e: e.memset(rd[:, :], 1.0), writes=["D_rd"])

                def indexer(s_, Q):
                    u = Q % 2
                    for jl in range(4):
                        j = 4 * Q + jl
                        Nj = NMETA + 128 * (j + 1)
                        for h in range(8):
                            S.op("dve", lambda e, h=h, j=j: e.tensor_scalar(out=dg[:, h, :], in0=ident_f[:, :], scalar1=wq[:, j, h:h + 1],
                                                                          scalar2=None, op0=ALU.mult),
                                 reads=["ident_f", "D_wq"], writes=["D_dg"])
                        for c0 in range(0, Nj, 512):
                            cw = min(512, Nj - c0)

                            def qk(h):
                                hh, hp, b_ = h % 2, h // 2, h % 2
                                S.op("pe", lambda e: e.matmul(
                                    pq[b_][:, :cw], lhsT=qi[64 * hh:64 * hh + 64, hp, 128 * j:128 * j + 128],
                                    rhs=ki[64 * hh:64 * hh + 64, c0:c0 + cw], start=True, stop=True),
                                    reads=["D_qi", "D_ki"], writes=[("D_pq", b_)])
                                if h % 2 == 0:
                                    S.op("act", lambda e: e.activation(out=Rb[b_][:, :cw], in_=pq[b_][:, :cw], func=AF.Relu),
                                         reads=[("D_pq", b_)], writes=[("D_Rb", b_)])
                                else:
                                    S.op("dve", lambda e: e.tensor_scalar(out=Rb[b_][:, :cw], in0=pq[b_][:, :cw], scalar1=0.0,
                                                                          scalar2=None, op0=ALU.max),
                                         reads=[("D_pq", b_)], writes=[("D_Rb", b_)])

                            def dgm(h):
                                b_ = h % 2
                                S.op("pe", lambda e: e.matmul(psc[:, :cw], lhsT=dg[:, h, :], rhs=Rb[b_][:, :cw],
                                                              start=(h == 0), stop=(h == 7)),
                                     reads=[("D_Rb", b_), "D_dg"], writes=["D_psc"])

                            qk(0)
                            qk(1)
                            for h in range(8):
                                dgm(h)
                                if h + 2 < 8:
                                    qk(h + 2)
                            S.op("act", lambda e, jl=jl, c0=c0, cw=cw: e.activation(out=sc[u][:, jl, c0:c0 + cw], in_=psc[:, :cw], func=AF.Copy),
                                 reads=["D_psc"], writes=[("D_sc", u, jl)])
                        S.op("dve", lambda e, jl=jl, Nj=Nj: e.tensor_reduce(out=lo[u][:, jl:jl + 1], in_=sc[u][:, jl, :Nj], axis=AX.X, op=ALU.min),
                             reads=[("D_sc", u, jl)], writes=[("D_lo", u)])
                        S.op("dve", lambda e, jl=jl, Nj=Nj: e.tensor_reduce(out=hi[:, jl:jl + 1], in_=sc[u][:, jl, :Nj], axis=AX.X, op=ALU.max),
                             reads=[("D_sc", u, jl)], writes=["D_hi"])
                        S.op("dve", lambda e, jl=jl, Nj=Nj: e.memset(sc[u][0:64, jl, Nj - 64:Nj], -1e30),
                             reads=[("D_sc", u, jl), ("D_lo", u), "D_hi"], writes=[("D_sc", u, jl)])

                def bisect_steps(Q):
                    u = Q % 2
                    L_ = lo[u]
                    steps = []

                    def init():
                        S.op("dve", lambda e: e.tensor_tensor(out=W0[:, :], in0=hi[:, :], in1=L_[:, :], op=ALU.subtract),
                             reads=[("D_lo", u), "D_hi"], writes=["D_W0"])
                    steps.append(init)

                    def mk(it):
                        def f():
                            S.op("dve", lambda e: e.tensor_scalar(out=Wk[:, :], in0=W0[:, :], scalar1=2.0 ** (-(it + 1)), scalar2=None,
                                                                  op0=ALU.mult), reads=["D_W0", "D_stp"], writes=["D_Wk"])
                            S.op("dve", lambda e: e.tensor_tensor(out=mid[:, :], in0=L_[:, :], in1=Wk[:, :], op=ALU.add),
                                 reads=[("D_lo", u), "D_Wk"], writes=["D_mid"])
                            for jl in range(4):
                                Nj = NMETA + 128 * (4 * Q + jl + 1)
                                S.op("dve", lambda e, jl=jl, Nj=Nj: e.tensor_scalar(
                                    out=junk[:, :Nj], in0=sc[u][:, jl, :Nj], scalar1=mid[:, jl:jl + 1], scalar2=0.0,
                                    op0=ALU.is_ge, op1=ALU.add, accum_out=cnt[:, jl:jl + 1]),
                                    reads=[("D_sc", u, jl), "D_mid"], writes=["D_junk", "D_cnt"])
                            S.op("dve", lambda e: e.tensor_scalar(out=stp[:, :], in0=cnt[:, :], scalar1=TOPK, scalar2=None, op0=ALU.is_ge),
                                 reads=["D_cnt"], writes=["D_stp"])
                            S.op("dve", lambda e: e.tensor_tensor(out=stp[:, :], in0=stp[:, :], in1=Wk[:, :], op=ALU.mult),
                                 reads=["D_stp", "D_Wk"], writes=["D_stp"])
                            S.op("dve", lambda e: e.tensor_tensor(out=L_[:, :], in0=L_[:, :], in1=stp[:, :], op=ALU.add),
                                 reads=[("D_lo", u), "D_stp"], writes=[("D_lo", u)])
                        return f
                    for it in range(NIT):
                        steps.append(mk(it))

                    def fin():
                        for jl in range(4):
                            Nj = NMETA + 128 * (4 * Q + jl + 1)
                            S.op("dve", lambda e, jl=jl, Nj=Nj: e.tensor_scalar(out=MA[u][:, jl, :Nj], in0=sc[u][:, jl, :Nj], scalar1=L_[:, jl:jl + 1],
                                                                              scalar2=-30000.0, op0=ALU.is_lt, op1=ALU.mult),
                                 reads=[("D_sc", u, jl), ("D_lo", u)], writes=[("D_MA", u)])
                    steps.append(fin)
                    return steps

                def attention(s_, Q, filler):
                    u = Q % 2
                    nblk = 4 * Q + 5
                    tiles = []
                    for h in range(8):
                        for b in range(nblk):
                            tiles.append((h, b))
                    NTL = len(tiles)

                    def geo(b):
                        w = NMETA if b == 0 else 128
                        pc0 = 0 if b == 0 else NMETA + 128 * (b - 1)
                        jl0 = max(0, b - 1 - 4 * Q)
                        return w, pc0, jl0

                    def stageA(n):
                        h, b = tiles[n]
                        hh, hp = h % 2, h // 2
                        w, pc0, jl0 = geo(b)
                        c0 = jl0 * 128
                        near = (b == 0 and Q == 0) or (b >= 1 and b - 1 >= 4 * Q - 1)
                        lb, pb_ = n % 2, n % 3
                        S.op("pe", lambda e: e.matmul(
                            pL[lb][:w, c0:512], lhsT=kk[64 * hh:64 * hh + 64, pc0:pc0 + w],
                            rhs=qq[64 * hh:64 * hh + 64, hp, 512 * Q + c0:512 * Q + 512], start=True, stop=False),
                            reads=["D_k", "D_q"], writes=[("D_pL", lb)])
                        if near:
                            if b == 0:
                                S.op("pe", lambda e: e.matmul(pL[lb][:NMETA, c0:512], lhsT=ident_b[:NMETA, :NMETA],
                                                              rhs=Mb[:NMETA, h, c0:512], start=False, stop=False),
                                     reads=["D_Mb", "ident_b"], writes=[("D_pL", lb)])
                            else:
                                z0 = 512 * Q + c0 - 128 * (b - 1) + 384
                                S.op("pe", lambda e: e.matmul(pL[lb][:, c0:512], lhsT=ident_b[:, :],
                                                              rhs=Gb[:, h, z0:z0 + 512 - c0], start=False, stop=False),
                                     reads=["D_Gb", "ident_b"], writes=[("D_pL", lb)])
                        for jl in range(jl0, 4):
                            S.op("pe", lambda e, jl=jl: e.matmul(pL[lb][:w, jl * 128:(jl + 1) * 128], lhsT=MA[u][:, jl, pc0:pc0 + w],
                                                                 rhs=ident_b[:, :], start=False, stop=(jl == 3)),
                                 reads=[("D_MA", u), "ident_b"], writes=[("D_pL", lb)])
                        S.op("act", lambda e: e.activation(out=Pt[pb_][:w, c0:512], in_=pL[lb][:w, c0:512], func=AF.Exp),
                             reads=[("D_pL", lb)], writes=[("D_Pt", pb_)])

                    def stageB(n):
                        h, b = tiles[n]
                        hh, hp = h % 2, h // 2
                        w, pc0, jl0 = geo(b)
                        c0 = jl0 * 128
                        pb_ = n % 3
                        ob = h % 2
                        S.op("pe", lambda e: e.matmul(pOD[ob][:, c0:512], lhsT=vd[:w, b, hh, :], rhs=Pt[pb_][:w, c0:512],
                                                      start=(b == 0), stop=(b == nblk - 1)),
                             reads=[("D_Pt", pb_), "D_vd", "D_vd1"], writes=[("D_pOD", ob)])
                        if b == nblk - 1:
                            orow = slice(64 * hh, 64 * hh + 64)
                            drow = slice(64 * (1 - hh), 64 * (1 - hh) + 64)
                            S.op("dve", lambda e: e.reciprocal(out=rd[drow, :], in_=pOD[ob][drow, :]),
                                 reads=[("D_pOD", ob)], writes=["D_rd"])
                            S.op("pe", lambda e: e.matmul(pSh[orow, :], lhsT=ident_f[drow, drow], rhs=rd[drow, :], start=True, stop=True),
                                 reads=["D_rd", "ident_f"], writes=["D_pSh"])
                            S.op("act", lambda e: e.activation(out=rds[orow, :], in_=pSh[orow, :], func=AF.Copy),
                                 reads=["D_pSh"], writes=["D_rds"])
                            S.op("dve", lambda e: e.tensor_tensor(out=yb[orow, hp, :], in0=rds[orow, :], in1=pOD[ob][orow, :], op=ALU.mult),
                                 reads=["D_rds", ("D_pOD", ob)], writes=["D_yb"])
                            for _ in range(3):
                                if filler:
                                    filler.pop(0)()

                    stageA(0)
                    stageA(1)
                    for n in range(NTL):
                        stageB(n)
                        if n + 2 < NTL:
                            stageA(n + 2)
                    while filler:
                        filler.pop(0)()
                    S.dma("sp", "o", ybT[s_, :, :, 512 * Q:512 * Q + 512], yb[:, :, :], reads=["D_yb"], writes=[("ybT", s_, Q)])

                for s_ in range(NSEQ):
                    S.dma("sp", "x", qi[:, :, :], qiT[s_, :, :, :], reads=[("qiT", s_, t) for t in range(NT)], writes=["D_qi"])
                    S.dma("sp", "x", ki[:, :], kiT[s_, :, :], reads=[("kiT", s_, t) for t in ALLT], writes=["D_ki"])
                    S.dma("sp", "x", qq[:, :, :], qT[s_, :, :, :], reads=[("qT", s_, t) for t in range(NT)], writes=["D_q"])
                    S.dma("sp", "x", kk[:, :], kT[s_, :, :], reads=[("kT", s_, t) for t in ALLT], writes=["D_k"])
                    vr = [("vwS", s_, t) for t in ALLT]
                    for half in range(2):
                        S.dma("pool", "x", vd[:, 1:17, half, 64 * half:64 * half + 64],
                              vwS[s_, NMETA:LP, 0:64].rearrange("(b p) c -> p b c", p=128), reads=vr, writes=["D_vd"])
                        S.dma("pool", "x", vd[:NMETA, 0, half, 64 * half:64 * half + 64], vwS[s_, 0:NMETA, 0:64], reads=vr, writes=["D_vd"])
                    S.dma("sp", "x", wq[:, :, :], vwS[s_, NMETA:LP, 64:72].rearrange("(b p) c -> p b c", p=128), reads=vr, writes=["D_wq"])
                    indexer(s_, 0)
                    for f_ in bisect_steps(0):
                        f_()
                    for Q in range(4):
                        filler = []
                        if Q + 1 < 4:
                            indexer(s_, Q + 1)
                            filler = bisect_steps(Q + 1)
                        attention(s_, Q, filler)

        if debug not in ("A", "B", "C"):
            phase_D()
            barrier()

        if debug == "D":
            with ExitStack() as st:
                tmp = sb(st, "dbgtmp", [128, 4, SEQ], BF16)
                tmp2 = sb(st, "dbgtmp2", [128, 4, SEQ], F32)
                S.dma("sp", "x", tmp[:, :, :], ybT[0, :, :, :], reads=[("ybT", 0, t) for t in range(NT)], writes=["dbgtmp"])
                S.op("dve", lambda e: e.tensor_copy(out=tmp2[:, :, :], in_=tmp[:, :, :]), reads=["dbgtmp"], writes=["dbgtmp2"])
                S.dma("sp", "o", outT[0, :, 0:4, 0:SEQ], tmp2[:, :, :], reads=["dbgtmp2"], writes=["out"])

        w_glu_d = din("w_glu", [128, 6, 768])
        w_a_d = din("w_a", [128, 6, D])
        w_b_d = din("w_b", [128, 4, D])
        w_o_d = din("w_o", [128, DC, D])
        w_g_d = din("w_g", [128, DC, 2 * D])
        h2T = dscr("h2T", [NSEQ, 128, DC, SEQ])

        def phase_E():
            with ExitStack() as st:
                wglu = sb(st, "E_wglu", [128, 6, 768], BF16)
                wa = sb(st, "E_wa", [128, 6, D], BF16)
                wb = sb(st, "E_wb", [128, 4, D], BF16)
                wo = sb(st, "E_wo", [128, DC, D], BF16)
                wgt = sb(st, "E_wg", [128, DC, 2 * D], BF16)
                S.dma("pool", "w", wglu[:, :, :], w_glu_d[:, :, :], writes=["E_w"], max_dma_last_dim=3072)
                for k in range(6):
                    S.dma("pool", "w", wa[:, k, :], w_a_d[:, k, :], writes=["E_w"])
                for k in range(4):
                    S.dma("pool", "w", wb[:, k, :], w_b_d[:, k, :], writes=["E_w"])
                for k in range(DC):
                    S.dma("pool", "w", wo[:, k, :], w_o_d[:, k, :], writes=["E_w"])
                    S.dma("pool", "w", wgt[:, k, :], w_g_d[:, k, :], writes=["E_w"], max_dma_last_dim=4096)
                hn = sb(st, "E_hn", [128, DC, TT], BF16)
                ya = sb(st, "E_ya", [128, 6, TT], BF16)
                yg = sb(st, "E_yg", [128, 6, TT], BF16)
                ybt = sb(st, "E_yb", [128, 4, TT], BF16)
                h1t = sb(st, "E_h1", [128, DC, TT])
                sgl = sb(st, "E_sgl", [128, TT])
                ga = sb(st, "E_ga", [128, TT])
                gb = sb(st, "E_gb", [128, TT])
                t1 = sb(st, "E_t1", [128, TT])
                t2 = sb(st, "E_t2", [128, TT])
                mg = sb(st, "E_mg", [128, DC, TT], BF16)
                ysb = sb(st, "E_y", [128, DC, TT])
                sq = sb(st, "E_sq", [128, 2, TT])
                rs = sb(st, "E_rs", [128, TT])
                pga = ps(st, "E_pga", [128, TT])
                pgb = ps(st, "E_pgb", [128, TT])
                pa = ps(st, "E_pa", [128, TT])
                pb = ps(st, "E_pb", [128, TT])
                py = [ps(st, "E_py%d" % i, [128, TT]) for i in range(2)]
                pss = ps(st, "E_pss", [128, TT])
                for s_ in range(NSEQ):
                    for t_ in range(NT):
                        tsl = slice(t_ * TT, (t_ + 1) * TT)
                        S.dma("sp", "x", hn[:, :, :], hnT[s_, :, :, tsl], reads=[("hnT", s_, t_)], writes=["E_hn"])
                        S.dma("sp", "x", ya[:, :, :], yaT[s_, :, :, tsl], reads=[("yaT", s_)], writes=["E_ya"])
                        S.dma("sp", "x", ybt[:, :, :], ybT[s_, :, :, tsl], reads=[("ybT", s_, t_)], writes=["E_yb"])
                        S.dma("sp", "x", h1t[:, :, :], h1T[s_, :, :, tsl], reads=[("h1T", s_, t_)], writes=["E_h1"])
                        for oc in range(6):
                            b_ = oc % 2
                            mms(py[b_][:, :], [(wglu[:, k, oc * 128:(oc + 1) * 128], ya[:, k, :]) for k in range(6)],
                                ["E_w", "E_ya"], ("E_py", b_))
                            S.op("act", lambda e, b_=b_: e.activation(out=sgl[:, :], in_=py[b_][:, :], func=AF.Sigmoid),
                                 reads=[("E_py", b_)], writes=["E_sgl"])
                            S.op("dve", lambda e, oc=oc: e.tensor_tensor(out=yg[:, oc, :], in0=sgl[:, :], in1=ya[:, oc, :], op=ALU.mult),
                                 reads=["E_sgl", "E_ya"], writes=["E_yg"])
                        for dc in range(DC):
                            mms(pga[:, :], [(wgt[:, k, dc * 128:(dc + 1) * 128], hn[:, k, :]) for k in range(DC)], ["E_w", "E_hn"], "E_pga")
                            mms(pgb[:, :], [(wgt[:, k, D + dc * 128:D + (dc + 1) * 128], hn[:, k, :]) for k in range(DC)], ["E_w", "E_hn"], "E_pgb")
                            mms(pa[:, :], [(wa[:, k, dc * 128:(dc + 1) * 128], yg[:, k, :]) for k in range(6)], ["E_w", "E_yg"], "E_pa")
                            mms(pb[:, :], [(wb[:, k, dc * 128:(dc + 1) * 128], ybt[:, k, :]) for k in range(4)], ["E_w", "E_yb"], "E_pb")
                            S.op("act", lambda e: e.activation(out=ga[:, :], in_=pga[:, :], func=AF.Sigmoid), reads=["E_pga"], writes=["E_ga"])
                            S.op("act", lambda e: e.activation(out=gb[:, :], in_=pgb[:, :], func=AF.Sigmoid), reads=["E_pgb"], writes=["E_gb"])
                            S.op("dve", lambda e: e.tensor_tensor(out=t1[:, :], in0=ga[:, :], in1=pa[:, :], op=ALU.mult), reads=["E_ga", "E_pa"], writes=["E_t1"])
                            S.op("dve", lambda e: e.tensor_tensor(out=t2[:, :], in0=gb[:, :], in1=pb[:, :], op=ALU.mult), reads=["E_gb", "E_pb"], writes=["E_t2"])
                            S.op("pool", lambda e, dc=dc: e.tensor_tensor(out=mg[:, dc, :], in0=t1[:, :], in1=t2[:, :], op=ALU.add),
                                 reads=["E_t1", "E_t2"], writes=["E_mg"])
                        for c in range(DC):
                            b_ = c % 2
                            mms(py[b_][:, :], [(wo[:, k, c * 128:(c + 1) * 128], mg[:, k, :]) for k in range(DC)], ["E_w", "E_mg"], ("E_py", b_))
                            S.op("act", lambda e, c=c, b_=b_: e.activation(out=ysb[:, c, :], in_=py[b_][:, :], func=AF.Copy),
                                 reads=[("E_py", b_)], writes=[("E_y", c)])
                            S.op("act", lambda e, c=c, b_=b_: e.activation(out=sq[:, c % 2, :], in_=py[b_][:, :], func=AF.Square),
                                 reads=[("E_py", b_)], writes=[("E_sq", c % 2)])
                            S.op("pe", lambda e, c=c: e.matmul(pss[:, :], lhsT=ones_f[:, :], rhs=sq[:, c % 2, :], start=(c == 0), stop=(c == DC - 1)),
                                 reads=[("E_sq", c % 2), "ones_f"], writes=["E_pss"])
                        rstd_from_sumsq(pss, rs, TT, "E_pss", "E_rs")
                        for c in range(DC):
                            S.op("dve", lambda e, c=c: e.scalar_tensor_tensor(
                                out=ysb[:, c, :], in0=ysb[:, c, :], scalar=gains_sb[:, 24 + c:24 + c + 1], in1=rs[:, :],
                                op0=ALU.mult, op1=ALU.mult), reads=[("E_y", c), "E_rs", "gains"], writes=[("E_y", c)])
                            S.op("pool", lambda e, c=c: e.tensor_tensor(out=ysb[:, c, :], in0=ysb[:, c, :], in1=h1t[:, c, :], op=ALU.add),
                                 reads=[("E_y", c), "E_h1"], writes=[("E_y", c)])
                        S.dma("sp", "o", h2T[s_, :, :, tsl], ysb[:, :, :], reads=[("E_y", c) for c in range(DC)], writes=[("h2T", s_, t_)])

        if debug not in ("A", "B", "C", "D"):
            phase_E()
            barrier()
            ff2_wg = din("ff2_wg", [128, DC, DFF])
            ff2_wu = din("ff2_wu", [128, DC, DFF])
            ff2_wd = din("ff2_wd", [128, FC, D])
            tilesF = []
            for s in range(NSEQ):
                for t in range(NFT):
                    tilesF.append((h2T[s, :, :, t * FT:(t + 1) * FT], outT[s, :, :, t * FT:(t + 1) * FT], FT,
                                   [("h2T", s, t * FT // TT)], [("out", s, t)]))
            ffn_phase("F", ff2_wg, ff2_wu, ff2_wd, 32, 40, tilesF)

        if debug == "A":
            with ExitStack() as st:
                tmp = sb(st, "dbgtmp", [128, DC, FT])
                S.dma("sp", "x", tmp[:, :, :], h1T[0, :, :, 0:FT], reads=[("h1T", 0, 0)], writes=["dbgtmp"])
                S.dma("sp", "o", outT[0, :, :, 0:FT], tmp[:, :, :], reads=["dbgtmp"], writes=["out"])
                S.dma("sp", "x", tmp[:, :, :NMETA], h1m[:, :, :], reads=["h1m", "out"], writes=["dbgtmp"])
                S.dma("sp", "o", outT[1, :, :, 0:NMETA], tmp[:, :, :NMETA], reads=["dbgtmp"], writes=["out"])

        S.drain("sp")
        print("instructions:", S.ninst)
    return nc


def _rel_bucket(rel):
    half, me = 16, 8
    base = np.where(rel > 0, half, 0)
    n = np.abs(rel)
    nf = np.maximum(n, 1).astype(np.float32)
    large = me + (np.log(nf / me) / math.log(128 / me) * (half - me)).astype(np.int32)
    large = np.minimum(large, half - 1)
    return base + np.where(n < me, n, large)


def prep_inputs(inp):
    f = lambda a: np.ascontiguousarray(np.asarray(a, dtype=np.float32))
    x = f(inp["x"])
    B = x.shape[0]
    xT = np.ascontiguousarray(x.reshape(B, SEQ, DC, 128).transpose(0, 3, 2, 1))
    metaT = np.ascontiguousarray(f(inp["meta_tokens"]).reshape(NMETA, DC, 128).transpose(2, 1, 0))
    gl = [inp[k] for k in ("ff1_norm_pre", "ff1_norm_post", "mix_norm_pre", "mix_norm_post", "ff2_norm_pre",
                           "ff2_norm_post")]
    gains = np.ascontiguousarray(np.concatenate([f(g)[0].reshape(DC, 128).T for g in gl], axis=1))

    def wk(w, kc):
        w = f(w)
        return np.ascontiguousarray(w.reshape(kc, 128, w.shape[-1]).transpose(1, 0, 2))

    shared = {
        "metaT": metaT, "gains": gains,
        "ff1_wg": wk(inp["ff1_w_gate"][0], DC), "ff1_wu": wk(inp["ff1_w_up"][0], DC),
        "ff1_wd": wk(inp["ff1_w_down"][0], FC),
    }
    win = f(inp["w_in"][0])
    upad = np.zeros((D, 6, 128), np.float32)
    for c6 in range(6):
        w_ = min(96, 512 - 96 * c6)
        upad[:, c6, :w_] = win[:, 96 * c6:96 * c6 + w_]
    winA = np.concatenate([upad.reshape(D, 768), win[:, 512:1024], win[:, 1096:1608], win[:, 1024:1088], win[:, 1024:1088],
                           win[:, 1608:1672], win[:, 1608:1672]], axis=1)
    winB = np.concatenate([win[:, 1672:1736], win[:, 1088:1096]], axis=1)
    shared["w_inA"] = wk(winA, DC)
    shared["w_inB"] = wk(winB, DC)
    lre, lim, ldt = f(inp["ssm_lambda_re"][0]), f(inp["ssm_lambda_im"][0]), f(inp["ssm_log_dt"][0])
    bre, bim = f(inp["ssm_b_re"][0]), f(inp["ssm_b_im"][0])
    cre, cim = f(inp["ssm_c_re"][0]), f(inp["ssm_c_im"][0])
    r = np.arange(128)
    sidx = np.arange(128)
    cc = np.arange(6)
    i_rc = 3 * cc[None, :] + (r[:, None] // 32)
    val_rc = (r[:, None] < 96) & (i_rc < 16)
    i_rc = np.where(val_rc, i_rc, 0)
    g_rcs = 2 * i_rc[:, :, None] + (sidx[None, None, :] // 64)
    p_s = sidx % 64
    pcm = np.stack([lre[g_rcs, p_s[None, None, :]], lim[g_rcs, p_s[None, None, :]], ldt[g_rcs]], axis=2)
    glr = (r % 32) // 16
    m_r = r % 16
    msk = (glr[:, None, None] == (sidx[None, None, :] // 64)) & val_rc[:, :, None]
    bcm = np.stack([np.where(msk, bre[g_rcs, p_s[None, None, :], m_r[:, None, None]], 0.0),
                    np.where(msk, bim[g_rcs, p_s[None, None, :], m_r[:, None, None]], 0.0)], axis=2)
    ii = np.arange(16)
    g_si = 2 * ii[None, :] + (sidx[:, None] // 64)
    psm = np.stack([lre[g_si, p_s[:, None]], lim[g_si, p_s[:, None]], ldt[g_si]], axis=1)
    q = np.arange(32)
    mq = q % 16
    mskq = ((q[None, None, :] // 16) == (sidx[:, None, None] // 64))
    bsm = np.stack([np.where(mskq, bre[g_si[:, :, None], p_s[:, None, None], mq[None, None, :]], 0.0),
                    np.where(mskq, bim[g_si[:, :, None], p_s[:, None, None], mq[None, None, :]], 0.0)], axis=2)
    csm = np.stack([np.where(mskq, cre[g_si[:, :, None], mq[None, None, :], p_s[:, None, None]], 0.0),
                    np.where(mskq, cim[g_si[:, :, None], mq[None, None, :], p_s[:, None, None]], 0.0)], axis=2)
    dflat = f(inp["ssm_d"][0]).reshape(512)
    ch_rc = 96 * cc[None, :] + r[:, None]
    vch = (r[:, None] < 96) & (ch_rc < 512)
    dsk = np.where(vch, dflat[np.where(vch, ch_rc, 0)], 0.0)
    shared["s5_pcm"] = f(pcm)
    shared["s5_bcm"] = f(bcm)
    shared["s5_psm"] = f(psm)
    shared["s5_bsm"] = f(bsm)
    shared["s5_csm"] = f(csm)
    shared["s5_d"] = f(dsk)
    shared["ident"] = np.eye(128, dtype=np.float32)
    rb = f(inp["rel_bias"])
    sl = np.arange(128)[:, None]
    zi = np.arange(1024)[None, :]
    shared["biasG"] = f(rb[_rel_bucket(sl - (zi - 384))].transpose(0, 2, 1))
    mm_ = np.arange(16)[:, None]
    tq = np.arange(512)[None, :]
    shared["biasM"] = f(rb[_rel_bucket(mm_ - 16 - tq)].transpose(0, 2, 1))
    shared["cvec"] = f(np.broadcast_to(rb[15][None, :], (128, 8)))
    def pad6rows(w):
        o = np.zeros((128, 6, w.shape[1]), np.float32)
        for c6 in range(6):
            w_ = min(96, 512 - 96 * c6)
            o[:w_, c6, :] = w[96 * c6:96 * c6 + w_]
        return o
    wg_ = f(inp["ssm_w_glu"][0])
    wgp = np.zeros((512, 6, 128), np.float32)
    for c6 in range(6):
        w_ = min(96, 512 - 96 * c6)
        wgp[:, c6, :w_] = wg_[:, 96 * c6:96 * c6 + w_]
    shared["w_glu"] = pad6rows(wgp.reshape(512, 768))
    shared["w_a"] = pad6rows(f(inp["w_branch_a"][0]))
    shared["w_b"] = wk(inp["w_branch_b"][0], 4)
    shared["w_o"] = wk(inp["w_out"][0], DC)
    shared["w_g"] = wk(win[:, 1736:3784], DC)
    shared["ff2_wg"] = wk(inp["ff2_w_gate"][0], DC)
    shared["ff2_wu"] = wk(inp["ff2_w_up"][0], DC)
    shared["ff2_wd"] = wk(inp["ff2_w_down"][0], FC)
    maps = []
    for c in range(NCORES):
        m = dict(shared)
        m["xT"] = xT[c * NSEQ:(c + 1) * NSEQ]
        maps.append(m)
    return maps


def kernel(**inputs):
    maps = prep_inputs(inputs)
    nc = build()
    res = run_bass_kernel_spmd(nc, maps, core_ids=list(range(NCORES)))
    outs = [r["outT"] for r in res.results]
    o = np.concatenate(outs, axis=0)
    out = o.transpose(0, 3, 2, 1).reshape(o.shape[0], SEQ, D)
    return np.ascontiguousarray(out.astype(np.float32))
```

```python
import math
from contextlib import ExitStack
import numpy as np
import ml_dtypes
import concourse.bass as bass
import concourse.mybir as mybir
from concourse.bass_utils import run_bass_kernel_spmd

F32 = mybir.dt.float32
BF16 = mybir.dt.bfloat16
AF = mybir.ActivationFunctionType
ALU = mybir.AluOpType
AX = mybir.AxisListType

NCORES = 8
D = 1024
DC = 8
SEQ = 2048
NSEQ = 2
NMETA = 16
DFF = 2816
FC = 22
EPS = 1e-6
TT = 512
NT = SEQ // TT
FT = 256
NFT = SEQ // FT


class Sync:
    def __init__(self, nc, es):
        self.nc = nc
        self.eng = {"pe": nc.tensor, "act": nc.scalar, "dve": nc.vector, "pool": nc.gpsimd, "sp": nc.sync}
        self.sem = {k: es.enter_context(nc.semaphore("s_" + k)) for k in self.eng}
        self.cnt = {k: 0 for k in self.eng}
        self.dsem = {}
        self.dcnt = {}
        self.es = es
        self.seen = {k: {} for k in self.eng}
        self.lastw = {}
        self.readers = {}
        self.ninst = 0

    NPOOL = {"sp": 12, "pool": 12, "act": 8}

    def dma_sem(self, q):
        if q not in self.dsem:
            self.dsem[q] = [self.es.enter_context(self.nc.semaphore("d_%s%d" % (q, i))) for i in range(self.NPOOL[q])]
            self.dcnt[q] = [0] * self.NPOOL[q]
            self.drr = getattr(self, "drr", {})
            self.drr[q] = 0
        i = self.drr[q]
        self.drr[q] = (i + 1) % self.NPOOL[q]
        return i

    def _wait(self, e, reads, writes):
        need = {}
        for k in reads:
            lw = self.lastw.get(k)
            if lw is not None:
                need[lw[0]] = max(need.get(lw[0], (0, None))[0], lw[1]), lw[2]
        for k in writes:
            lw = self.lastw.get(k)
            if lw is not None:
                need[lw[0]] = max(need.get(lw[0], (0, None))[0], lw[1]), lw[2]
            for r in self.readers.get(k, ()):
                need[r[0]] = max(need.get(r[0], (0, None))[0], r[1]), r[2]
        E = self.eng[e]
        for semid, (val, semobj) in need.items():
            if semid == "e_" + e and e == "pe":
                continue
            if self.seen[e].get(semid, 0) >= val:
                continue
            E.wait_ge(semobj, val)
            self.seen[e][semid] = val

    def _record(self, rec, reads, writes):
        for k in reads:
            self.readers.setdefault(k, []).append(rec)
        for k in writes:
            self.lastw[k] = rec
            self.readers[k] = []

    def op(self, e, fn, reads=(), writes=()):
        self._wait(e, reads, writes)
        inst = fn(self.eng[e])
        self.cnt[e] += 1
        inst.then_inc(self.sem[e], 1)
        self.ninst += 1
        self._record(("e_" + e, self.cnt[e], self.sem[e]), reads, writes)

    def dma(self, q, semname, out, in_, reads=(), writes=(), **kw):
        self._wait(q, reads, writes)
        i = self.dma_sem(q)
        sem = self.dsem[q][i]
        semid = "d_%s%d" % (q, i)
        if self.dcnt[q][i] > 0 and self.seen[q].get(semid, 0) < self.dcnt[q][i]:
            self.eng[q].wait_ge(sem, self.dcnt[q][i])
            self.seen[q][semid] = self.dcnt[q][i]
        inst = self.eng[q].dma_start(out=out, in_=in_, **kw)
        self.dcnt[q][i] += 16
        inst.then_inc(sem, 16)
        self.ninst += 1
        self._record((semid, self.dcnt[q][i], sem), reads, writes)

    def drain(self, e):
        E = self.eng[e]
        for k in self.eng:
            if k != e and self.cnt[k] > 0:
                E.wait_ge(self.sem[k], self.cnt[k])
        for q, sems in self.dsem.items():
            for i, sm in enumerate(sems):
                if self.dcnt[q][i] > 0:
                    E.wait_ge(sm, self.dcnt[q][i])


def build(debug=None):
    nc = bass.Bass("TRN2", target_bir_lowering=False)
    es = ExitStack()
    with es:
        S = Sync(nc, es)

        def din(name, shape, dt=F32):
            return nc.dram_tensor(name, list(shape), dt, kind="ExternalInput").ap()

        def dscr(name, shape, dt=F32):
            return nc.dram_tensor(name, list(shape), dt, kind="Internal").ap()

        xT = din("xT", [NSEQ, 128, DC, SEQ])
        metaT = din("metaT", [128, DC, NMETA])
        gains = din("gains", [128, 48])
        ff1_wg = din("ff1_wg", [128, DC, DFF])
        ff1_wu = din("ff1_wu", [128, DC, DFF])
        ff1_wd = din("ff1_wd", [128, FC, D])
        outT = nc.dram_tensor("outT", [NSEQ, 128, DC, SEQ], F32, kind="ExternalOutput").ap()
        h1T = dscr("h1T", [NSEQ, 128, DC, SEQ])
        h1m = dscr("h1m", [128, DC, NMETA])

        def sb(stack, name, shape, dt=F32):
            return stack.enter_context(nc.sbuf_tensor(name, list(shape), dt))

        def ps(stack, name, shape, dt=F32):
            return stack.enter_context(nc.psum_tensor(name, list(shape), dt))

        gains_sb = sb(es, "gains_sb", [128, 48])
        ones_f = sb(es, "ones_f", [128, 128])
        S.dma("sp", "c", gains_sb[:, :], gains[:, :], writes=["gains"])
        S.op("dve", lambda e: e.memset(ones_f[:, :], 1.0), writes=["ones_f"])
        zeros_b = sb(es, "zeros_b", [128, 128], BF16)
        S.op("dve", lambda e: e.memset(zeros_b[:, :], 0.0), writes=["zeros_b"])

        def rstd_from_sumsq(pss, rs, n, key_pss, key_rs, half=False):
            S.op("act", lambda e: e.activation(out=rs[:, :n], in_=pss[:, :n], func=AF.Sqrt,
                                               bias=eps_sb[:, (1 if half else 0):(2 if half else 1)],
                                               scale=(4.0 if half else 1.0) / D),
                 reads=[key_pss, "eps"], writes=[key_rs])
            S.op("dve", lambda e: e.reciprocal(out=rs[:, :n], in_=rs[:, :n]), reads=[key_rs], writes=[key_rs])

        eps_sb = sb(es, "eps_sb", [128, 2])
        S.op("dve", lambda e: e.memset(eps_sb[:, 0:1], EPS), writes=["eps"])
        S.op("dve", lambda e: e.memset(eps_sb[:, 1:2], 4.0 * EPS), writes=["eps"])

        def ffn_phase(tag, wg, wu, wd, gpre_col, gpost_col, tiles):
            with ExitStack() as st:
                wg_sb = sb(st, tag + "wg", [128, DC, DFF], BF16)
                wu_sb = sb(st, tag + "wu", [128, DC, DFF], BF16)
                wd_sb = sb(st, tag + "wd", [128, FC, D], BF16)
                xt = [sb(st, tag + "xt%d" % i, [128, DC, FT]) for i in range(2)]
                sq = sb(st, tag + "sq", [128, 2, FT])
                hn = sb(st, tag + "hn", [128, DC, FT], BF16)
                act = sb(st, tag + "act", [128, FC, FT], BF16)
                sg = [sb(st, tag + "sg%d" % i, [128, FT]) for i in range(2)]
                ysb = sb(st, tag + "y", [128, DC, FT])
                rs = sb(st, tag + "rs", [128, FT])
                psg = [ps(st, tag + "psg%d" % i, [128, FT]) for i in range(2)]
                psu = [ps(st, tag + "psu%d" % i, [128, FT]) for i in range(2)]
                psy = [ps(st, tag + "psy%d" % i, [128, FT]) for i in range(2)]
                pss = ps(st, tag + "pss", [128, FT])
                for k in range(DC):
                    S.dma("pool", "w", wg_sb[:, k, :], wg[:, k, :], writes=[(tag, "wg", k)], max_dma_last_dim=5632)
                    S.dma("pool", "w", wu_sb[:, k, :], wu[:, k, :], writes=[(tag, "wu", k)], max_dma_last_dim=5632)
                for j in range(FC):
                    S.dma("pool", "w", wd_sb[:, j, :], wd[:, j, :], writes=[(tag, "wd", j)], max_dma_last_dim=4096)

                def load(i):
                    src, dst, n, sr, dw = tiles[i]
                    S.dma("sp", "x", xt[i % 2][:, :, :n], src, reads=sr, writes=[(tag, "xt", i % 2)])

                load(0)
                for i, (src, dst, n, sr, dw) in enumerate(tiles):
                    if i + 1 < len(tiles):
                        load(i + 1)
                    x = xt[i % 2]
                    kx = (tag, "xt", i % 2)
                    for c in range(DC):
                        S.op("act", lambda e, c=c: e.activation(out=sq[:, c % 2, :n], in_=x[:, c, :n], func=AF.Square),
                             reads=[kx], writes=[(tag, "sq", c % 2)])
                        S.op("pe", lambda e, c=c: e.matmul(pss[:, :n], lhsT=ones_f[:, :], rhs=sq[:, c % 2, :n],
                                                          start=(c == 0), stop=(c == DC - 1)),
                             reads=[(tag, "sq", c % 2), "ones_f"], writes=[(tag, "pss")])
                    rstd_from_sumsq(pss, rs, n, (tag, "pss"), (tag, "rs"))
                    for c in range(DC):
                        S.op("dve", lambda e, c=c: e.scalar_tensor_tensor(
                            out=hn[:, c, :n], in0=x[:, c, :n], scalar=gains_sb[:, gpre_col + c:gpre_col + c + 1],
                            in1=rs[:, :n], op0=ALU.mult, op1=ALU.mult),
                            reads=[kx, (tag, "rs"), "gains"], writes=[(tag, "hn", c)])
                    for j in range(FC):
                        b = j % 2
                        for k in range(DC):
                            S.op("pe", lambda e, k=k, j=j, b=b: e.matmul(
                                psg[b][:, :n], lhsT=wg_sb[:, k, j * 128:(j + 1) * 128], rhs=hn[:, k, :n],
                                start=(k == 0), stop=(k == DC - 1)),
                                reads=[(tag, "wg", k), (tag, "hn", k)], writes=[(tag, "psg", b)])
                        for k in range(DC):
                            S.op("pe", lambda e, k=k, j=j, b=b: e.matmul(
                                psu[b][:, :n], lhsT=wu_sb[:, k, j * 128:(j + 1) * 128], rhs=hn[:, k, :n],
                                start=(k == 0), stop=(k == DC - 1)),
                                reads=[(tag, "wu", k), (tag, "hn", k)], writes=[(tag, "psu", b)])
                        S.op("act", lambda e, b=b: e.activation(out=sg[b][:, :n], in_=psg[b][:, :n], func=AF.Silu),
                             reads=[(tag, "psg", b)], writes=[(tag, "sg", b)])
                        S.op("dve", lambda e, b=b, j=j: e.tensor_tensor(out=act[:, j, :n], in0=sg[b][:, :n],
                                                                        in1=psu[b][:, :n], op=ALU.mult),
                             reads=[(tag, "sg", b), (tag, "psu", b)], writes=[(tag, "act", j)])
                    for c in range(DC):
                        b = c % 2
                        for j in range(FC):
                            S.op("pe", lambda e, c=c, j=j, b=b: e.matmul(
                                psy[b][:, :n], lhsT=wd_sb[:, j, c * 128:(c + 1) * 128], rhs=act[:, j, :n],
                                start=(j == 0), stop=(j == FC - 1)),
                                reads=[(tag, "wd", j), (tag, "act", j)], writes=[(tag, "psy", b)])
                        S.op("act", lambda e, c=c, b=b: e.activation(out=ysb[:, c, :n], in_=psy[b][:, :n], func=AF.Copy),
                             reads=[(tag, "psy", b)], writes=[(tag, "y", c)])
                        S.op("act", lambda e, c=c, b=b: e.activation(out=sq[:, c % 2, :n], in_=psy[b][:, :n], func=AF.Square),
                             reads=[(tag, "psy", b)], writes=[(tag, "sq", c % 2)])
                        S.op("pe", lambda e, c=c: e.matmul(pss[:, :n], lhsT=ones_f[:, :], rhs=sq[:, c % 2, :n],
                                                          start=(c == 0), stop=(c == DC - 1)),
                             reads=[(tag, "sq", c % 2), "ones_f"], writes=[(tag, "pss")])
                    rstd_from_sumsq(pss, rs, n, (tag, "pss"), (tag, "rs"), half=True)
                    for c in range(DC):
                        S.op("dve", lambda e, c=c: e.scalar_tensor_tensor(
                            out=ysb[:, c, :n], in0=ysb[:, c, :n], scalar=gains_sb[:, gpost_col + c:gpost_col + c + 1],
                            in1=rs[:, :n], op0=ALU.mult, op1=ALU.mult),
                            reads=[(tag, "y", c), (tag, "rs"), "gains"], writes=[(tag, "y", c)])
                        S.op("pool", lambda e, c=c: e.tensor_tensor(
                            out=ysb[:, c, :n], in0=ysb[:, c, :n], in1=x[:, c, :n], op=ALU.add),
                            reads=[(tag, "y", c), kx], writes=[(tag, "y", c)])
                    S.dma("sp", "o", dst, ysb[:, :, :n], reads=[(tag, "y", c) for c in range(DC)], writes=dw)

        def barrier():
            for e in S.eng:
                S.drain(e)

        S.barrier = barrier

        def mms(out, pairs, reads, wkey):
            n_ = len(pairs)
            for idx, (l, r) in enumerate(pairs):
                S.op("pe", lambda e, l=l, r=r, idx=idx: e.matmul(out, lhsT=l, rhs=r, start=(idx == 0),
                                                                 stop=(idx == n_ - 1)),
                     reads=reads, writes=[wkey])

        def prenorm(tag, x, kx, n, gcol, hn, sq, pss, rs):
            for c in range(DC):
                S.op("act", lambda e, c=c: e.activation(out=sq[:, c % 2, :n], in_=x[:, c, :n], func=AF.Square),
                     reads=[kx], writes=[(tag, "sq", c % 2)])
                S.op("pe", lambda e, c=c: e.matmul(pss[:, :n], lhsT=ones_f[:, :], rhs=sq[:, c % 2, :n],
                                                  start=(c == 0), stop=(c == DC - 1)),
                     reads=[(tag, "sq", c % 2), "ones_f"], writes=[(tag, "pss")])
            rstd_from_sumsq(pss, rs, n, (tag, "pss"), (tag, "rs"))
            for c in range(DC):
                S.op("dve", lambda e, c=c: e.scalar_tensor_tensor(
                    out=hn[:, c, :n], in0=x[:, c, :n], scalar=gains_sb[:, gcol + c:gcol + c + 1],
                    in1=rs[:, :n], op0=ALU.mult, op1=ALU.mult),
                    reads=[kx, (tag, "rs"), "gains"], writes=[(tag, "hn")])

        tilesA = [(metaT[:, :, :], h1m[:, :, :], NMETA, [], ["h1m"])]
        for s in range(NSEQ):
            for t in range(NFT):
                tilesA.append((xT[s, :, :, t * FT:(t + 1) * FT], h1T[s, :, :, t * FT:(t + 1) * FT], FT, [],
                               [("h1T", s, t * FT // TT)]))
        if debug == "A":
            tilesA = tilesA[:2]
        ffn_phase("A", ff1_wg, ff1_wu, ff1_wd, 0, 8, tilesA)
        barrier()

        LP = NMETA + SEQ
        w_inA = din("w_inA", [128, DC, 2048])
        w_inB = din("w_inB", [128, DC, 72])
        uT = dscr("uT", [NSEQ, 128, 6, LP], BF16)
        kiT = dscr("kiT", [NSEQ, 128, LP], BF16)
        kT = dscr("kT", [NSEQ, 128, LP], BF16)
        qiT = dscr("qiT", [NSEQ, 128, 4, SEQ], BF16)
        qT = dscr("qT", [NSEQ, 128, 4, SEQ], BF16)
        vwS = dscr("vwS", [NSEQ, LP, 72], F32)
        hnT = dscr("hnT", [NSEQ, 128, DC, SEQ], BF16)

        def phase_B():
            tag = "B"
            with ExitStack() as st:
                wA = sb(st, "BwA", [128, DC, 2048], BF16)
                wB = sb(st, "BwB", [128, DC, 72], BF16)
                for k in range(DC):
                    S.dma("pool", "w", wA[:, k, :], w_inA[:, k, :], writes=[("B", "wA")], max_dma_last_dim=4096)
                S.dma("pool", "w", wB[:, :, :], w_inB[:, :, :], writes=[("B", "wB")])
                xt = [sb(st, "Bxt%d" % i, [128, DC, TT]) for i in range(2)]
                hn = sb(st, "Bhn", [128, DC, TT], BF16)
                sq = sb(st, "Bsq", [128, 2, TT])
                rs = sb(st, "Brs", [128, TT])
                stage = sb(st, "Bstage", [128, 16, TT], BF16)
                vw = sb(st, "Bvw", [128, 4, 72])
                pss = ps(st, "Bpss", [128, TT])
                pp = [ps(st, "Bpp%d" % i, [128, TT]) for i in range(2)]
                pv = [ps(st, "Bpv%d" % i, [128, 72]) for i in range(2)]
                tiles = [("m", 0)] + [(s, t) for s in range(NSEQ) for t in range(NT)]

                def load(i):
                    s_, t_ = tiles[i]
                    if s_ == "m":
                        S.dma("sp", "x", xt[i % 2][:, :, :NMETA], h1m[:, :, :], reads=["h1m"], writes=[("B", "xt", i % 2)])
                    else:
                        S.dma("sp", "x", xt[i % 2][:, :, :], h1T[s_, :, :, t_ * TT:(t_ + 1) * TT],
                              reads=[("h1T", s_, t_)], writes=[("B", "xt", i % 2)])

                load(0)
                for i, (s_, t_) in enumerate(tiles):
                    if i + 1 < len(tiles):
                        load(i + 1)
                    n = NMETA if s_ == "m" else TT
                    x = xt[i % 2]
                    kx = ("B", "xt", i % 2)
                    prenorm("B", x, kx, n, 16, hn, sq, pss, rs)
                    if s_ != "m":
                        S.dma("act", "o", hnT[s_, :, :, t_ * TT:(t_ + 1) * TT], hn[:, :, :], reads=[("B", "hn")],
                              writes=[("hnT", s_, t_)])
                    for cc in range(16):
                        b_ = cc % 2
                        mms(pp[b_][:, :n], [(wA[:, k, cc * 128:(cc + 1) * 128], hn[:, k, :n]) for k in range(DC)],
                            [("B", "wA"), ("B", "hn")], ("B", "pp", b_))
                        sc_ = 0.125 if 10 <= cc < 14 else 1.0
                        S.op("act", lambda e, cc=cc, b_=b_, sc_=sc_: e.activation(
                            out=stage[:, cc, :n], in_=pp[b_][:, :n], func=AF.Copy, scale=sc_),
                            reads=[("B", "pp", b_)], writes=[("B", "stage")])
                    if s_ == "m":
                        for s2 in range(NSEQ):
                            S.dma("sp", "o", uT[s2, :, :, 0:NMETA], stage[:, 0:6, :NMETA], reads=[("B", "stage")],
                                  writes=[("uT", s2, "m")])
                            S.dma("sp", "o", kiT[s2, :, 0:NMETA], stage[:, 14, :NMETA], reads=[("B", "stage")],
                                  writes=[("kiT", s2, "m")])
                            S.dma("sp", "o", kT[s2, :, 0:NMETA], stage[:, 15, :NMETA], reads=[("B", "stage")],
                                  writes=[("kT", s2, "m")])
                    else:
                        t0 = t_ * TT
                        S.dma("sp", "o", uT[s_, :, :, NMETA + t0:NMETA + t0 + TT], stage[:, 0:6, :],
                              reads=[("B", "stage")], writes=[("uT", s_, t_)])
                        S.dma("sp", "o", qiT[s_, :, :, t0:t0 + TT], stage[:, 6:10, :], reads=[("B", "stage")],
                              writes=[("qiT", s_, t_)])
                        S.dma("sp", "o", qT[s_, :, :, t0:t0 + TT], stage[:, 10:14, :], reads=[("B", "stage")],
                              writes=[("qT", s_, t_)])
                        S.dma("sp", "o", kiT[s_, :, NMETA + t0:NMETA + t0 + TT], stage[:, 14, :],
                              reads=[("B", "stage")], writes=[("kiT", s_, t_)])
                        S.dma("sp", "o", kT[s_, :, NMETA + t0:NMETA + t0 + TT], stage[:, 15, :],
                              reads=[("B", "stage")], writes=[("kT", s_, t_)])
                    nb = max(1, n // 128)
                    rows = min(n, 128)
                    for blk in range(nb):
                        b_ = blk % 2
                        mms(pv[b_][:rows, :], [(hn[:, k, blk * 128:blk * 128 + rows], wB[:, k, :]) for k in range(DC)],
                            [("B", "wB"), ("B", "hn")], ("B", "pv", b_))
                        S.op("dve", lambda e, blk=blk, b_=b_: e.tensor_copy(out=vw[:rows, blk, :], in_=pv[b_][:rows, :]),
                             reads=[("B", "pv", b_)], writes=[("B", "vw")])
                    if s_ == "m":
                        for s2 in range(NSEQ):
                            S.dma("sp", "o", vwS[s2, 0:NMETA, :], vw[:NMETA, 0, :], reads=[("B", "vw")],
                                  writes=[("vwS", s2, "m")])
                    else:
                        S.dma("sp", "o", vwS[s_, NMETA + t0:NMETA + t0 + TT, :].rearrange("(b p) c -> p b c", p=128),
                              vw[:, :, :], reads=[("B", "vw")], writes=[("vwS", s_, t_)])

        if debug != "A":
            phase_B()
            barrier()

        ALLT = ["m"] + list(range(NT))

        if debug == "B":
            with ExitStack() as st:
                tmp = sb(st, "dbgtmp", [128, 6, LP], BF16)
                tmp2 = sb(st, "dbgtmp2", [128, 6, LP], F32)
                S.dma("sp", "x", tmp[:, :, :], uT[0, :, :, :], reads=[("uT", 0, t) for t in ALLT], writes=["dbgtmp"])
                S.op("dve", lambda e: e.tensor_copy(out=tmp2[:, :, :], in_=tmp[:, :, :]), reads=["dbgtmp"], writes=["dbgtmp2"])
                S.dma("sp", "o", outT[0, :, 0:6, 0:SEQ], tmp2[:, :, NMETA:LP], reads=["dbgtmp2"], writes=["out"])
                S.dma("sp", "x", tmp2[:, 0, 0:72 * 16].rearrange("p (b c) -> p b c", c=72),
                      vwS[0, NMETA:NMETA + 2048, :].rearrange("(b p) c -> p b c", p=128),
                      reads=[("vwS", 0, t) for t in ALLT] + ["out"], writes=["dbgtmp2"])
                S.dma("sp", "o", outT[1, :, 0, 0:72 * 16], tmp2[:, 0, 0:72 * 16], reads=["dbgtmp2"], writes=["out"])


        s5_pcm = din("s5_pcm", [128, 6, 3, 128])
        s5_bcm = din("s5_bcm", [128, 6, 2, 128])
        s5_psm = din("s5_psm", [128, 3, 16])
        s5_bsm = din("s5_bsm", [128, 16, 2, 32])
        s5_csm = din("s5_csm", [128, 16, 2, 32])
        s5_d = din("s5_d", [128, 6])
        ident_d = din("ident", [128, 128])
        yaT = dscr("yaT", [NSEQ, 128, 6, SEQ], BF16)
        NCH = LP // 16
        ident_f = sb(es, "ident_f", [128, 128])
        ident_b = sb(es, "ident_b", [128, 128], BF16)
        S.dma("sp", "c", ident_f[:, :], ident_d[:, :], writes=["ident_f"])
        S.op("dve", lambda e: e.tensor_copy(out=ident_b[:, :], in_=ident_f[:, :]), reads=["ident_f"], writes=["ident_b"])

        def phase_C():
            K_ = "Cprep"
            R_, W_ = [K_], [K_]

            def tt(eng, out, a, b_, op):
                S.op(eng, lambda e: e.tensor_tensor(out=out, in0=a, in1=b_, op=op), reads=R_, writes=W_)

            def tsc(eng, out, a, s1, op0, s2=None, op1=None):
                if op1 is None:
                    S.op(eng, lambda e: e.tensor_scalar(out=out, in0=a, scalar1=s1, scalar2=None, op0=op0), reads=R_, writes=W_)
                else:
                    S.op(eng, lambda e: e.tensor_scalar(out=out, in0=a, scalar1=s1, scalar2=s2, op0=op0, op1=op1),
                         reads=R_, writes=W_)

            def stt(out, a, sc_, b_, op0, op1):
                S.op("dve", lambda e: e.scalar_tensor_tensor(out=out, in0=a, scalar=sc_, in1=b_, op0=op0, op1=op1),
                     reads=R_, writes=W_)

            def actf(out, a, func, scale=1.0):
                S.op("act", lambda e: e.activation(out=out, in_=a, func=func, scale=scale), reads=R_, writes=W_)

            def cparams(st, nm, lr, li, ldt_, shp):
                T = lambda n_: sb(st, "C%s_%s" % (nm, n_), shp)
                dt, mag, th, sh, c, s_, t1, t2, t3 = [T(n_) for n_ in ("dt", "mag", "th", "sh", "c", "s", "t1", "t2", "t3")]
                are, aim, cre_, cim_ = [T(n_) for n_ in ("are", "aim", "cre", "cim")]
                A = lambda t_: t_[tuple(slice(None) for _ in shp)]
                actf(A(dt), ldt_, AF.Exp)
                tt("dve", A(t1), lr, A(dt), ALU.mult)
                actf(A(mag), A(t1), AF.Exp)
                tt("dve", A(th), li, A(dt), ALU.mult)
                actf(A(sh), A(th), AF.Sin, scale=1.0 / 32)
                actf(A(s_), A(th), AF.Sin, scale=1.0 / 16)
                tt("dve", A(t1), A(sh), A(sh), ALU.mult)
                tsc("dve", A(c), A(t1), -2.0, ALU.mult, 1.0, ALU.add)
                for _ in range(4):
                    tt("dve", A(t1), A(c), A(c), ALU.mult)
                    tt("dve", A(t2), A(s_), A(s_), ALU.mult)
                    tt("dve", A(t3), A(c), A(s_), ALU.mult)
                    tt("dve", A(c), A(t1), A(t2), ALU.subtract)
                    tsc("dve", A(s_), A(t3), 2.0, ALU.mult)
                tt("dve", A(are), A(mag), A(c), ALU.mult)
                tt("dve", A(aim), A(mag), A(s_), ALU.mult)
                tt("dve", A(t1), lr, lr, ALU.mult)
                tt("dve", A(t2), li, li, ALU.mult)
                tt("dve", A(t1), A(t1), A(t2), ALU.add)
                S.op("dve", lambda e: e.reciprocal(out=A(t1), in_=A(t1)), reads=R_, writes=W_)
                tsc("dve", A(t2), A(are), -1.0, ALU.add)
                tt("dve", A(t3), A(t2), lr, ALU.mult)
                tt("dve", A(c), A(aim), li, ALU.mult)
                tt("dve", A(t3), A(t3), A(c), ALU.add)
                tt("dve", A(cre_), A(t3), A(t1), ALU.mult)
                tt("dve", A(t3), A(aim), lr, ALU.mult)
                tt("dve", A(c), A(t2), li, ALU.mult)
                tt("dve", A(t3), A(t3), A(c), ALU.subtract)
                tt("dve", A(cim_), A(t3), A(t1), ALU.mult)
                return are, aim, cre_, cim_

            with ExitStack() as st:
                WS = sb(st, "C_WS", [128, 16, 6, 2, 128], BF16)
                WO = sb(st, "C_WO", [128, 16, 16, 2, 32], BF16)
                WK = sb(st, "C_WK", [128, 16, 6, 128], BF16)
                a16 = sb(st, "C_a16", [128, 2, 16])
                with ExitStack() as st2:
                    pc = sb(st2, "C_pc", [128, 6, 3, 128])
                    bc = sb(st2, "C_bc", [128, 6, 2, 128])
                    S.dma("sp", "c", pc[:, :, :, :], s5_pcm[:, :, :, :], writes=W_)
                    S.dma("sp", "c", bc[:, :, :, :], s5_bcm[:, :, :, :], writes=W_)
                    are, aim, cre_, cim_ = cparams(st2, "cm", pc[:, :, 0, :], pc[:, :, 1, :], pc[:, :, 2, :], [128, 6, 128])
                    wr = sb(st2, "C_wr", [128, 6, 128])
                    wi = sb(st2, "C_wi", [128, 6, 128])
                    u1 = sb(st2, "C_u1", [128, 6, 128])
                    u2 = sb(st2, "C_u2", [128, 6, 128])
                    F3 = (slice(None),) * 3

                    def cmul(orr, oi, xr, xi, yr, yi):
                        tt("dve", u1[F3], xr, yr, ALU.mult)
                        tt("pool", u2[F3], xi, yi, ALU.mult)
                        tt("dve", u1[F3], u1[F3], u2[F3], ALU.subtract)
                        tt("pool", u2[F3], xr, yi, ALU.mult)
                        tt("dve", oi, xi, yr, ALU.mult)
                        tt("dve", oi, oi, u2[F3], ALU.add)
                        S.op("dve", lambda e: e.tensor_copy(out=orr, in_=u1[F3]), reads=R_, writes=W_)

                    cmul(wr[F3], wi[F3], cre_[F3], cim_[F3], bc[:, :, 0, :], bc[:, :, 1, :])
                    for lag in range(16):
                        S.op("act", lambda e, lag=lag: e.activation(out=WS[:, lag, :, 0, :], in_=wr[F3], func=AF.Copy),
                             reads=R_, writes=W_)
                        S.op("act", lambda e, lag=lag: e.activation(out=WS[:, lag, :, 1, :], in_=wi[F3], func=AF.Copy),
                             reads=R_, writes=W_)
                        if lag < 15:
                            cmul(wr[F3], wi[F3], wr[F3], wi[F3], are[F3], aim[F3])
                barrier()
                with ExitStack() as st2:
                    pm = sb(st2, "C_pm", [128, 3, 16])
                    bs = sb(st2, "C_bs", [128, 16, 2, 32])
                    cs = sb(st2, "C_cs", [128, 16, 2, 32])
                    dsb = sb(st2, "C_d", [128, 6])
                    S.dma("sp", "c", pm[:, :, :], s5_psm[:, :, :], writes=W_)
                    S.dma("sp", "c", bs[:, :, :, :], s5_bsm[:, :, :, :], writes=W_)
                    S.dma("sp", "c", cs[:, :, :, :], s5_csm[:, :, :, :], writes=W_)
                    S.dma("sp", "c", dsb[:, :], s5_d[:, :], writes=W_)
                    sre, sim, scr, sci = cparams(st2, "sm", pm[:, 0, :], pm[:, 1, :], pm[:, 2, :], [128, 16])
                    apr = sb(st2, "C_apr", [128, 17, 16])
                    api = sb(st2, "C_api", [128, 17, 16])
                    napi = sb(st2, "C_napi", [128, 17, 16])
                    v1 = sb(st2, "C_v1", [128, 16])
                    v2 = sb(st2, "C_v2", [128, 16])
                    S.op("dve", lambda e: e.memset(apr[:, 0, :], 1.0), reads=R_, writes=W_)
                    S.op("dve", lambda e: e.memset(api[:, 0, :], 0.0), reads=R_, writes=W_)
                    for k in range(1, 17):
                        tt("dve", v1[:, :], apr[:, k - 1, :], sre[:, :], ALU.mult)
                        tt("dve", v2[:, :], api[:, k - 1, :], sim[:, :], ALU.mult)
                        tt("dve", apr[:, k, :], v1[:, :], v2[:, :], ALU.subtract)
                        tt("dve", v1[:, :], apr[:, k - 1, :], sim[:, :], ALU.mult)
                        tt("dve", v2[:, :], api[:, k - 1, :], sre[:, :], ALU.mult)
                        tt("dve", api[:, k, :], v1[:, :], v2[:, :], ALU.add)
                    tsc("dve", napi[:, :, :], api[:, :, :], -1.0, ALU.mult)
                    napr = sb(st2, "C_napr", [128, 17, 16])
                    tsc("dve", napr[:, :, :], apr[:, :, :], -1.0, ALU.mult)
                    S.op("dve", lambda e: e.tensor_copy(out=a16[:, 0, :], in_=apr[:, 16, :]), reads=R_, writes=W_)
                    S.op("dve", lambda e: e.tensor_copy(out=a16[:, 1, :], in_=api[:, 16, :]), reads=R_, writes=W_)
                    nsci = sb(st2, "C_nsci", [128, 16])
                    tsc("dve", nsci[:, :], sci[:, :], -1.0, ALU.mult)
                    bb = sb(st2, "C_bb", [128, 16, 2, 32])
                    x1 = sb(st2, "C_x1", [128, 32])
                    for i in range(16):
                        tsc("dve", x1[:, :], bs[:, i, 0, :], scr[:, i:i + 1], ALU.mult)
                        stt(bb[:, i, 0, :], bs[:, i, 1, :], nsci[:, i:i + 1], x1[:, :], ALU.mult, ALU.add)
                        tsc("dve", x1[:, :], bs[:, i, 1, :], scr[:, i:i + 1], ALU.mult)
                        stt(bb[:, i, 1, :], bs[:, i, 0, :], sci[:, i:i + 1], x1[:, :], ALU.mult, ALU.add)
                    Bs = sb(st2, "C_Bs", [128, 16, 2, 32])
                    tsc("dve", Bs[:, :, 0, :], bb[:, :, 1, :], -1.0, ALU.mult)
                    S.op("dve", lambda e: e.tensor_copy(out=Bs[:, :, 1, :], in_=bb[:, :, 0, :]), reads=R_, writes=W_)
                    Cp2 = sb(st2, "C_Cp2", [128, 16, 2, 32])
                    Cs2 = sb(st2, "C_Cs2", [128, 16, 2, 32])
                    S.op("dve", lambda e: e.tensor_copy(out=Cp2[:, :, 0, :], in_=cs[:, :, 0, :]), reads=R_, writes=W_)
                    tsc("dve", Cp2[:, :, 1, :], cs[:, :, 1, :], -1.0, ALU.mult)
                    tsc("dve", Cs2[:, :, 0, :], cs[:, :, 1, :], -1.0, ALU.mult)
                    tsc("dve", Cs2[:, :, 1, :], cs[:, :, 0, :], -1.0, ALU.mult)
                    crb = sb(st2, "C_crb", [128, 16, 2, 32], BF16)
                    S.op("dve", lambda e: e.tensor_copy(out=crb[:, :, :, :], in_=Cp2[:, :, :, :]), reads=R_, writes=W_)
                    S.op("pool", lambda e: e.memset(WK[:, :, :, :], 0.0), reads=R_, writes=W_)
                    barrier()
                    AB = sb(st2, "C_AB", [128, 2, 16, 2, 32], BF16)
                    tA = [sb(st2, "C_tA%d" % i, [128, 2, 32]) for i in range(2)]
                    tC = [sb(st2, "C_tC%d" % i, [128, 2, 32]) for i in range(2)]
                    psK = [ps(st2, "C_psK%d" % i, [128, 192]) for i in range(2)]
                    for lag in range(16):
                        lp_ = lag % 2
                        for i in range(16):
                            ip = i % 2
                            r0 = 32 * (i % 3)
                            cc_ = i // 3
                            S.op("act", lambda e, i=i, ip=ip, lag=lag: e.activation(out=tA[ip][:, :, :], in_=bb[:, i, :, :], func=AF.Copy,
                                                                                  scale=apr[:, lag, i:i + 1]),
                                 reads=[], writes=[("C_tA", ip)])
                            S.op("dve", lambda e, i=i, ip=ip, lag=lag, lp_=lp_: e.scalar_tensor_tensor(
                                out=AB[:, lp_, i, :, :], in0=Bs[:, i, :, :], scalar=api[:, lag, i:i + 1], in1=tA[ip][:, :, :],
                                op0=ALU.mult, op1=ALU.add), reads=[("C_tA", ip)], writes=[("C_AB", lp_, i)])
                            S.op("act", lambda e, i=i, ip=ip, lag=lag: e.activation(out=tC[ip][:, :, :], in_=Cp2[:, i, :, :], func=AF.Copy,
                                                                                  scale=apr[:, lag + 1, i:i + 1]),
                                 reads=[], writes=[("C_tC", ip)])
                            S.op("dve", lambda e, i=i, ip=ip, lag=lag: e.scalar_tensor_tensor(
                                out=WO[:, lag, i, :, :], in0=Cs2[:, i, :, :], scalar=api[:, lag + 1, i:i + 1], in1=tC[ip][:, :, :],
                                op0=ALU.mult, op1=ALU.add), reads=[("C_tC", ip)], writes=["C_WOw"])
                            S.op("pe", lambda e, i=i, r0=r0, cc_=cc_, lp_=lp_: e.matmul(
                                psK[lp_][r0:r0 + 32, cc_ * 32:(cc_ + 1) * 32], lhsT=AB[:, lp_, i, 0, :], rhs=crb[:, i, 0, :],
                                start=True, stop=False), reads=[("C_AB", lp_, i)], writes=[("C_psK", lp_, i)])
                            S.op("pe", lambda e, i=i, r0=r0, cc_=cc_, lp_=lp_: e.matmul(
                                psK[lp_][r0:r0 + 32, cc_ * 32:(cc_ + 1) * 32], lhsT=AB[:, lp_, i, 1, :], rhs=crb[:, i, 1, :],
                                start=False, stop=True), reads=[("C_AB", lp_, i)], writes=[("C_psK", lp_, i)])
                            S.op("pool" if False else "act", lambda e, i=i, r0=r0, cc_=cc_, lag=lag, lp_=lp_: e.activation(
                                out=WK[r0:r0 + 32, lag, cc_, r0:r0 + 32], in_=psK[lp_][r0:r0 + 32, cc_ * 32:(cc_ + 1) * 32],
                                func=AF.Copy), reads=[("C_psK", lp_, i)], writes=["C_WKw"])
                    barrier()
                    for cc_ in range(6):
                        stt(WK[:, 0, cc_, :], ident_f[:, :], dsb[:, cc_:cc_ + 1], WK[:, 0, cc_, :], ALU.mult, ALU.add)
                barrier()
                usb = sb(st, "C_u", [128, 6, LP], BF16)
                Ssb = sb(st, "C_S", [128, 16, 2, NCH])
                Xbf = sb(st, "C_Xbf", [128, 16, 2, NCH], BF16)
                ypre = sb(st, "C_ypre", [128, SEQ])
                g1 = sb(st, "C_g1", [128, SEQ])
                ya = sb(st, "C_ya", [128, 6, SEQ], BF16)
                w1 = sb(st, "C_w1", [128, 16])
                w2 = sb(st, "C_w2", [128, 16])
                w3 = sb(st, "C_w3", [128, 16])
                w4 = sb(st, "C_w4", [128, 16])
                psS = [ps(st, "C_psS%d" % i, [128, NCH]) for i in range(2)]
                psY = [ps(st, "C_psY%d" % i, [128, NCH]) for i in range(2)]
                for s_ in range(NSEQ):
                    S.dma("sp", "x", usb[:, :, :], uT[s_, :, :, :], reads=[("uT", s_, t) for t in ALLT], writes=["C_u"])
                    n_ = 0
                    for i in range(16):
                        r0 = 32 * (i % 3)
                        cc_ = i // 3
                        for ri in range(2):
                            b_ = n_ % 2
                            n_ += 1
                            mms(psS[b_][:, :], [(WS[r0:r0 + 32, 15 - j, cc_, ri, :], usb[r0:r0 + 32, cc_, j:LP:16])
                                                for j in range(16)], ["C_u", K_], ("C_psS", b_))
                            S.op("act", lambda e, i=i, ri=ri, b_=b_: e.activation(out=Ssb[:, i, ri, :], in_=psS[b_][:, :],
                                                                               func=AF.Copy),
                                 reads=[("C_psS", b_)], writes=["C_S"])
                    for c in range(1, NCH - 1):
                        S.op("dve", lambda e, c=c: e.tensor_tensor(out=w1[:, :], in0=Ssb[:, :, 0, c - 1], in1=a16[:, 0, :], op=ALU.mult),
                             reads=["C_S", K_], writes=["C_w1"])
                        S.op("dve", lambda e, c=c: e.tensor_tensor(out=w2[:, :], in0=Ssb[:, :, 1, c - 1], in1=a16[:, 1, :], op=ALU.mult),
                             reads=["C_S"], writes=["C_w2"])
                        S.op("pool", lambda e, c=c: e.tensor_tensor(out=w3[:, :], in0=Ssb[:, :, 1, c - 1], in1=a16[:, 0, :], op=ALU.mult),
                             reads=["C_S", K_], writes=["C_w3"])
                        S.op("pool", lambda e, c=c: e.tensor_tensor(out=w4[:, :], in0=Ssb[:, :, 0, c - 1], in1=a16[:, 1, :], op=ALU.mult),
                             reads=["C_S"], writes=["C_w4"])
                        S.op("dve", lambda e: e.tensor_tensor(out=w1[:, :], in0=w1[:, :], in1=w2[:, :], op=ALU.subtract),
                             reads=["C_w1", "C_w2"], writes=["C_w1"])
                        S.op("pool", lambda e: e.tensor_tensor(out=w3[:, :], in0=w3[:, :], in1=w4[:, :], op=ALU.add),
                             reads=["C_w3", "C_w4"], writes=["C_w3"])
                        S.op("dve", lambda e, c=c: e.tensor_tensor(out=Ssb[:, :, 0, c], in0=w1[:, :], in1=Ssb[:, :, 0, c], op=ALU.add),
                             reads=["C_w1", "C_w3", "C_S"], writes=["C_Sa"])
                        S.op("pool", lambda e, c=c: e.tensor_tensor(out=Ssb[:, :, 1, c], in0=w3[:, :], in1=Ssb[:, :, 1, c], op=ALU.add),
                             reads=["C_w3", "C_Sa", "C_S"], writes=["C_S"])
                    S.op("dve", lambda e: e.memset(Xbf[:, :, :, 0], 0.0), reads=["C_Xbf"], writes=["C_Xbf"])
                    S.op("dve", lambda e: e.tensor_copy(out=Xbf[:, :, :, 1:NCH], in_=Ssb[:, :, :, 0:NCH - 1]), reads=["C_S", "C_Xbf"], writes=["C_Xbf"])
                    n_ = 0
                    for cc_ in range(6):
                        tiles_cc = [i for i in range(3 * cc_, min(3 * cc_ + 3, 16))]
                        for tau in range(16):
                            b_ = n_ % 2
                            n_ += 1
                            pairs = [(WK[:, tau - j, cc_, :], usb[:, cc_, j:LP:16]) for j in range(tau + 1)]
                            if tau == 0:
                                pairs.append((zeros_b[:, :], usb[:, cc_, 0:LP:16]))
                            mms_out = psY[b_]
                            np_ = len(pairs)

                            def intra(idx):
                                l, r_ = pairs[idx]
                                S.op("pe", lambda e: e.matmul(mms_out[:, :], lhsT=l, rhs=r_, start=(idx == 0), stop=(idx == np_ - 1)),
                                     reads=["C_u", K_, "zeros_b"], writes=[("C_psY", b_)])
                            intra(0)
                            for i in tiles_cc:
                                r0 = 32 * (i % 3)
                                for ri in range(2):
                                    S.op("pe", lambda e, i=i, ri=ri, r0=r0, tau=tau: e.matmul(
                                        mms_out[r0:r0 + 32, :], lhsT=WO[:, tau, i, ri, :], rhs=Xbf[:, i, ri, :], start=False, stop=False),
                                        reads=["C_Xbf", K_], writes=[("C_psY", b_)])
                            for idx in range(1, np_):
                                intra(idx)
                            S.op("act", lambda e, tau=tau, b_=b_: e.activation(
                                out=ypre[:, tau:SEQ:16], in_=psY[b_][:, 1:NCH], func=AF.Copy),
                                reads=[("C_psY", b_)], writes=["C_ypre"])
                        S.op("act", lambda e: e.activation(out=g1[:, :], in_=ypre[:, :], func=AF.Square), reads=["C_ypre"], writes=["C_g1"])
                        S.op("dve", lambda e: e.tensor_scalar(out=g1[:, :], in0=g1[:, :], scalar1=0.0713548162726, scalar2=1.5957691216,
                                                              op0=ALU.mult, op1=ALU.add), reads=["C_g1"], writes=["C_g1"])
                        S.op("dve", lambda e: e.tensor_tensor(out=g1[:, :], in0=g1[:, :], in1=ypre[:, :], op=ALU.mult), reads=["C_g1", "C_ypre"], writes=["C_g1"])
                        S.op("act", lambda e: e.activation(out=g1[:, :], in_=g1[:, :], func=AF.Sigmoid), reads=["C_g1"], writes=["C_g1"])
                        S.op("dve", lambda e, cc_=cc_: e.tensor_tensor(out=ya[:, cc_, :], in0=g1[:, :], in1=ypre[:, :], op=ALU.mult),
                             reads=["C_g1", "C_ypre"], writes=["C_ya"])
                    S.dma("sp", "o", yaT[s_, :, :, :], ya[:, :, :], reads=["C_ya"], writes=[("yaT", s_)])

        if debug not in ("A", "B"):
            phase_C()
            barrier()

        if debug == "C":
            with ExitStack() as st:
                tmp = sb(st, "dbgtmp", [128, 6, SEQ], BF16)
                tmp2 = sb(st, "dbgtmp2", [128, 6, SEQ], F32)
                S.dma("sp", "x", tmp[:, :, :], yaT[0, :, :, :], reads=[("yaT", 0)], writes=["dbgtmp"])
                S.op("dve", lambda e: e.tensor_copy(out=tmp2[:, :, :], in_=tmp[:, :, :]), reads=["dbgtmp"], writes=["dbgtmp2"])
                S.dma("sp", "o", outT[0, :, 0:6, 0:SEQ], tmp2[:, :, :], reads=["dbgtmp2"], writes=["out"])

        biasG_d = din("biasG", [128, 8, 1024])
        biasM_d = din("biasM", [16, 8, 512])
        cvec_d = din("cvec", [128, 8])
        ybT = dscr("ybT", [NSEQ, 128, 4, SEQ], BF16)
        ones_b = sb(es, "ones_b", [128, 128], BF16)
        S.op("dve", lambda e: e.memset(ones_b[:, :], 1.0), writes=["ones_b"])
        NIT = 22
        TOPK = 256.0

        def phase_D():
            with ExitStack() as st:
                Gb = sb(st, "D_Gb", [128, 8, 1024], BF16)
                Mb = sb(st, "D_Mb", [16, 8, 512], BF16)
                cv = sb(st, "D_cv", [128, 8])
                S.dma("sp", "c", cv[:, :], cvec_d[:, :], writes=["D_cv"])
                with ExitStack() as st2:
                    Gf = sb(st2, "D_Gf", [128, 8, 1024])
                    Mf = sb(st2, "D_Mf", [16, 8, 512])
                    S.dma("sp", "c", Gf[:, :, :], biasG_d[:, :, :], writes=["D_Gf"])
                    S.dma("sp", "c", Mf[:, :, :], biasM_d[:, :, :], writes=["D_Mf"])
                    for h in range(8):
                        S.op("dve", lambda e, h=h: e.tensor_scalar(out=Gb[:, h, :], in0=Gf[:, h, :], scalar1=cv[:, h:h + 1],
                                                                   scalar2=None, op0=ALU.subtract),
                             reads=["D_Gf", "D_cv"], writes=["D_Gb"])
                        S.op("dve", lambda e, h=h: e.tensor_scalar(out=Mb[:, h, :], in0=Mf[:, h, :], scalar1=cv[:16, h:h + 1],
                                                                   scalar2=None, op0=ALU.subtract),
                             reads=["D_Mf", "D_cv"], writes=["D_Mb"])
                    barrier()
                qi = sb(st, "D_qi", [128, 4, SEQ], BF16)
                ki = sb(st, "D_ki", [128, LP], BF16)
                qq = sb(st, "D_q", [128, 4, SEQ], BF16)
                kk = sb(st, "D_k", [128, LP], BF16)
                vd = sb(st, "D_vd", [128, 17, 2, 128], BF16)
                wq = sb(st, "D_wq", [128, 16, 8])
                sc = [sb(st, "D_sc%d" % i, [128, 4, LP]) for i in range(2)]
                MA = [sb(st, "D_MA%d" % i, [128, 4, LP], BF16) for i in range(2)]
                junk = sb(st, "D_junk", [128, LP], BF16)
                Rb = [sb(st, "D_Rb%d" % i, [128, 512], BF16) for i in range(2)]
                dg = sb(st, "D_dg", [128, 8, 128], BF16)
                Pt = [sb(st, "D_Pt%d" % i, [128, 512], BF16) for i in range(3)]
                rd = sb(st, "D_rd", [128, 512])
                rds = sb(st, "D_rds", [128, 512])
                yb = sb(st, "D_yb", [128, 4, 512], BF16)
                lo = [sb(st, "D_lo%d" % i, [128, 4]) for i in range(2)]
                hi = sb(st, "D_hi", [128, 4])
                W0 = sb(st, "D_W0", [128, 4])
                Wk = sb(st, "D_Wk", [128, 4])
                mid = sb(st, "D_mid", [128, 4])
                cnt = sb(st, "D_cnt", [128, 4])
                stp = sb(st, "D_stp", [128, 4])
                pq = [ps(st, "D_pq%d" % i, [128, 512]) for i in range(2)]
                psc = ps(st, "D_psc", [128, 512])
                pL = [ps(st, "D_pL%d" % i, [128, 512]) for i in range(2)]
                pOD = [ps(st, "D_pOD%d" % i, [128, 512]) for i in range(2)]
                pSh = ps(st, "D_pSh", [128, 512])
                S.op("dve", lambda e: e.memset(vd[:, :, 0, 64:128], 1.0), writes=["D_vd1"])
                S.op("dve", lambda e: e.memset(vd[:, :, 1, 0:64], 1.0), writes=["D_vd1"])
                S.op("dve", lambda e: e.memset(rd[:, :], 1.0), writes=["D_rd"])
                ACT_JL = (2, 3)
                nmid = sb(st, "D_nmid", [128, 4])
                junkA = sb(st, "D_junkA", [128, LP], BF16)
                thrc = sb(st, "D_thrc", [128, 4, 4])
                for Q_ in range(4):
                    for jl_ in range(4):
                        v_ = (510.5 - (NMETA + 128 * (4 * Q_ + jl_ + 1))) if jl_ in ACT_JL else TOPK
                        S.op("dve", lambda e, Q_=Q_, jl_=jl_, v_=v_: e.memset(thrc[:, Q_, jl_:jl_ + 1], float(v_)), writes=["D_thrc"])

                def indexer(s_, Q):
                    u = Q % 2
                    for jl in range(4):
                        j = 4 * Q + jl
                        Nj = NMETA + 128 * (j + 1)
                        for h in range(8):
                            S.op("pool", lambda e, h=h, j=j: e.tensor_scalar(out=dg[:, h, :], in0=ident_f[:, :], scalar1=wq[:, j, h:h + 1],
                                                                           scalar2=None, op0=ALU.mult),
                                 reads=["ident_f", "D_wq"], writes=["D_dg"])
                        for c0 in range(0, Nj, 512):
                            cw = min(512, Nj - c0)

                            def qk(h):
                                hh, hp, b_ = h % 2, h // 2, h % 2
                                S.op("pe", lambda e: e.matmul(
                                    pq[b_][:, :cw], lhsT=qi[64 * hh:64 * hh + 64, hp, 128 * j:128 * j + 128],
                                    rhs=ki[64 * hh:64 * hh + 64, c0:c0 + cw], start=True, stop=True),
                                    reads=["D_qi", "D_ki"], writes=[("D_pq", b_)])
                                S.op("act", lambda e: e.activation(out=Rb[b_][:, :cw], in_=pq[b_][:, :cw], func=AF.Relu),
                                     reads=[("D_pq", b_)], writes=[("D_Rb", b_)])

                            def dgm(h):
                                b_ = h % 2
                                S.op("pe", lambda e: e.matmul(psc[:, :cw], lhsT=dg[:, h, :], rhs=Rb[b_][:, :cw],
                                                              start=(h == 0), stop=(h == 7)),
                                     reads=[("D_Rb", b_), "D_dg"], writes=["D_psc"])

                            qk(0)
                            qk(1)
                            for h in range(8):
                                dgm(h)
                                if h + 2 < 8:
                                    qk(h + 2)
                            S.op("act", lambda e, jl=jl, c0=c0, cw=cw: e.activation(out=sc[u][:, jl, c0:c0 + cw], in_=psc[:, :cw], func=AF.Copy),
                                 reads=["D_psc"], writes=[("D_sc", u, jl)])
                        S.op("dve", lambda e, jl=jl, Nj=Nj: e.tensor_reduce(out=lo[u][:, jl:jl + 1], in_=sc[u][:, jl, :Nj], axis=AX.X, op=ALU.min),
                             reads=[("D_sc", u, jl)], writes=[("D_lo", u)])
                        S.op("dve", lambda e, jl=jl, Nj=Nj: e.tensor_reduce(out=hi[:, jl:jl + 1], in_=sc[u][:, jl, :Nj], axis=AX.X, op=ALU.max),
                             reads=[("D_sc", u, jl)], writes=["D_hi"])
                        S.op("dve", lambda e, jl=jl, Nj=Nj: e.memset(sc[u][0:64, jl, Nj - 64:Nj], -1e30),
                             reads=[("D_sc", u, jl), ("D_lo", u), "D_hi"], writes=[("D_sc", u, jl)])

                def bisect_steps(Q):
                    u = Q % 2
                    L_ = lo[u]
                    steps = []

                    def init():
                        S.op("dve", lambda e: e.tensor_tensor(out=W0[:, :], in0=hi[:, :], in1=L_[:, :], op=ALU.subtract),
                             reads=[("D_lo", u), "D_hi"], writes=["D_W0"])
                    steps.append(init)

                    def mk(it):
                        def f():
                            S.op("dve", lambda e: e.tensor_scalar(out=Wk[:, :], in0=W0[:, :], scalar1=2.0 ** (-(it + 1)), scalar2=None,
                                                                  op0=ALU.mult), reads=["D_W0", "D_stp"], writes=["D_Wk"])
                            S.op("dve", lambda e: e.tensor_tensor(out=mid[:, :], in0=L_[:, :], in1=Wk[:, :], op=ALU.add),
                                 reads=[("D_lo", u), "D_Wk"], writes=["D_mid"])
                            if ACT_JL:
                                S.op("dve", lambda e: e.tensor_scalar(out=nmid[:, :], in0=mid[:, :], scalar1=-1.0, scalar2=None, op0=ALU.mult),
                                     reads=["D_mid"], writes=["D_nmid"])
                            for jl in range(4):
                                Nj = NMETA + 128 * (4 * Q + jl + 1)
                                if jl in ACT_JL:
                                    S.op("act", lambda e, jl=jl, Nj=Nj: e.activation(
                                        out=junkA[:, :Nj], in_=sc[u][:, jl, :Nj], func=AF.Sign, bias=nmid[:, jl:jl + 1], scale=1.0,
                                        accum_out=cnt[:, jl:jl + 1]),
                                        reads=[("D_sc", u, jl), "D_nmid"], writes=["D_junkA", ("D_cnt", jl)])
                                else:
                                    S.op("dve", lambda e, jl=jl, Nj=Nj: e.tensor_scalar(
                                        out=junk[:, :Nj], in0=sc[u][:, jl, :Nj], scalar1=mid[:, jl:jl + 1], scalar2=0.0,
                                        op0=ALU.is_ge, op1=ALU.add, accum_out=cnt[:, jl:jl + 1]),
                                        reads=[("D_sc", u, jl), "D_mid"], writes=["D_junk", ("D_cnt", jl)])
                            S.op("dve", lambda e: e.tensor_tensor(out=stp[:, :], in0=cnt[:, :], in1=thrc[:, Q, :], op=ALU.is_ge),
                                 reads=[("D_cnt", jl) for jl in range(4)] + ["D_thrc"], writes=["D_stp"])
                            S.op("dve", lambda e: e.tensor_tensor(out=stp[:, :], in0=stp[:, :], in1=Wk[:, :], op=ALU.mult),
                                 reads=["D_stp", "D_Wk"], writes=["D_stp"])
                            S.op("dve", lambda e: e.tensor_tensor(out=L_[:, :], in0=L_[:, :], in1=stp[:, :], op=ALU.add),
                                 reads=[("D_lo", u), "D_stp"], writes=[("D_lo", u)])
                        return f
                    for it in range(NIT):
                        steps.append(mk(it))

                    def fin():
                        for jl in range(4):
                            Nj = NMETA + 128 * (4 * Q + jl + 1)
                            S.op("dve", lambda e, jl=jl, Nj=Nj: e.tensor_scalar(out=MA[u][:, jl, :Nj], in0=sc[u][:, jl, :Nj], scalar1=L_[:, jl:jl + 1],
                                                                              scalar2=-30000.0, op0=ALU.is_lt, op1=ALU.mult),
                                 reads=[("D_sc", u, jl), ("D_lo", u)], writes=[("D_MA", u)])
                    steps.append(fin)
                    return steps

                def attention(s_, Q, filler):
                    u = Q % 2
                    nblk = 4 * Q + 5
                    tiles = []
                    for h in range(8):
                        for b in range(nblk):
                            tiles.append((h, b))
                    NTL = len(tiles)

                    def geo(b):
                        w = NMETA if b == 0 else 128
                        pc0 = 0 if b == 0 else NMETA + 128 * (b - 1)
                        jl0 = max(0, b - 1 - 4 * Q)
                        return w, pc0, jl0

                    def stageA(n):
                        h, b = tiles[n]
                        hh, hp = h % 2, h // 2
                        w, pc0, jl0 = geo(b)
                        c0 = jl0 * 128
                        near = (b == 0 and Q == 0) or (b >= 1 and b - 1 >= 4 * Q - 1)
                        lb, pb_ = n % 2, n % 3
                        S.op("pe", lambda e: e.matmul(
                            pL[lb][:w, c0:512], lhsT=kk[64 * hh:64 * hh + 64, pc0:pc0 + w],
                            rhs=qq[64 * hh:64 * hh + 64, hp, 512 * Q + c0:512 * Q + 512], start=True, stop=False),
                            reads=["D_k", "D_q"], writes=[("D_pL", lb)])
                        if near:
                            if b == 0:
                                S.op("pe", lambda e: e.matmul(pL[lb][:NMETA, c0:512], lhsT=ident_b[:NMETA, :NMETA],
                                                              rhs=Mb[:NMETA, h, c0:512], start=False, stop=False),
                                     reads=["D_Mb", "ident_b"], writes=[("D_pL", lb)])
                            else:
                                z0 = 512 * Q + c0 - 128 * (b - 1) + 384
                                S.op("pe", lambda e: e.matmul(pL[lb][:, c0:512], lhsT=ident_b[:, :],
                                                              rhs=Gb[:, h, z0:z0 + 512 - c0], start=False, stop=False),
                                     reads=["D_Gb", "ident_b"], writes=[("D_pL", lb)])
                        for jl in range(jl0, 4):
                            S.op("pe", lambda e, jl=jl: e.matmul(pL[lb][:w, jl * 128:(jl + 1) * 128], lhsT=MA[u][:, jl, pc0:pc0 + w],
                                                                 rhs=ident_b[:, :], start=False, stop=(jl == 3)),
                                 reads=[("D_MA", u), "ident_b"], writes=[("D_pL", lb)])
                        S.op("act", lambda e: e.activation(out=Pt[pb_][:w, c0:512], in_=pL[lb][:w, c0:512], func=AF.Exp),
                             reads=[("D_pL", lb)], writes=[("D_Pt", pb_)])

                    def stageB(n):
                        h, b = tiles[n]
                        hh, hp = h % 2, h // 2
                        w, pc0, jl0 = geo(b)
                        c0 = jl0 * 128
                        pb_ = n % 3
                        ob = h % 2
                        S.op("pe", lambda e: e.matmul(pOD[ob][:, c0:512], lhsT=vd[:w, b, hh, :], rhs=Pt[pb_][:w, c0:512],
                                                      start=(b == 0), stop=(b == nblk - 1)),
                             reads=[("D_Pt", pb_), "D_vd", "D_vd1"], writes=[("D_pOD", ob)])
                        if b == nblk - 1:
                            orow = slice(64 * hh, 64 * hh + 64)
                            drow = slice(64 * (1 - hh), 64 * (1 - hh) + 64)
                            S.op("dve", lambda e: e.reciprocal(out=rd[drow, :], in_=pOD[ob][drow, :]),
                                 reads=[("D_pOD", ob)], writes=["D_rd"])
                            S.op("pe", lambda e: e.matmul(pSh[orow, :], lhsT=ident_f[drow, drow], rhs=rd[drow, :], start=True, stop=True),
                                 reads=["D_rd", "ident_f"], writes=["D_pSh"])
                            S.op("act", lambda e: e.activation(out=rds[orow, :], in_=pSh[orow, :], func=AF.Copy),
                                 reads=["D_pSh"], writes=["D_rds"])
                            S.op("dve", lambda e: e.tensor_tensor(out=yb[orow, hp, :], in0=rds[orow, :], in1=pOD[ob][orow, :], op=ALU.mult),
                                 reads=["D_rds", ("D_pOD", ob)], writes=["D_yb"])
                            for _ in range(3):
                                if filler:
                                    filler.pop(0)()

                    stageA(0)
                    stageA(1)
                    for n in range(NTL):
                        stageB(n)
                        if n + 2 < NTL:
                            stageA(n + 2)
                    while filler:
                        filler.pop(0)()
                    S.dma("sp", "o", ybT[s_, :, :, 512 * Q:512 * Q + 512], yb[:, :, :], reads=["D_yb"], writes=[("ybT", s_, Q)])

                for s_ in range(NSEQ):
                    S.dma("sp", "x", qi[:, :, :], qiT[s_, :, :, :], reads=[("qiT", s_, t) for t in range(NT)], writes=["D_qi"])
                    S.dma("sp", "x", ki[:, :], kiT[s_, :, :], reads=[("kiT", s_, t) for t in ALLT], writes=["D_ki"])
                    S.dma("sp", "x", qq[:, :, :], qT[s_, :, :, :], reads=[("qT", s_, t) for t in range(NT)], writes=["D_q"])
                    S.dma("sp", "x", kk[:, :], kT[s_, :, :], reads=[("kT", s_, t) for t in ALLT], writes=["D_k"])
                    vr = [("vwS", s_, t) for t in ALLT]
                    for half in range(2):
                        S.dma("pool", "x", vd[:, 1:17, half, 64 * half:64 * half + 64],
                              vwS[s_, NMETA:LP, 0:64].rearrange("(b p) c -> p b c", p=128), reads=vr, writes=["D_vd"])
                        S.dma("pool", "x", vd[:NMETA, 0, half, 64 * half:64 * half + 64], vwS[s_, 0:NMETA, 0:64], reads=vr, writes=["D_vd"])
                    S.dma("sp", "x", wq[:, :, :], vwS[s_, NMETA:LP, 64:72].rearrange("(b p) c -> p b c", p=128), reads=vr, writes=["D_wq"])
                    indexer(s_, 0)
                    for f_ in bisect_steps(0):
                        f_()
                    for Q in range(4):
                        filler = []
                        if Q + 1 < 4:
                            indexer(s_, Q + 1)
                            filler = bisect_steps(Q + 1)
                        attention(s_, Q, filler)

        if debug not in ("A", "B", "C"):
            phase_D()
            barrier()

        if debug == "D":
            with ExitStack() as st:
                tmp = sb(st, "dbgtmp", [128, 4, SEQ], BF16)
                tmp2 = sb(st, "dbgtmp2", [128, 4, SEQ], F32)
                S.dma("sp", "x", tmp[:, :, :], ybT[0, :, :, :], reads=[("ybT", 0, t) for t in range(NT)], writes=["dbgtmp"])
                S.op("dve", lambda e: e.tensor_copy(out=tmp2[:, :, :], in_=tmp[:, :, :]), reads=["dbgtmp"], writes=["dbgtmp2"])
                S.dma("sp", "o", outT[0, :, 0:4, 0:SEQ], tmp2[:, :, :], reads=["dbgtmp2"], writes=["out"])

        w_glu_d = din("w_glu", [128, 6, 768])
        w_a_d = din("w_a", [128, 6, D])
        w_b_d = din("w_b", [128, 4, D])
        w_o_d = din("w_o", [128, DC, D])
        w_g_d = din("w_g", [128, DC, 2 * D])
        h2T = dscr("h2T", [NSEQ, 128, DC, SEQ])

        def phase_E():
            with ExitStack() as st:
                wglu = sb(st, "E_wglu", [128, 6, 768], BF16)
                wa = sb(st, "E_wa", [128, 6, D], BF16)
                wb = sb(st, "E_wb", [128, 4, D], BF16)
                wo = sb(st, "E_wo", [128, DC, D], BF16)
                wgt = sb(st, "E_wg", [128, DC, 2 * D], BF16)
                S.dma("pool", "w", wglu[:, :, :], w_glu_d[:, :, :], writes=["E_w"], max_dma_last_dim=3072)
                for k in range(6):
                    S.dma("pool", "w", wa[:, k, :], w_a_d[:, k, :], writes=["E_w"])
                for k in range(4):
                    S.dma("pool", "w", wb[:, k, :], w_b_d[:, k, :], writes=["E_w"])
                for k in range(DC):
                    S.dma("pool", "w", wo[:, k, :], w_o_d[:, k, :], writes=["E_w"])
                    S.dma("pool", "w", wgt[:, k, :], w_g_d[:, k, :], writes=["E_w"], max_dma_last_dim=4096)
                hn = sb(st, "E_hn", [128, DC, TT], BF16)
                ya = sb(st, "E_ya", [128, 6, TT], BF16)
                yg = sb(st, "E_yg", [128, 6, TT], BF16)
                ybt = sb(st, "E_yb", [128, 4, TT], BF16)
                h1t = sb(st, "E_h1", [128, DC, TT])
                sgl = sb(st, "E_sgl", [128, TT])
                ga = sb(st, "E_ga", [128, TT])
                gb = sb(st, "E_gb", [128, TT])
                t1 = sb(st, "E_t1", [128, TT])
                t2 = sb(st, "E_t2", [128, TT])
                mg = sb(st, "E_mg", [128, DC, TT], BF16)
                ysb = sb(st, "E_y", [128, DC, TT])
                sq = sb(st, "E_sq", [128, 2, TT])
                rs = sb(st, "E_rs", [128, TT])
                pga = ps(st, "E_pga", [128, TT])
                pgb = ps(st, "E_pgb", [128, TT])
                pa = ps(st, "E_pa", [128, TT])
                pb = ps(st, "E_pb", [128, TT])
                py = [ps(st, "E_py%d" % i, [128, TT]) for i in range(2)]
                pss = ps(st, "E_pss", [128, TT])
                for s_ in range(NSEQ):
                    for t_ in range(NT):
                        tsl = slice(t_ * TT, (t_ + 1) * TT)
                        S.dma("sp", "x", hn[:, :, :], hnT[s_, :, :, tsl], reads=[("hnT", s_, t_)], writes=["E_hn"])
                        S.dma("sp", "x", ya[:, :, :], yaT[s_, :, :, tsl], reads=[("yaT", s_)], writes=["E_ya"])
                        S.dma("sp", "x", ybt[:, :, :], ybT[s_, :, :, tsl], reads=[("ybT", s_, t_)], writes=["E_yb"])
                        S.dma("sp", "x", h1t[:, :, :], h1T[s_, :, :, tsl], reads=[("h1T", s_, t_)], writes=["E_h1"])
                        for oc in range(6):
                            b_ = oc % 2
                            mms(py[b_][:, :], [(wglu[:, k, oc * 128:(oc + 1) * 128], ya[:, k, :]) for k in range(6)],
                                ["E_w", "E_ya"], ("E_py", b_))
                            S.op("act", lambda e, b_=b_: e.activation(out=sgl[:, :], in_=py[b_][:, :], func=AF.Sigmoid),
                                 reads=[("E_py", b_)], writes=["E_sgl"])
                            S.op("dve", lambda e, oc=oc: e.tensor_tensor(out=yg[:, oc, :], in0=sgl[:, :], in1=ya[:, oc, :], op=ALU.mult),
                                 reads=["E_sgl", "E_ya"], writes=["E_yg"])
                        for dc in range(DC):
                            mms(pga[:, :], [(wgt[:, k, dc * 128:(dc + 1) * 128], hn[:, k, :]) for k in range(DC)], ["E_w", "E_hn"], "E_pga")
                            mms(pgb[:, :], [(wgt[:, k, D + dc * 128:D + (dc + 1) * 128], hn[:, k, :]) for k in range(DC)], ["E_w", "E_hn"], "E_pgb")
                            mms(pa[:, :], [(wa[:, k, dc * 128:(dc + 1) * 128], yg[:, k, :]) for k in range(6)], ["E_w", "E_yg"], "E_pa")
                            mms(pb[:, :], [(wb[:, k, dc * 128:(dc + 1) * 128], ybt[:, k, :]) for k in range(4)], ["E_w", "E_yb"], "E_pb")
                            S.op("act", lambda e: e.activation(out=ga[:, :], in_=pga[:, :], func=AF.Sigmoid), reads=["E_pga"], writes=["E_ga"])
                            S.op("act", lambda e: e.activation(out=gb[:, :], in_=pgb[:, :], func=AF.Sigmoid), reads=["E_pgb"], writes=["E_gb"])
                            S.op("dve", lambda e: e.tensor_tensor(out=t1[:, :], in0=ga[:, :], in1=pa[:, :], op=ALU.mult), reads=["E_ga", "E_pa"], writes=["E_t1"])
                            S.op("dve", lambda e: e.tensor_tensor(out=t2[:, :], in0=gb[:, :], in1=pb[:, :], op=ALU.mult), reads=["E_gb", "E_pb"], writes=["E_t2"])
                            S.op("pool", lambda e, dc=dc: e.tensor_tensor(out=mg[:, dc, :], in0=t1[:, :], in1=t2[:, :], op=ALU.add),
                                 reads=["E_t1", "E_t2"], writes=["E_mg"])
                        for c in range(DC):
                            b_ = c % 2
                            mms(py[b_][:, :], [(wo[:, k, c * 128:(c + 1) * 128], mg[:, k, :]) for k in range(DC)], ["E_w", "E_mg"], ("E_py", b_))
                            S.op("act", lambda e, c=c, b_=b_: e.activation(out=ysb[:, c, :], in_=py[b_][:, :], func=AF.Copy),
                                 reads=[("E_py", b_)], writes=[("E_y", c)])
                            S.op("act", lambda e, c=c, b_=b_: e.activation(out=sq[:, c % 2, :], in_=py[b_][:, :], func=AF.Square),
                                 reads=[("E_py", b_)], writes=[("E_sq", c % 2)])
                            S.op("pe", lambda e, c=c: e.matmul(pss[:, :], lhsT=ones_f[:, :], rhs=sq[:, c % 2, :], start=(c == 0), stop=(c == DC - 1)),
                                 reads=[("E_sq", c % 2), "ones_f"], writes=["E_pss"])
                        rstd_from_sumsq(pss, rs, TT, "E_pss", "E_rs")
                        for c in range(DC):
                            S.op("dve", lambda e, c=c: e.scalar_tensor_tensor(
                                out=ysb[:, c, :], in0=ysb[:, c, :], scalar=gains_sb[:, 24 + c:24 + c + 1], in1=rs[:, :],
                                op0=ALU.mult, op1=ALU.mult), reads=[("E_y", c), "E_rs", "gains"], writes=[("E_y", c)])
                            S.op("pool", lambda e, c=c: e.tensor_tensor(out=ysb[:, c, :], in0=ysb[:, c, :], in1=h1t[:, c, :], op=ALU.add),
                                 reads=[("E_y", c), "E_h1"], writes=[("E_y", c)])
                        S.dma("sp", "o", h2T[s_, :, :, tsl], ysb[:, :, :], reads=[("E_y", c) for c in range(DC)], writes=[("h2T", s_, t_)])

        if debug not in ("A", "B", "C", "D"):
            phase_E()
            barrier()
            ff2_wg = din("ff2_wg", [128, DC, DFF])
            ff2_wu = din("ff2_wu", [128, DC, DFF])
            ff2_wd = din("ff2_wd", [128, FC, D])
            tilesF = []
            for s in range(NSEQ):
                for t in range(NFT):
                    tilesF.append((h2T[s, :, :, t * FT:(t + 1) * FT], outT[s, :, :, t * FT:(t + 1) * FT], FT,
                                   [("h2T", s, t * FT // TT)], [("out", s, t)]))
            ffn_phase("F", ff2_wg, ff2_wu, ff2_wd, 32, 40, tilesF)

        if debug == "A":
            with ExitStack() as st:
                tmp = sb(st, "dbgtmp", [128, DC, FT])
                S.dma("sp", "x", tmp[:, :, :], h1T[0, :, :, 0:FT], reads=[("h1T", 0, 0)], writes=["dbgtmp"])
                S.dma("sp", "o", outT[0, :, :, 0:FT], tmp[:, :, :], reads=["dbgtmp"], writes=["out"])
                S.dma("sp", "x", tmp[:, :, :NMETA], h1m[:, :, :], reads=["h1m", "out"], writes=["dbgtmp"])
                S.dma("sp", "o", outT[1, :, :, 0:NMETA], tmp[:, :, :NMETA], reads=["dbgtmp"], writes=["out"])

        S.drain("sp")
        print("instructions:", S.ninst)
    return nc


def _rel_bucket(rel):
    half, me = 16, 8
    base = np.where(rel > 0, half, 0)
    n = np.abs(rel)
    nf = np.maximum(n, 1).astype(np.float32)
    large = me + (np.log(nf / me) / math.log(128 / me) * (half - me)).astype(np.int32)
    large = np.minimum(large, half - 1)
    return base + np.where(n < me, n, large)


def prep_inputs(inp):
    f = lambda a: np.ascontiguousarray(np.asarray(a, dtype=np.float32))
    x = f(inp["x"])
    B = x.shape[0]
    xT = np.ascontiguousarray(x.reshape(B, SEQ, DC, 128).transpose(0, 3, 2, 1))
    metaT = np.ascontiguousarray(f(inp["meta_tokens"]).reshape(NMETA, DC, 128).transpose(2, 1, 0))
    gl = [inp[k] for k in ("ff1_norm_pre", "ff1_norm_post", "mix_norm_pre", "mix_norm_post", "ff2_norm_pre",
                           "ff2_norm_post")]
    gains = np.ascontiguousarray(np.concatenate([f(g)[0].reshape(DC, 128).T for g in gl], axis=1))

    def wk(w, kc):
        w = f(w)
        return np.ascontiguousarray(w.reshape(kc, 128, w.shape[-1]).transpose(1, 0, 2))

    shared = {
        "metaT": metaT, "gains": gains,
        "ff1_wg": wk(inp["ff1_w_gate"][0], DC), "ff1_wu": wk(inp["ff1_w_up"][0], DC),
        "ff1_wd": wk(inp["ff1_w_down"][0], FC),
    }
    win = f(inp["w_in"][0])
    upad = np.zeros((D, 6, 128), np.float32)
    for c6 in range(6):
        w_ = min(96, 512 - 96 * c6)
        upad[:, c6, :w_] = win[:, 96 * c6:96 * c6 + w_]
    winA = np.concatenate([upad.reshape(D, 768), win[:, 512:1024], win[:, 1096:1608], win[:, 1024:1088], win[:, 1024:1088],
                           win[:, 1608:1672], win[:, 1608:1672]], axis=1)
    winB = np.concatenate([win[:, 1672:1736], win[:, 1088:1096]], axis=1)
    shared["w_inA"] = wk(winA, DC)
    shared["w_inB"] = wk(winB, DC)
    lre, lim, ldt = f(inp["ssm_lambda_re"][0]), f(inp["ssm_lambda_im"][0]), f(inp["ssm_log_dt"][0])
    bre, bim = f(inp["ssm_b_re"][0]), f(inp["ssm_b_im"][0])
    cre, cim = f(inp["ssm_c_re"][0]), f(inp["ssm_c_im"][0])
    r = np.arange(128)
    sidx = np.arange(128)
    cc = np.arange(6)
    i_rc = 3 * cc[None, :] + (r[:, None] // 32)
    val_rc = (r[:, None] < 96) & (i_rc < 16)
    i_rc = np.where(val_rc, i_rc, 0)
    g_rcs = 2 * i_rc[:, :, None] + (sidx[None, None, :] // 64)
    p_s = sidx % 64
    pcm = np.stack([lre[g_rcs, p_s[None, None, :]], lim[g_rcs, p_s[None, None, :]], ldt[g_rcs]], axis=2)
    glr = (r % 32) // 16
    m_r = r % 16
    msk = (glr[:, None, None] == (sidx[None, None, :] // 64)) & val_rc[:, :, None]
    bcm = np.stack([np.where(msk, bre[g_rcs, p_s[None, None, :], m_r[:, None, None]], 0.0),
                    np.where(msk, bim[g_rcs, p_s[None, None, :], m_r[:, None, None]], 0.0)], axis=2)
    ii = np.arange(16)
    g_si = 2 * ii[None, :] + (sidx[:, None] // 64)
    psm = np.stack([lre[g_si, p_s[:, None]], lim[g_si, p_s[:, None]], ldt[g_si]], axis=1)
    q = np.arange(32)
    mq = q % 16
    mskq = ((q[None, None, :] // 16) == (sidx[:, None, None] // 64))
    bsm = np.stack([np.where(mskq, bre[g_si[:, :, None], p_s[:, None, None], mq[None, None, :]], 0.0),
                    np.where(mskq, bim[g_si[:, :, None], p_s[:, None, None], mq[None, None, :]], 0.0)], axis=2)
    csm = np.stack([np.where(mskq, cre[g_si[:, :, None], mq[None, None, :], p_s[:, None, None]], 0.0),
                    np.where(mskq, cim[g_si[:, :, None], mq[None, None, :], p_s[:, None, None]], 0.0)], axis=2)
    dflat = f(inp["ssm_d"][0]).reshape(512)
    ch_rc = 96 * cc[None, :] + r[:, None]
    vch = (r[:, None] < 96) & (ch_rc < 512)
    dsk = np.where(vch, dflat[np.where(vch, ch_rc, 0)], 0.0)
    shared["s5_pcm"] = f(pcm)
    shared["s5_bcm"] = f(bcm)
    shared["s5_psm"] = f(psm)
    shared["s5_bsm"] = f(bsm)
    shared["s5_csm"] = f(csm)
    shared["s5_d"] = f(dsk)
    shared["ident"] = np.eye(128, dtype=np.float32)
    rb = f(inp["rel_bias"])
    sl = np.arange(128)[:, None]
    zi = np.arange(1024)[None, :]
    shared["biasG"] = f(rb[_rel_bucket(sl - (zi - 384))].transpose(0, 2, 1))
    mm_ = np.arange(16)[:, None]
    tq = np.arange(512)[None, :]
    shared["biasM"] = f(rb[_rel_bucket(mm_ - 16 - tq)].transpose(0, 2, 1))
    shared["cvec"] = f(np.broadcast_to(rb[15][None, :], (128, 8)))
    def pad6rows(w):
        o = np.zeros((128, 6, w.shape[1]), np.float32)
        for c6 in range(6):
            w_ = min(96, 512 - 96 * c6)
            o[:w_, c6, :] = w[96 * c6:96 * c6 + w_]
        return o
    wg_ = f(inp["ssm_w_glu"][0])
    wgp = np.zeros((512, 6, 128), np.float32)
    for c6 in range(6):
        w_ = min(96, 512 - 96 * c6)
        wgp[:, c6, :w_] = wg_[:, 96 * c6:96 * c6 + w_]
    shared["w_glu"] = pad6rows(wgp.reshape(512, 768))
    shared["w_a"] = pad6rows(f(inp["w_branch_a"][0]))
    shared["w_b"] = wk(inp["w_branch_b"][0], 4)
    shared["w_o"] = wk(inp["w_out"][0], DC)
    shared["w_g"] = wk(win[:, 1736:3784], DC)
    shared["ff2_wg"] = wk(inp["ff2_w_gate"][0], DC)
    shared["ff2_wu"] = wk(inp["ff2_w_up"][0], DC)
    shared["ff2_wd"] = wk(inp["ff2_w_down"][0], FC)
    maps = []
    for c in range(NCORES):
        m = dict(shared)
        m["xT"] = xT[c * NSEQ:(c + 1) * NSEQ]
        maps.append(m)
    return maps


def kernel(**inputs):
    maps = prep_inputs(inputs)
    nc = build()
    res = run_bass_kernel_spmd(nc, maps, core_ids=list(range(NCORES)))
    outs = [r["outT"] for r in res.results]
    o = np.concatenate(outs, axis=0)
    out = o.transpose(0, 3, 2, 1).reshape(o.shape[0], SEQ, D)
    return np.ascontiguousarray(out.astype(np.float32))
```

```python
import math
from contextlib import ExitStack
import numpy as np
import ml_dtypes
import concourse.bass as bass
import concourse.mybir as mybir
from concourse.bass_utils import run_bass_kernel_spmd

F32 = mybir.dt.float32
BF16 = mybir.dt.bfloat16
AF = mybir.ActivationFunctionType
ALU = mybir.AluOpType
AX = mybir.AxisListType

NCORES = 8
D = 1024
DC = 8
SEQ = 2048
NSEQ = 2
NMETA = 16
DFF = 2816
FC = 22
EPS = 1e-6
TT = 512
NT = SEQ // TT
FT = 256
NFT = SEQ // FT


class Sync:
    def __init__(self, nc, es):
        self.nc = nc
        self.eng = {"pe": nc.tensor, "act": nc.scalar, "dve": nc.vector, "pool": nc.gpsimd, "sp": nc.sync}
        self.sem = {k: es.enter_context(nc.semaphore("s_" + k)) for k in self.eng}
        self.cnt = {k: 0 for k in self.eng}
        self.dsem = {}
        self.dcnt = {}
        self.es = es
        self.seen = {k: {} for k in self.eng}
        self.lastw = {}
        self.readers = {}
        self.ninst = 0

    NPOOL = {"sp": 12, "pool": 12, "act": 8}

    def dma_sem(self, q):
        if q not in self.dsem:
            self.dsem[q] = [self.es.enter_context(self.nc.semaphore("d_%s%d" % (q, i))) for i in range(self.NPOOL[q])]
            self.dcnt[q] = [0] * self.NPOOL[q]
            self.drr = getattr(self, "drr", {})
            self.drr[q] = 0
        i = self.drr[q]
        self.drr[q] = (i + 1) % self.NPOOL[q]
        return i

    def _wait(self, e, reads, writes):
        need = {}
        for k in reads:
            lw = self.lastw.get(k)
            if lw is not None:
                need[lw[0]] = max(need.get(lw[0], (0, None))[0], lw[1]), lw[2]
        for k in writes:
            lw = self.lastw.get(k)
            if lw is not None:
                need[lw[0]] = max(need.get(lw[0], (0, None))[0], lw[1]), lw[2]
            for r in self.readers.get(k, ()):
                need[r[0]] = max(need.get(r[0], (0, None))[0], r[1]), r[2]
        E = self.eng[e]
        for semid, (val, semobj) in need.items():
            if semid == "e_" + e and e == "pe":
                continue
            if self.seen[e].get(semid, 0) >= val:
                continue
            E.wait_ge(semobj, val)
            self.seen[e][semid] = val

    def _record(self, rec, reads, writes):
        for k in reads:
            self.readers.setdefault(k, []).append(rec)
        for k in writes:
            self.lastw[k] = rec
            self.readers[k] = []

    def op(self, e, fn, reads=(), writes=()):
        self._wait(e, reads, writes)
        inst = fn(self.eng[e])
        self.cnt[e] += 1
        inst.then_inc(self.sem[e], 1)
        self.ninst += 1
        self._record(("e_" + e, self.cnt[e], self.sem[e]), reads, writes)

    def dma(self, q, semname, out, in_, reads=(), writes=(), **kw):
        self._wait(q, reads, writes)
        i = self.dma_sem(q)
        sem = self.dsem[q][i]
        semid = "d_%s%d" % (q, i)
        if self.dcnt[q][i] > 0 and self.seen[q].get(semid, 0) < self.dcnt[q][i]:
            self.eng[q].wait_ge(sem, self.dcnt[q][i])
            self.seen[q][semid] = self.dcnt[q][i]
        inst = self.eng[q].dma_start(out=out, in_=in_, **kw)
        self.dcnt[q][i] += 16
        inst.then_inc(sem, 16)
        self.ninst += 1
        self._record((semid, self.dcnt[q][i], sem), reads, writes)

    def drain(self, e):
        E = self.eng[e]
        for k in self.eng:
            if k != e and self.cnt[k] > 0:
                E.wait_ge(self.sem[k], self.cnt[k])
        for q, sems in self.dsem.items():
            for i, sm in enumerate(sems):
                if self.dcnt[q][i] > 0:
                    E.wait_ge(sm, self.dcnt[q][i])


def build(debug=None):
    nc = bass.Bass("TRN2", target_bir_lowering=False)
    es = ExitStack()
    with es:
        S = Sync(nc, es)

        def din(name, shape, dt=F32):
            return nc.dram_tensor(name, list(shape), dt, kind="ExternalInput").ap()

        def dscr(name, shape, dt=F32):
            return nc.dram_tensor(name, list(shape), dt, kind="Internal").ap()

        xT = din("xT", [NSEQ, 128, DC, SEQ])
        metaT = din("metaT", [128, DC, NMETA])
        gains = din("gains", [128, 48])
        ff1_wg = din("ff1_wg", [128, DC, DFF])
        ff1_wu = din("ff1_wu", [128, DC, DFF])
        ff1_wd = din("ff1_wd", [128, FC, D])
        outT = nc.dram_tensor("outT", [NSEQ, 128, DC, SEQ], F32, kind="ExternalOutput").ap()
        h1T = dscr("h1T", [NSEQ, 128, DC, SEQ])
        h1m = dscr("h1m", [128, DC, NMETA])

        def sb(stack, name, shape, dt=F32):
            return stack.enter_context(nc.sbuf_tensor(name, list(shape), dt))

        def ps(stack, name, shape, dt=F32):
            return stack.enter_context(nc.psum_tensor(name, list(shape), dt))

        gains_sb = sb(es, "gains_sb", [128, 48])
        ones_f = sb(es, "ones_f", [128, 128])
        S.dma("sp", "c", gains_sb[:, :], gains[:, :], writes=["gains"])
        S.op("dve", lambda e: e.memset(ones_f[:, :], 1.0), writes=["ones_f"])
        zeros_b = sb(es, "zeros_b", [128, 128], BF16)
        S.op("dve", lambda e: e.memset(zeros_b[:, :], 0.0), writes=["zeros_b"])

        def rstd_from_sumsq(pss, rs, n, key_pss, key_rs, half=False):
            S.op("act", lambda e: e.activation(out=rs[:, :n], in_=pss[:, :n], func=AF.Sqrt,
                                               bias=eps_sb[:, (1 if half else 0):(2 if half else 1)],
                                               scale=(4.0 if half else 1.0) / D),
                 reads=[key_pss, "eps"], writes=[key_rs])
            S.op("dve", lambda e: e.reciprocal(out=rs[:, :n], in_=rs[:, :n]), reads=[key_rs], writes=[key_rs])

        eps_sb = sb(es, "eps_sb", [128, 2])
        S.op("dve", lambda e: e.memset(eps_sb[:, 0:1], EPS), writes=["eps"])
        S.op("dve", lambda e: e.memset(eps_sb[:, 1:2], 4.0 * EPS), writes=["eps"])

        def ffn_phase(tag, wg, wu, wd, gpre_col, gpost_col, tiles):
            with ExitStack() as st:
                wg_sb = sb(st, tag + "wg", [128, DC, DFF], BF16)
                wu_sb = sb(st, tag + "wu", [128, DC, DFF], BF16)
                wd_sb = sb(st, tag + "wd", [128, FC, D], BF16)
                xt = [sb(st, tag + "xt%d" % i, [128, DC, FT]) for i in range(2)]
                sq = sb(st, tag + "sq", [128, 2, FT])
                hn = sb(st, tag + "hn", [128, DC, FT], BF16)
                act = sb(st, tag + "act", [128, FC, FT], BF16)
                sg = [sb(st, tag + "sg%d" % i, [128, FT]) for i in range(2)]
                ysb = sb(st, tag + "y", [128, DC, FT])
                rs = sb(st, tag + "rs", [128, FT])
                psg = [ps(st, tag + "psg%d" % i, [128, FT]) for i in range(2)]
                psu = [ps(st, tag + "psu%d" % i, [128, FT]) for i in range(2)]
                psy = [ps(st, tag + "psy%d" % i, [128, FT]) for i in range(2)]
                pss = ps(st, tag + "pss", [128, FT])
                for k in range(DC):
                    S.dma("pool", "w", wg_sb[:, k, :], wg[:, k, :], writes=[(tag, "wg", k)], max_dma_last_dim=5632)
                    S.dma("pool", "w", wu_sb[:, k, :], wu[:, k, :], writes=[(tag, "wu", k)], max_dma_last_dim=5632)
                for j in range(FC):
                    S.dma("pool", "w", wd_sb[:, j, :], wd[:, j, :], writes=[(tag, "wd", j)], max_dma_last_dim=4096)

                def load(i):
                    src, dst, n, sr, dw = tiles[i]
                    S.dma("sp", "x", xt[i % 2][:, :, :n], src, reads=sr, writes=[(tag, "xt", i % 2)])

                load(0)
                for i, (src, dst, n, sr, dw) in enumerate(tiles):
                    if i + 1 < len(tiles):
                        load(i + 1)
                    x = xt[i % 2]
                    kx = (tag, "xt", i % 2)
                    for c in range(DC):
                        S.op("act", lambda e, c=c: e.activation(out=sq[:, c % 2, :n], in_=x[:, c, :n], func=AF.Square),
                             reads=[kx], writes=[(tag, "sq", c % 2)])
                        S.op("pe", lambda e, c=c: e.matmul(pss[:, :n], lhsT=ones_f[:, :], rhs=sq[:, c % 2, :n],
                                                          start=(c == 0), stop=(c == DC - 1)),
                             reads=[(tag, "sq", c % 2), "ones_f"], writes=[(tag, "pss")])
                    rstd_from_sumsq(pss, rs, n, (tag, "pss"), (tag, "rs"))
                    for c in range(DC):
                        S.op("dve", lambda e, c=c: e.scalar_tensor_tensor(
                            out=hn[:, c, :n], in0=x[:, c, :n], scalar=gains_sb[:, gpre_col + c:gpre_col + c + 1],
                            in1=rs[:, :n], op0=ALU.mult, op1=ALU.mult),
                            reads=[kx, (tag, "rs"), "gains"], writes=[(tag, "hn", c)])
                    for j in range(FC):
                        b = j % 2
                        for k in range(DC):
                            S.op("pe", lambda e, k=k, j=j, b=b: e.matmul(
                                psg[b][:, :n], lhsT=wg_sb[:, k, j * 128:(j + 1) * 128], rhs=hn[:, k, :n],
                                start=(k == 0), stop=(k == DC - 1)),
                                reads=[(tag, "wg", k), (tag, "hn", k)], writes=[(tag, "psg", b)])
                        for k in range(DC):
                            S.op("pe", lambda e, k=k, j=j, b=b: e.matmul(
                                psu[b][:, :n], lhsT=wu_sb[:, k, j * 128:(j + 1) * 128], rhs=hn[:, k, :n],
                                start=(k == 0), stop=(k == DC - 1)),
                                reads=[(tag, "wu", k), (tag, "hn", k)], writes=[(tag, "psu", b)])
                        S.op("act", lambda e, b=b: e.activation(out=sg[b][:, :n], in_=psg[b][:, :n], func=AF.Silu),
                             reads=[(tag, "psg", b)], writes=[(tag, "sg", b)])
                        S.op("dve", lambda e, b=b, j=j: e.tensor_tensor(out=act[:, j, :n], in0=sg[b][:, :n],
                                                                        in1=psu[b][:, :n], op=ALU.mult),
                             reads=[(tag, "sg", b), (tag, "psu", b)], writes=[(tag, "act", j)])
                    for c in range(DC):
                        b = c % 2
                        for j in range(FC):
                            S.op("pe", lambda e, c=c, j=j, b=b: e.matmul(
                                psy[b][:, :n], lhsT=wd_sb[:, j, c * 128:(c + 1) * 128], rhs=act[:, j, :n],
                                start=(j == 0), stop=(j == FC - 1)),
                                reads=[(tag, "wd", j), (tag, "act", j)], writes=[(tag, "psy", b)])
                        S.op("act", lambda e, c=c, b=b: e.activation(out=ysb[:, c, :n], in_=psy[b][:, :n], func=AF.Copy),
                             reads=[(tag, "psy", b)], writes=[(tag, "y", c)])
                        S.op("act", lambda e, c=c, b=b: e.activation(out=sq[:, c % 2, :n], in_=psy[b][:, :n], func=AF.Square),
                             reads=[(tag, "psy", b)], writes=[(tag, "sq", c % 2)])
                        S.op("pe", lambda e, c=c: e.matmul(pss[:, :n], lhsT=ones_f[:, :], rhs=sq[:, c % 2, :n],
                                                          start=(c == 0), stop=(c == DC - 1)),
                             reads=[(tag, "sq", c % 2), "ones_f"], writes=[(tag, "pss")])
                    rstd_from_sumsq(pss, rs, n, (tag, "pss"), (tag, "rs"), half=True)
                    for c in range(DC):
                        S.op("dve", lambda e, c=c: e.scalar_tensor_tensor(
                            out=ysb[:, c, :n], in0=ysb[:, c, :n], scalar=gains_sb[:, gpost_col + c:gpost_col + c + 1],
                            in1=rs[:, :n], op0=ALU.mult, op1=ALU.mult),
                            reads=[(tag, "y", c), (tag, "rs"), "gains"], writes=[(tag, "y", c)])
                        S.op("pool", lambda e, c=c: e.tensor_tensor(
                            out=ysb[:, c, :n], in0=ysb[:, c, :n], in1=x[:, c, :n], op=ALU.add),
                            reads=[(tag, "y", c), kx], writes=[(tag, "y", c)])
                    S.dma("sp", "o", dst, ysb[:, :, :n], reads=[(tag, "y", c) for c in range(DC)], writes=dw)

        def barrier():
            for e in S.eng:
                S.drain(e)

        S.barrier = barrier

        def mms(out, pairs, reads, wkey):
            n_ = len(pairs)
            for idx, (l, r) in enumerate(pairs):
                S.op("pe", lambda e, l=l, r=r, idx=idx: e.matmul(out, lhsT=l, rhs=r, start=(idx == 0),
                                                                 stop=(idx == n_ - 1)),
                     reads=reads, writes=[wkey])

        def prenorm(tag, x, kx, n, gcol, hn, sq, pss, rs):
            for c in range(DC):
                S.op("act", lambda e, c=c: e.activation(out=sq[:, c % 2, :n], in_=x[:, c, :n], func=AF.Square),
                     reads=[kx], writes=[(tag, "sq", c % 2)])
                S.op("pe", lambda e, c=c: e.matmul(pss[:, :n], lhsT=ones_f[:, :], rhs=sq[:, c % 2, :n],
                                                  start=(c == 0), stop=(c == DC - 1)),
                     reads=[(tag, "sq", c % 2), "ones_f"], writes=[(tag, "pss")])
            rstd_from_sumsq(pss, rs, n, (tag, "pss"), (tag, "rs"))
            for c in range(DC):
                S.op("dve", lambda e, c=c: e.scalar_tensor_tensor(
                    out=hn[:, c, :n], in0=x[:, c, :n], scalar=gains_sb[:, gcol + c:gcol + c + 1],
                    in1=rs[:, :n], op0=ALU.mult, op1=ALU.mult),
                    reads=[kx, (tag, "rs"), "gains"], writes=[(tag, "hn")])

        tilesA = [(metaT[:, :, :], h1m[:, :, :], NMETA, [], ["h1m"])]
        for s in range(NSEQ):
            for t in range(NFT):
                tilesA.append((xT[s, :, :, t * FT:(t + 1) * FT], h1T[s, :, :, t * FT:(t + 1) * FT], FT, [],
                               [("h1T", s, t * FT // TT)]))
        if debug == "A":
            tilesA = tilesA[:2]
        ffn_phase("A", ff1_wg, ff1_wu, ff1_wd, 0, 8, tilesA)
        barrier()

        LP = NMETA + SEQ
        w_inA = din("w_inA", [128, DC, 2048])
        w_inB = din("w_inB", [128, DC, 72])
        uT = dscr("uT", [NSEQ, 128, 6, LP], BF16)
        kiT = dscr("kiT", [NSEQ, 128, LP], BF16)
        kT = dscr("kT", [NSEQ, 128, LP], BF16)
        qiT = dscr("qiT", [NSEQ, 128, 4, SEQ], BF16)
        qT = dscr("qT", [NSEQ, 128, 4, SEQ], BF16)
        vwS = dscr("vwS", [NSEQ, LP, 72], F32)
        hnT = dscr("hnT", [NSEQ, 128, DC, SEQ], BF16)

        def phase_B():
            tag = "B"
            with ExitStack() as st:
                wA = sb(st, "BwA", [128, DC, 2048], BF16)
                wB = sb(st, "BwB", [128, DC, 72], BF16)
                for k in range(DC):
                    S.dma("pool", "w", wA[:, k, :], w_inA[:, k, :], writes=[("B", "wA")], max_dma_last_dim=4096)
                S.dma("pool", "w", wB[:, :, :], w_inB[:, :, :], writes=[("B", "wB")])
                xt = [sb(st, "Bxt%d" % i, [128, DC, TT]) for i in range(2)]
                hn = sb(st, "Bhn", [128, DC, TT], BF16)
                sq = sb(st, "Bsq", [128, 2, TT])
                rs = sb(st, "Brs", [128, TT])
                stage = sb(st, "Bstage", [128, 16, TT], BF16)
                vw = sb(st, "Bvw", [128, 4, 72])
                pss = ps(st, "Bpss", [128, TT])
                pp = [ps(st, "Bpp%d" % i, [128, TT]) for i in range(2)]
                pv = [ps(st, "Bpv%d" % i, [128, 72]) for i in range(2)]
                tiles = [("m", 0)] + [(s, t) for s in range(NSEQ) for t in range(NT)]

                def load(i):
                    s_, t_ = tiles[i]
                    if s_ == "m":
                        S.dma("sp", "x", xt[i % 2][:, :, :NMETA], h1m[:, :, :], reads=["h1m"], writes=[("B", "xt", i % 2)])
                    else:
                        S.dma("sp", "x", xt[i % 2][:, :, :], h1T[s_, :, :, t_ * TT:(t_ + 1) * TT],
                              reads=[("h1T", s_, t_)], writes=[("B", "xt", i % 2)])

                load(0)
                for i, (s_, t_) in enumerate(tiles):
                    if i + 1 < len(tiles):
                        load(i + 1)
                    n = NMETA if s_ == "m" else TT
                    x = xt[i % 2]
                    kx = ("B", "xt", i % 2)
                    prenorm("B", x, kx, n, 16, hn, sq, pss, rs)
                    if s_ != "m":
                        S.dma("act", "o", hnT[s_, :, :, t_ * TT:(t_ + 1) * TT], hn[:, :, :], reads=[("B", "hn")],
                              writes=[("hnT", s_, t_)])
                    for cc in range(16):
                        b_ = cc % 2
                        mms(pp[b_][:, :n], [(wA[:, k, cc * 128:(cc + 1) * 128], hn[:, k, :n]) for k in range(DC)],
                            [("B", "wA"), ("B", "hn")], ("B", "pp", b_))
                        sc_ = 0.125 if 10 <= cc < 14 else 1.0
                        S.op("act", lambda e, cc=cc, b_=b_, sc_=sc_: e.activation(
                            out=stage[:, cc, :n], in_=pp[b_][:, :n], func=AF.Copy, scale=sc_),
                            reads=[("B", "pp", b_)], writes=[("B", "stage")])
                    if s_ == "m":
                        for s2 in range(NSEQ):
                            S.dma("sp", "o", uT[s2, :, :, 0:NMETA], stage[:, 0:6, :NMETA], reads=[("B", "stage")],
                                  writes=[("uT", s2, "m")])
                            S.dma("sp", "o", kiT[s2, :, 0:NMETA], stage[:, 14, :NMETA], reads=[("B", "stage")],
                                  writes=[("kiT", s2, "m")])
                            S.dma("sp", "o", kT[s2, :, 0:NMETA], stage[:, 15, :NMETA], reads=[("B", "stage")],
                                  writes=[("kT", s2, "m")])
                    else:
                        t0 = t_ * TT
                        S.dma("sp", "o", uT[s_, :, :, NMETA + t0:NMETA + t0 + TT], stage[:, 0:6, :],
                              reads=[("B", "stage")], writes=[("uT", s_, t_)])
                        S.dma("sp", "o", qiT[s_, :, :, t0:t0 + TT], stage[:, 6:10, :], reads=[("B", "stage")],
                              writes=[("qiT", s_, t_)])
                        S.dma("sp", "o", qT[s_, :, :, t0:t0 + TT], stage[:, 10:14, :], reads=[("B", "stage")],
                              writes=[("qT", s_, t_)])
                        S.dma("sp", "o", kiT[s_, :, NMETA + t0:NMETA + t0 + TT], stage[:, 14, :],
                              reads=[("B", "stage")], writes=[("kiT", s_, t_)])
                        S.dma("sp", "o", kT[s_, :, NMETA + t0:NMETA + t0 + TT], stage[:, 15, :],
                              reads=[("B", "stage")], writes=[("kT", s_, t_)])
                    nb = max(1, n // 128)
                    rows = min(n, 128)
                    for blk in range(nb):
                        b_ = blk % 2
                        mms(pv[b_][:rows, :], [(hn[:, k, blk * 128:blk * 128 + rows], wB[:, k, :]) for k in range(DC)],
                            [("B", "wB"), ("B", "hn")], ("B", "pv", b_))
                        S.op("dve", lambda e, blk=blk, b_=b_: e.tensor_copy(out=vw[:rows, blk, :], in_=pv[b_][:rows, :]),
                             reads=[("B", "pv", b_)], writes=[("B", "vw")])
                    if s_ == "m":
                        for s2 in range(NSEQ):
                            S.dma("sp", "o", vwS[s2, 0:NMETA, :], vw[:NMETA, 0, :], reads=[("B", "vw")],
                                  writes=[("vwS", s2, "m")])
                    else:
                        S.dma("sp", "o", vwS[s_, NMETA + t0:NMETA + t0 + TT, :].rearrange("(b p) c -> p b c", p=128),
                              vw[:, :, :], reads=[("B", "vw")], writes=[("vwS", s_, t_)])

        if debug != "A":
            phase_B()
            barrier()

        ALLT = ["m"] + list(range(NT))

        if debug == "B":
            with ExitStack() as st:
                tmp = sb(st, "dbgtmp", [128, 6, LP], BF16)
                tmp2 = sb(st, "dbgtmp2", [128, 6, LP], F32)
                S.dma("sp", "x", tmp[:, :, :], uT[0, :, :, :], reads=[("uT", 0, t) for t in ALLT], writes=["dbgtmp"])
                S.op("dve", lambda e: e.tensor_copy(out=tmp2[:, :, :], in_=tmp[:, :, :]), reads=["dbgtmp"], writes=["dbgtmp2"])
                S.dma("sp", "o", outT[0, :, 0:6, 0:SEQ], tmp2[:, :, NMETA:LP], reads=["dbgtmp2"], writes=["out"])
                S.dma("sp", "x", tmp2[:, 0, 0:72 * 16].rearrange("p (b c) -> p b c", c=72),
                      vwS[0, NMETA:NMETA + 2048, :].rearrange("(b p) c -> p b c", p=128),
                      reads=[("vwS", 0, t) for t in ALLT] + ["out"], writes=["dbgtmp2"])
                S.dma("sp", "o", outT[1, :, 0, 0:72 * 16], tmp2[:, 0, 0:72 * 16], reads=["dbgtmp2"], writes=["out"])


        s5_pcm = din("s5_pcm", [128, 6, 3, 128])
        s5_bcm = din("s5_bcm", [128, 6, 2, 128])
        s5_psm = din("s5_psm", [128, 3, 16])
        s5_bsm = din("s5_bsm", [128, 16, 2, 32])
        s5_csm = din("s5_csm", [128, 16, 2, 32])
        s5_d = din("s5_d", [128, 6])
        ident_d = din("ident", [128, 128])
        yaT = dscr("yaT", [NSEQ, 128, 6, SEQ], BF16)
        NCH = LP // 16
        ident_f = sb(es, "ident_f", [128, 128])
        ident_b = sb(es, "ident_b", [128, 128], BF16)
        S.dma("sp", "c", ident_f[:, :], ident_d[:, :], writes=["ident_f"])
        S.op("dve", lambda e: e.tensor_copy(out=ident_b[:, :], in_=ident_f[:, :]), reads=["ident_f"], writes=["ident_b"])

        def phase_C():
            K_ = "Cprep"
            R_, W_ = [K_], [K_]

            def tt(eng, out, a, b_, op):
                S.op(eng, lambda e: e.tensor_tensor(out=out, in0=a, in1=b_, op=op), reads=R_, writes=W_)

            def tsc(eng, out, a, s1, op0, s2=None, op1=None):
                if op1 is None:
                    S.op(eng, lambda e: e.tensor_scalar(out=out, in0=a, scalar1=s1, scalar2=None, op0=op0), reads=R_, writes=W_)
                else:
                    S.op(eng, lambda e: e.tensor_scalar(out=out, in0=a, scalar1=s1, scalar2=s2, op0=op0, op1=op1),
                         reads=R_, writes=W_)

            def stt(out, a, sc_, b_, op0, op1):
                S.op("dve", lambda e: e.scalar_tensor_tensor(out=out, in0=a, scalar=sc_, in1=b_, op0=op0, op1=op1),
                     reads=R_, writes=W_)

            def actf(out, a, func, scale=1.0):
                S.op("act", lambda e: e.activation(out=out, in_=a, func=func, scale=scale), reads=R_, writes=W_)

            def cparams(st, nm, lr, li, ldt_, shp):
                T = lambda n_: sb(st, "C%s_%s" % (nm, n_), shp)
                dt, mag, th, sh, c, s_, t1, t2, t3 = [T(n_) for n_ in ("dt", "mag", "th", "sh", "c", "s", "t1", "t2", "t3")]
                are, aim, cre_, cim_ = [T(n_) for n_ in ("are", "aim", "cre", "cim")]
                A = lambda t_: t_[tuple(slice(None) for _ in shp)]
                actf(A(dt), ldt_, AF.Exp)
                tt("dve", A(t1), lr, A(dt), ALU.mult)
                actf(A(mag), A(t1), AF.Exp)
                tt("dve", A(th), li, A(dt), ALU.mult)
                actf(A(sh), A(th), AF.Sin, scale=1.0 / 32)
                actf(A(s_), A(th), AF.Sin, scale=1.0 / 16)
                tt("dve", A(t1), A(sh), A(sh), ALU.mult)
                tsc("dve", A(c), A(t1), -2.0, ALU.mult, 1.0, ALU.add)
                for _ in range(4):
                    tt("dve", A(t1), A(c), A(c), ALU.mult)
                    tt("dve", A(t2), A(s_), A(s_), ALU.mult)
                    tt("dve", A(t3), A(c), A(s_), ALU.mult)
                    tt("dve", A(c), A(t1), A(t2), ALU.subtract)
                    tsc("dve", A(s_), A(t3), 2.0, ALU.mult)
                tt("dve", A(are), A(mag), A(c), ALU.mult)
                tt("dve", A(aim), A(mag), A(s_), ALU.mult)
                tt("dve", A(t1), lr, lr, ALU.mult)
                tt("dve", A(t2), li, li, ALU.mult)
                tt("dve", A(t1), A(t1), A(t2), ALU.add)
                S.op("dve", lambda e: e.reciprocal(out=A(t1), in_=A(t1)), reads=R_, writes=W_)
                tsc("dve", A(t2), A(are), -1.0, ALU.add)
                tt("dve", A(t3), A(t2), lr, ALU.mult)
                tt("dve", A(c), A(aim), li, ALU.mult)
                tt("dve", A(t3), A(t3), A(c), ALU.add)
                tt("dve", A(cre_), A(t3), A(t1), ALU.mult)
                tt("dve", A(t3), A(aim), lr, ALU.mult)
                tt("dve", A(c), A(t2), li, ALU.mult)
                tt("dve", A(t3), A(t3), A(c), ALU.subtract)
                tt("dve", A(cim_), A(t3), A(t1), ALU.mult)
                return are, aim, cre_, cim_

            with ExitStack() as st:
                WS = sb(st, "C_WS", [128, 16, 6, 2, 128], BF16)
                WO = sb(st, "C_WO", [128, 16, 16, 2, 32], BF16)
                WK = sb(st, "C_WK", [128, 16, 6, 128], BF16)
                a16 = sb(st, "C_a16", [128, 2, 16])
                with ExitStack() as st2:
                    pc = sb(st2, "C_pc", [128, 6, 3, 128])
                    bc = sb(st2, "C_bc", [128, 6, 2, 128])
                    S.dma("sp", "c", pc[:, :, :, :], s5_pcm[:, :, :, :], writes=W_)
                    S.dma("sp", "c", bc[:, :, :, :], s5_bcm[:, :, :, :], writes=W_)
                    are, aim, cre_, cim_ = cparams(st2, "cm", pc[:, :, 0, :], pc[:, :, 1, :], pc[:, :, 2, :], [128, 6, 128])
                    wr = sb(st2, "C_wr", [128, 6, 128])
                    wi = sb(st2, "C_wi", [128, 6, 128])
                    u1 = sb(st2, "C_u1", [128, 6, 128])
                    u2 = sb(st2, "C_u2", [128, 6, 128])
                    F3 = (slice(None),) * 3

                    def cmul(orr, oi, xr, xi, yr, yi):
                        tt("dve", u1[F3], xr, yr, ALU.mult)
                        tt("dve", u2[F3], xi, yi, ALU.mult)
                        tt("dve", u1[F3], u1[F3], u2[F3], ALU.subtract)
                        tt("dve", u2[F3], xr, yi, ALU.mult)
                        tt("dve", oi, xi, yr, ALU.mult)
                        tt("dve", oi, oi, u2[F3], ALU.add)
                        S.op("dve", lambda e: e.tensor_copy(out=orr, in_=u1[F3]), reads=R_, writes=W_)

                    cmul(wr[F3], wi[F3], cre_[F3], cim_[F3], bc[:, :, 0, :], bc[:, :, 1, :])
                    for lag in range(16):
                        S.op("act", lambda e, lag=lag: e.activation(out=WS[:, lag, :, 0, :], in_=wr[F3], func=AF.Copy),
                             reads=R_, writes=W_)
                        S.op("act", lambda e, lag=lag: e.activation(out=WS[:, lag, :, 1, :], in_=wi[F3], func=AF.Copy),
                             reads=R_, writes=W_)
                        if lag < 15:
                            cmul(wr[F3], wi[F3], wr[F3], wi[F3], are[F3], aim[F3])
                barrier()
                with ExitStack() as st2:
                    pm = sb(st2, "C_pm", [128, 3, 16])
                    bs = sb(st2, "C_bs", [128, 16, 2, 32])
                    cs = sb(st2, "C_cs", [128, 16, 2, 32])
                    dsb = sb(st2, "C_d", [128, 6])
                    S.dma("sp", "c", pm[:, :, :], s5_psm[:, :, :], writes=W_)
                    S.dma("sp", "c", bs[:, :, :, :], s5_bsm[:, :, :, :], writes=W_)
                    S.dma("sp", "c", cs[:, :, :, :], s5_csm[:, :, :, :], writes=W_)
                    S.dma("sp", "c", dsb[:, :], s5_d[:, :], writes=W_)
                    sre, sim, scr, sci = cparams(st2, "sm", pm[:, 0, :], pm[:, 1, :], pm[:, 2, :], [128, 16])
                    apr = sb(st2, "C_apr", [128, 17, 16])
                    api = sb(st2, "C_api", [128, 17, 16])
                    napi = sb(st2, "C_napi", [128, 17, 16])
                    v1 = sb(st2, "C_v1", [128, 16])
                    v2 = sb(st2, "C_v2", [128, 16])
                    S.op("dve", lambda e: e.memset(apr[:, 0, :], 1.0), reads=R_, writes=W_)
                    S.op("dve", lambda e: e.memset(api[:, 0, :], 0.0), reads=R_, writes=W_)
                    for k in range(1, 17):
                        tt("dve", v1[:, :], apr[:, k - 1, :], sre[:, :], ALU.mult)
                        tt("dve", v2[:, :], api[:, k - 1, :], sim[:, :], ALU.mult)
                        tt("dve", apr[:, k, :], v1[:, :], v2[:, :], ALU.subtract)
                        tt("dve", v1[:, :], apr[:, k - 1, :], sim[:, :], ALU.mult)
                        tt("dve", v2[:, :], api[:, k - 1, :], sre[:, :], ALU.mult)
                        tt("dve", api[:, k, :], v1[:, :], v2[:, :], ALU.add)
                    tsc("dve", napi[:, :, :], api[:, :, :], -1.0, ALU.mult)
                    napr = sb(st2, "C_napr", [128, 17, 16])
                    tsc("dve", napr[:, :, :], apr[:, :, :], -1.0, ALU.mult)
                    S.op("dve", lambda e: e.tensor_copy(out=a16[:, 0, :], in_=apr[:, 16, :]), reads=R_, writes=W_)
                    S.op("dve", lambda e: e.tensor_copy(out=a16[:, 1, :], in_=api[:, 16, :]), reads=R_, writes=W_)
                    nsci = sb(st2, "C_nsci", [128, 16])
                    tsc("dve", nsci[:, :], sci[:, :], -1.0, ALU.mult)
                    bb = sb(st2, "C_bb", [128, 16, 2, 32])
                    x1 = sb(st2, "C_x1", [128, 32])
                    for i in range(16):
                        tsc("dve", x1[:, :], bs[:, i, 0, :], scr[:, i:i + 1], ALU.mult)
                        stt(bb[:, i, 0, :], bs[:, i, 1, :], nsci[:, i:i + 1], x1[:, :], ALU.mult, ALU.add)
                        tsc("dve", x1[:, :], bs[:, i, 1, :], scr[:, i:i + 1], ALU.mult)
                        stt(bb[:, i, 1, :], bs[:, i, 0, :], sci[:, i:i + 1], x1[:, :], ALU.mult, ALU.add)
                    Bs = sb(st2, "C_Bs", [128, 16, 2, 32])
                    tsc("dve", Bs[:, :, 0, :], bb[:, :, 1, :], -1.0, ALU.mult)
                    S.op("dve", lambda e: e.tensor_copy(out=Bs[:, :, 1, :], in_=bb[:, :, 0, :]), reads=R_, writes=W_)
                    Cp2 = sb(st2, "C_Cp2", [128, 16, 2, 32])
                    Cs2 = sb(st2, "C_Cs2", [128, 16, 2, 32])
                    S.op("dve", lambda e: e.tensor_copy(out=Cp2[:, :, 0, :], in_=cs[:, :, 0, :]), reads=R_, writes=W_)
                    tsc("dve", Cp2[:, :, 1, :], cs[:, :, 1, :], -1.0, ALU.mult)
                    tsc("dve", Cs2[:, :, 0, :], cs[:, :, 1, :], -1.0, ALU.mult)
                    tsc("dve", Cs2[:, :, 1, :], cs[:, :, 0, :], -1.0, ALU.mult)
                    crb = sb(st2, "C_crb", [128, 16, 2, 32], BF16)
                    S.op("dve", lambda e: e.tensor_copy(out=crb[:, :, :, :], in_=Cp2[:, :, :, :]), reads=R_, writes=W_)
                    S.op("pool", lambda e: e.memset(WK[:, :, :, :], 0.0), reads=R_, writes=W_)
                    barrier()
                    AB = sb(st2, "C_AB", [128, 2, 16, 2, 32], BF16)
                    tA = [sb(st2, "C_tA%d" % i, [128, 2, 32]) for i in range(2)]
                    tC = [sb(st2, "C_tC%d" % i, [128, 2, 32]) for i in range(2)]
                    psK = [ps(st2, "C_psK%d" % i, [128, 192]) for i in range(2)]
                    for lag in range(16):
                        lp_ = lag % 2
                        for i in range(16):
                            ip = i % 2
                            r0 = 32 * (i % 3)
                            cc_ = i // 3
                            S.op("act", lambda e, i=i, ip=ip, lag=lag: e.activation(out=tA[ip][:, :, :], in_=bb[:, i, :, :], func=AF.Copy,
                                                                                  scale=apr[:, lag, i:i + 1]),
                                 reads=[], writes=[("C_tA", ip)])
                            S.op("dve", lambda e, i=i, ip=ip, lag=lag, lp_=lp_: e.scalar_tensor_tensor(
                                out=AB[:, lp_, i, :, :], in0=Bs[:, i, :, :], scalar=api[:, lag, i:i + 1], in1=tA[ip][:, :, :],
                                op0=ALU.mult, op1=ALU.add), reads=[("C_tA", ip)], writes=[("C_AB", lp_, i)])
                            S.op("pool", lambda e, i=i, ip=ip, lag=lag: e.tensor_scalar(out=tC[ip][:, :, :], in0=Cp2[:, i, :, :],
                                                                                      scalar1=apr[:, lag + 1, i:i + 1], scalar2=1.0,
                                                                                      op0=ALU.mult, op1=ALU.mult),
                                 reads=[], writes=[("C_tC", ip)])
                            S.op("dve", lambda e, i=i, ip=ip, lag=lag: e.scalar_tensor_tensor(
                                out=WO[:, lag, i, :, :], in0=Cs2[:, i, :, :], scalar=api[:, lag + 1, i:i + 1], in1=tC[ip][:, :, :],
                                op0=ALU.mult, op1=ALU.add), reads=[("C_tC", ip)], writes=["C_WOw"])
                            S.op("pe", lambda e, i=i, r0=r0, cc_=cc_, lp_=lp_: e.matmul(
                                psK[lp_][r0:r0 + 32, cc_ * 32:(cc_ + 1) * 32], lhsT=AB[:, lp_, i, 0, :], rhs=crb[:, i, 0, :],
                                start=True, stop=False), reads=[("C_AB", lp_, i)], writes=[("C_psK", lp_, i)])
                            S.op("pe", lambda e, i=i, r0=r0, cc_=cc_, lp_=lp_: e.matmul(
                                psK[lp_][r0:r0 + 32, cc_ * 32:(cc_ + 1) * 32], lhsT=AB[:, lp_, i, 1, :], rhs=crb[:, i, 1, :],
                                start=False, stop=True), reads=[("C_AB", lp_, i)], writes=[("C_psK", lp_, i)])
                            S.op("pool" if False else "act", lambda e, i=i, r0=r0, cc_=cc_, lag=lag, lp_=lp_: e.activation(
                                out=WK[r0:r0 + 32, lag, cc_, r0:r0 + 32], in_=psK[lp_][r0:r0 + 32, cc_ * 32:(cc_ + 1) * 32],
                                func=AF.Copy), reads=[("C_psK", lp_, i)], writes=["C_WKw"])
                    barrier()
                    for cc_ in range(6):
                        stt(WK[:, 0, cc_, :], ident_f[:, :], dsb[:, cc_:cc_ + 1], WK[:, 0, cc_, :], ALU.mult, ALU.add)
                barrier()
                usb = sb(st, "C_u", [128, 6, LP], BF16)
                Ssb = sb(st, "C_S", [128, 16, 2, NCH])
                Xbf = sb(st, "C_Xbf", [128, 16, 2, NCH], BF16)
                ypre = sb(st, "C_ypre", [128, SEQ])
                g1 = sb(st, "C_g1", [128, SEQ])
                ya = sb(st, "C_ya", [128, 6, SEQ], BF16)
                w1 = sb(st, "C_w1", [128, 16])
                w2 = sb(st, "C_w2", [128, 16])
                w3 = sb(st, "C_w3", [128, 16])
                w4 = sb(st, "C_w4", [128, 16])
                psS = [ps(st, "C_psS%d" % i, [128, NCH]) for i in range(2)]
                psY = [ps(st, "C_psY%d" % i, [128, NCH]) for i in range(2)]
                for s_ in range(NSEQ):
                    S.dma("sp", "x", usb[:, :, :], uT[s_, :, :, :], reads=[("uT", s_, t) for t in ALLT], writes=["C_u"])
                    n_ = 0
                    for i in range(16):
                        r0 = 32 * (i % 3)
                        cc_ = i // 3
                        for ri in range(2):
                            b_ = n_ % 2
                            n_ += 1
                            mms(psS[b_][:, :], [(WS[r0:r0 + 32, 15 - j, cc_, ri, :], usb[r0:r0 + 32, cc_, j:LP:16])
                                                for j in range(16)], ["C_u", K_], ("C_psS", b_))
                            S.op("act", lambda e, i=i, ri=ri, b_=b_: e.activation(out=Ssb[:, i, ri, :], in_=psS[b_][:, :],
                                                                               func=AF.Copy),
                                 reads=[("C_psS", b_)], writes=["C_S"])
                    for c in range(1, NCH - 1):
                        S.op("dve", lambda e, c=c: e.tensor_tensor(out=w1[:, :], in0=Ssb[:, :, 0, c - 1], in1=a16[:, 0, :], op=ALU.mult),
                             reads=["C_S", K_], writes=["C_w1"])
                        S.op("dve", lambda e, c=c: e.tensor_tensor(out=w2[:, :], in0=Ssb[:, :, 1, c - 1], in1=a16[:, 1, :], op=ALU.mult),
                             reads=["C_S"], writes=["C_w2"])
                        S.op("pool", lambda e, c=c: e.tensor_tensor(out=w3[:, :], in0=Ssb[:, :, 1, c - 1], in1=a16[:, 0, :], op=ALU.mult),
                             reads=["C_S", K_], writes=["C_w3"])
                        S.op("pool", lambda e, c=c: e.tensor_tensor(out=w4[:, :], in0=Ssb[:, :, 0, c - 1], in1=a16[:, 1, :], op=ALU.mult),
                             reads=["C_S"], writes=["C_w4"])
                        S.op("dve", lambda e: e.tensor_tensor(out=w1[:, :], in0=w1[:, :], in1=w2[:, :], op=ALU.subtract),
                             reads=["C_w1", "C_w2"], writes=["C_w1"])
                        S.op("pool", lambda e: e.tensor_tensor(out=w3[:, :], in0=w3[:, :], in1=w4[:, :], op=ALU.add),
                             reads=["C_w3", "C_w4"], writes=["C_w3"])
                        S.op("dve", lambda e, c=c: e.tensor_tensor(out=Ssb[:, :, 0, c], in0=w1[:, :], in1=Ssb[:, :, 0, c], op=ALU.add),
                             reads=["C_w1", "C_w3", "C_S"], writes=["C_Sa"])
                        S.op("pool", lambda e, c=c: e.tensor_tensor(out=Ssb[:, :, 1, c], in0=w3[:, :], in1=Ssb[:, :, 1, c], op=ALU.add),
                             reads=["C_w3", "C_Sa", "C_S"], writes=["C_S"])
                    S.op("dve", lambda e: e.memset(Xbf[:, :, :, 0], 0.0), reads=["C_Xbf"], writes=["C_Xbf"])
                    S.op("dve", lambda e: e.tensor_copy(out=Xbf[:, :, :, 1:NCH], in_=Ssb[:, :, :, 0:NCH - 1]), reads=["C_S", "C_Xbf"], writes=["C_Xbf"])
                    n_ = 0
                    for cc_ in range(6):
                        tiles_cc = [i for i in range(3 * cc_, min(3 * cc_ + 3, 16))]
                        for tau in range(16):
                            b_ = n_ % 2
                            n_ += 1
                            pairs = [(WK[:, tau - j, cc_, :], usb[:, cc_, j:LP:16]) for j in range(tau + 1)]
                            if tau == 0:
                                pairs.append((zeros_b[:, :], usb[:, cc_, 0:LP:16]))
                            mms_out = psY[b_]
                            np_ = len(pairs)

                            def intra(idx):
                                l, r_ = pairs[idx]
                                S.op("pe", lambda e: e.matmul(mms_out[:, :], lhsT=l, rhs=r_, start=(idx == 0), stop=(idx == np_ - 1)),
                                     reads=["C_u", K_, "zeros_b"], writes=[("C_psY", b_)])
                            intra(0)
                            for i in tiles_cc:
                                r0 = 32 * (i % 3)
                                for ri in range(2):
                                    S.op("pe", lambda e, i=i, ri=ri, r0=r0, tau=tau: e.matmul(
                                        mms_out[r0:r0 + 32, :], lhsT=WO[:, tau, i, ri, :], rhs=Xbf[:, i, ri, :], start=False, stop=False),
                                        reads=["C_Xbf", K_], writes=[("C_psY", b_)])
                            for idx in range(1, np_):
                                intra(idx)
                            S.op("act", lambda e, tau=tau, b_=b_: e.activation(
                                out=ypre[:, tau:SEQ:16], in_=psY[b_][:, 1:NCH], func=AF.Copy),
                                reads=[("C_psY", b_)], writes=["C_ypre"])
                        S.op("act", lambda e: e.activation(out=g1[:, :], in_=ypre[:, :], func=AF.Square), reads=["C_ypre"], writes=["C_g1"])
                        S.op("dve", lambda e: e.tensor_scalar(out=g1[:, :], in0=g1[:, :], scalar1=0.0713548162726, scalar2=1.5957691216,
                                                              op0=ALU.mult, op1=ALU.add), reads=["C_g1"], writes=["C_g1"])
                        S.op("dve", lambda e: e.tensor_tensor(out=g1[:, :], in0=g1[:, :], in1=ypre[:, :], op=ALU.mult), reads=["C_g1", "C_ypre"], writes=["C_g1"])
                        S.op("act", lambda e: e.activation(out=g1[:, :], in_=g1[:, :], func=AF.Sigmoid), reads=["C_g1"], writes=["C_g1"])
                        S.op("dve", lambda e, cc_=cc_: e.tensor_tensor(out=ya[:, cc_, :], in0=g1[:, :], in1=ypre[:, :], op=ALU.mult),
                             reads=["C_g1", "C_ypre"], writes=["C_ya"])
                    S.dma("sp", "o", yaT[s_, :, :, :], ya[:, :, :], reads=["C_ya"], writes=[("yaT", s_)])

        if debug not in ("A", "B"):
            phase_C()
            barrier()

        if debug == "C":
            with ExitStack() as st:
                tmp = sb(st, "dbgtmp", [128, 6, SEQ], BF16)
                tmp2 = sb(st, "dbgtmp2", [128, 6, SEQ], F32)
                S.dma("sp", "x", tmp[:, :, :], yaT[0, :, :, :], reads=[("yaT", 0)], writes=["dbgtmp"])
                S.op("dve", lambda e: e.tensor_copy(out=tmp2[:, :, :], in_=tmp[:, :, :]), reads=["dbgtmp"], writes=["dbgtmp2"])
                S.dma("sp", "o", outT[0, :, 0:6, 0:SEQ], tmp2[:, :, :], reads=["dbgtmp2"], writes=["out"])

        biasG_d = din("biasG", [128, 8, 1024])
        biasM_d = din("biasM", [16, 8, 512])
        cvec_d = din("cvec", [128, 8])
        ybT = dscr("ybT", [NSEQ, 128, 4, SEQ], BF16)
        ones_b = sb(es, "ones_b", [128, 128], BF16)
        S.op("dve", lambda e: e.memset(ones_b[:, :], 1.0), writes=["ones_b"])
        NIT = 22
        TOPK = 256.0

        def phase_D():
            with ExitStack() as st:
                Gb = sb(st, "D_Gb", [128, 8, 1024], BF16)
                Mb = sb(st, "D_Mb", [16, 8, 512], BF16)
                cv = sb(st, "D_cv", [128, 8])
                S.dma("sp", "c", cv[:, :], cvec_d[:, :], writes=["D_cv"])
                with ExitStack() as st2:
                    Gf = sb(st2, "D_Gf", [128, 8, 1024])
                    Mf = sb(st2, "D_Mf", [16, 8, 512])
                    S.dma("sp", "c", Gf[:, :, :], biasG_d[:, :, :], writes=["D_Gf"])
                    S.dma("sp", "c", Mf[:, :, :], biasM_d[:, :, :], writes=["D_Mf"])
                    for h in range(8):
                        S.op("dve", lambda e, h=h: e.tensor_scalar(out=Gb[:, h, :], in0=Gf[:, h, :], scalar1=cv[:, h:h + 1],
                                                                   scalar2=None, op0=ALU.subtract),
                             reads=["D_Gf", "D_cv"], writes=["D_Gb"])
                        S.op("dve", lambda e, h=h: e.tensor_scalar(out=Mb[:, h, :], in0=Mf[:, h, :], scalar1=cv[:16, h:h + 1],
                                                                   scalar2=None, op0=ALU.subtract),
                             reads=["D_Mf", "D_cv"], writes=["D_Mb"])
                    barrier()
                qi = sb(st, "D_qi", [128, 4, SEQ], BF16)
                ki = sb(st, "D_ki", [128, LP], BF16)
                qq = sb(st, "D_q", [128, 4, SEQ], BF16)
                kk = sb(st, "D_k", [128, LP], BF16)
                vd = sb(st, "D_vd", [128, 17, 2, 128], BF16)
                wq = sb(st, "D_wq", [128, 16, 8])
                sc = [sb(st, "D_sc%d" % i, [128, 4, LP]) for i in range(2)]
                MA = [sb(st, "D_MA%d" % i, [128, 4, LP], BF16) for i in range(2)]
                junk = sb(st, "D_junk", [128, LP], BF16)
                Rb = [sb(st, "D_Rb%d" % i, [128, 512], BF16) for i in range(2)]
                dg = sb(st, "D_dg", [128, 8, 128], BF16)
                Pt = [sb(st, "D_Pt%d" % i, [128, 512], BF16) for i in range(3)]
                rd = sb(st, "D_rd", [128, 512])
                rds = sb(st, "D_rds", [128, 512])
                Osb = sb(st, "D_Osb", [128, 512])
                yb = sb(st, "D_yb", [128, 4, 512], BF16)
                lo = [sb(st, "D_lo%d" % i, [128, 4]) for i in range(2)]
                hi = sb(st, "D_hi", [128, 4])
                W0 = sb(st, "D_W0", [128, 4])
                Wk = sb(st, "D_Wk", [128, 4])
                mid = sb(st, "D_mid", [128, 4])
                cnt = sb(st, "D_cnt", [128, 4])
                stp = sb(st, "D_stp", [128, 4])
                pq = [ps(st, "D_pq%d" % i, [128, 512]) for i in range(2)]
                psc = ps(st, "D_psc", [128, 512])
                pL = [ps(st, "D_pL%d" % i, [128, 512]) for i in range(2)]
                pOD = [ps(st, "D_pOD%d" % i, [128, 512]) for i in range(2)]
                pSh = ps(st, "D_pSh", [128, 512])
                S.op("dve", lambda e: e.memset(vd[:, :, 0, 64:128], 1.0), writes=["D_vd1"])
                S.op("dve", lambda e: e.memset(vd[:, :, 1, 0:64], 1.0), writes=["D_vd1"])
                ACT_JL = ()
                nmid = sb(st, "D_nmid", [128, 4])
                thrc = sb(st, "D_thrc", [128, 4, 4])
                for Q_ in range(4):
                    for jl_ in range(4):
                        v_ = (510.5 - (NMETA + 128 * (4 * Q_ + jl_ + 1))) if jl_ in ACT_JL else TOPK
                        S.op("dve", lambda e, Q_=Q_, jl_=jl_, v_=v_: e.memset(thrc[:, Q_, jl_:jl_ + 1], float(v_)), writes=["D_thrc"])

                def indexer(s_, Q):
                    u = Q % 2
                    for jl in range(4):
                        j = 4 * Q + jl
                        Nj = NMETA + 128 * (j + 1)
                        for h in range(8):
                            S.op("pool", lambda e, h=h, j=j: e.tensor_scalar(out=dg[:, h, :], in0=ident_f[:, :], scalar1=wq[:, j, h:h + 1],
                                                                           scalar2=1.0, op0=ALU.mult, op1=ALU.mult),
                                 reads=["ident_f", "D_wq"], writes=["D_dg"])
                        for c0 in range(0, Nj, 512):
                            cw = min(512, Nj - c0)

                            def qk(h):
                                hh, hp, b_ = h % 2, h // 2, h % 2
                                S.op("pe", lambda e: e.matmul(
                                    pq[b_][:, :cw], lhsT=qi[64 * hh:64 * hh + 64, hp, 128 * j:128 * j + 128],
                                    rhs=ki[64 * hh:64 * hh + 64, c0:c0 + cw], start=True, stop=True),
                                    reads=["D_qi", "D_ki"], writes=[("D_pq", b_)])
                                S.op("act", lambda e: e.activation(out=Rb[b_][:, :cw], in_=pq[b_][:, :cw], func=AF.Relu),
                                     reads=[("D_pq", b_)], writes=[("D_Rb", b_)])

                            def dgm(h):
                                b_ = h % 2
                                S.op("pe", lambda e: e.matmul(psc[:, :cw], lhsT=dg[:, h, :], rhs=Rb[b_][:, :cw],
                                                              start=(h == 0), stop=(h == 7)),
                                     reads=[("D_Rb", b_), "D_dg"], writes=["D_psc"])

                            qk(0)
                            qk(1)
                            for h in range(8):
                                dgm(h)
                                if h + 2 < 8:
                                    qk(h + 2)
                            S.op("act", lambda e, jl=jl, c0=c0, cw=cw: e.activation(out=sc[u][:, jl, c0:c0 + cw], in_=psc[:, :cw], func=AF.Copy),
                                 reads=["D_psc"], writes=[("D_sc", u, jl)])
                        S.op("dve", lambda e, jl=jl, Nj=Nj: e.tensor_reduce(out=lo[u][:, jl:jl + 1], in_=sc[u][:, jl, :Nj], axis=AX.X, op=ALU.min),
                             reads=[("D_sc", u, jl)], writes=[("D_lo", u)])
                        S.op("dve", lambda e, jl=jl, Nj=Nj: e.tensor_reduce(out=hi[:, jl:jl + 1], in_=sc[u][:, jl, :Nj], axis=AX.X, op=ALU.max),
                             reads=[("D_sc", u, jl)], writes=["D_hi"])
                        S.op("dve", lambda e, jl=jl, Nj=Nj: e.memset(sc[u][0:64, jl, Nj - 64:Nj], -1e30),
                             reads=[("D_sc", u, jl), ("D_lo", u), "D_hi"], writes=[("D_sc", u, jl)])

                def bisect_steps(Q):
                    u = Q % 2
                    L_ = lo[u]
                    steps = []

                    def init():
                        S.op("dve", lambda e: e.tensor_tensor(out=W0[:, :], in0=hi[:, :], in1=L_[:, :], op=ALU.subtract),
                             reads=[("D_lo", u), "D_hi"], writes=["D_W0"])
                    steps.append(init)

                    def mk(it):
                        def f():
                            S.op("dve", lambda e: e.tensor_scalar(out=Wk[:, :], in0=W0[:, :], scalar1=2.0 ** (-(it + 1)), scalar2=None,
                                                                  op0=ALU.mult), reads=["D_W0", "D_stp"], writes=["D_Wk"])
                            S.op("dve", lambda e: e.tensor_tensor(out=mid[:, :], in0=L_[:, :], in1=Wk[:, :], op=ALU.add),
                                 reads=[("D_lo", u), "D_Wk"], writes=["D_mid"])
                            if ACT_JL:
                                S.op("dve", lambda e: e.tensor_scalar(out=nmid[:, :], in0=mid[:, :], scalar1=-1.0, scalar2=None, op0=ALU.mult),
                                     reads=["D_mid"], writes=["D_nmid"])
                            for jl in range(4):
                                Nj = NMETA + 128 * (4 * Q + jl + 1)
                                if jl in ACT_JL:
                                    S.op("act", lambda e, jl=jl, Nj=Nj: e.activation(
                                        out=junkA[:, :Nj], in_=sc[u][:, jl, :Nj], func=AF.Sign, bias=nmid[:, jl:jl + 1], scale=1.0,
                                        accum_out=cnt[:, jl:jl + 1]),
                                        reads=[("D_sc", u, jl), "D_nmid"], writes=["D_junkA", ("D_cnt", jl)])
                                else:
                                    S.op("dve", lambda e, jl=jl, Nj=Nj: e.tensor_scalar(
                                        out=junk[:, :Nj], in0=sc[u][:, jl, :Nj], scalar1=mid[:, jl:jl + 1], scalar2=0.0,
                                        op0=ALU.is_ge, op1=ALU.add, accum_out=cnt[:, jl:jl + 1]),
                                        reads=[("D_sc", u, jl), "D_mid"], writes=["D_junk", ("D_cnt", jl)])
                            S.op("dve", lambda e: e.tensor_tensor(out=stp[:, :], in0=cnt[:, :], in1=thrc[:, Q, :], op=ALU.is_ge),
                                 reads=[("D_cnt", jl) for jl in range(4)] + ["D_thrc"], writes=["D_stp"])
                            S.op("dve", lambda e: e.tensor_tensor(out=stp[:, :], in0=stp[:, :], in1=Wk[:, :], op=ALU.mult),
                                 reads=["D_stp", "D_Wk"], writes=["D_stp"])
                            S.op("dve", lambda e: e.tensor_tensor(out=L_[:, :], in0=L_[:, :], in1=stp[:, :], op=ALU.add),
                                 reads=[("D_lo", u), "D_stp"], writes=[("D_lo", u)])
                        return f
                    for it in range(NIT):
                        steps.append(mk(it))

                    def fin():
                        for jl in range(4):
                            Nj = NMETA + 128 * (4 * Q + jl + 1)
                            S.op("dve", lambda e, jl=jl, Nj=Nj: e.tensor_scalar(out=MA[u][:, jl, :Nj], in0=sc[u][:, jl, :Nj], scalar1=L_[:, jl:jl + 1],
                                                                              scalar2=-30000.0, op0=ALU.is_lt, op1=ALU.mult),
                                 reads=[("D_sc", u, jl), ("D_lo", u)], writes=[("D_MA", u)])
                    steps.append(fin)
                    return steps

                def attention(s_, Q, filler):
                    u = Q % 2
                    nblk = 4 * Q + 5
                    tiles = []
                    for h in range(8):
                        for b in range(nblk):
                            tiles.append((h, b))
                    NTL = len(tiles)

                    def geo(b):
                        w = NMETA if b == 0 else 128
                        pc0 = 0 if b == 0 else NMETA + 128 * (b - 1)
                        jl0 = max(0, b - 1 - 4 * Q)
                        return w, pc0, jl0

                    def stageA(n):
                        h, b = tiles[n]
                        hh, hp = h % 2, h // 2
                        w, pc0, jl0 = geo(b)
                        c0 = jl0 * 128
                        near = (b == 0 and Q == 0) or (b >= 1 and b - 1 >= 4 * Q - 1)
                        lb, pb_ = n % 2, n % 3
                        S.op("pe", lambda e: e.matmul(
                            pL[lb][:w, c0:512], lhsT=kk[64 * hh:64 * hh + 64, pc0:pc0 + w],
                            rhs=qq[64 * hh:64 * hh + 64, hp, 512 * Q + c0:512 * Q + 512], start=True, stop=False),
                            reads=["D_k", "D_q"], writes=[("D_pL", lb)])
                        if near:
                            if b == 0:
                                S.op("pe", lambda e: e.matmul(pL[lb][:NMETA, c0:512], lhsT=ident_b[:NMETA, :NMETA],
                                                              rhs=Mb[:NMETA, h, c0:512], start=False, stop=False),
                                     reads=["D_Mb", "ident_b"], writes=[("D_pL", lb)])
                            else:
                                z0 = 512 * Q + c0 - 128 * (b - 1) + 384
                                S.op("pe", lambda e: e.matmul(pL[lb][:, c0:512], lhsT=ident_b[:, :],
                                                              rhs=Gb[:, h, z0:z0 + 512 - c0], start=False, stop=False),
                                     reads=["D_Gb", "ident_b"], writes=[("D_pL", lb)])
                        for jl in range(jl0, 4):
                            S.op("pe", lambda e, jl=jl: e.matmul(pL[lb][:w, jl * 128:(jl + 1) * 128], lhsT=MA[u][:, jl, pc0:pc0 + w],
                                                                 rhs=ident_b[:, :], start=False, stop=(jl == 3)),
                                 reads=[("D_MA", u), "ident_b"], writes=[("D_pL", lb)])
                        S.op("act", lambda e: e.activation(out=Pt[pb_][:w, c0:512], in_=pL[lb][:w, c0:512], func=AF.Exp),
                             reads=[("D_pL", lb)], writes=[("D_Pt", pb_)])

                    def stageB(n):
                        h, b = tiles[n]
                        hh, hp = h % 2, h // 2
                        w, pc0, jl0 = geo(b)
                        c0 = jl0 * 128
                        pb_ = n % 3
                        ob = h % 2
                        S.op("pe", lambda e: e.matmul(pOD[ob][:, c0:512], lhsT=vd[:w, b, hh, :], rhs=Pt[pb_][:w, c0:512],
                                                      start=(b == 0), stop=(b == nblk - 1)),
                             reads=[("D_Pt", pb_), "D_vd", "D_vd1"], writes=[("D_pOD", ob)])
                        if b == nblk - 1:
                            orow = slice(64 * hh, 64 * hh + 64)
                            drow = slice(64 * (1 - hh), 64 * (1 - hh) + 64)
                            S.op("act", lambda e: e.activation(out=rd[drow, :], in_=pOD[ob][drow, :], func=AF.Ln),
                                 reads=[("D_pOD", ob)], writes=[("D_rd", hh)])
                            S.op("act", lambda e: e.activation(out=rd[drow, :], in_=rd[drow, :], func=AF.Exp, scale=-1.0),
                                 reads=[("D_rd", hh)], writes=[("D_rd", hh)])
                            S.op("pe", lambda e: e.matmul(pSh[orow, :], lhsT=ident_f[drow, drow], rhs=rd[drow, :], start=True, stop=True),
                                 reads=[("D_rd", hh), "ident_f"], writes=[("D_pSh", hh)])
                            S.op("act", lambda e: e.activation(out=rds[orow, :], in_=pSh[orow, :], func=AF.Copy),
                                 reads=[("D_pSh", hh)], writes=[("D_rds", hh)])
                            S.op("act", lambda e: e.activation(out=Osb[orow, :], in_=pOD[ob][orow, :], func=AF.Copy),
                                 reads=[("D_pOD", ob)], writes=[("D_Osb", hh)])
                            S.op("pool", lambda e: e.tensor_tensor(out=yb[orow, hp, :], in0=Osb[orow, :], in1=rds[orow, :], op=ALU.mult),
                                 reads=[("D_rds", hh), ("D_Osb", hh)], writes=["D_yb"])

                    stageA(0)
                    stageA(1)
                    for n in range(NTL):
                        stageB(n)
                        if n + 2 < NTL:
                            stageA(n + 2)
                    while filler:
                        filler.pop(0)()
                    S.dma("sp", "o", ybT[s_, :, :, 512 * Q:512 * Q + 512], yb[:, :, :], reads=["D_yb"], writes=[("ybT", s_, Q)])

                for s_ in range(NSEQ):
                    S.dma("sp", "x", qi[:, :, :], qiT[s_, :, :, :], reads=[("qiT", s_, t) for t in range(NT)], writes=["D_qi"])
                    S.dma("sp", "x", ki[:, :], kiT[s_, :, :], reads=[("kiT", s_, t) for t in ALLT], writes=["D_ki"])
                    S.dma("sp", "x", qq[:, :, :], qT[s_, :, :, :], reads=[("qT", s_, t) for t in range(NT)], writes=["D_q"])
                    S.dma("sp", "x", kk[:, :], kT[s_, :, :], reads=[("kT", s_, t) for t in ALLT], writes=["D_k"])
                    vr = [("vwS", s_, t) for t in ALLT]
                    for half in range(2):
                        S.dma("pool", "x", vd[:, 1:17, half, 64 * half:64 * half + 64],
                              vwS[s_, NMETA:LP, 0:64].rearrange("(b p) c -> p b c", p=128), reads=vr, writes=["D_vd"])
                        S.dma("pool", "x", vd[:NMETA, 0, half, 64 * half:64 * half + 64], vwS[s_, 0:NMETA, 0:64], reads=vr, writes=["D_vd"])
                    S.dma("sp", "x", wq[:, :, :], vwS[s_, NMETA:LP, 64:72].rearrange("(b p) c -> p b c", p=128), reads=vr, writes=["D_wq"])
                    indexer(s_, 0)
                    for f_ in bisect_steps(0):
                        f_()
                    for Q in range(4):
                        if Q + 1 < 4:
                            indexer(s_, Q + 1)
                            for f_ in bisect_steps(Q + 1):
                                f_()
                        attention(s_, Q, [])

        if debug not in ("A", "B", "C"):
            phase_D()
            barrier()

        if debug == "D":
            with ExitStack() as st:
                tmp = sb(st, "dbgtmp", [128, 4, SEQ], BF16)
                tmp2 = sb(st, "dbgtmp2", [128, 4, SEQ], F32)
                S.dma("sp", "x", tmp[:, :, :], ybT[0, :, :, :], reads=[("ybT", 0, t) for t in range(NT)], writes=["dbgtmp"])
                S.op("dve", lambda e: e.tensor_copy(out=tmp2[:, :, :], in_=tmp[:, :, :]), reads=["dbgtmp"], writes=["dbgtmp2"])
                S.dma("sp", "o", outT[0, :, 0:4, 0:SEQ], tmp2[:, :, :], reads=["dbgtmp2"], writes=["out"])

        w_glu_d = din("w_glu", [128, 6, 768])
        w_a_d = din("w_a", [128, 6, D])
        w_b_d = din("w_b", [128, 4, D])
        w_o_d = din("w_o", [128, DC, D])
        w_g_d = din("w_g", [128, DC, 2 * D])
        h2T = dscr("h2T", [NSEQ, 128, DC, SEQ])

        def phase_E():
            with ExitStack() as st:
                wglu = sb(st, "E_wglu", [128, 6, 768], BF16)
                wa = sb(st, "E_wa", [128, 6, D], BF16)
                wb = sb(st, "E_wb", [128, 4, D], BF16)
                wo = sb(st, "E_wo", [128, DC, D], BF16)
                wgt = sb(st, "E_wg", [128, DC, 2 * D], BF16)
                S.dma("pool", "w", wglu[:, :, :], w_glu_d[:, :, :], writes=["E_w"], max_dma_last_dim=3072)
                for k in range(6):
                    S.dma("pool", "w", wa[:, k, :], w_a_d[:, k, :], writes=["E_w"])
                for k in range(4):
                    S.dma("pool", "w", wb[:, k, :], w_b_d[:, k, :], writes=["E_w"])
                for k in range(DC):
                    S.dma("pool", "w", wo[:, k, :], w_o_d[:, k, :], writes=["E_w"])
                    S.dma("pool", "w", wgt[:, k, :], w_g_d[:, k, :], writes=["E_w"], max_dma_last_dim=4096)
                hn = sb(st, "E_hn", [128, DC, TT], BF16)
                ya = sb(st, "E_ya", [128, 6, TT], BF16)
                yg = sb(st, "E_yg", [128, 6, TT], BF16)
                ybt = sb(st, "E_yb", [128, 4, TT], BF16)
                h1t = sb(st, "E_h1", [128, DC, TT])
                sgl = sb(st, "E_sgl", [128, TT])
                ga = sb(st, "E_ga", [128, TT])
                gb = sb(st, "E_gb", [128, TT])
                t1 = sb(st, "E_t1", [128, TT])
                t2 = sb(st, "E_t2", [128, TT])
                mg = sb(st, "E_mg", [128, DC, TT], BF16)
                ysb = sb(st, "E_y", [128, DC, TT])
                sq = sb(st, "E_sq", [128, 2, TT])
                rs = sb(st, "E_rs", [128, TT])
                pga = ps(st, "E_pga", [128, TT])
                pgb = ps(st, "E_pgb", [128, TT])
                pa = ps(st, "E_pa", [128, TT])
                pb = ps(st, "E_pb", [128, TT])
                py = [ps(st, "E_py%d" % i, [128, TT]) for i in range(2)]
                pss = ps(st, "E_pss", [128, TT])
                for s_ in range(NSEQ):
                    for t_ in range(NT):
                        tsl = slice(t_ * TT, (t_ + 1) * TT)
                        S.dma("sp", "x", hn[:, :, :], hnT[s_, :, :, tsl], reads=[("hnT", s_, t_)], writes=["E_hn"])
                        S.dma("sp", "x", ya[:, :, :], yaT[s_, :, :, tsl], reads=[("yaT", s_)], writes=["E_ya"])
                        S.dma("sp", "x", ybt[:, :, :], ybT[s_, :, :, tsl], reads=[("ybT", s_, t_)], writes=["E_yb"])
                        S.dma("sp", "x", h1t[:, :, :], h1T[s_, :, :, tsl], reads=[("h1T", s_, t_)], writes=["E_h1"])
                        for oc in range(6):
                            b_ = oc % 2
                            mms(py[b_][:, :], [(wglu[:, k, oc * 128:(oc + 1) * 128], ya[:, k, :]) for k in range(6)],
                                ["E_w", "E_ya"], ("E_py", b_))
                            S.op("act", lambda e, b_=b_: e.activation(out=sgl[:, :], in_=py[b_][:, :], func=AF.Sigmoid),
                                 reads=[("E_py", b_)], writes=["E_sgl"])
                            S.op("dve", lambda e, oc=oc: e.tensor_tensor(out=yg[:, oc, :], in0=sgl[:, :], in1=ya[:, oc, :], op=ALU.mult),
                                 reads=["E_sgl", "E_ya"], writes=["E_yg"])
                        for dc in range(DC):
                            mms(pga[:, :], [(wgt[:, k, dc * 128:(dc + 1) * 128], hn[:, k, :]) for k in range(DC)], ["E_w", "E_hn"], "E_pga")
                            mms(pgb[:, :], [(wgt[:, k, D + dc * 128:D + (dc + 1) * 128], hn[:, k, :]) for k in range(DC)], ["E_w", "E_hn"], "E_pgb")
                            mms(pa[:, :], [(wa[:, k, dc * 128:(dc + 1) * 128], yg[:, k, :]) for k in range(6)], ["E_w", "E_yg"], "E_pa")
                            mms(pb[:, :], [(wb[:, k, dc * 128:(dc + 1) * 128], ybt[:, k, :]) for k in range(4)], ["E_w", "E_yb"], "E_pb")
                            S.op("act", lambda e: e.activation(out=ga[:, :], in_=pga[:, :], func=AF.Sigmoid), reads=["E_pga"], writes=["E_ga"])
                            S.op("act", lambda e: e.activation(out=gb[:, :], in_=pgb[:, :], func=AF.Sigmoid), reads=["E_pgb"], writes=["E_gb"])
                            S.op("dve", lambda e: e.tensor_tensor(out=t1[:, :], in0=ga[:, :], in1=pa[:, :], op=ALU.mult), reads=["E_ga", "E_pa"], writes=["E_t1"])
                            S.op("dve", lambda e: e.tensor_tensor(out=t2[:, :], in0=gb[:, :], in1=pb[:, :], op=ALU.mult), reads=["E_gb", "E_pb"], writes=["E_t2"])
                            S.op("pool", lambda e, dc=dc: e.tensor_tensor(out=mg[:, dc, :], in0=t1[:, :], in1=t2[:, :], op=ALU.add),
                                 reads=["E_t1", "E_t2"], writes=["E_mg"])
                        for c in range(DC):
                            b_ = c % 2
                            mms(py[b_][:, :], [(wo[:, k, c * 128:(c + 1) * 128], mg[:, k, :]) for k in range(DC)], ["E_w", "E_mg"], ("E_py", b_))
                            S.op("act", lambda e, c=c, b_=b_: e.activation(out=ysb[:, c, :], in_=py[b_][:, :], func=AF.Copy),
                                 reads=[("E_py", b_)], writes=[("E_y", c)])
                            S.op("act", lambda e, c=c, b_=b_: e.activation(out=sq[:, c % 2, :], in_=py[b_][:, :], func=AF.Square),
                                 reads=[("E_py", b_)], writes=[("E_sq", c % 2)])
                            S.op("pe", lambda e, c=c: e.matmul(pss[:, :], lhsT=ones_f[:, :], rhs=sq[:, c % 2, :], start=(c == 0), stop=(c == DC - 1)),
                                 reads=[("E_sq", c % 2), "ones_f"], writes=["E_pss"])
                        rstd_from_sumsq(pss, rs, TT, "E_pss", "E_rs")
                        for c in range(DC):
                            S.op("dve", lambda e, c=c: e.scalar_tensor_tensor(
                                out=ysb[:, c, :], in0=ysb[:, c, :], scalar=gains_sb[:, 24 + c:24 + c + 1], in1=rs[:, :],
                                op0=ALU.mult, op1=ALU.mult), reads=[("E_y", c), "E_rs", "gains"], writes=[("E_y", c)])
                            S.op("pool", lambda e, c=c: e.tensor_tensor(out=ysb[:, c, :], in0=ysb[:, c, :], in1=h1t[:, c, :], op=ALU.add),
                                 reads=[("E_y", c), "E_h1"], writes=[("E_y", c)])
                        S.dma("sp", "o", h2T[s_, :, :, tsl], ysb[:, :, :], reads=[("E_y", c) for c in range(DC)], writes=[("h2T", s_, t_)])

        if debug not in ("A", "B", "C", "D"):
            phase_E()
            barrier()
            ff2_wg = din("ff2_wg", [128, DC, DFF])
            ff2_wu = din("ff2_wu", [128, DC, DFF])
            ff2_wd = din("ff2_wd", [128, FC, D])
            tilesF = []
            for s in range(NSEQ):
                for t in range(NFT):
                    tilesF.append((h2T[s, :, :, t * FT:(t + 1) * FT], outT[s, :, :, t * FT:(t + 1) * FT], FT,
                                   [("h2T", s, t * FT // TT)], [("out", s, t)]))
            ffn_phase("F", ff2_wg, ff2_wu, ff2_wd, 32, 40, tilesF)

        if debug == "A":
            with ExitStack() as st:
                tmp = sb(st, "dbgtmp", [128, DC, FT])
                S.dma("sp", "x", tmp[:, :, :], h1T[0, :, :, 0:FT], reads=[("h1T", 0, 0)], writes=["dbgtmp"])
                S.dma("sp", "o", outT[0, :, :, 0:FT], tmp[:, :, :], reads=["dbgtmp"], writes=["out"])
                S.dma("sp", "x", tmp[:, :, :NMETA], h1m[:, :, :], reads=["h1m", "out"], writes=["dbgtmp"])
                S.dma("sp", "o", outT[1, :, :, 0:NMETA], tmp[:, :, :NMETA], reads=["dbgtmp"], writes=["out"])

        S.drain("sp")
        print("instructions:", S.ninst)
    return nc


def _rel_bucket(rel):
    half, me = 16, 8
    base = np.where(rel > 0, half, 0)
    n = np.abs(rel)
    nf = np.maximum(n, 1).astype(np.float32)
    large = me + (np.log(nf / me) / math.log(128 / me) * (half - me)).astype(np.int32)
    large = np.minimum(large, half - 1)
    return base + np.where(n < me, n, large)


def prep_inputs(inp):
    f = lambda a: np.ascontiguousarray(np.asarray(a, dtype=np.float32))
    x = f(inp["x"])
    B = x.shape[0]
    xT = np.ascontiguousarray(x.reshape(B, SEQ, DC, 128).transpose(0, 3, 2, 1))
    metaT = np.ascontiguousarray(f(inp["meta_tokens"]).reshape(NMETA, DC, 128).transpose(2, 1, 0))
    gl = [inp[k] for k in ("ff1_norm_pre", "ff1_norm_post", "mix_norm_pre", "mix_norm_post", "ff2_norm_pre",
                           "ff2_norm_post")]
    gains = np.ascontiguousarray(np.concatenate([f(g)[0].reshape(DC, 128).T for g in gl], axis=1))

    def wk(w, kc):
        w = f(w)
        return np.ascontiguousarray(w.reshape(kc, 128, w.shape[-1]).transpose(1, 0, 2))

    shared = {
        "metaT": metaT, "gains": gains,
        "ff1_wg": wk(inp["ff1_w_gate"][0], DC), "ff1_wu": wk(inp["ff1_w_up"][0], DC),
        "ff1_wd": wk(inp["ff1_w_down"][0], FC),
    }
    win = f(inp["w_in"][0])
    upad = np.zeros((D, 6, 128), np.float32)
    for c6 in range(6):
        w_ = min(96, 512 - 96 * c6)
        upad[:, c6, :w_] = win[:, 96 * c6:96 * c6 + w_]
    winA = np.concatenate([upad.reshape(D, 768), win[:, 512:1024], win[:, 1096:1608], win[:, 1024:1088], win[:, 1024:1088],
                           win[:, 1608:1672], win[:, 1608:1672]], axis=1)
    winB = np.concatenate([win[:, 1672:1736], win[:, 1088:1096]], axis=1)
    shared["w_inA"] = wk(winA, DC)
    shared["w_inB"] = wk(winB, DC)
    lre, lim, ldt = f(inp["ssm_lambda_re"][0]), f(inp["ssm_lambda_im"][0]), f(inp["ssm_log_dt"][0])
    bre, bim = f(inp["ssm_b_re"][0]), f(inp["ssm_b_im"][0])
    cre, cim = f(inp["ssm_c_re"][0]), f(inp["ssm_c_im"][0])
    r = np.arange(128)
    sidx = np.arange(128)
    cc = np.arange(6)
    i_rc = 3 * cc[None, :] + (r[:, None] // 32)
    val_rc = (r[:, None] < 96) & (i_rc < 16)
    i_rc = np.where(val_rc, i_rc, 0)
    g_rcs = 2 * i_rc[:, :, None] + (sidx[None, None, :] // 64)
    p_s = sidx % 64
    pcm = np.stack([lre[g_rcs, p_s[None, None, :]], lim[g_rcs, p_s[None, None, :]], ldt[g_rcs]], axis=2)
    glr = (r % 32) // 16
    m_r = r % 16
    msk = (glr[:, None, None] == (sidx[None, None, :] // 64)) & val_rc[:, :, None]
    bcm = np.stack([np.where(msk, bre[g_rcs, p_s[None, None, :], m_r[:, None, None]], 0.0),
                    np.where(msk, bim[g_rcs, p_s[None, None, :], m_r[:, None, None]], 0.0)], axis=2)
    ii = np.arange(16)
    g_si = 2 * ii[None, :] + (sidx[:, None] // 64)
    psm = np.stack([lre[g_si, p_s[:, None]], lim[g_si, p_s[:, None]], ldt[g_si]], axis=1)
    q = np.arange(32)
    mq = q % 16
    mskq = ((q[None, None, :] // 16) == (sidx[:, None, None] // 64))
    bsm = np.stack([np.where(mskq, bre[g_si[:, :, None], p_s[:, None, None], mq[None, None, :]], 0.0),
                    np.where(mskq, bim[g_si[:, :, None], p_s[:, None, None], mq[None, None, :]], 0.0)], axis=2)
    csm = np.stack([np.where(mskq, cre[g_si[:, :, None], mq[None, None, :], p_s[:, None, None]], 0.0),
                    np.where(mskq, cim[g_si[:, :, None], mq[None, None, :], p_s[:, None, None]], 0.0)], axis=2)
    dflat = f(inp["ssm_d"][0]).reshape(512)
    ch_rc = 96 * cc[None, :] + r[:, None]
    vch = (r[:, None] < 96) & (ch_rc < 512)
    dsk = np.where(vch, dflat[np.where(vch, ch_rc, 0)], 0.0)
    shared["s5_pcm"] = f(pcm)
    shared["s5_bcm"] = f(bcm)
    shared["s5_psm"] = f(psm)
    shared["s5_bsm"] = f(bsm)
    shared["s5_csm"] = f(csm)
    shared["s5_d"] = f(dsk)
    shared["ident"] = np.eye(128, dtype=np.float32)
    rb = f(inp["rel_bias"])
    sl = np.arange(128)[:, None]
    zi = np.arange(1024)[None, :]
    shared["biasG"] = f(rb[_rel_bucket(sl - (zi - 384))].transpose(0, 2, 1))
    mm_ = np.arange(16)[:, None]
    tq = np.arange(512)[None, :]
    shared["biasM"] = f(rb[_rel_bucket(mm_ - 16 - tq)].transpose(0, 2, 1))
    shared["cvec"] = f(np.broadcast_to(rb[15][None, :], (128, 8)))
    def pad6rows(w):
        o = np.zeros((128, 6, w.shape[1]), np.float32)
        for c6 in range(6):
            w_ = min(96, 512 - 96 * c6)
            o[:w_, c6, :] = w[96 * c6:96 * c6 + w_]
        return o
    wg_ = f(inp["ssm_w_glu"][0])
    wgp = np.zeros((512, 6, 128), np.float32)
    for c6 in range(6):
        w_ = min(96, 512 - 96 * c6)
        wgp[:, c6, :w_] = wg_[:, 96 * c6:96 * c6 + w_]
    shared["w_glu"] = pad6rows(wgp.reshape(512, 768))
    shared["w_a"] = pad6rows(f(inp["w_branch_a"][0]))
    shared["w_b"] = wk(inp["w_branch_b"][0], 4)
    shared["w_o"] = wk(inp["w_out"][0], DC)
    shared["w_g"] = wk(win[:, 1736:3784], DC)
    shared["ff2_wg"] = wk(inp["ff2_w_gate"][0], DC)
    shared["ff2_wu"] = wk(inp["ff2_w_up"][0], DC)
    shared["ff2_wd"] = wk(inp["ff2_w_down"][0], FC)
    maps = []
    for c in range(NCORES):
        m = dict(shared)
        m["xT"] = xT[c * NSEQ:(c + 1) * NSEQ]
        maps.append(m)
    return maps


def kernel(**inputs):
    maps = prep_inputs(inputs)
    nc = build()
    res = run_bass_kernel_spmd(nc, maps, core_ids=list(range(NCORES)))
    outs = [r["outT"] for r in res.results]
    o = np.concatenate(outs, axis=0)
    out = o.transpose(0, 3, 2, 1).reshape(o.shape[0], SEQ, D)
    return np.ascontiguousarray(out.astype(np.float32))
```

```python
import math
from contextlib import ExitStack
import numpy as np
import ml_dtypes
import concourse.bass as bass
import concourse.mybir as mybir
from concourse.bass_utils import run_bass_kernel_spmd

F32 = mybir.dt.float32
BF16 = mybir.dt.bfloat16
AF = mybir.ActivationFunctionType
ALU = mybir.AluOpType
AX = mybir.AxisListType

NCORES = 8
D = 1024
DC = 8
SEQ = 2048
NSEQ = 2
NMETA = 16
DFF = 2816
FC = 22
EPS = 1e-6
TT = 512
NT = SEQ // TT
FT = 256
NFT = SEQ // FT


class Sync:
    def __init__(self, nc, es):
        self.nc = nc
        self.eng = {"pe": nc.tensor, "act": nc.scalar, "dve": nc.vector, "pool": nc.gpsimd, "sp": nc.sync}
        self.sem = {k: es.enter_context(nc.semaphore("s_" + k)) for k in self.eng}
        self.cnt = {k: 0 for k in self.eng}
        self.dsem = {}
        self.dcnt = {}
        self.es = es
        self.seen = {k: {} for k in self.eng}
        self.lastw = {}
        self.readers = {}
        self.ninst = 0

    NPOOL = {"sp": 12, "pool": 12, "act": 8}

    def dma_sem(self, q):
        if q not in self.dsem:
            self.dsem[q] = [self.es.enter_context(self.nc.semaphore("d_%s%d" % (q, i))) for i in range(self.NPOOL[q])]
            self.dcnt[q] = [0] * self.NPOOL[q]
            self.drr = getattr(self, "drr", {})
            self.drr[q] = 0
        i = self.drr[q]
        self.drr[q] = (i + 1) % self.NPOOL[q]
        return i

    def _wait(self, e, reads, writes):
        need = {}
        for k in reads:
            lw = self.lastw.get(k)
            if lw is not None:
                need[lw[0]] = max(need.get(lw[0], (0, None))[0], lw[1]), lw[2]
        for k in writes:
            lw = self.lastw.get(k)
            if lw is not None:
                need[lw[0]] = max(need.get(lw[0], (0, None))[0], lw[1]), lw[2]
            for r in self.readers.get(k, ()):
                need[r[0]] = max(need.get(r[0], (0, None))[0], r[1]), r[2]
        E = self.eng[e]
        for semid, (val, semobj) in need.items():
            if semid == "e_" + e and e == "pe":
                continue
            if self.seen[e].get(semid, 0) >= val:
                continue
            E.wait_ge(semobj, val)
            self.seen[e][semid] = val

    def _record(self, rec, reads, writes):
        for k in reads:
            self.readers.setdefault(k, []).append(rec)
        for k in writes:
            self.lastw[k] = rec
            self.readers[k] = []

    def op(self, e, fn, reads=(), writes=()):
        self._wait(e, reads, writes)
        inst = fn(self.eng[e])
        self.cnt[e] += 1
        inst.then_inc(self.sem[e], 1)
        self.ninst += 1
        self._record(("e_" + e, self.cnt[e], self.sem[e]), reads, writes)

    def dma(self, q, semname, out, in_, reads=(), writes=(), **kw):
        self._wait(q, reads, writes)
        i = self.dma_sem(q)
        sem = self.dsem[q][i]
        semid = "d_%s%d" % (q, i)
        if self.dcnt[q][i] > 0 and self.seen[q].get(semid, 0) < self.dcnt[q][i]:
            self.eng[q].wait_ge(sem, self.dcnt[q][i])
            self.seen[q][semid] = self.dcnt[q][i]
        inst = self.eng[q].dma_start(out=out, in_=in_, **kw)
        self.dcnt[q][i] += 16
        inst.then_inc(sem, 16)
        self.ninst += 1
        self._record((semid, self.dcnt[q][i], sem), reads, writes)

    def drain(self, e):
        E = self.eng[e]
        for k in self.eng:
            if k != e and self.cnt[k] > 0:
                E.wait_ge(self.sem[k], self.cnt[k])
        for q, sems in self.dsem.items():
            for i, sm in enumerate(sems):
                if self.dcnt[q][i] > 0:
                    E.wait_ge(sm, self.dcnt[q][i])


def build(debug=None):
    nc = bass.Bass("TRN2", target_bir_lowering=False)
    es = ExitStack()
    with es:
        S = Sync(nc, es)

        def din(name, shape, dt=F32):
            return nc.dram_tensor(name, list(shape), dt, kind="ExternalInput").ap()

        def dscr(name, shape, dt=F32):
            return nc.dram_tensor(name, list(shape), dt, kind="Internal").ap()

        xT = din("xT", [NSEQ, 128, DC, SEQ])
        metaT = din("metaT", [128, DC, NMETA])
        gains = din("gains", [128, 48])
        ff1_wg = din("ff1_wg", [128, DC, DFF])
        ff1_wu = din("ff1_wu", [128, DC, DFF])
        ff1_wd = din("ff1_wd", [128, FC, D])
        outT = nc.dram_tensor("outT", [NSEQ, 128, DC, SEQ], F32, kind="ExternalOutput").ap()
        h1T = dscr("h1T", [NSEQ, 128, DC, SEQ])
        h1m = dscr("h1m", [128, DC, NMETA])

        def sb(stack, name, shape, dt=F32):
            return stack.enter_context(nc.sbuf_tensor(name, list(shape), dt))

        def ps(stack, name, shape, dt=F32):
            return stack.enter_context(nc.psum_tensor(name, list(shape), dt))

        gains_sb = sb(es, "gains_sb", [128, 48])
        ones_f = sb(es, "ones_f", [128, 128])
        S.dma("sp", "c", gains_sb[:, :], gains[:, :], writes=["gains"])
        S.op("dve", lambda e: e.memset(ones_f[:, :], 1.0), writes=["ones_f"])
        zeros_b = sb(es, "zeros_b", [128, 128], BF16)
        S.op("dve", lambda e: e.memset(zeros_b[:, :], 0.0), writes=["zeros_b"])

        def rstd_from_sumsq(pss, rs, n, key_pss, key_rs, half=False):
            S.op("act", lambda e: e.activation(out=rs[:, :n], in_=pss[:, :n], func=AF.Sqrt,
                                               bias=eps_sb[:, (1 if half else 0):(2 if half else 1)],
                                               scale=(4.0 if half else 1.0) / D),
                 reads=[key_pss, "eps"], writes=[key_rs])
            S.op("dve", lambda e: e.reciprocal(out=rs[:, :n], in_=rs[:, :n]), reads=[key_rs], writes=[key_rs])

        eps_sb = sb(es, "eps_sb", [128, 2])
        S.op("dve", lambda e: e.memset(eps_sb[:, 0:1], EPS), writes=["eps"])
        S.op("dve", lambda e: e.memset(eps_sb[:, 1:2], 4.0 * EPS), writes=["eps"])

        def stats_step(tag, c, src, n, sq, acc, rkeys):
            S.op("act", lambda e: e.activation(out=sq[:, c % 2, :n], in_=src, func=AF.Square),
                 reads=rkeys, writes=[(tag, "sq", c % 2)])
            if c == 1:
                S.op("pool", lambda e: e.tensor_tensor(out=acc[:, :n], in0=sq[:, 0, :n], in1=sq[:, 1, :n], op=ALU.add),
                     reads=[(tag, "sq", 0), (tag, "sq", 1)], writes=[(tag, "acc")])
            elif c >= 2:
                S.op("pool", lambda e: e.tensor_tensor(out=acc[:, :n], in0=acc[:, :n], in1=sq[:, c % 2, :n], op=ALU.add),
                     reads=[(tag, "sq", c % 2), (tag, "acc")], writes=[(tag, "acc")])

        def stats_finish(tag, n, acc, pss, rs, half=False):
            S.op("pe", lambda e: e.matmul(pss[:, :n], lhsT=ones_f[:, :], rhs=acc[:, :n], start=True, stop=True),
                 reads=[(tag, "acc"), "ones_f"], writes=[(tag, "pss")])
            rstd_from_sumsq(pss, rs, n, (tag, "pss"), (tag, "rs"), half=half)

        def ffn_phase(tag, wg, wu, wd, gpre_col, gpost_col, tiles):
            with ExitStack() as st:
                wg_sb = sb(st, tag + "wg", [128, DC, DFF], BF16)
                wu_sb = sb(st, tag + "wu", [128, DC, DFF], BF16)
                wd_sb = sb(st, tag + "wd", [128, FC, D], BF16)
                xt = [sb(st, tag + "xt%d" % i, [128, DC, FT]) for i in range(2)]
                sq = sb(st, tag + "sq", [128, 2, FT])
                acc = sb(st, tag + "acc", [128, FT])
                hn = sb(st, tag + "hn", [128, DC, FT], BF16)
                act = sb(st, tag + "act", [128, FC, FT], BF16)
                sg = [sb(st, tag + "sg%d" % i, [128, FT]) for i in range(2)]
                ysb = sb(st, tag + "y", [128, DC, FT])
                rs = sb(st, tag + "rs", [128, FT])
                psg = [ps(st, tag + "psg%d" % i, [128, FT]) for i in range(2)]
                psu = [ps(st, tag + "psu%d" % i, [128, FT]) for i in range(2)]
                psy = [ps(st, tag + "psy%d" % i, [128, FT]) for i in range(2)]
                pss = ps(st, tag + "pss", [128, FT])
                for g in range(FC // 2):
                    cs_ = slice(g * 256, (g + 1) * 256)
                    S.dma("pool", "w", wg_sb[:, :, cs_], wg[:, :, cs_], writes=[(tag, "wg", g)])
                    S.dma("pool", "w", wu_sb[:, :, cs_], wu[:, :, cs_], writes=[(tag, "wu", g)])
                for j in range(FC):
                    S.dma("pool", "w", wd_sb[:, j, :], wd[:, j, :], writes=[(tag, "wd", j)], max_dma_last_dim=4096)

                def load(i):
                    src, dst, n, sr, dw = tiles[i]
                    S.dma("sp", "x", xt[i % 2][:, :, :n], src, reads=sr, writes=[(tag, "xt", i % 2)])

                load(0)
                for i, (src, dst, n, sr, dw) in enumerate(tiles):
                    if i + 1 < len(tiles):
                        load(i + 1)
                    x = xt[i % 2]
                    kx = (tag, "xt", i % 2)
                    for c in range(DC):
                        stats_step(tag, c, x[:, c, :n], n, sq, acc, [kx])
                    stats_finish(tag, n, acc, pss, rs)
                    for c in range(DC):
                        S.op("dve", lambda e, c=c: e.scalar_tensor_tensor(
                            out=hn[:, c, :n], in0=x[:, c, :n], scalar=gains_sb[:, gpre_col + c:gpre_col + c + 1],
                            in1=rs[:, :n], op0=ALU.mult, op1=ALU.mult),
                            reads=[kx, (tag, "rs"), "gains"], writes=[(tag, "hn", c)])
                    for j in range(FC):
                        b = j % 2
                        for k in range(DC):
                            S.op("pe", lambda e, k=k, j=j, b=b: e.matmul(
                                psg[b][:, :n], lhsT=wg_sb[:, k, j * 128:(j + 1) * 128], rhs=hn[:, k, :n],
                                start=(k == 0), stop=(k == DC - 1)),
                                reads=[(tag, "wg", j // 2), (tag, "hn", k)], writes=[(tag, "psg", b)])
                        for k in range(DC):
                            S.op("pe", lambda e, k=k, j=j, b=b: e.matmul(
                                psu[b][:, :n], lhsT=wu_sb[:, k, j * 128:(j + 1) * 128], rhs=hn[:, k, :n],
                                start=(k == 0), stop=(k == DC - 1)),
                                reads=[(tag, "wu", j // 2), (tag, "hn", k)], writes=[(tag, "psu", b)])
                        S.op("act", lambda e, b=b: e.activation(out=sg[b][:, :n], in_=psg[b][:, :n], func=AF.Silu),
                             reads=[(tag, "psg", b)], writes=[(tag, "sg", b)])
                        S.op("dve", lambda e, b=b, j=j: e.tensor_tensor(out=act[:, j, :n], in0=sg[b][:, :n],
                                                                        in1=psu[b][:, :n], op=ALU.mult),
                             reads=[(tag, "sg", b), (tag, "psu", b)], writes=[(tag, "act", j)])
                    for c in range(DC):
                        b = c % 2
                        for j in range(FC):
                            S.op("pe", lambda e, c=c, j=j, b=b: e.matmul(
                                psy[b][:, :n], lhsT=wd_sb[:, j, c * 128:(c + 1) * 128], rhs=act[:, j, :n],
                                start=(j == 0), stop=(j == FC - 1)),
                                reads=[(tag, "wd", j), (tag, "act", j)], writes=[(tag, "psy", b)])
                        S.op("act", lambda e, c=c, b=b: e.activation(out=ysb[:, c, :n], in_=psy[b][:, :n], func=AF.Copy),
                             reads=[(tag, "psy", b)], writes=[(tag, "y", c)])
                        stats_step(tag, c, psy[b][:, :n], n, sq, acc, [(tag, "psy", b)])
                    stats_finish(tag, n, acc, pss, rs, half=True)
                    for c in range(DC):
                        S.op("dve", lambda e, c=c: e.scalar_tensor_tensor(
                            out=ysb[:, c, :n], in0=ysb[:, c, :n], scalar=gains_sb[:, gpost_col + c:gpost_col + c + 1],
                            in1=rs[:, :n], op0=ALU.mult, op1=ALU.mult),
                            reads=[(tag, "y", c), (tag, "rs"), "gains"], writes=[(tag, "y", c)])
                        S.op("pool", lambda e, c=c: e.tensor_tensor(
                            out=ysb[:, c, :n], in0=ysb[:, c, :n], in1=x[:, c, :n], op=ALU.add),
                            reads=[(tag, "y", c), kx], writes=[(tag, "y", c)])
                    S.dma("sp", "o", dst, ysb[:, :, :n], reads=[(tag, "y", c) for c in range(DC)], writes=dw)

        def barrier():
            for e in S.eng:
                S.drain(e)

        S.barrier = barrier

        def mms(out, pairs, reads, wkey):
            n_ = len(pairs)
            for idx, (l, r) in enumerate(pairs):
                S.op("pe", lambda e, l=l, r=r, idx=idx: e.matmul(out, lhsT=l, rhs=r, start=(idx == 0),
                                                                 stop=(idx == n_ - 1)),
                     reads=reads, writes=[wkey])

        def prenorm(tag, x, kx, n, gcol, hn, sq, pss, rs, acc):
            for c in range(DC):
                stats_step(tag, c, x[:, c, :n], n, sq, acc, [kx])
            stats_finish(tag, n, acc, pss, rs)
            for c in range(DC):
                S.op("dve", lambda e, c=c: e.scalar_tensor_tensor(
                    out=hn[:, c, :n], in0=x[:, c, :n], scalar=gains_sb[:, gcol + c:gcol + c + 1],
                    in1=rs[:, :n], op0=ALU.mult, op1=ALU.mult),
                    reads=[kx, (tag, "rs"), "gains"], writes=[(tag, "hn")])

        tilesA = []
        for s in range(NSEQ):
            for t in range(NFT):
                tilesA.append((xT[s, :, :, t * FT:(t + 1) * FT], h1T[s, :, :, t * FT:(t + 1) * FT], FT, [],
                               [("h1T", s, t * FT // TT)]))
        tilesA.append((metaT[:, :, :], h1m[:, :, :], NMETA, [], ["h1m"]))
        if debug == "A":
            tilesA = tilesA[:2]
        ffn_phase("A", ff1_wg, ff1_wu, ff1_wd, 0, 8, tilesA)
        barrier()

        LP = NMETA + SEQ
        w_inA = din("w_inA", [128, DC, 2048])
        w_inB = din("w_inB", [128, DC, 72])
        uT = dscr("uT", [NSEQ, 128, 6, LP], BF16)
        kiT = dscr("kiT", [NSEQ, 128, LP], BF16)
        kT = dscr("kT", [NSEQ, 128, LP], BF16)
        qiT = dscr("qiT", [NSEQ, 128, 4, SEQ], BF16)
        qT = dscr("qT", [NSEQ, 128, 4, SEQ], BF16)
        vwS = dscr("vwS", [NSEQ, LP, 72], F32)
        hnT = dscr("hnT", [NSEQ, 128, DC, SEQ], BF16)

        def phase_B():
            tag = "B"
            with ExitStack() as st:
                wA = sb(st, "BwA", [128, DC, 2048], BF16)
                wB = sb(st, "BwB", [128, DC, 72], BF16)
                for k in range(DC):
                    S.dma("pool", "w", wA[:, k, :], w_inA[:, k, :], writes=[("B", "wA")], max_dma_last_dim=4096)
                S.dma("pool", "w", wB[:, :, :], w_inB[:, :, :], writes=[("B", "wB")])
                xt = [sb(st, "Bxt%d" % i, [128, DC, TT]) for i in range(2)]
                hn = sb(st, "Bhn", [128, DC, TT], BF16)
                sq = sb(st, "Bsq", [128, 2, TT])
                accB = sb(st, "Bacc", [128, TT])
                rs = sb(st, "Brs", [128, TT])
                stage = sb(st, "Bstage", [128, 16, TT], BF16)
                vw = sb(st, "Bvw", [128, 4, 72])
                pss = ps(st, "Bpss", [128, TT])
                pp = [ps(st, "Bpp%d" % i, [128, TT]) for i in range(2)]
                pv = [ps(st, "Bpv%d" % i, [128, 72]) for i in range(2)]
                tiles = [("m", 0)] + [(s, t) for s in range(NSEQ) for t in range(NT)]

                def load(i):
                    s_, t_ = tiles[i]
                    if s_ == "m":
                        S.dma("sp", "x", xt[i % 2][:, :, :NMETA], h1m[:, :, :], reads=["h1m"], writes=[("B", "xt", i % 2)])
                    else:
                        S.dma("sp", "x", xt[i % 2][:, :, :], h1T[s_, :, :, t_ * TT:(t_ + 1) * TT],
                              reads=[("h1T", s_, t_)], writes=[("B", "xt", i % 2)])

                load(0)
                for i, (s_, t_) in enumerate(tiles):
                    if i + 1 < len(tiles):
                        load(i + 1)
                    n = NMETA if s_ == "m" else TT
                    x = xt[i % 2]
                    kx = ("B", "xt", i % 2)
                    prenorm("B", x, kx, n, 16, hn, sq, pss, rs, accB)
                    if s_ != "m":
                        S.dma("act", "o", hnT[s_, :, :, t_ * TT:(t_ + 1) * TT], hn[:, :, :], reads=[("B", "hn")],
                              writes=[("hnT", s_, t_)])
                    for cc in range(16):
                        b_ = cc % 2
                        mms(pp[b_][:, :n], [(wA[:, k, cc * 128:(cc + 1) * 128], hn[:, k, :n]) for k in range(DC)],
                            [("B", "wA"), ("B", "hn")], ("B", "pp", b_))
                        sc_ = 0.125 if 10 <= cc < 14 else 1.0
                        S.op("act", lambda e, cc=cc, b_=b_, sc_=sc_: e.activation(
                            out=stage[:, cc, :n], in_=pp[b_][:, :n], func=AF.Copy, scale=sc_),
                            reads=[("B", "pp", b_)], writes=[("B", "stage")])
                    if s_ == "m":
                        for s2 in range(NSEQ):
                            S.dma("sp", "o", uT[s2, :, :, 0:NMETA], stage[:, 0:6, :NMETA], reads=[("B", "stage")],
                                  writes=[("uT", s2, "m")])
                            S.dma("sp", "o", kiT[s2, :, 0:NMETA], stage[:, 14, :NMETA], reads=[("B", "stage")],
                                  writes=[("kiT", s2, "m")])
                            S.dma("sp", "o", kT[s2, :, 0:NMETA], stage[:, 15, :NMETA], reads=[("B", "stage")],
                                  writes=[("kT", s2, "m")])
                    else:
                        t0 = t_ * TT
                        S.dma("sp", "o", uT[s_, :, :, NMETA + t0:NMETA + t0 + TT], stage[:, 0:6, :],
                              reads=[("B", "stage")], writes=[("uT", s_, t_)])
                        S.dma("sp", "o", qiT[s_, :, :, t0:t0 + TT], stage[:, 6:10, :], reads=[("B", "stage")],
                              writes=[("qiT", s_, t_)])
                        S.dma("sp", "o", qT[s_, :, :, t0:t0 + TT], stage[:, 10:14, :], reads=[("B", "stage")],
                              writes=[("qT", s_, t_)])
                        S.dma("sp", "o", kiT[s_, :, NMETA + t0:NMETA + t0 + TT], stage[:, 14, :],
                              reads=[("B", "stage")], writes=[("kiT", s_, t_)])
                        S.dma("sp", "o", kT[s_, :, NMETA + t0:NMETA + t0 + TT], stage[:, 15, :],
                              reads=[("B", "stage")], writes=[("kT", s_, t_)])
                    nb = max(1, n // 128)
                    rows = min(n, 128)
                    for blk in range(nb):
                        b_ = blk % 2
                        mms(pv[b_][:rows, :], [(hn[:, k, blk * 128:blk * 128 + rows], wB[:, k, :]) for k in range(DC)],
                            [("B", "wB"), ("B", "hn")], ("B", "pv", b_))
                        S.op("dve", lambda e, blk=blk, b_=b_: e.tensor_copy(out=vw[:rows, blk, :], in_=pv[b_][:rows, :]),
                             reads=[("B", "pv", b_)], writes=[("B", "vw")])
                    if s_ == "m":
                        for s2 in range(NSEQ):
                            S.dma("sp", "o", vwS[s2, 0:NMETA, :], vw[:NMETA, 0, :], reads=[("B", "vw")],
                                  writes=[("vwS", s2, "m")])
                    else:
                        S.dma("sp", "o", vwS[s_, NMETA + t0:NMETA + t0 + TT, :].rearrange("(b p) c -> p b c", p=128),
                              vw[:, :, :], reads=[("B", "vw")], writes=[("vwS", s_, t_)])

        if debug != "A":
            phase_B()
            barrier()

        ALLT = ["m"] + list(range(NT))

        if debug == "B":
            with ExitStack() as st:
                tmp = sb(st, "dbgtmp", [128, 6, LP], BF16)
                tmp2 = sb(st, "dbgtmp2", [128, 6, LP], F32)
                S.dma("sp", "x", tmp[:, :, :], uT[0, :, :, :], reads=[("uT", 0, t) for t in ALLT], writes=["dbgtmp"])
                S.op("dve", lambda e: e.tensor_copy(out=tmp2[:, :, :], in_=tmp[:, :, :]), reads=["dbgtmp"], writes=["dbgtmp2"])
                S.dma("sp", "o", outT[0, :, 0:6, 0:SEQ], tmp2[:, :, NMETA:LP], reads=["dbgtmp2"], writes=["out"])
                S.dma("sp", "x", tmp2[:, 0, 0:72 * 16].rearrange("p (b c) -> p b c", c=72),
                      vwS[0, NMETA:NMETA + 2048, :].rearrange("(b p) c -> p b c", p=128),
                      reads=[("vwS", 0, t) for t in ALLT] + ["out"], writes=["dbgtmp2"])
                S.dma("sp", "o", outT[1, :, 0, 0:72 * 16], tmp2[:, 0, 0:72 * 16], reads=["dbgtmp2"], writes=["out"])


        s5_pcm = din("s5_pcm", [128, 6, 3, 128])
        s5_bcm = din("s5_bcm", [128, 6, 2, 128])
        s5_psm = din("s5_psm", [128, 3, 16])
        s5_bsm = din("s5_bsm", [128, 16, 2, 32])
        s5_csm = din("s5_csm", [128, 16, 2, 32])
        s5_d = din("s5_d", [128, 6])
        ident_d = din("ident", [128, 128])
        yaT = dscr("yaT", [NSEQ, 128, 6, SEQ], BF16)
        NCH = LP // 16
        ident_f = sb(es, "ident_f", [128, 128])
        ident_b = sb(es, "ident_b", [128, 128], BF16)
        S.dma("sp", "c", ident_f[:, :], ident_d[:, :], writes=["ident_f"])
        S.op("dve", lambda e: e.tensor_copy(out=ident_b[:, :], in_=ident_f[:, :]), reads=["ident_f"], writes=["ident_b"])

        def phase_C():
            K_ = "Cprep"
            R_, W_ = [K_], [K_]

            def tt(eng, out, a, b_, op):
                S.op(eng, lambda e: e.tensor_tensor(out=out, in0=a, in1=b_, op=op), reads=R_, writes=W_)

            def tsc(eng, out, a, s1, op0, s2=None, op1=None):
                if op1 is None:
                    S.op(eng, lambda e: e.tensor_scalar(out=out, in0=a, scalar1=s1, scalar2=None, op0=op0), reads=R_, writes=W_)
                else:
                    S.op(eng, lambda e: e.tensor_scalar(out=out, in0=a, scalar1=s1, scalar2=s2, op0=op0, op1=op1),
                         reads=R_, writes=W_)

            def stt(out, a, sc_, b_, op0, op1):
                S.op("dve", lambda e: e.scalar_tensor_tensor(out=out, in0=a, scalar=sc_, in1=b_, op0=op0, op1=op1),
                     reads=R_, writes=W_)

            def actf(out, a, func, scale=1.0):
                S.op("act", lambda e: e.activation(out=out, in_=a, func=func, scale=scale), reads=R_, writes=W_)

            def cparams(st, nm, lr, li, ldt_, shp):
                T = lambda n_: sb(st, "C%s_%s" % (nm, n_), shp)
                dt, mag, th, sh, c, s_, t1, t2, t3 = [T(n_) for n_ in ("dt", "mag", "th", "sh", "c", "s", "t1", "t2", "t3")]
                are, aim, cre_, cim_ = [T(n_) for n_ in ("are", "aim", "cre", "cim")]
                A = lambda t_: t_[tuple(slice(None) for _ in shp)]
                actf(A(dt), ldt_, AF.Exp)
                tt("dve", A(t1), lr, A(dt), ALU.mult)
                actf(A(mag), A(t1), AF.Exp)
                tt("dve", A(th), li, A(dt), ALU.mult)
                actf(A(sh), A(th), AF.Sin, scale=1.0 / 32)
                actf(A(s_), A(th), AF.Sin, scale=1.0 / 16)
                tt("dve", A(t1), A(sh), A(sh), ALU.mult)
                tsc("dve", A(c), A(t1), -2.0, ALU.mult, 1.0, ALU.add)
                for _ in range(4):
                    tt("dve", A(t1), A(c), A(c), ALU.mult)
                    tt("dve", A(t2), A(s_), A(s_), ALU.mult)
                    tt("dve", A(t3), A(c), A(s_), ALU.mult)
                    tt("dve", A(c), A(t1), A(t2), ALU.subtract)
                    tsc("dve", A(s_), A(t3), 2.0, ALU.mult)
                tt("dve", A(are), A(mag), A(c), ALU.mult)
                tt("dve", A(aim), A(mag), A(s_), ALU.mult)
                tt("dve", A(t1), lr, lr, ALU.mult)
                tt("dve", A(t2), li, li, ALU.mult)
                tt("dve", A(t1), A(t1), A(t2), ALU.add)
                S.op("dve", lambda e: e.reciprocal(out=A(t1), in_=A(t1)), reads=R_, writes=W_)
                tsc("dve", A(t2), A(are), -1.0, ALU.add)
                tt("dve", A(t3), A(t2), lr, ALU.mult)
                tt("dve", A(c), A(aim), li, ALU.mult)
                tt("dve", A(t3), A(t3), A(c), ALU.add)
                tt("dve", A(cre_), A(t3), A(t1), ALU.mult)
                tt("dve", A(t3), A(aim), lr, ALU.mult)
                tt("dve", A(c), A(t2), li, ALU.mult)
                tt("dve", A(t3), A(t3), A(c), ALU.subtract)
                tt("dve", A(cim_), A(t3), A(t1), ALU.mult)
                return are, aim, cre_, cim_

            with ExitStack() as st:
                WS = sb(st, "C_WS", [128, 16, 6, 2, 128], BF16)
                WO = sb(st, "C_WO", [128, 16, 16, 2, 32], BF16)
                WK = sb(st, "C_WK", [128, 16, 6, 128], BF16)
                a16 = sb(st, "C_a16", [128, 2, 16])
                with ExitStack() as st2:
                    pc = sb(st2, "C_pc", [128, 6, 3, 128])
                    bc = sb(st2, "C_bc", [128, 6, 2, 128])
                    S.dma("sp", "c", pc[:, :, :, :], s5_pcm[:, :, :, :], writes=W_)
                    S.dma("sp", "c", bc[:, :, :, :], s5_bcm[:, :, :, :], writes=W_)
                    are, aim, cre_, cim_ = cparams(st2, "cm", pc[:, :, 0, :], pc[:, :, 1, :], pc[:, :, 2, :], [128, 6, 128])
                    wr = sb(st2, "C_wr", [128, 6, 128])
                    wi = sb(st2, "C_wi", [128, 6, 128])
                    u1 = sb(st2, "C_u1", [128, 6, 128])
                    u2 = sb(st2, "C_u2", [128, 6, 128])
                    F3 = (slice(None),) * 3

                    def cmul(orr, oi, xr, xi, yr, yi):
                        tt("dve", u1[F3], xr, yr, ALU.mult)
                        tt("dve", u2[F3], xi, yi, ALU.mult)
                        tt("dve", u1[F3], u1[F3], u2[F3], ALU.subtract)
                        tt("dve", u2[F3], xr, yi, ALU.mult)
                        tt("dve", oi, xi, yr, ALU.mult)
                        tt("dve", oi, oi, u2[F3], ALU.add)
                        S.op("dve", lambda e: e.tensor_copy(out=orr, in_=u1[F3]), reads=R_, writes=W_)

                    cmul(wr[F3], wi[F3], cre_[F3], cim_[F3], bc[:, :, 0, :], bc[:, :, 1, :])
                    for lag in range(16):
                        S.op("act", lambda e, lag=lag: e.activation(out=WS[:, lag, :, 0, :], in_=wr[F3], func=AF.Copy),
                             reads=R_, writes=W_)
                        S.op("act", lambda e, lag=lag: e.activation(out=WS[:, lag, :, 1, :], in_=wi[F3], func=AF.Copy),
                             reads=R_, writes=W_)
                        if lag < 15:
                            cmul(wr[F3], wi[F3], wr[F3], wi[F3], are[F3], aim[F3])
                barrier()
                with ExitStack() as st2:
                    pm = sb(st2, "C_pm", [128, 3, 16])
                    bs = sb(st2, "C_bs", [128, 16, 2, 32])
                    cs = sb(st2, "C_cs", [128, 16, 2, 32])
                    dsb = sb(st2, "C_d", [128, 6])
                    S.dma("sp", "c", pm[:, :, :], s5_psm[:, :, :], writes=W_)
                    S.dma("sp", "c", bs[:, :, :, :], s5_bsm[:, :, :, :], writes=W_)
                    S.dma("sp", "c", cs[:, :, :, :], s5_csm[:, :, :, :], writes=W_)
                    S.dma("sp", "c", dsb[:, :], s5_d[:, :], writes=W_)
                    sre, sim, scr, sci = cparams(st2, "sm", pm[:, 0, :], pm[:, 1, :], pm[:, 2, :], [128, 16])
                    apr = sb(st2, "C_apr", [128, 17, 16])
                    api = sb(st2, "C_api", [128, 17, 16])
                    napi = sb(st2, "C_napi", [128, 17, 16])
                    v1 = sb(st2, "C_v1", [128, 16])
                    v2 = sb(st2, "C_v2", [128, 16])
                    S.op("dve", lambda e: e.memset(apr[:, 0, :], 1.0), reads=R_, writes=W_)
                    S.op("dve", lambda e: e.memset(api[:, 0, :], 0.0), reads=R_, writes=W_)
                    for k in range(1, 17):
                        tt("dve", v1[:, :], apr[:, k - 1, :], sre[:, :], ALU.mult)
                        tt("dve", v2[:, :], api[:, k - 1, :], sim[:, :], ALU.mult)
                        tt("dve", apr[:, k, :], v1[:, :], v2[:, :], ALU.subtract)
                        tt("dve", v1[:, :], apr[:, k - 1, :], sim[:, :], ALU.mult)
                        tt("dve", v2[:, :], api[:, k - 1, :], sre[:, :], ALU.mult)
                        tt("dve", api[:, k, :], v1[:, :], v2[:, :], ALU.add)
                    tsc("dve", napi[:, :, :], api[:, :, :], -1.0, ALU.mult)
                    napr = sb(st2, "C_napr", [128, 17, 16])
                    tsc("dve", napr[:, :, :], apr[:, :, :], -1.0, ALU.mult)
                    S.op("dve", lambda e: e.tensor_copy(out=a16[:, 0, :], in_=apr[:, 16, :]), reads=R_, writes=W_)
                    S.op("dve", lambda e: e.tensor_copy(out=a16[:, 1, :], in_=api[:, 16, :]), reads=R_, writes=W_)
                    nsci = sb(st2, "C_nsci", [128, 16])
                    tsc("dve", nsci[:, :], sci[:, :], -1.0, ALU.mult)
                    bb = sb(st2, "C_bb", [128, 16, 2, 32])
                    x1 = sb(st2, "C_x1", [128, 32])
                    for i in range(16):
                        tsc("dve", x1[:, :], bs[:, i, 0, :], scr[:, i:i + 1], ALU.mult)
                        stt(bb[:, i, 0, :], bs[:, i, 1, :], nsci[:, i:i + 1], x1[:, :], ALU.mult, ALU.add)
                        tsc("dve", x1[:, :], bs[:, i, 1, :], scr[:, i:i + 1], ALU.mult)
                        stt(bb[:, i, 1, :], bs[:, i, 0, :], sci[:, i:i + 1], x1[:, :], ALU.mult, ALU.add)
                    Bs = sb(st2, "C_Bs", [128, 16, 2, 32])
                    tsc("dve", Bs[:, :, 0, :], bb[:, :, 1, :], -1.0, ALU.mult)
                    S.op("dve", lambda e: e.tensor_copy(out=Bs[:, :, 1, :], in_=bb[:, :, 0, :]), reads=R_, writes=W_)
                    Cp2 = sb(st2, "C_Cp2", [128, 16, 2, 32])
                    Cs2 = sb(st2, "C_Cs2", [128, 16, 2, 32])
                    S.op("dve", lambda e: e.tensor_copy(out=Cp2[:, :, 0, :], in_=cs[:, :, 0, :]), reads=R_, writes=W_)
                    tsc("dve", Cp2[:, :, 1, :], cs[:, :, 1, :], -1.0, ALU.mult)
                    tsc("dve", Cs2[:, :, 0, :], cs[:, :, 1, :], -1.0, ALU.mult)
                    tsc("dve", Cs2[:, :, 1, :], cs[:, :, 0, :], -1.0, ALU.mult)
                    crb = sb(st2, "C_crb", [128, 16, 2, 32], BF16)
                    S.op("dve", lambda e: e.tensor_copy(out=crb[:, :, :, :], in_=Cp2[:, :, :, :]), reads=R_, writes=W_)
                    S.op("pool", lambda e: e.memset(WK[:, :, :, :], 0.0), reads=R_, writes=W_)
                    barrier()
                    AB = sb(st2, "C_AB", [128, 2, 16, 2, 32], BF16)
                    tA = [sb(st2, "C_tA%d" % i, [128, 2, 32]) for i in range(2)]
                    tC = [sb(st2, "C_tC%d" % i, [128, 2, 32]) for i in range(2)]
                    psK = [ps(st2, "C_psK%d" % i, [128, 192]) for i in range(2)]
                    for lag in range(16):
                        lp_ = lag % 2
                        for i in range(16):
                            ip = i % 2
                            r0 = 32 * (i % 3)
                            cc_ = i // 3
                            S.op("act", lambda e, i=i, ip=ip, lag=lag: e.activation(out=tA[ip][:, :, :], in_=bb[:, i, :, :], func=AF.Copy,
                                                                                  scale=apr[:, lag, i:i + 1]),
                                 reads=[], writes=[("C_tA", ip)])
                            S.op("dve", lambda e, i=i, ip=ip, lag=lag, lp_=lp_: e.scalar_tensor_tensor(
                                out=AB[:, lp_, i, :, :], in0=Bs[:, i, :, :], scalar=api[:, lag, i:i + 1], in1=tA[ip][:, :, :],
                                op0=ALU.mult, op1=ALU.add), reads=[("C_tA", ip)], writes=[("C_AB", lp_, i)])
                            S.op("pool", lambda e, i=i, ip=ip, lag=lag: e.tensor_scalar(out=tC[ip][:, :, :], in0=Cp2[:, i, :, :],
                                                                                      scalar1=apr[:, lag + 1, i:i + 1], scalar2=1.0,
                                                                                      op0=ALU.mult, op1=ALU.mult),
                                 reads=[], writes=[("C_tC", ip)])
                            S.op("dve", lambda e, i=i, ip=ip, lag=lag: e.scalar_tensor_tensor(
                                out=WO[:, lag, i, :, :], in0=Cs2[:, i, :, :], scalar=api[:, lag + 1, i:i + 1], in1=tC[ip][:, :, :],
                                op0=ALU.mult, op1=ALU.add), reads=[("C_tC", ip)], writes=["C_WOw"])
                            S.op("pe", lambda e, i=i, r0=r0, cc_=cc_, lp_=lp_: e.matmul(
                                psK[lp_][r0:r0 + 32, cc_ * 32:(cc_ + 1) * 32], lhsT=AB[:, lp_, i, 0, :], rhs=crb[:, i, 0, :],
                                start=True, stop=False), reads=[("C_AB", lp_, i)], writes=[("C_psK", lp_, i)])
                            S.op("pe", lambda e, i=i, r0=r0, cc_=cc_, lp_=lp_: e.matmul(
                                psK[lp_][r0:r0 + 32, cc_ * 32:(cc_ + 1) * 32], lhsT=AB[:, lp_, i, 1, :], rhs=crb[:, i, 1, :],
                                start=False, stop=True), reads=[("C_AB", lp_, i)], writes=[("C_psK", lp_, i)])
                            S.op("pool" if False else "act", lambda e, i=i, r0=r0, cc_=cc_, lag=lag, lp_=lp_: e.activation(
                                out=WK[r0:r0 + 32, lag, cc_, r0:r0 + 32], in_=psK[lp_][r0:r0 + 32, cc_ * 32:(cc_ + 1) * 32],
                                func=AF.Copy), reads=[("C_psK", lp_, i)], writes=["C_WKw"])
                    barrier()
                    for cc_ in range(6):
                        stt(WK[:, 0, cc_, :], ident_f[:, :], dsb[:, cc_:cc_ + 1], WK[:, 0, cc_, :], ALU.mult, ALU.add)
                barrier()
                usb = sb(st, "C_u", [128, 6, LP], BF16)
                Ssb = sb(st, "C_S", [128, 16, 2, NCH])
                Xbf = sb(st, "C_Xbf", [128, 16, 2, NCH], BF16)
                ypre = sb(st, "C_ypre", [128, SEQ])
                g1 = sb(st, "C_g1", [128, SEQ])
                ya = sb(st, "C_ya", [128, 6, SEQ], BF16)
                w1 = sb(st, "C_w1", [128, 16])
                w2 = sb(st, "C_w2", [128, 16])
                w3 = sb(st, "C_w3", [128, 16])
                w4 = sb(st, "C_w4", [128, 16])
                psS = [ps(st, "C_psS%d" % i, [128, NCH]) for i in range(2)]
                psY = [ps(st, "C_psY%d" % i, [128, NCH]) for i in range(2)]
                for s_ in range(NSEQ):
                    S.dma("sp", "x", usb[:, :, :], uT[s_, :, :, :], reads=[("uT", s_, t) for t in ALLT], writes=["C_u"])
                    n_ = 0
                    for i in range(16):
                        r0 = 32 * (i % 3)
                        cc_ = i // 3
                        for ri in range(2):
                            b_ = n_ % 2
                            n_ += 1
                            mms(psS[b_][:, :], [(WS[r0:r0 + 32, 15 - j, cc_, ri, :], usb[r0:r0 + 32, cc_, j:LP:16])
                                                for j in range(16)], ["C_u", K_], ("C_psS", b_))
                            S.op("act", lambda e, i=i, ri=ri, b_=b_: e.activation(out=Ssb[:, i, ri, :], in_=psS[b_][:, :],
                                                                               func=AF.Copy),
                                 reads=[("C_psS", b_)], writes=["C_S"])
                    for c in range(1, NCH - 1):
                        S.op("dve", lambda e, c=c: e.tensor_tensor(out=w1[:, :], in0=Ssb[:, :, 0, c - 1], in1=a16[:, 0, :], op=ALU.mult),
                             reads=["C_S", K_], writes=["C_w1"])
                        S.op("dve", lambda e, c=c: e.tensor_tensor(out=w2[:, :], in0=Ssb[:, :, 1, c - 1], in1=a16[:, 1, :], op=ALU.mult),
                             reads=["C_S"], writes=["C_w2"])
                        S.op("pool", lambda e, c=c: e.tensor_tensor(out=w3[:, :], in0=Ssb[:, :, 1, c - 1], in1=a16[:, 0, :], op=ALU.mult),
                             reads=["C_S", K_], writes=["C_w3"])
                        S.op("pool", lambda e, c=c: e.tensor_tensor(out=w4[:, :], in0=Ssb[:, :, 0, c - 1], in1=a16[:, 1, :], op=ALU.mult),
                             reads=["C_S"], writes=["C_w4"])
                        S.op("dve", lambda e: e.tensor_tensor(out=w1[:, :], in0=w1[:, :], in1=w2[:, :], op=ALU.subtract),
                             reads=["C_w1", "C_w2"], writes=["C_w1"])
                        S.op("pool", lambda e: e.tensor_tensor(out=w3[:, :], in0=w3[:, :], in1=w4[:, :], op=ALU.add),
                             reads=["C_w3", "C_w4"], writes=["C_w3"])
                        S.op("dve", lambda e, c=c: e.tensor_tensor(out=Ssb[:, :, 0, c], in0=w1[:, :], in1=Ssb[:, :, 0, c], op=ALU.add),
                             reads=["C_w1", "C_w3", "C_S"], writes=["C_Sa"])
                        S.op("pool", lambda e, c=c: e.tensor_tensor(out=Ssb[:, :, 1, c], in0=w3[:, :], in1=Ssb[:, :, 1, c], op=ALU.add),
                             reads=["C_w3", "C_Sa", "C_S"], writes=["C_S"])
                    S.op("dve", lambda e: e.memset(Xbf[:, :, :, 0], 0.0), reads=["C_Xbf"], writes=["C_Xbf"])
                    S.op("dve", lambda e: e.tensor_copy(out=Xbf[:, :, :, 1:NCH], in_=Ssb[:, :, :, 0:NCH - 1]), reads=["C_S", "C_Xbf"], writes=["C_Xbf"])
                    n_ = 0
                    for cc_ in range(6):
                        tiles_cc = [i for i in range(3 * cc_, min(3 * cc_ + 3, 16))]
                        for tau in range(16):
                            b_ = n_ % 2
                            n_ += 1
                            pairs = [(WK[:, tau - j, cc_, :], usb[:, cc_, j:LP:16]) for j in range(tau + 1)]
                            if tau == 0:
                                pairs.append((zeros_b[:, :], usb[:, cc_, 0:LP:16]))
                            mms_out = psY[b_]
                            np_ = len(pairs)

                            def intra(idx):
                                l, r_ = pairs[idx]
                                S.op("pe", lambda e: e.matmul(mms_out[:, :], lhsT=l, rhs=r_, start=(idx == 0), stop=(idx == np_ - 1)),
                                     reads=["C_u", K_, "zeros_b"], writes=[("C_psY", b_)])
                            intra(0)
                            for i in tiles_cc:
                                r0 = 32 * (i % 3)
                                for ri in range(2):
                                    S.op("pe", lambda e, i=i, ri=ri, r0=r0, tau=tau: e.matmul(
                                        mms_out[r0:r0 + 32, :], lhsT=WO[:, tau, i, ri, :], rhs=Xbf[:, i, ri, :], start=False, stop=False),
                                        reads=["C_Xbf", K_], writes=[("C_psY", b_)])
                            for idx in range(1, np_):
                                intra(idx)
                            S.op("act", lambda e, tau=tau, b_=b_: e.activation(
                                out=ypre[:, tau:SEQ:16], in_=psY[b_][:, 1:NCH], func=AF.Copy),
                                reads=[("C_psY", b_)], writes=["C_ypre"])
                        S.op("act", lambda e: e.activation(out=g1[:, :], in_=ypre[:, :], func=AF.Square), reads=["C_ypre"], writes=["C_g1"])
                        S.op("dve", lambda e: e.tensor_scalar(out=g1[:, :], in0=g1[:, :], scalar1=0.0713548162726, scalar2=1.5957691216,
                                                              op0=ALU.mult, op1=ALU.add), reads=["C_g1"], writes=["C_g1"])
                        S.op("dve", lambda e: e.tensor_tensor(out=g1[:, :], in0=g1[:, :], in1=ypre[:, :], op=ALU.mult), reads=["C_g1", "C_ypre"], writes=["C_g1"])
                        S.op("act", lambda e: e.activation(out=g1[:, :], in_=g1[:, :], func=AF.Sigmoid), reads=["C_g1"], writes=["C_g1"])
                        S.op("dve", lambda e, cc_=cc_: e.tensor_tensor(out=ya[:, cc_, :], in0=g1[:, :], in1=ypre[:, :], op=ALU.mult),
                             reads=["C_g1", "C_ypre"], writes=["C_ya"])
                    S.dma("sp", "o", yaT[s_, :, :, :], ya[:, :, :], reads=["C_ya"], writes=[("yaT", s_)])

        if debug not in ("A", "B"):
            phase_C()
            barrier()

        if debug == "C":
            with ExitStack() as st:
                tmp = sb(st, "dbgtmp", [128, 6, SEQ], BF16)
                tmp2 = sb(st, "dbgtmp2", [128, 6, SEQ], F32)
                S.dma("sp", "x", tmp[:, :, :], yaT[0, :, :, :], reads=[("yaT", 0)], writes=["dbgtmp"])
                S.op("dve", lambda e: e.tensor_copy(out=tmp2[:, :, :], in_=tmp[:, :, :]), reads=["dbgtmp"], writes=["dbgtmp2"])
                S.dma("sp", "o", outT[0, :, 0:6, 0:SEQ], tmp2[:, :, :], reads=["dbgtmp2"], writes=["out"])

        biasG_d = din("biasG", [128, 8, 1024])
        biasM_d = din("biasM", [16, 8, 512])
        cvec_d = din("cvec", [128, 8])
        ybT = dscr("ybT", [NSEQ, 128, 4, SEQ], BF16)
        ones_b = sb(es, "ones_b", [128, 128], BF16)
        S.op("dve", lambda e: e.memset(ones_b[:, :], 1.0), writes=["ones_b"])
        NIT = 22
        TOPK = 256.0

        def phase_D():
            with ExitStack() as st:
                Gb = sb(st, "D_Gb", [128, 8, 1024], BF16)
                Mb = sb(st, "D_Mb", [16, 8, 512], BF16)
                cv = sb(st, "D_cv", [128, 8])
                S.dma("sp", "c", cv[:, :], cvec_d[:, :], writes=["D_cv"])
                with ExitStack() as st2:
                    Gf = sb(st2, "D_Gf", [128, 8, 1024])
                    Mf = sb(st2, "D_Mf", [16, 8, 512])
                    S.dma("sp", "c", Gf[:, :, :], biasG_d[:, :, :], writes=["D_Gf"])
                    S.dma("sp", "c", Mf[:, :, :], biasM_d[:, :, :], writes=["D_Mf"])
                    for h in range(8):
                        S.op("dve", lambda e, h=h: e.tensor_scalar(out=Gb[:, h, :], in0=Gf[:, h, :], scalar1=cv[:, h:h + 1],
                                                                   scalar2=None, op0=ALU.subtract),
                             reads=["D_Gf", "D_cv"], writes=["D_Gb"])
                        S.op("dve", lambda e, h=h: e.tensor_scalar(out=Mb[:, h, :], in0=Mf[:, h, :], scalar1=cv[:16, h:h + 1],
                                                                   scalar2=None, op0=ALU.subtract),
                             reads=["D_Mf", "D_cv"], writes=["D_Mb"])
                    barrier()
                qi = sb(st, "D_qi", [128, 4, SEQ], BF16)
                ki = sb(st, "D_ki", [128, LP], BF16)
                qq = sb(st, "D_q", [128, 4, SEQ], BF16)
                kk = sb(st, "D_k", [128, LP], BF16)
                vd = sb(st, "D_vd", [128, 17, 2, 128], BF16)
                wq = sb(st, "D_wq", [128, 16, 8])
                sc = [sb(st, "D_sc%d" % i, [128, 4, LP]) for i in range(2)]
                MA = [sb(st, "D_MA%d" % i, [128, 4, LP], BF16) for i in range(2)]
                junk = sb(st, "D_junk", [128, LP], BF16)
                Rb = [sb(st, "D_Rb%d" % i, [128, 512], BF16) for i in range(2)]
                dg = sb(st, "D_dg", [128, 8, 128], BF16)
                Pt = [sb(st, "D_Pt%d" % i, [128, 512], BF16) for i in range(3)]
                rd = sb(st, "D_rd", [128, 512])
                rds = sb(st, "D_rds", [128, 512])
                Osb = sb(st, "D_Osb", [128, 512])
                yb = sb(st, "D_yb", [128, 4, 512], BF16)
                lo = [sb(st, "D_lo%d" % i, [128, 4]) for i in range(2)]
                hi = sb(st, "D_hi", [128, 4])
                W0 = sb(st, "D_W0", [128, 4])
                Wk = sb(st, "D_Wk", [128, 4])
                mid = sb(st, "D_mid", [128, 4])
                cnt = sb(st, "D_cnt", [128, 4])
                stp = sb(st, "D_stp", [128, 4])
                pq = [ps(st, "D_pq%d" % i, [128, 512]) for i in range(2)]
                psc = ps(st, "D_psc", [128, 512])
                pL = [ps(st, "D_pL%d" % i, [128, 512]) for i in range(2)]
                pOD = [ps(st, "D_pOD%d" % i, [128, 512]) for i in range(2)]
                pSh = ps(st, "D_pSh", [128, 512])
                S.op("dve", lambda e: e.memset(vd[:, :, 0, 64:128], 1.0), writes=["D_vd1"])
                S.op("dve", lambda e: e.memset(vd[:, :, 1, 0:64], 1.0), writes=["D_vd1"])
                ACT_JL = ()
                nmid = sb(st, "D_nmid", [128, 4])
                thrc = sb(st, "D_thrc", [128, 4, 4])
                for Q_ in range(4):
                    for jl_ in range(4):
                        v_ = (510.5 - (NMETA + 128 * (4 * Q_ + jl_ + 1))) if jl_ in ACT_JL else TOPK
                        S.op("dve", lambda e, Q_=Q_, jl_=jl_, v_=v_: e.memset(thrc[:, Q_, jl_:jl_ + 1], float(v_)), writes=["D_thrc"])

                def indexer(s_, Q):
                    u = Q % 2
                    for jl in range(4):
                        j = 4 * Q + jl
                        Nj = NMETA + 128 * (j + 1)
                        for h in range(8):
                            S.op("pool", lambda e, h=h, j=j: e.tensor_scalar(out=dg[:, h, :], in0=ident_f[:, :], scalar1=wq[:, j, h:h + 1],
                                                                           scalar2=1.0, op0=ALU.mult, op1=ALU.mult),
                                 reads=["ident_f", "D_wq"], writes=["D_dg"])
                        for c0 in range(0, Nj, 512):
                            cw = min(512, Nj - c0)

                            def qk(h):
                                hh, hp, b_ = h % 2, h // 2, h % 2
                                S.op("pe", lambda e: e.matmul(
                                    pq[b_][:, :cw], lhsT=qi[64 * hh:64 * hh + 64, hp, 128 * j:128 * j + 128],
                                    rhs=ki[64 * hh:64 * hh + 64, c0:c0 + cw], start=True, stop=True),
                                    reads=["D_qi", "D_ki"], writes=[("D_pq", b_)])
                                S.op("act", lambda e: e.activation(out=Rb[b_][:, :cw], in_=pq[b_][:, :cw], func=AF.Relu),
                                     reads=[("D_pq", b_)], writes=[("D_Rb", b_)])

                            def dgm(h):
                                b_ = h % 2
                                S.op("pe", lambda e: e.matmul(psc[:, :cw], lhsT=dg[:, h, :], rhs=Rb[b_][:, :cw],
                                                              start=(h == 0), stop=(h == 7)),
                                     reads=[("D_Rb", b_), "D_dg"], writes=["D_psc"])

                            qk(0)
                            qk(1)
                            for h in range(8):
                                dgm(h)
                                if h + 2 < 8:
                                    qk(h + 2)
                            S.op("act", lambda e, jl=jl, c0=c0, cw=cw: e.activation(out=sc[u][:, jl, c0:c0 + cw], in_=psc[:, :cw], func=AF.Copy),
                                 reads=["D_psc"], writes=[("D_sc", u, jl)])
                        S.op("dve", lambda e, jl=jl, Nj=Nj: e.tensor_reduce(out=lo[u][:, jl:jl + 1], in_=sc[u][:, jl, :Nj], axis=AX.X, op=ALU.min),
                             reads=[("D_sc", u, jl)], writes=[("D_lo", u)])
                        S.op("dve", lambda e, jl=jl, Nj=Nj: e.tensor_reduce(out=hi[:, jl:jl + 1], in_=sc[u][:, jl, :Nj], axis=AX.X, op=ALU.max),
                             reads=[("D_sc", u, jl)], writes=["D_hi"])
                        S.op("dve", lambda e, jl=jl, Nj=Nj: e.memset(sc[u][0:64, jl, Nj - 64:Nj], -1e30),
                             reads=[("D_sc", u, jl), ("D_lo", u), "D_hi"], writes=[("D_sc", u, jl)])

                def bisect_steps(Q):
                    u = Q % 2
                    L_ = lo[u]
                    steps = []

                    def init():
                        S.op("dve", lambda e: e.tensor_tensor(out=W0[:, :], in0=hi[:, :], in1=L_[:, :], op=ALU.subtract),
                             reads=[("D_lo", u), "D_hi"], writes=["D_W0"])
                    steps.append(init)

                    def mk(it):
                        def f():
                            S.op("dve", lambda e: e.tensor_scalar(out=Wk[:, :], in0=W0[:, :], scalar1=2.0 ** (-(it + 1)), scalar2=None,
                                                                  op0=ALU.mult), reads=["D_W0", "D_stp"], writes=["D_Wk"])
                            S.op("dve", lambda e: e.tensor_tensor(out=mid[:, :], in0=L_[:, :], in1=Wk[:, :], op=ALU.add),
                                 reads=[("D_lo", u), "D_Wk"], writes=["D_mid"])
                            if ACT_JL:
                                S.op("dve", lambda e: e.tensor_scalar(out=nmid[:, :], in0=mid[:, :], scalar1=-1.0, scalar2=None, op0=ALU.mult),
                                     reads=["D_mid"], writes=["D_nmid"])
                            for jl in range(4):
                                Nj = NMETA + 128 * (4 * Q + jl + 1)
                                if jl in ACT_JL:
                                    S.op("act", lambda e, jl=jl, Nj=Nj: e.activation(
                                        out=junkA[:, :Nj], in_=sc[u][:, jl, :Nj], func=AF.Sign, bias=nmid[:, jl:jl + 1], scale=1.0,
                                        accum_out=cnt[:, jl:jl + 1]),
                                        reads=[("D_sc", u, jl), "D_nmid"], writes=["D_junkA", ("D_cnt", jl)])
                                else:
                                    S.op("dve", lambda e, jl=jl, Nj=Nj: e.tensor_scalar(
                                        out=junk[:, :Nj], in0=sc[u][:, jl, :Nj], scalar1=mid[:, jl:jl + 1], scalar2=0.0,
                                        op0=ALU.is_ge, op1=ALU.add, accum_out=cnt[:, jl:jl + 1]),
                                        reads=[("D_sc", u, jl), "D_mid"], writes=["D_junk", ("D_cnt", jl)])
                            S.op("dve", lambda e: e.tensor_tensor(out=stp[:, :], in0=cnt[:, :], in1=thrc[:, Q, :], op=ALU.is_ge),
                                 reads=[("D_cnt", jl) for jl in range(4)] + ["D_thrc"], writes=["D_stp"])
                            S.op("dve", lambda e: e.tensor_tensor(out=stp[:, :], in0=stp[:, :], in1=Wk[:, :], op=ALU.mult),
                                 reads=["D_stp", "D_Wk"], writes=["D_stp"])
                            S.op("dve", lambda e: e.tensor_tensor(out=L_[:, :], in0=L_[:, :], in1=stp[:, :], op=ALU.add),
                                 reads=[("D_lo", u), "D_stp"], writes=[("D_lo", u)])
                        return f
                    for it in range(NIT):
                        steps.append(mk(it))

                    def fin():
                        for jl in range(4):
                            Nj = NMETA + 128 * (4 * Q + jl + 1)
                            S.op("dve", lambda e, jl=jl, Nj=Nj: e.tensor_scalar(out=MA[u][:, jl, :Nj], in0=sc[u][:, jl, :Nj], scalar1=L_[:, jl:jl + 1],
                                                                              scalar2=-30000.0, op0=ALU.is_lt, op1=ALU.mult),
                                 reads=[("D_sc", u, jl), ("D_lo", u)], writes=[("D_MA", u)])
                    steps.append(fin)
                    return steps

                def attention(s_, Q, filler):
                    u = Q % 2
                    nblk = 4 * Q + 5
                    tiles = []
                    for h in range(8):
                        for b in range(nblk):
                            tiles.append((h, b))
                    NTL = len(tiles)

                    def geo(b):
                        w = NMETA if b == 0 else 128
                        pc0 = 0 if b == 0 else NMETA + 128 * (b - 1)
                        jl0 = max(0, b - 1 - 4 * Q)
                        return w, pc0, jl0

                    def stageA(n):
                        h, b = tiles[n]
                        hh, hp = h % 2, h // 2
                        w, pc0, jl0 = geo(b)
                        c0 = jl0 * 128
                        near = (b == 0 and Q == 0) or (b >= 1 and b - 1 >= 4 * Q - 1)
                        lb, pb_ = n % 2, n % 3
                        S.op("pe", lambda e: e.matmul(
                            pL[lb][:w, c0:512], lhsT=kk[64 * hh:64 * hh + 64, pc0:pc0 + w],
                            rhs=qq[64 * hh:64 * hh + 64, hp, 512 * Q + c0:512 * Q + 512], start=True, stop=False),
                            reads=["D_k", "D_q"], writes=[("D_pL", lb)])
                        if near:
                            if b == 0:
                                S.op("pe", lambda e: e.matmul(pL[lb][:NMETA, c0:512], lhsT=ident_b[:NMETA, :NMETA],
                                                              rhs=Mb[:NMETA, h, c0:512], start=False, stop=False),
                                     reads=["D_Mb", "ident_b"], writes=[("D_pL", lb)])
                            else:
                                z0 = 512 * Q + c0 - 128 * (b - 1) + 384
                                S.op("pe", lambda e: e.matmul(pL[lb][:, c0:512], lhsT=ident_b[:, :],
                                                              rhs=Gb[:, h, z0:z0 + 512 - c0], start=False, stop=False),
                                     reads=["D_Gb", "ident_b"], writes=[("D_pL", lb)])
                        for jl in range(jl0, 4):
                            S.op("pe", lambda e, jl=jl: e.matmul(pL[lb][:w, jl * 128:(jl + 1) * 128], lhsT=MA[u][:, jl, pc0:pc0 + w],
                                                                 rhs=ident_b[:, :], start=False, stop=(jl == 3)),
                                 reads=[("D_MA", u), "ident_b"], writes=[("D_pL", lb)])
                        S.op("act", lambda e: e.activation(out=Pt[pb_][:w, c0:512], in_=pL[lb][:w, c0:512], func=AF.Exp),
                             reads=[("D_pL", lb)], writes=[("D_Pt", pb_)])

                    def stageB(n):
                        h, b = tiles[n]
                        hh, hp = h % 2, h // 2
                        w, pc0, jl0 = geo(b)
                        c0 = jl0 * 128
                        pb_ = n % 3
                        ob = h % 2
                        S.op("pe", lambda e: e.matmul(pOD[ob][:, c0:512], lhsT=vd[:w, b, hh, :], rhs=Pt[pb_][:w, c0:512],
                                                      start=(b == 0), stop=(b == nblk - 1)),
                             reads=[("D_Pt", pb_), "D_vd", "D_vd1"], writes=[("D_pOD", ob)])
                        if b == nblk - 1:
                            orow = slice(64 * hh, 64 * hh + 64)
                            drow = slice(64 * (1 - hh), 64 * (1 - hh) + 64)
                            S.op("act", lambda e: e.activation(out=rd[drow, :], in_=pOD[ob][drow, :], func=AF.Ln),
                                 reads=[("D_pOD", ob)], writes=[("D_rd", hh)])
                            S.op("act", lambda e: e.activation(out=rd[drow, :], in_=rd[drow, :], func=AF.Exp, scale=-1.0),
                                 reads=[("D_rd", hh)], writes=[("D_rd", hh)])
                            S.op("pe", lambda e: e.matmul(pSh[orow, :], lhsT=ident_f[drow, drow], rhs=rd[drow, :], start=True, stop=True),
                                 reads=[("D_rd", hh), "ident_f"], writes=[("D_pSh", hh)])
                            S.op("act", lambda e: e.activation(out=rds[orow, :], in_=pSh[orow, :], func=AF.Copy),
                                 reads=[("D_pSh", hh)], writes=[("D_rds", hh)])
                            S.op("act", lambda e: e.activation(out=Osb[orow, :], in_=pOD[ob][orow, :], func=AF.Copy),
                                 reads=[("D_pOD", ob)], writes=[("D_Osb", hh)])
                            S.op("pool", lambda e: e.tensor_tensor(out=yb[orow, hp, :], in0=Osb[orow, :], in1=rds[orow, :], op=ALU.mult),
                                 reads=[("D_rds", hh), ("D_Osb", hh)], writes=["D_yb"])

                    stageA(0)
                    stageA(1)
                    for n in range(NTL):
                        stageB(n)
                        if n + 2 < NTL:
                            stageA(n + 2)
                    while filler:
                        filler.pop(0)()
                    S.dma("sp", "o", ybT[s_, :, :, 512 * Q:512 * Q + 512], yb[:, :, :], reads=["D_yb"], writes=[("ybT", s_, Q)])

                for s_ in range(NSEQ):
                    S.dma("sp", "x", qi[:, :, :], qiT[s_, :, :, :], reads=[("qiT", s_, t) for t in range(NT)], writes=["D_qi"])
                    S.dma("sp", "x", ki[:, :], kiT[s_, :, :], reads=[("kiT", s_, t) for t in ALLT], writes=["D_ki"])
                    S.dma("sp", "x", qq[:, :, :], qT[s_, :, :, :], reads=[("qT", s_, t) for t in range(NT)], writes=["D_q"])
                    S.dma("sp", "x", kk[:, :], kT[s_, :, :], reads=[("kT", s_, t) for t in ALLT], writes=["D_k"])
                    vr = [("vwS", s_, t) for t in ALLT]
                    for half in range(2):
                        S.dma("pool", "x", vd[:, 1:17, half, 64 * half:64 * half + 64],
                              vwS[s_, NMETA:LP, 0:64].rearrange("(b p) c -> p b c", p=128), reads=vr, writes=["D_vd"])
                        S.dma("pool", "x", vd[:NMETA, 0, half, 64 * half:64 * half + 64], vwS[s_, 0:NMETA, 0:64], reads=vr, writes=["D_vd"])
                    S.dma("sp", "x", wq[:, :, :], vwS[s_, NMETA:LP, 64:72].rearrange("(b p) c -> p b c", p=128), reads=vr, writes=["D_wq"])
                    indexer(s_, 0)
                    for f_ in bisect_steps(0):
                        f_()
                    for Q in range(4):
                        if Q + 1 < 4:
                            indexer(s_, Q + 1)
                            for f_ in bisect_steps(Q + 1):
                                f_()
                        attention(s_, Q, [])

        if debug not in ("A", "B", "C"):
            phase_D()
            barrier()

        if debug == "D":
            with ExitStack() as st:
                tmp = sb(st, "dbgtmp", [128, 4, SEQ], BF16)
                tmp2 = sb(st, "dbgtmp2", [128, 4, SEQ], F32)
                S.dma("sp", "x", tmp[:, :, :], ybT[0, :, :, :], reads=[("ybT", 0, t) for t in range(NT)], writes=["dbgtmp"])
                S.op("dve", lambda e: e.tensor_copy(out=tmp2[:, :, :], in_=tmp[:, :, :]), reads=["dbgtmp"], writes=["dbgtmp2"])
                S.dma("sp", "o", outT[0, :, 0:4, 0:SEQ], tmp2[:, :, :], reads=["dbgtmp2"], writes=["out"])

        w_glu_d = din("w_glu", [128, 6, 768])
        w_a_d = din("w_a", [128, 6, D])
        w_b_d = din("w_b", [128, 4, D])
        w_o_d = din("w_o", [128, DC, D])
        w_g_d = din("w_g", [128, DC, 2 * D])
        h2T = dscr("h2T", [NSEQ, 128, DC, SEQ])

        def phase_E():
            with ExitStack() as st:
                wglu = sb(st, "E_wglu", [128, 6, 768], BF16)
                wa = sb(st, "E_wa", [128, 6, D], BF16)
                wb = sb(st, "E_wb", [128, 4, D], BF16)
                wo = sb(st, "E_wo", [128, DC, D], BF16)
                wgt = sb(st, "E_wg", [128, DC, 2 * D], BF16)
                S.dma("pool", "w", wglu[:, :, :], w_glu_d[:, :, :], writes=["E_w"], max_dma_last_dim=3072)
                for k in range(6):
                    S.dma("pool", "w", wa[:, k, :], w_a_d[:, k, :], writes=["E_w"])
                for k in range(4):
                    S.dma("pool", "w", wb[:, k, :], w_b_d[:, k, :], writes=["E_w"])
                for k in range(DC):
                    S.dma("pool", "w", wo[:, k, :], w_o_d[:, k, :], writes=["E_w"])
                    S.dma("pool", "w", wgt[:, k, :], w_g_d[:, k, :], writes=["E_w"], max_dma_last_dim=4096)
                hn2 = [sb(st, "E_hn%d" % i, [128, DC, TT], BF16) for i in range(2)]
                ya2 = [sb(st, "E_ya%d" % i, [128, 6, TT], BF16) for i in range(2)]
                yb2 = [sb(st, "E_yb%d" % i, [128, 4, TT], BF16) for i in range(2)]
                h12 = [sb(st, "E_h1%d" % i, [128, DC, TT]) for i in range(2)]
                yg = sb(st, "E_yg", [128, 6, TT], BF16)
                sgl = sb(st, "E_sgl", [128, TT])
                ga = sb(st, "E_ga", [128, TT])
                gb = sb(st, "E_gb", [128, TT])
                t1 = sb(st, "E_t1", [128, TT])
                t2 = sb(st, "E_t2", [128, TT])
                mg = sb(st, "E_mg", [128, DC, TT], BF16)
                ysb = sb(st, "E_y", [128, DC, TT])
                sq = sb(st, "E_sq", [128, 2, TT])
                accE = sb(st, "E_acc", [128, TT])
                rs = sb(st, "E_rs", [128, TT])
                pga = ps(st, "E_pga", [128, TT])
                pgb = ps(st, "E_pgb", [128, TT])
                pa = ps(st, "E_pa", [128, TT])
                pb = ps(st, "E_pb", [128, TT])
                py = [ps(st, "E_py%d" % i, [128, TT]) for i in range(2)]
                pss = ps(st, "E_pss", [128, TT])
                tl = [(s_, t_) for s_ in range(NSEQ) for t_ in range(NT)]

                def loadE(i):
                    s_, t_ = tl[i]
                    u = i % 2
                    tsl = slice(t_ * TT, (t_ + 1) * TT)
                    S.dma("sp", "x", hn2[u][:, :, :], hnT[s_, :, :, tsl], reads=[("hnT", s_, t_)], writes=[("E_hn", u)])
                    S.dma("sp", "x", ya2[u][:, :, :], yaT[s_, :, :, tsl], reads=[("yaT", s_)], writes=[("E_ya", u)])
                    S.dma("sp", "x", yb2[u][:, :, :], ybT[s_, :, :, tsl], reads=[("ybT", s_, t_)], writes=[("E_yb", u)])
                    S.dma("sp", "x", h12[u][:, :, :], h1T[s_, :, :, tsl], reads=[("h1T", s_, t_)], writes=[("E_h1", u)])

                loadE(0)
                for i, (s_, t_) in enumerate(tl):
                    if i + 1 < len(tl):
                        loadE(i + 1)
                    u = i % 2
                    hn, ya, ybt, h1t = hn2[u], ya2[u], yb2[u], h12[u]
                    khn, kya, kyb, kh1 = ("E_hn", u), ("E_ya", u), ("E_yb", u), ("E_h1", u)
                    tsl = slice(t_ * TT, (t_ + 1) * TT)
                    for oc in range(6):
                        b_ = oc % 2
                        mms(py[b_][:, :], [(wglu[:, k, oc * 128:(oc + 1) * 128], ya[:, k, :]) for k in range(6)],
                            ["E_w", kya], ("E_py", b_))
                        S.op("act", lambda e, b_=b_: e.activation(out=sgl[:, :], in_=py[b_][:, :], func=AF.Sigmoid),
                             reads=[("E_py", b_)], writes=["E_sgl"])
                        S.op("dve", lambda e, oc=oc: e.tensor_tensor(out=yg[:, oc, :], in0=sgl[:, :], in1=ya[:, oc, :], op=ALU.mult),
                             reads=["E_sgl", kya], writes=["E_yg"])
                    for dc in range(DC):
                        mms(pga[:, :], [(wgt[:, k, dc * 128:(dc + 1) * 128], hn[:, k, :]) for k in range(DC)], ["E_w", khn], "E_pga")
                        mms(pa[:, :], [(wa[:, k, dc * 128:(dc + 1) * 128], yg[:, k, :]) for k in range(6)], ["E_w", "E_yg"], "E_pa")
                        S.op("act", lambda e: e.activation(out=ga[:, :], in_=pga[:, :], func=AF.Sigmoid), reads=["E_pga"], writes=["E_ga"])
                        S.op("dve", lambda e: e.tensor_tensor(out=t1[:, :], in0=ga[:, :], in1=pa[:, :], op=ALU.mult), reads=["E_ga", "E_pa"], writes=["E_t1"])
                        mms(pgb[:, :], [(wgt[:, k, D + dc * 128:D + (dc + 1) * 128], hn[:, k, :]) for k in range(DC)], ["E_w", khn], "E_pgb")
                        mms(pb[:, :], [(wb[:, k, dc * 128:(dc + 1) * 128], ybt[:, k, :]) for k in range(4)], ["E_w", kyb], "E_pb")
                        S.op("act", lambda e: e.activation(out=gb[:, :], in_=pgb[:, :], func=AF.Sigmoid), reads=["E_pgb"], writes=["E_gb"])
                        S.op("dve", lambda e: e.tensor_tensor(out=t2[:, :], in0=gb[:, :], in1=pb[:, :], op=ALU.mult), reads=["E_gb", "E_pb"], writes=["E_t2"])
                        S.op("pool", lambda e, dc=dc: e.tensor_tensor(out=mg[:, dc, :], in0=t1[:, :], in1=t2[:, :], op=ALU.add),
                             reads=["E_t1", "E_t2"], writes=["E_mg"])
                    for c in range(DC):
                        b_ = c % 2
                        mms(py[b_][:, :], [(wo[:, k, c * 128:(c + 1) * 128], mg[:, k, :]) for k in range(DC)], ["E_w", "E_mg"], ("E_py", b_))
                        S.op("act", lambda e, c=c, b_=b_: e.activation(out=ysb[:, c, :], in_=py[b_][:, :], func=AF.Copy),
                             reads=[("E_py", b_)], writes=[("E_y", c)])
                        stats_step("E", c, py[b_][:, :], TT, sq, accE, [("E_py", b_)])
                    stats_finish("E", TT, accE, pss, rs)
                    for c in range(DC):
                        S.op("dve", lambda e, c=c: e.scalar_tensor_tensor(
                            out=ysb[:, c, :], in0=ysb[:, c, :], scalar=gains_sb[:, 24 + c:24 + c + 1], in1=rs[:, :],
                            op0=ALU.mult, op1=ALU.mult), reads=[("E_y", c), ("E", "rs"), "gains"], writes=[("E_y", c)])
                        S.op("pool", lambda e, c=c: e.tensor_tensor(out=ysb[:, c, :], in0=ysb[:, c, :], in1=h1t[:, c, :], op=ALU.add),
                             reads=[("E_y", c), kh1], writes=[("E_y", c)])
                    S.dma("sp", "o", h2T[s_, :, :, tsl], ysb[:, :, :], reads=[("E_y", c) for c in range(DC)], writes=[("h2T", s_, t_)])

        if debug not in ("A", "B", "C", "D"):
            phase_E()
            barrier()
            ff2_wg = din("ff2_wg", [128, DC, DFF])
            ff2_wu = din("ff2_wu", [128, DC, DFF])
            ff2_wd = din("ff2_wd", [128, FC, D])
            tilesF = []
            for s in range(NSEQ):
                for t in range(NFT):
                    tilesF.append((h2T[s, :, :, t * FT:(t + 1) * FT], outT[s, :, :, t * FT:(t + 1) * FT], FT,
                                   [("h2T", s, t * FT // TT)], [("out", s, t)]))
            ffn_phase("F", ff2_wg, ff2_wu, ff2_wd, 32, 40, tilesF)

        if debug == "A":
            with ExitStack() as st:
                tmp = sb(st, "dbgtmp", [128, DC, FT])
                S.dma("sp", "x", tmp[:, :, :], h1T[0, :, :, 0:FT], reads=[("h1T", 0, 0)], writes=["dbgtmp"])
                S.dma("sp", "o", outT[0, :, :, 0:FT], tmp[:, :, :], reads=["dbgtmp"], writes=["out"])
                S.dma("sp", "x", tmp[:, :, :NMETA], h1m[:, :, :], reads=["h1m", "out"], writes=["dbgtmp"])
                S.dma("sp", "o", outT[1, :, :, 0:NMETA], tmp[:, :, :NMETA], reads=["dbgtmp"], writes=["out"])

        S.drain("sp")
        print("instructions:", S.ninst)
    return nc


def _rel_bucket(rel):
    half, me = 16, 8
    base = np.where(rel > 0, half, 0)
    n = np.abs(rel)
    nf = np.maximum(n, 1).astype(np.float32)
    large = me + (np.log(nf / me) / math.log(128 / me) * (half - me)).astype(np.int32)
    large = np.minimum(large, half - 1)
    return base + np.where(n < me, n, large)


def prep_inputs(inp):
    f = lambda a: np.ascontiguousarray(np.asarray(a, dtype=np.float32))
    x = f(inp["x"])
    B = x.shape[0]
    xT = np.ascontiguousarray(x.reshape(B, SEQ, DC, 128).transpose(0, 3, 2, 1))
    metaT = np.ascontiguousarray(f(inp["meta_tokens"]).reshape(NMETA, DC, 128).transpose(2, 1, 0))
    gl = [inp[k] for k in ("ff1_norm_pre", "ff1_norm_post", "mix_norm_pre", "mix_norm_post", "ff2_norm_pre",
                           "ff2_norm_post")]
    gains = np.ascontiguousarray(np.concatenate([f(g)[0].reshape(DC, 128).T for g in gl], axis=1))

    def wk(w, kc):
        w = f(w)
        return np.ascontiguousarray(w.reshape(kc, 128, w.shape[-1]).transpose(1, 0, 2))

    shared = {
        "metaT": metaT, "gains": gains,
        "ff1_wg": wk(inp["ff1_w_gate"][0], DC), "ff1_wu": wk(inp["ff1_w_up"][0], DC),
        "ff1_wd": wk(inp["ff1_w_down"][0], FC),
    }
    win = f(inp["w_in"][0])
    upad = np.zeros((D, 6, 128), np.float32)
    for c6 in range(6):
        w_ = min(96, 512 - 96 * c6)
        upad[:, c6, :w_] = win[:, 96 * c6:96 * c6 + w_]
    winA = np.concatenate([upad.reshape(D, 768), win[:, 512:1024], win[:, 1096:1608], win[:, 1024:1088], win[:, 1024:1088],
                           win[:, 1608:1672], win[:, 1608:1672]], axis=1)
    winB = np.concatenate([win[:, 1672:1736], win[:, 1088:1096]], axis=1)
    shared["w_inA"] = wk(winA, DC)
    shared["w_inB"] = wk(winB, DC)
    lre, lim, ldt = f(inp["ssm_lambda_re"][0]), f(inp["ssm_lambda_im"][0]), f(inp["ssm_log_dt"][0])
    bre, bim = f(inp["ssm_b_re"][0]), f(inp["ssm_b_im"][0])
    cre, cim = f(inp["ssm_c_re"][0]), f(inp["ssm_c_im"][0])
    r = np.arange(128)
    sidx = np.arange(128)
    cc = np.arange(6)
    i_rc = 3 * cc[None, :] + (r[:, None] // 32)
    val_rc = (r[:, None] < 96) & (i_rc < 16)
    i_rc = np.where(val_rc, i_rc, 0)
    g_rcs = 2 * i_rc[:, :, None] + (sidx[None, None, :] // 64)
    p_s = sidx % 64
    pcm = np.stack([lre[g_rcs, p_s[None, None, :]], lim[g_rcs, p_s[None, None, :]], ldt[g_rcs]], axis=2)
    glr = (r % 32) // 16
    m_r = r % 16
    msk = (glr[:, None, None] == (sidx[None, None, :] // 64)) & val_rc[:, :, None]
    bcm = np.stack([np.where(msk, bre[g_rcs, p_s[None, None, :], m_r[:, None, None]], 0.0),
                    np.where(msk, bim[g_rcs, p_s[None, None, :], m_r[:, None, None]], 0.0)], axis=2)
    ii = np.arange(16)
    g_si = 2 * ii[None, :] + (sidx[:, None] // 64)
    psm = np.stack([lre[g_si, p_s[:, None]], lim[g_si, p_s[:, None]], ldt[g_si]], axis=1)
    q = np.arange(32)
    mq = q % 16
    mskq = ((q[None, None, :] // 16) == (sidx[:, None, None] // 64))
    bsm = np.stack([np.where(mskq, bre[g_si[:, :, None], p_s[:, None, None], mq[None, None, :]], 0.0),
                    np.where(mskq, bim[g_si[:, :, None], p_s[:, None, None], mq[None, None, :]], 0.0)], axis=2)
    csm = np.stack([np.where(mskq, cre[g_si[:, :, None], mq[None, None, :], p_s[:, None, None]], 0.0),
                    np.where(mskq, cim[g_si[:, :, None], mq[None, None, :], p_s[:, None, None]], 0.0)], axis=2)
    dflat = f(inp["ssm_d"][0]).reshape(512)
    ch_rc = 96 * cc[None, :] + r[:, None]
    vch = (r[:, None] < 96) & (ch_rc < 512)
    dsk = np.where(vch, dflat[np.where(vch, ch_rc, 0)], 0.0)
    shared["s5_pcm"] = f(pcm)
    shared["s5_bcm"] = f(bcm)
    shared["s5_psm"] = f(psm)
    shared["s5_bsm"] = f(bsm)
    shared["s5_csm"] = f(csm)
    shared["s5_d"] = f(dsk)
    shared["ident"] = np.eye(128, dtype=np.float32)
    rb = f(inp["rel_bias"])
    sl = np.arange(128)[:, None]
    zi = np.arange(1024)[None, :]
    shared["biasG"] = f(rb[_rel_bucket(sl - (zi - 384))].transpose(0, 2, 1))
    mm_ = np.arange(16)[:, None]
    tq = np.arange(512)[None, :]
    shared["biasM"] = f(rb[_rel_bucket(mm_ - 16 - tq)].transpose(0, 2, 1))
    shared["cvec"] = f(np.broadcast_to(rb[15][None, :], (128, 8)))
    def pad6rows(w):
        o = np.zeros((128, 6, w.shape[1]), np.float32)
        for c6 in range(6):
            w_ = min(96, 512 - 96 * c6)
            o[:w_, c6, :] = w[96 * c6:96 * c6 + w_]
        return o
    wg_ = f(inp["ssm_w_glu"][0])
    wgp = np.zeros((512, 6, 128), np.float32)
    for c6 in range(6):
        w_ = min(96, 512 - 96 * c6)
        wgp[:, c6, :w_] = wg_[:, 96 * c6:96 * c6 + w_]
    shared["w_glu"] = pad6rows(wgp.reshape(512, 768))
    shared["w_a"] = pad6rows(f(inp["w_branch_a"][0]))
    shared["w_b"] = wk(inp["w_branch_b"][0], 4)
    shared["w_o"] = wk(inp["w_out"][0], DC)
    shared["w_g"] = wk(win[:, 1736:3784], DC)
    shared["ff2_wg"] = wk(inp["ff2_w_gate"][0], DC)
    shared["ff2_wu"] = wk(inp["ff2_w_up"][0], DC)
    shared["ff2_wd"] = wk(inp["ff2_w_down"][0], FC)
    maps = []
    for c in range(NCORES):
        m = dict(shared)
        m["xT"] = xT[c * NSEQ:(c + 1) * NSEQ]
        maps.append(m)
    return maps


def kernel(**inputs):
    maps = prep_inputs(inputs)
    nc = build()
    res = run_bass_kernel_spmd(nc, maps, core_ids=list(range(NCORES)))
    outs = [r["outT"] for r in res.results]
    o = np.concatenate(outs, axis=0)
    out = o.transpose(0, 3, 2, 1).reshape(o.shape[0], SEQ, D)
    return np.ascontiguousarray(out.astype(np.float32))
```

```python
import math
from contextlib import ExitStack
import numpy as np
import ml_dtypes
import concourse.bass as bass
import concourse.mybir as mybir
from concourse.bass_utils import run_bass_kernel_spmd

F32 = mybir.dt.float32
BF16 = mybir.dt.bfloat16
AF = mybir.ActivationFunctionType
ALU = mybir.AluOpType
AX = mybir.AxisListType

NCORES = 8
D = 1024
DC = 8
SEQ = 2048
NSEQ = 2
NMETA = 16
DFF = 2816
FC = 22
EPS = 1e-6
TT = 512
NT = SEQ // TT
FT = 256
NFT = SEQ // FT


class Sync:
    def __init__(self, nc, es):
        self.nc = nc
        self.eng = {"pe": nc.tensor, "act": nc.scalar, "dve": nc.vector, "pool": nc.gpsimd, "sp": nc.sync}
        self.sem = {k: es.enter_context(nc.semaphore("s_" + k)) for k in self.eng}
        self.cnt = {k: 0 for k in self.eng}
        self.dsem = {}
        self.dcnt = {}
        self.es = es
        self.seen = {k: {} for k in self.eng}
        self.lastw = {}
        self.readers = {}
        self.ninst = 0

    NPOOL = {"sp": 12, "pool": 12, "act": 8}

    def dma_sem(self, q):
        if q not in self.dsem:
            self.dsem[q] = [self.es.enter_context(self.nc.semaphore("d_%s%d" % (q, i))) for i in range(self.NPOOL[q])]
            self.dcnt[q] = [0] * self.NPOOL[q]
            self.drr = getattr(self, "drr", {})
            self.drr[q] = 0
        i = self.drr[q]
        self.drr[q] = (i + 1) % self.NPOOL[q]
        return i

    def _wait(self, e, reads, writes):
        need = {}
        for k in reads:
            lw = self.lastw.get(k)
            if lw is not None:
                need[lw[0]] = max(need.get(lw[0], (0, None))[0], lw[1]), lw[2]
        for k in writes:
            lw = self.lastw.get(k)
            if lw is not None:
                need[lw[0]] = max(need.get(lw[0], (0, None))[0], lw[1]), lw[2]
            for r in self.readers.get(k, ()):
                need[r[0]] = max(need.get(r[0], (0, None))[0], r[1]), r[2]
        E = self.eng[e]
        for semid, (val, semobj) in need.items():
            if semid == "e_" + e and e == "pe":
                continue
            if self.seen[e].get(semid, 0) >= val:
                continue
            E.wait_ge(semobj, val)
            self.seen[e][semid] = val

    def _record(self, rec, reads, writes):
        for k in reads:
            self.readers.setdefault(k, []).append(rec)
        for k in writes:
            self.lastw[k] = rec
            self.readers[k] = []

    def op(self, e, fn, reads=(), writes=()):
        self._wait(e, reads, writes)
        inst = fn(self.eng[e])
        self.cnt[e] += 1
        inst.then_inc(self.sem[e], 1)
        self.ninst += 1
        self._record(("e_" + e, self.cnt[e], self.sem[e]), reads, writes)

    def dma(self, q, semname, out, in_, reads=(), writes=(), **kw):
        self._wait(q, reads, writes)
        i = self.dma_sem(q)
        sem = self.dsem[q][i]
        semid = "d_%s%d" % (q, i)
        if self.dcnt[q][i] > 0 and self.seen[q].get(semid, 0) < self.dcnt[q][i]:
            self.eng[q].wait_ge(sem, self.dcnt[q][i])
            self.seen[q][semid] = self.dcnt[q][i]
        inst = self.eng[q].dma_start(out=out, in_=in_, **kw)
        self.dcnt[q][i] += 16
        inst.then_inc(sem, 16)
        self.ninst += 1
        self._record((semid, self.dcnt[q][i], sem), reads, writes)

    def drain(self, e):
        E = self.eng[e]
        for k in self.eng:
            if self.cnt[k] > 0:
                E.wait_ge(self.sem[k], self.cnt[k])
        for q, sems in self.dsem.items():
            for i, sm in enumerate(sems):
                if self.dcnt[q][i] > 0:
                    E.wait_ge(sm, self.dcnt[q][i])


def build(debug=None):
    nc = bass.Bass("TRN2", target_bir_lowering=False)
    es = ExitStack()
    with es:
        S = Sync(nc, es)

        def din(name, shape, dt=F32):
            return nc.dram_tensor(name, list(shape), dt, kind="ExternalInput").ap()

        def dscr(name, shape, dt=F32):
            return nc.dram_tensor(name, list(shape), dt, kind="Internal").ap()

        xT = din("xT", [NSEQ, 128, DC, SEQ])
        metaT = din("metaT", [128, DC, NMETA])
        gains = din("gains", [128, 48])
        ff1_wg = din("ff1_wg", [128, DC, DFF])
        ff1_wu = din("ff1_wu", [128, DC, DFF])
        ff1_wd = din("ff1_wd", [128, FC, D])
        outT = nc.dram_tensor("outT", [NSEQ, 128, DC, SEQ], F32, kind="ExternalOutput").ap()
        h1T = dscr("h1T", [NSEQ, 128, DC, SEQ])
        h1m = dscr("h1m", [128, DC, NMETA])

        def sb(stack, name, shape, dt=F32):
            return stack.enter_context(nc.sbuf_tensor(name, list(shape), dt))

        def ps(stack, name, shape, dt=F32):
            return stack.enter_context(nc.psum_tensor(name, list(shape), dt))

        gains_sb = sb(es, "gains_sb", [128, 48])
        ones_f = sb(es, "ones_f", [128, 128])
        S.dma("sp", "c", gains_sb[:, :], gains[:, :], writes=["gains"])
        S.op("dve", lambda e: e.memset(ones_f[:, :], 1.0), writes=["ones_f"])
        zeros_b = sb(es, "zeros_b", [128, 128], BF16)
        S.op("dve", lambda e: e.memset(zeros_b[:, :], 0.0), writes=["zeros_b"])

        def rstd_from_sumsq(pss, rs, n, key_pss, key_rs, half=False):
            S.op("act", lambda e: e.activation(out=rs[:, :n], in_=pss[:, :n], func=AF.Sqrt,
                                               bias=eps_sb[:, (1 if half else 0):(2 if half else 1)],
                                               scale=(4.0 if half else 1.0) / D),
                 reads=[key_pss, "eps"], writes=[key_rs])
            S.op("dve", lambda e: e.reciprocal(out=rs[:, :n], in_=rs[:, :n]), reads=[key_rs], writes=[key_rs])

        eps_sb = sb(es, "eps_sb", [128, 2])
        S.op("dve", lambda e: e.memset(eps_sb[:, 0:1], EPS), writes=["eps"])
        S.op("dve", lambda e: e.memset(eps_sb[:, 1:2], 4.0 * EPS), writes=["eps"])

        def stats_step(tag, c, src, n, sq, acc, rkeys):
            S.op("act", lambda e: e.activation(out=sq[:, c % 2, :n], in_=src, func=AF.Square),
                 reads=rkeys, writes=[(tag, "sq", c % 2)])
            if c == 1:
                S.op("pool", lambda e: e.tensor_tensor(out=acc[:, :n], in0=sq[:, 0, :n], in1=sq[:, 1, :n], op=ALU.add),
                     reads=[(tag, "sq", 0), (tag, "sq", 1)], writes=[(tag, "acc")])
            elif c >= 2:
                S.op("pool", lambda e: e.tensor_tensor(out=acc[:, :n], in0=acc[:, :n], in1=sq[:, c % 2, :n], op=ALU.add),
                     reads=[(tag, "sq", c % 2), (tag, "acc")], writes=[(tag, "acc")])

        def stats_finish(tag, n, acc, pss, rs, half=False):
            S.op("pe", lambda e: e.matmul(pss[:, :n], lhsT=ones_f[:, :], rhs=acc[:, :n], start=True, stop=True),
                 reads=[(tag, "acc"), "ones_f"], writes=[(tag, "pss")])
            rstd_from_sumsq(pss, rs, n, (tag, "pss"), (tag, "rs"), half=half)

        def ffn_phase(tag, wg, wu, wd, gpre_col, gpost_col, tiles):
            with ExitStack() as st:
                wg_sb = sb(st, tag + "wg", [128, DC, DFF], BF16)
                wu_sb = sb(st, tag + "wu", [128, DC, DFF], BF16)
                wd_sb = sb(st, tag + "wd", [128, FC, D], BF16)
                xt = [sb(st, tag + "xt%d" % i, [128, DC, FT]) for i in range(2)]
                sq = sb(st, tag + "sq", [128, 2, FT])
                acc = sb(st, tag + "acc", [128, FT])
                hn = sb(st, tag + "hn", [128, DC, FT], BF16)
                act = sb(st, tag + "act", [128, FC, FT], BF16)
                sg = [sb(st, tag + "sg%d" % i, [128, FT]) for i in range(2)]
                ysb = sb(st, tag + "y", [128, DC, FT])
                rs = sb(st, tag + "rs", [128, FT])
                psg = [ps(st, tag + "psg%d" % i, [128, FT]) for i in range(2)]
                psu = [ps(st, tag + "psu%d" % i, [128, FT]) for i in range(2)]
                psy = [ps(st, tag + "psy%d" % i, [128, FT]) for i in range(2)]
                pss = ps(st, tag + "pss", [128, FT])
                for g in range(FC // 2):
                    cs_ = slice(g * 256, (g + 1) * 256)
                    S.dma("pool", "w", wg_sb[:, :, cs_], wg[:, :, cs_], writes=[(tag, "wg", g)])
                    S.dma("pool", "w", wu_sb[:, :, cs_], wu[:, :, cs_], writes=[(tag, "wu", g)])
                for j in range(FC):
                    S.dma("pool", "w", wd_sb[:, j, :], wd[:, j, :], writes=[(tag, "wd", j)], max_dma_last_dim=4096)

                def load(i):
                    src, dst, n, sr, dw = tiles[i]
                    S.dma("sp", "x", xt[i % 2][:, :, :n], src, reads=sr, writes=[(tag, "xt", i % 2)])

                load(0)
                for i, (src, dst, n, sr, dw) in enumerate(tiles):
                    if i + 1 < len(tiles):
                        load(i + 1)
                    x = xt[i % 2]
                    kx = (tag, "xt", i % 2)
                    for c in range(DC):
                        stats_step(tag, c, x[:, c, :n], n, sq, acc, [kx])
                    stats_finish(tag, n, acc, pss, rs)
                    for c in range(DC):
                        S.op("dve", lambda e, c=c: e.scalar_tensor_tensor(
                            out=hn[:, c, :n], in0=x[:, c, :n], scalar=gains_sb[:, gpre_col + c:gpre_col + c + 1],
                            in1=rs[:, :n], op0=ALU.mult, op1=ALU.mult),
                            reads=[kx, (tag, "rs"), "gains"], writes=[(tag, "hn", c)])
                    for j in range(FC):
                        b = j % 2
                        for k in range(DC):
                            S.op("pe", lambda e, k=k, j=j, b=b: e.matmul(
                                psg[b][:, :n], lhsT=wg_sb[:, k, j * 128:(j + 1) * 128], rhs=hn[:, k, :n],
                                start=(k == 0), stop=(k == DC - 1)),
                                reads=[(tag, "wg", j // 2), (tag, "hn", k)], writes=[(tag, "psg", b)])
                        for k in range(DC):
                            S.op("pe", lambda e, k=k, j=j, b=b: e.matmul(
                                psu[b][:, :n], lhsT=wu_sb[:, k, j * 128:(j + 1) * 128], rhs=hn[:, k, :n],
                                start=(k == 0), stop=(k == DC - 1)),
                                reads=[(tag, "wu", j // 2), (tag, "hn", k)], writes=[(tag, "psu", b)])
                        S.op("act", lambda e, b=b: e.activation(out=sg[b][:, :n], in_=psg[b][:, :n], func=AF.Silu),
                             reads=[(tag, "psg", b)], writes=[(tag, "sg", b)])
                        S.op("dve", lambda e, b=b, j=j: e.tensor_tensor(out=act[:, j, :n], in0=sg[b][:, :n],
                                                                        in1=psu[b][:, :n], op=ALU.mult),
                             reads=[(tag, "sg", b), (tag, "psu", b)], writes=[(tag, "act", j)])
                    for c in range(DC):
                        b = c % 2
                        for j in range(FC):
                            S.op("pe", lambda e, c=c, j=j, b=b: e.matmul(
                                psy[b][:, :n], lhsT=wd_sb[:, j, c * 128:(c + 1) * 128], rhs=act[:, j, :n],
                                start=(j == 0), stop=(j == FC - 1)),
                                reads=[(tag, "wd", j), (tag, "act", j)], writes=[(tag, "psy", b)])
                        S.op("act", lambda e, c=c, b=b: e.activation(out=ysb[:, c, :n], in_=psy[b][:, :n], func=AF.Copy),
                             reads=[(tag, "psy", b)], writes=[(tag, "y", c)])
                        stats_step(tag, c, psy[b][:, :n], n, sq, acc, [(tag, "psy", b)])
                    stats_finish(tag, n, acc, pss, rs, half=True)
                    for c in range(DC):
                        S.op("dve", lambda e, c=c: e.scalar_tensor_tensor(
                            out=ysb[:, c, :n], in0=ysb[:, c, :n], scalar=gains_sb[:, gpost_col + c:gpost_col + c + 1],
                            in1=rs[:, :n], op0=ALU.mult, op1=ALU.mult),
                            reads=[(tag, "y", c), (tag, "rs"), "gains"], writes=[(tag, "y", c)])
                        S.op("pool", lambda e, c=c: e.tensor_tensor(
                            out=ysb[:, c, :n], in0=ysb[:, c, :n], in1=x[:, c, :n], op=ALU.add),
                            reads=[(tag, "y", c), kx], writes=[(tag, "y", c)])
                    S.dma("sp", "o", dst, ysb[:, :, :n], reads=[(tag, "y", c) for c in range(DC)], writes=dw)

        def barrier():
            for e in S.eng:
                S.drain(e)

        S.barrier = barrier

        def mms(out, pairs, reads, wkey):
            n_ = len(pairs)
            for idx, (l, r) in enumerate(pairs):
                S.op("pe", lambda e, l=l, r=r, idx=idx: e.matmul(out, lhsT=l, rhs=r, start=(idx == 0),
                                                                 stop=(idx == n_ - 1)),
                     reads=reads, writes=[wkey])

        def prenorm(tag, x, kx, n, gcol, hn, sq, pss, rs, acc):
            for c in range(DC):
                stats_step(tag, c, x[:, c, :n], n, sq, acc, [kx])
            stats_finish(tag, n, acc, pss, rs)
            for c in range(DC):
                S.op("dve", lambda e, c=c: e.scalar_tensor_tensor(
                    out=hn[:, c, :n], in0=x[:, c, :n], scalar=gains_sb[:, gcol + c:gcol + c + 1],
                    in1=rs[:, :n], op0=ALU.mult, op1=ALU.mult),
                    reads=[kx, (tag, "rs"), "gains"], writes=[(tag, "hn")])

        tilesA = []
        for s in range(NSEQ):
            for t in range(NFT):
                tilesA.append((xT[s, :, :, t * FT:(t + 1) * FT], h1T[s, :, :, t * FT:(t + 1) * FT], FT, [],
                               [("h1T", s, t * FT // TT)]))
        tilesA.append((metaT[:, :, :], h1m[:, :, :], NMETA, [], ["h1m"]))
        if debug == "A":
            tilesA = tilesA[:2]
        ffn_phase("A", ff1_wg, ff1_wu, ff1_wd, 0, 8, tilesA)
        barrier()

        LP = NMETA + SEQ
        w_inA = din("w_inA", [128, DC, 2048])
        w_inB = din("w_inB", [128, DC, 72])
        uT = dscr("uT", [NSEQ, 128, 6, LP], BF16)
        kiT = dscr("kiT", [NSEQ, 128, LP], BF16)
        kT = dscr("kT", [NSEQ, 128, LP], BF16)
        qiT = dscr("qiT", [NSEQ, 128, 4, SEQ], BF16)
        qT = dscr("qT", [NSEQ, 128, 4, SEQ], BF16)
        vwS = dscr("vwS", [NSEQ, LP, 72], F32)
        hnT = dscr("hnT", [NSEQ, 128, DC, SEQ], BF16)

        def phase_B():
            tag = "B"
            with ExitStack() as st:
                wA = sb(st, "BwA", [128, DC, 2048], BF16)
                wB = sb(st, "BwB", [128, DC, 72], BF16)
                for k in range(DC):
                    S.dma("pool", "w", wA[:, k, :], w_inA[:, k, :], writes=[("B", "wA")], max_dma_last_dim=4096)
                S.dma("pool", "w", wB[:, :, :], w_inB[:, :, :], writes=[("B", "wB")])
                xt = [sb(st, "Bxt%d" % i, [128, DC, TT]) for i in range(2)]
                hn = sb(st, "Bhn", [128, DC, TT], BF16)
                sq = sb(st, "Bsq", [128, 2, TT])
                accB = sb(st, "Bacc", [128, TT])
                rs = sb(st, "Brs", [128, TT])
                stage = sb(st, "Bstage", [128, 16, TT], BF16)
                vw = sb(st, "Bvw", [128, 4, 72])
                pss = ps(st, "Bpss", [128, TT])
                pp = [ps(st, "Bpp%d" % i, [128, TT]) for i in range(2)]
                pv = [ps(st, "Bpv%d" % i, [128, 72]) for i in range(2)]
                tiles = [("m", 0)] + [(s, t) for s in range(NSEQ) for t in range(NT)]

                def load(i):
                    s_, t_ = tiles[i]
                    if s_ == "m":
                        S.dma("sp", "x", xt[i % 2][:, :, :NMETA], h1m[:, :, :], reads=["h1m"], writes=[("B", "xt", i % 2)])
                    else:
                        S.dma("sp", "x", xt[i % 2][:, :, :], h1T[s_, :, :, t_ * TT:(t_ + 1) * TT],
                              reads=[("h1T", s_, t_)], writes=[("B", "xt", i % 2)])

                load(0)
                for i, (s_, t_) in enumerate(tiles):
                    if i + 1 < len(tiles):
                        load(i + 1)
                    n = NMETA if s_ == "m" else TT
                    x = xt[i % 2]
                    kx = ("B", "xt", i % 2)
                    prenorm("B", x, kx, n, 16, hn, sq, pss, rs, accB)
                    if s_ != "m":
                        S.dma("act", "o", hnT[s_, :, :, t_ * TT:(t_ + 1) * TT], hn[:, :, :], reads=[("B", "hn")],
                              writes=[("hnT", s_, t_)])
                    for cc in range(16):
                        b_ = cc % 2
                        mms(pp[b_][:, :n], [(wA[:, k, cc * 128:(cc + 1) * 128], hn[:, k, :n]) for k in range(DC)],
                            [("B", "wA"), ("B", "hn")], ("B", "pp", b_))
                        sc_ = 0.125 if 10 <= cc < 14 else 1.0
                        S.op("act", lambda e, cc=cc, b_=b_, sc_=sc_: e.activation(
                            out=stage[:, cc, :n], in_=pp[b_][:, :n], func=AF.Copy, scale=sc_),
                            reads=[("B", "pp", b_)], writes=[("B", "stage")])
                    if s_ == "m":
                        for s2 in range(NSEQ):
                            S.dma("sp", "o", uT[s2, :, :, 0:NMETA], stage[:, 0:6, :NMETA], reads=[("B", "stage")],
                                  writes=[("uT", s2, "m")])
                            S.dma("sp", "o", kiT[s2, :, 0:NMETA], stage[:, 14, :NMETA], reads=[("B", "stage")],
                                  writes=[("kiT", s2, "m")])
                            S.dma("sp", "o", kT[s2, :, 0:NMETA], stage[:, 15, :NMETA], reads=[("B", "stage")],
                                  writes=[("kT", s2, "m")])
                    else:
                        t0 = t_ * TT
                        S.dma("sp", "o", uT[s_, :, :, NMETA + t0:NMETA + t0 + TT], stage[:, 0:6, :],
                              reads=[("B", "stage")], writes=[("uT", s_, t_)])
                        S.dma("sp", "o", qiT[s_, :, :, t0:t0 + TT], stage[:, 6:10, :], reads=[("B", "stage")],
                              writes=[("qiT", s_, t_)])
                        S.dma("sp", "o", qT[s_, :, :, t0:t0 + TT], stage[:, 10:14, :], reads=[("B", "stage")],
                              writes=[("qT", s_, t_)])
                        S.dma("sp", "o", kiT[s_, :, NMETA + t0:NMETA + t0 + TT], stage[:, 14, :],
                              reads=[("B", "stage")], writes=[("kiT", s_, t_)])
                        S.dma("sp", "o", kT[s_, :, NMETA + t0:NMETA + t0 + TT], stage[:, 15, :],
                              reads=[("B", "stage")], writes=[("kT", s_, t_)])
                    nb = max(1, n // 128)
                    rows = min(n, 128)
                    for blk in range(nb):
                        b_ = blk % 2
                        mms(pv[b_][:rows, :], [(hn[:, k, blk * 128:blk * 128 + rows], wB[:, k, :]) for k in range(DC)],
                            [("B", "wB"), ("B", "hn")], ("B", "pv", b_))
                        S.op("dve", lambda e, blk=blk, b_=b_: e.tensor_copy(out=vw[:rows, blk, :], in_=pv[b_][:rows, :]),
                             reads=[("B", "pv", b_)], writes=[("B", "vw")])
                    if s_ == "m":
                        for s2 in range(NSEQ):
                            S.dma("sp", "o", vwS[s2, 0:NMETA, :], vw[:NMETA, 0, :], reads=[("B", "vw")],
                                  writes=[("vwS", s2, "m")])
                    else:
                        S.dma("sp", "o", vwS[s_, NMETA + t0:NMETA + t0 + TT, :].rearrange("(b p) c -> p b c", p=128),
                              vw[:, :, :], reads=[("B", "vw")], writes=[("vwS", s_, t_)])

        if debug != "A":
            phase_B()
            barrier()

        ALLT = ["m"] + list(range(NT))

        if debug == "B":
            with ExitStack() as st:
                tmp = sb(st, "dbgtmp", [128, 6, LP], BF16)
                tmp2 = sb(st, "dbgtmp2", [128, 6, LP], F32)
                S.dma("sp", "x", tmp[:, :, :], uT[0, :, :, :], reads=[("uT", 0, t) for t in ALLT], writes=["dbgtmp"])
                S.op("dve", lambda e: e.tensor_copy(out=tmp2[:, :, :], in_=tmp[:, :, :]), reads=["dbgtmp"], writes=["dbgtmp2"])
                S.dma("sp", "o", outT[0, :, 0:6, 0:SEQ], tmp2[:, :, NMETA:LP], reads=["dbgtmp2"], writes=["out"])
                S.dma("sp", "x", tmp2[:, 0, 0:72 * 16].rearrange("p (b c) -> p b c", c=72),
                      vwS[0, NMETA:NMETA + 2048, :].rearrange("(b p) c -> p b c", p=128),
                      reads=[("vwS", 0, t) for t in ALLT] + ["out"], writes=["dbgtmp2"])
                S.dma("sp", "o", outT[1, :, 0, 0:72 * 16], tmp2[:, 0, 0:72 * 16], reads=["dbgtmp2"], writes=["out"])


        s5_pcm = din("s5_pcm", [128, 6, 3, 128])
        s5_bcm = din("s5_bcm", [128, 6, 2, 128])
        s5_psm = din("s5_psm", [128, 3, 16])
        s5_bsm = din("s5_bsm", [128, 16, 2, 32])
        s5_csm = din("s5_csm", [128, 16, 2, 32])
        s5_d = din("s5_d", [128, 6])
        ident_d = din("ident", [128, 128])
        yaT = dscr("yaT", [NSEQ, 128, 6, SEQ], BF16)
        NCH = LP // 16
        ident_f = sb(es, "ident_f", [128, 128])
        ident_b = sb(es, "ident_b", [128, 128], BF16)
        S.dma("sp", "c", ident_f[:, :], ident_d[:, :], writes=["ident_f"])
        S.op("dve", lambda e: e.tensor_copy(out=ident_b[:, :], in_=ident_f[:, :]), reads=["ident_f"], writes=["ident_b"])

        def phase_C():
            K_ = "Cprep"
            R_, W_ = [K_], [K_]

            def tt(eng, out, a, b_, op):
                S.op(eng, lambda e: e.tensor_tensor(out=out, in0=a, in1=b_, op=op), reads=R_, writes=W_)

            def tsc(eng, out, a, s1, op0, s2=None, op1=None):
                if op1 is None:
                    S.op(eng, lambda e: e.tensor_scalar(out=out, in0=a, scalar1=s1, scalar2=None, op0=op0), reads=R_, writes=W_)
                else:
                    S.op(eng, lambda e: e.tensor_scalar(out=out, in0=a, scalar1=s1, scalar2=s2, op0=op0, op1=op1),
                         reads=R_, writes=W_)

            def stt(out, a, sc_, b_, op0, op1):
                S.op("dve", lambda e: e.scalar_tensor_tensor(out=out, in0=a, scalar=sc_, in1=b_, op0=op0, op1=op1),
                     reads=R_, writes=W_)

            def actf(out, a, func, scale=1.0):
                S.op("act", lambda e: e.activation(out=out, in_=a, func=func, scale=scale), reads=R_, writes=W_)

            def cparams(st, nm, lr, li, ldt_, shp):
                T = lambda n_: sb(st, "C%s_%s" % (nm, n_), shp)
                dt, mag, th, sh, c, s_, t1, t2, t3 = [T(n_) for n_ in ("dt", "mag", "th", "sh", "c", "s", "t1", "t2", "t3")]
                are, aim, cre_, cim_ = [T(n_) for n_ in ("are", "aim", "cre", "cim")]
                A = lambda t_: t_[tuple(slice(None) for _ in shp)]
                actf(A(dt), ldt_, AF.Exp)
                tt("dve", A(t1), lr, A(dt), ALU.mult)
                actf(A(mag), A(t1), AF.Exp)
                tt("dve", A(th), li, A(dt), ALU.mult)
                actf(A(sh), A(th), AF.Sin, scale=1.0 / 32)
                actf(A(s_), A(th), AF.Sin, scale=1.0 / 16)
                tt("dve", A(t1), A(sh), A(sh), ALU.mult)
                tsc("dve", A(c), A(t1), -2.0, ALU.mult, 1.0, ALU.add)
                for _ in range(4):
                    tt("dve", A(t1), A(c), A(c), ALU.mult)
                    tt("dve", A(t2), A(s_), A(s_), ALU.mult)
                    tt("dve", A(t3), A(c), A(s_), ALU.mult)
                    tt("dve", A(c), A(t1), A(t2), ALU.subtract)
                    tsc("dve", A(s_), A(t3), 2.0, ALU.mult)
                tt("dve", A(are), A(mag), A(c), ALU.mult)
                tt("dve", A(aim), A(mag), A(s_), ALU.mult)
                tt("dve", A(t1), lr, lr, ALU.mult)
                tt("dve", A(t2), li, li, ALU.mult)
                tt("dve", A(t1), A(t1), A(t2), ALU.add)
                S.op("dve", lambda e: e.reciprocal(out=A(t1), in_=A(t1)), reads=R_, writes=W_)
                tsc("dve", A(t2), A(are), -1.0, ALU.add)
                tt("dve", A(t3), A(t2), lr, ALU.mult)
                tt("dve", A(c), A(aim), li, ALU.mult)
                tt("dve", A(t3), A(t3), A(c), ALU.add)
                tt("dve", A(cre_), A(t3), A(t1), ALU.mult)
                tt("dve", A(t3), A(aim), lr, ALU.mult)
                tt("dve", A(c), A(t2), li, ALU.mult)
                tt("dve", A(t3), A(t3), A(c), ALU.subtract)
                tt("dve", A(cim_), A(t3), A(t1), ALU.mult)
                return are, aim, cre_, cim_

            with ExitStack() as st:
                WS = sb(st, "C_WS", [128, 16, 6, 2, 128], BF16)
                WO = sb(st, "C_WO", [128, 16, 16, 2, 32], BF16)
                WK = sb(st, "C_WK", [128, 16, 6, 128], BF16)
                a16 = sb(st, "C_a16", [128, 2, 16])
                a32 = sb(st, "C_a32", [128, 2, 16])
                with ExitStack() as st2:
                    pc = sb(st2, "C_pc", [128, 6, 3, 128])
                    bc = sb(st2, "C_bc", [128, 6, 2, 128])
                    S.dma("sp", "c", pc[:, :, :, :], s5_pcm[:, :, :, :], writes=W_)
                    S.dma("sp", "c", bc[:, :, :, :], s5_bcm[:, :, :, :], writes=W_)
                    are, aim, cre_, cim_ = cparams(st2, "cm", pc[:, :, 0, :], pc[:, :, 1, :], pc[:, :, 2, :], [128, 6, 128])
                    wr = sb(st2, "C_wr", [128, 6, 128])
                    wi = sb(st2, "C_wi", [128, 6, 128])
                    u1 = sb(st2, "C_u1", [128, 6, 128])
                    u2 = sb(st2, "C_u2", [128, 6, 128])
                    F3 = (slice(None),) * 3

                    def cmul(orr, oi, xr, xi, yr, yi):
                        tt("dve", u1[F3], xr, yr, ALU.mult)
                        tt("dve", u2[F3], xi, yi, ALU.mult)
                        tt("dve", u1[F3], u1[F3], u2[F3], ALU.subtract)
                        tt("dve", u2[F3], xr, yi, ALU.mult)
                        tt("dve", oi, xi, yr, ALU.mult)
                        tt("dve", oi, oi, u2[F3], ALU.add)
                        S.op("dve", lambda e: e.tensor_copy(out=orr, in_=u1[F3]), reads=R_, writes=W_)

                    cmul(wr[F3], wi[F3], cre_[F3], cim_[F3], bc[:, :, 0, :], bc[:, :, 1, :])
                    for lag in range(16):
                        S.op("act", lambda e, lag=lag: e.activation(out=WS[:, lag, :, 0, :], in_=wr[F3], func=AF.Copy),
                             reads=R_, writes=W_)
                        S.op("act", lambda e, lag=lag: e.activation(out=WS[:, lag, :, 1, :], in_=wi[F3], func=AF.Copy),
                             reads=R_, writes=W_)
                        if lag < 15:
                            cmul(wr[F3], wi[F3], wr[F3], wi[F3], are[F3], aim[F3])
                barrier()
                with ExitStack() as st2:
                    pm = sb(st2, "C_pm", [128, 3, 16])
                    bs = sb(st2, "C_bs", [128, 16, 2, 32])
                    cs = sb(st2, "C_cs", [128, 16, 2, 32])
                    dsb = sb(st2, "C_d", [128, 6])
                    S.dma("sp", "c", pm[:, :, :], s5_psm[:, :, :], writes=W_)
                    S.dma("sp", "c", bs[:, :, :, :], s5_bsm[:, :, :, :], writes=W_)
                    S.dma("sp", "c", cs[:, :, :, :], s5_csm[:, :, :, :], writes=W_)
                    S.dma("sp", "c", dsb[:, :], s5_d[:, :], writes=W_)
                    sre, sim, scr, sci = cparams(st2, "sm", pm[:, 0, :], pm[:, 1, :], pm[:, 2, :], [128, 16])
                    apr = sb(st2, "C_apr", [128, 17, 16])
                    api = sb(st2, "C_api", [128, 17, 16])
                    napi = sb(st2, "C_napi", [128, 17, 16])
                    v1 = sb(st2, "C_v1", [128, 16])
                    v2 = sb(st2, "C_v2", [128, 16])
                    S.op("dve", lambda e: e.memset(apr[:, 0, :], 1.0), reads=R_, writes=W_)
                    S.op("dve", lambda e: e.memset(api[:, 0, :], 0.0), reads=R_, writes=W_)
                    for k in range(1, 17):
                        tt("dve", v1[:, :], apr[:, k - 1, :], sre[:, :], ALU.mult)
                        tt("dve", v2[:, :], api[:, k - 1, :], sim[:, :], ALU.mult)
                        tt("dve", apr[:, k, :], v1[:, :], v2[:, :], ALU.subtract)
                        tt("dve", v1[:, :], apr[:, k - 1, :], sim[:, :], ALU.mult)
                        tt("dve", v2[:, :], api[:, k - 1, :], sre[:, :], ALU.mult)
                        tt("dve", api[:, k, :], v1[:, :], v2[:, :], ALU.add)
                    tsc("dve", napi[:, :, :], api[:, :, :], -1.0, ALU.mult)
                    napr = sb(st2, "C_napr", [128, 17, 16])
                    tsc("dve", napr[:, :, :], apr[:, :, :], -1.0, ALU.mult)
                    S.op("dve", lambda e: e.tensor_copy(out=a16[:, 0, :], in_=apr[:, 16, :]), reads=R_, writes=W_)
                    S.op("dve", lambda e: e.tensor_copy(out=a16[:, 1, :], in_=api[:, 16, :]), reads=R_, writes=W_)
                    tt("dve", v1[:, :], apr[:, 16, :], apr[:, 16, :], ALU.mult)
                    tt("dve", v2[:, :], api[:, 16, :], api[:, 16, :], ALU.mult)
                    tt("dve", a32[:, 0, :], v1[:, :], v2[:, :], ALU.subtract)
                    tt("dve", v1[:, :], apr[:, 16, :], api[:, 16, :], ALU.mult)
                    tsc("dve", a32[:, 1, :], v1[:, :], 2.0, ALU.mult)
                    nsci = sb(st2, "C_nsci", [128, 16])
                    tsc("dve", nsci[:, :], sci[:, :], -1.0, ALU.mult)
                    bb = sb(st2, "C_bb", [128, 16, 2, 32])
                    x1 = sb(st2, "C_x1", [128, 32])
                    for i in range(16):
                        tsc("dve", x1[:, :], bs[:, i, 0, :], scr[:, i:i + 1], ALU.mult)
                        stt(bb[:, i, 0, :], bs[:, i, 1, :], nsci[:, i:i + 1], x1[:, :], ALU.mult, ALU.add)
                        tsc("dve", x1[:, :], bs[:, i, 1, :], scr[:, i:i + 1], ALU.mult)
                        stt(bb[:, i, 1, :], bs[:, i, 0, :], sci[:, i:i + 1], x1[:, :], ALU.mult, ALU.add)
                    Bs = sb(st2, "C_Bs", [128, 16, 2, 32])
                    tsc("dve", Bs[:, :, 0, :], bb[:, :, 1, :], -1.0, ALU.mult)
                    S.op("dve", lambda e: e.tensor_copy(out=Bs[:, :, 1, :], in_=bb[:, :, 0, :]), reads=R_, writes=W_)
                    Cp2 = sb(st2, "C_Cp2", [128, 16, 2, 32])
                    Cs2 = sb(st2, "C_Cs2", [128, 16, 2, 32])
                    S.op("dve", lambda e: e.tensor_copy(out=Cp2[:, :, 0, :], in_=cs[:, :, 0, :]), reads=R_, writes=W_)
                    tsc("dve", Cp2[:, :, 1, :], cs[:, :, 1, :], -1.0, ALU.mult)
                    tsc("dve", Cs2[:, :, 0, :], cs[:, :, 1, :], -1.0, ALU.mult)
                    tsc("dve", Cs2[:, :, 1, :], cs[:, :, 0, :], -1.0, ALU.mult)
                    crb = sb(st2, "C_crb", [128, 16, 2, 32], BF16)
                    S.op("dve", lambda e: e.tensor_copy(out=crb[:, :, :, :], in_=Cp2[:, :, :, :]), reads=R_, writes=W_)
                    S.op("pool", lambda e: e.memset(WK[:, :, :, :], 0.0), reads=R_, writes=W_)
                    barrier()
                    AB = sb(st2, "C_AB", [128, 2, 16, 2, 32], BF16)
                    f1 = [sb(st2, "C_f1%d" % i, [128, 16, 64]) for i in range(2)]
                    f2 = [sb(st2, "C_f2%d" % i, [128, 16, 64]) for i in range(2)]
                    psK = [ps(st2, "C_psK%d" % i, [128, 192]) for i in range(2)]
                    for lp_ in range(2):
                        S.op("pe", lambda e, lp_=lp_: e.matmul(psK[lp_][:, :], lhsT=zeros_b[:, :], rhs=crb[:, 0:3, :, :].rearrange("p i r q -> p (i r q)"),
                                                             start=True, stop=True), reads=["zeros_b"], writes=[("C_psK", lp_)])

                    def bc(t, k):
                        return t[:, k, :].unsqueeze(2).to_broadcast([128, 16, 64])

                    Bp3 = bb[:, :, :, :].rearrange("p i r q -> p i (r q)")
                    Bs3 = Bs[:, :, :, :].rearrange("p i r q -> p i (r q)")
                    Cp3 = Cp2[:, :, :, :].rearrange("p i r q -> p i (r q)")
                    Cs3 = Cs2[:, :, :, :].rearrange("p i r q -> p i (r q)")
                    for lag in range(16):
                        lp_ = lag % 2
                        AB3 = AB[:, lp_, :, :, :].rearrange("p i r q -> p i (r q)")
                        WO3 = WO[:, lag, :, :, :].rearrange("p i r q -> p i (r q)")
                        S.op("dve", lambda e, lag=lag, lp_=lp_: e.tensor_tensor(out=f1[0][:, :, :], in0=Bp3, in1=bc(apr, lag), op=ALU.mult),
                             reads=[], writes=[("C_f1", 0)])
                        S.op("pool", lambda e, lag=lag, lp_=lp_: e.tensor_tensor(out=f2[0][:, :, :], in0=Bs3, in1=bc(api, lag), op=ALU.mult),
                             reads=[], writes=[("C_f2", 0)])
                        S.op("dve", lambda e, AB3=AB3: e.tensor_tensor(out=AB3, in0=f1[0][:, :, :], in1=f2[0][:, :, :], op=ALU.add),
                             reads=[("C_f1", 0), ("C_f2", 0)], writes=[("C_AB", lp_)])
                        S.op("dve", lambda e, lag=lag: e.tensor_tensor(out=f1[1][:, :, :], in0=Cp3, in1=bc(apr, lag + 1), op=ALU.mult),
                             reads=[], writes=[("C_f1", 1)])
                        S.op("pool", lambda e, lag=lag: e.tensor_tensor(out=f2[1][:, :, :], in0=Cs3, in1=bc(api, lag + 1), op=ALU.mult),
                             reads=[], writes=[("C_f2", 1)])
                        S.op("dve", lambda e, WO3=WO3: e.tensor_tensor(out=WO3, in0=f1[1][:, :, :], in1=f2[1][:, :, :], op=ALU.add),
                             reads=[("C_f1", 1), ("C_f2", 1)], writes=["C_WOw"])
                        for i in range(16):
                            r0 = 32 * (i % 3)
                            cc_ = i // 3
                            S.op("pe", lambda e, i=i, r0=r0, cc_=cc_, lp_=lp_: e.matmul(
                                psK[lp_][r0:r0 + 32, cc_ * 32:(cc_ + 1) * 32], lhsT=AB[:, lp_, i, 0, :], rhs=crb[:, i, 0, :],
                                start=True, stop=False), reads=[("C_AB", lp_)], writes=[("C_psK", lp_)])
                            S.op("pe", lambda e, i=i, r0=r0, cc_=cc_, lp_=lp_: e.matmul(
                                psK[lp_][r0:r0 + 32, cc_ * 32:(cc_ + 1) * 32], lhsT=AB[:, lp_, i, 1, :], rhs=crb[:, i, 1, :],
                                start=False, stop=True), reads=[("C_AB", lp_)], writes=[("C_psK", lp_)])
                        for rb in range(3):
                            S.op("act", lambda e, rb=rb, lag=lag, lp_=lp_: e.activation(
                                out=WK[32 * rb:32 * rb + 32, lag, :, 32 * rb:32 * rb + 32],
                                in_=psK[lp_][32 * rb:32 * rb + 32, :].rearrange("p (c q) -> p c q", q=32),
                                func=AF.Copy), reads=[("C_psK", lp_)], writes=["C_WKw"])
                    barrier()
                    for cc_ in range(6):
                        stt(WK[:, 0, cc_, :], ident_f[:, :], dsb[:, cc_:cc_ + 1], WK[:, 0, cc_, :], ALU.mult, ALU.add)
                barrier()
                usb = sb(st, "C_u", [128, 6, LP], BF16)
                Ssb = sb(st, "C_S", [128, 16, 2, NCH])
                Xbf = sb(st, "C_Xbf", [128, 16, 2, NCH], BF16)
                ypre = sb(st, "C_ypre", [128, SEQ])
                g1 = sb(st, "C_g1", [128, SEQ])
                ya = sb(st, "C_ya", [128, 6, SEQ], BF16)
                w1 = sb(st, "C_w1", [128, 16])
                w2 = sb(st, "C_w2", [128, 16])
                w3 = sb(st, "C_w3", [128, 16])
                w4 = sb(st, "C_w4", [128, 16])
                psS = [ps(st, "C_psS%d" % i, [128, NCH]) for i in range(2)]
                psY = [ps(st, "C_psY%d" % i, [128, NCH]) for i in range(2)]
                for s_ in range(NSEQ):
                    S.dma("sp", "x", usb[:, :, :], uT[s_, :, :, :], reads=[("uT", s_, t) for t in ALLT], writes=["C_u"])
                    n_ = 0
                    for i in range(16):
                        r0 = 32 * (i % 3)
                        cc_ = i // 3
                        for ri in range(2):
                            b_ = n_ % 2
                            n_ += 1
                            mms(psS[b_][:, :], [(WS[r0:r0 + 32, 15 - j, cc_, ri, :], usb[r0:r0 + 32, cc_, j:LP:16])
                                                for j in range(16)], ["C_u", K_], ("C_psS", b_))
                            S.op("act", lambda e, i=i, ri=ri, b_=b_: e.activation(out=Ssb[:, i, ri, :], in_=psS[b_][:, :],
                                                                               func=AF.Copy),
                                 reads=[("C_psS", b_)], writes=["C_S"])
                    T1 = ypre[:, 0:1024].rearrange("p (i c) -> p i c", c=64)
                    T2 = ypre[:, 1024:2048].rearrange("p (i c) -> p i c", c=64)
                    T3 = g1[:, 0:1024].rearrange("p (i c) -> p i c", c=64)
                    T4 = g1[:, 1024:2048].rearrange("p (i c) -> p i c", c=64)
                    TK = ["C_ypre", "C_g1"]

                    def bulk(dst_lo, dst_hi, src_lo, src_hi, nn):
                        ARb = a16[:, 0, :].unsqueeze(2).to_broadcast([128, 16, nn])
                        AIb = a16[:, 1, :].unsqueeze(2).to_broadcast([128, 16, nn])
                        Sr, Si = Ssb[:, :, 0, src_lo:src_hi:2], Ssb[:, :, 1, src_lo:src_hi:2]
                        Dr, Di = Ssb[:, :, 0, dst_lo:dst_hi:2], Ssb[:, :, 1, dst_lo:dst_hi:2]
                        t1_, t2_, t3_, t4_ = T1[:, :, :nn], T2[:, :, :nn], T3[:, :, :nn], T4[:, :, :nn]
                        S.op("dve", lambda e: e.tensor_tensor(out=t1_, in0=Sr, in1=ARb, op=ALU.mult), reads=["C_S", K_] + TK, writes=["C_T1"])
                        S.op("pool", lambda e: e.tensor_tensor(out=t2_, in0=Si, in1=AIb, op=ALU.mult), reads=["C_S", K_] + TK, writes=["C_T2"])
                        S.op("dve", lambda e: e.tensor_tensor(out=t3_, in0=Si, in1=ARb, op=ALU.mult), reads=["C_S", K_] + TK, writes=["C_T3"])
                        S.op("pool", lambda e: e.tensor_tensor(out=t4_, in0=Sr, in1=AIb, op=ALU.mult), reads=["C_S", K_] + TK, writes=["C_T4"])
                        S.op("dve", lambda e: e.tensor_tensor(out=t1_, in0=t1_, in1=t2_, op=ALU.subtract), reads=["C_T1", "C_T2"], writes=["C_T1"])
                        S.op("pool", lambda e: e.tensor_tensor(out=t3_, in0=t3_, in1=t4_, op=ALU.add), reads=["C_T3", "C_T4"], writes=["C_T3"])
                        S.op("dve", lambda e: e.tensor_tensor(out=Dr, in0=Dr, in1=t1_, op=ALU.add), reads=["C_T1", "C_T3", "C_S"], writes=["C_Sa"])
                        S.op("pool", lambda e: e.tensor_tensor(out=Di, in0=Di, in1=t3_, op=ALU.add), reads=["C_T3", "C_Sa", "C_S"], writes=["C_S"] + TK)

                    bulk(1, 128, 0, 127, 64)
                    for c in range(3, NCH - 1, 2):
                        S.op("dve", lambda e, c=c: e.tensor_tensor(out=w1[:, :], in0=Ssb[:, :, 0, c - 2], in1=a32[:, 0, :], op=ALU.mult),
                             reads=["C_S", K_], writes=["C_w1"])
                        S.op("dve", lambda e, c=c: e.tensor_tensor(out=w2[:, :], in0=Ssb[:, :, 1, c - 2], in1=a32[:, 1, :], op=ALU.mult),
                             reads=["C_S"], writes=["C_w2"])
                        S.op("pool", lambda e, c=c: e.tensor_tensor(out=w3[:, :], in0=Ssb[:, :, 1, c - 2], in1=a32[:, 0, :], op=ALU.mult),
                             reads=["C_S", K_], writes=["C_w3"])
                        S.op("pool", lambda e, c=c: e.tensor_tensor(out=w4[:, :], in0=Ssb[:, :, 0, c - 2], in1=a32[:, 1, :], op=ALU.mult),
                             reads=["C_S"], writes=["C_w4"])
                        S.op("dve", lambda e: e.tensor_tensor(out=w1[:, :], in0=w1[:, :], in1=w2[:, :], op=ALU.subtract),
                             reads=["C_w1", "C_w2"], writes=["C_w1"])
                        S.op("pool", lambda e: e.tensor_tensor(out=w3[:, :], in0=w3[:, :], in1=w4[:, :], op=ALU.add),
                             reads=["C_w3", "C_w4"], writes=["C_w3"])
                        S.op("dve", lambda e, c=c: e.tensor_tensor(out=Ssb[:, :, 0, c], in0=w1[:, :], in1=Ssb[:, :, 0, c], op=ALU.add),
                             reads=["C_w1", "C_w3", "C_S"], writes=["C_Sa"])
                        S.op("pool", lambda e, c=c: e.tensor_tensor(out=Ssb[:, :, 1, c], in0=w3[:, :], in1=Ssb[:, :, 1, c], op=ALU.add),
                             reads=["C_w3", "C_Sa", "C_S"], writes=["C_S"])
                    bulk(2, 127, 1, 126, 63)
                    S.op("dve", lambda e: e.memset(Xbf[:, :, :, 0], 0.0), reads=["C_Xbf"], writes=["C_Xbf"])
                    S.op("dve", lambda e: e.tensor_copy(out=Xbf[:, :, :, 1:NCH], in_=Ssb[:, :, :, 0:NCH - 1]), reads=["C_S", "C_Xbf"], writes=["C_Xbf"])
                    n_ = 0
                    for cc_ in range(6):
                        tiles_cc = [i for i in range(3 * cc_, min(3 * cc_ + 3, 16))]
                        for tau in range(16):
                            b_ = n_ % 2
                            n_ += 1
                            pairs = [(WK[:, tau - j, cc_, :], usb[:, cc_, j:LP:16]) for j in range(tau + 1)]
                            if tau == 0:
                                pairs.append((zeros_b[:, :], usb[:, cc_, 0:LP:16]))
                            mms_out = psY[b_]
                            np_ = len(pairs)

                            def intra(idx):
                                l, r_ = pairs[idx]
                                S.op("pe", lambda e: e.matmul(mms_out[:, :], lhsT=l, rhs=r_, start=(idx == 0), stop=(idx == np_ - 1)),
                                     reads=["C_u", K_, "zeros_b"], writes=[("C_psY", b_)])
                            intra(0)
                            for i in tiles_cc:
                                r0 = 32 * (i % 3)
                                for ri in range(2):
                                    S.op("pe", lambda e, i=i, ri=ri, r0=r0, tau=tau: e.matmul(
                                        mms_out[r0:r0 + 32, :], lhsT=WO[:, tau, i, ri, :], rhs=Xbf[:, i, ri, :], start=False, stop=False),
                                        reads=["C_Xbf", K_], writes=[("C_psY", b_)])
                            for idx in range(1, np_):
                                intra(idx)
                            S.op("act", lambda e, tau=tau, b_=b_: e.activation(
                                out=ypre[:, tau:SEQ:16], in_=psY[b_][:, 1:NCH], func=AF.Copy),
                                reads=[("C_psY", b_)], writes=["C_ypre"])
                        S.op("act", lambda e: e.activation(out=g1[:, :], in_=ypre[:, :], func=AF.Square), reads=["C_ypre"], writes=["C_g1"])
                        S.op("dve", lambda e: e.tensor_scalar(out=g1[:, :], in0=g1[:, :], scalar1=0.0713548162726, scalar2=1.5957691216,
                                                              op0=ALU.mult, op1=ALU.add), reads=["C_g1"], writes=["C_g1"])
                        S.op("dve", lambda e: e.tensor_tensor(out=g1[:, :], in0=g1[:, :], in1=ypre[:, :], op=ALU.mult), reads=["C_g1", "C_ypre"], writes=["C_g1"])
                        S.op("act", lambda e: e.activation(out=g1[:, :], in_=g1[:, :], func=AF.Sigmoid), reads=["C_g1"], writes=["C_g1"])
                        S.op("dve", lambda e, cc_=cc_: e.tensor_tensor(out=ya[:, cc_, :], in0=g1[:, :], in1=ypre[:, :], op=ALU.mult),
                             reads=["C_g1", "C_ypre"], writes=["C_ya"])
                    S.dma("sp", "o", yaT[s_, :, :, :], ya[:, :, :], reads=["C_ya"], writes=[("yaT", s_)])

        if debug not in ("A", "B"):
            phase_C()
            barrier()

        if debug == "C":
            with ExitStack() as st:
                tmp = sb(st, "dbgtmp", [128, 6, SEQ], BF16)
                tmp2 = sb(st, "dbgtmp2", [128, 6, SEQ], F32)
                S.dma("sp", "x", tmp[:, :, :], yaT[0, :, :, :], reads=[("yaT", 0)], writes=["dbgtmp"])
                S.op("dve", lambda e: e.tensor_copy(out=tmp2[:, :, :], in_=tmp[:, :, :]), reads=["dbgtmp"], writes=["dbgtmp2"])
                S.dma("sp", "o", outT[0, :, 0:6, 0:SEQ], tmp2[:, :, :], reads=["dbgtmp2"], writes=["out"])

        biasG_d = din("biasG", [128, 8, 1024])
        biasM_d = din("biasM", [16, 8, 512])
        cvec_d = din("cvec", [128, 8])
        ybT = dscr("ybT", [NSEQ, 128, 4, SEQ], BF16)
        ones_b = sb(es, "ones_b", [128, 128], BF16)
        S.op("dve", lambda e: e.memset(ones_b[:, :], 1.0), writes=["ones_b"])
        NIT = 22
        TOPK = 256.0

        def phase_D():
            with ExitStack() as st:
                Gb = sb(st, "D_Gb", [128, 8, 1024], BF16)
                Mb = sb(st, "D_Mb", [16, 8, 512], BF16)
                cv = sb(st, "D_cv", [128, 8])
                S.dma("sp", "c", cv[:, :], cvec_d[:, :], writes=["D_cv"])
                with ExitStack() as st2:
                    Gf = sb(st2, "D_Gf", [128, 8, 1024])
                    Mf = sb(st2, "D_Mf", [16, 8, 512])
                    S.dma("sp", "c", Gf[:, :, :], biasG_d[:, :, :], writes=["D_Gf"])
                    S.dma("sp", "c", Mf[:, :, :], biasM_d[:, :, :], writes=["D_Mf"])
                    for h in range(8):
                        S.op("dve", lambda e, h=h: e.tensor_scalar(out=Gb[:, h, :], in0=Gf[:, h, :], scalar1=cv[:, h:h + 1],
                                                                   scalar2=None, op0=ALU.subtract),
                             reads=["D_Gf", "D_cv"], writes=["D_Gb"])
                        S.op("dve", lambda e, h=h: e.tensor_scalar(out=Mb[:, h, :], in0=Mf[:, h, :], scalar1=cv[:16, h:h + 1],
                                                                   scalar2=None, op0=ALU.subtract),
                             reads=["D_Mf", "D_cv"], writes=["D_Mb"])
                    barrier()
                qi = sb(st, "D_qi", [128, 4, SEQ], BF16)
                ki = sb(st, "D_ki", [128, LP], BF16)
                qq = sb(st, "D_q", [128, 4, SEQ], BF16)
                kk = sb(st, "D_k", [128, LP], BF16)
                vd = sb(st, "D_vd", [128, 17, 2, 128], BF16)
                wq = sb(st, "D_wq", [128, 16, 8])
                sc = [sb(st, "D_sc%d" % i, [128, 4, LP]) for i in range(2)]
                MA = [sb(st, "D_MA%d" % i, [128, 4, LP], BF16) for i in range(2)]
                junk = sb(st, "D_junk", [128, LP], BF16)
                Rb = [sb(st, "D_Rb%d" % i, [128, 512], BF16) for i in range(2)]
                dg = sb(st, "D_dg", [128, 8, 128], BF16)
                Pt = [sb(st, "D_Pt%d" % i, [128, 512], BF16) for i in range(3)]
                rd = sb(st, "D_rd", [128, 512])
                rds = sb(st, "D_rds", [128, 512])
                Osb = sb(st, "D_Osb", [128, 512])
                yb = sb(st, "D_yb", [128, 4, 512], BF16)
                lo = [sb(st, "D_lo%d" % i, [128, 4]) for i in range(2)]
                hi = sb(st, "D_hi", [128, 4])
                W0 = sb(st, "D_W0", [128, 4])
                Wk = sb(st, "D_Wk", [128, 4])
                mid = sb(st, "D_mid", [128, 4])
                cnt = sb(st, "D_cnt", [128, 4])
                stp = sb(st, "D_stp", [128, 4])
                pq = [ps(st, "D_pq%d" % i, [128, 512]) for i in range(2)]
                psc = ps(st, "D_psc", [128, 512])
                pL = [ps(st, "D_pL%d" % i, [128, 512]) for i in range(2)]
                pOD = [ps(st, "D_pOD%d" % i, [128, 512]) for i in range(2)]
                pSh = ps(st, "D_pSh", [128, 512])
                S.op("dve", lambda e: e.memset(vd[:, :, 0, 64:128], 1.0), writes=["D_vd1"])
                S.op("dve", lambda e: e.memset(vd[:, :, 1, 0:64], 1.0), writes=["D_vd1"])
                ACT_JL = ()
                nmid = sb(st, "D_nmid", [128, 4])
                thrc = sb(st, "D_thrc", [128, 4, 4])
                for Q_ in range(4):
                    for jl_ in range(4):
                        v_ = (510.5 - (NMETA + 128 * (4 * Q_ + jl_ + 1))) if jl_ in ACT_JL else TOPK
                        S.op("dve", lambda e, Q_=Q_, jl_=jl_, v_=v_: e.memset(thrc[:, Q_, jl_:jl_ + 1], float(v_)), writes=["D_thrc"])

                def indexer(s_, Q):
                    u = Q % 2
                    for jl in range(4):
                        j = 4 * Q + jl
                        Nj = NMETA + 128 * (j + 1)
                        for h in range(8):
                            S.op("pool", lambda e, h=h, j=j: e.tensor_scalar(out=dg[:, h, :], in0=ident_f[:, :], scalar1=wq[:, j, h:h + 1],
                                                                           scalar2=1.0, op0=ALU.mult, op1=ALU.mult),
                                 reads=["ident_f", "D_wq"], writes=["D_dg"])
                        for c0 in range(0, Nj, 512):
                            cw = min(512, Nj - c0)

                            def qk(h):
                                hh, hp, b_ = h % 2, h // 2, h % 2
                                S.op("pe", lambda e: e.matmul(
                                    pq[b_][:, :cw], lhsT=qi[64 * hh:64 * hh + 64, hp, 128 * j:128 * j + 128],
                                    rhs=ki[64 * hh:64 * hh + 64, c0:c0 + cw], start=True, stop=True),
                                    reads=["D_qi", "D_ki"], writes=[("D_pq", b_)])
                                S.op("act", lambda e: e.activation(out=Rb[b_][:, :cw], in_=pq[b_][:, :cw], func=AF.Relu),
                                     reads=[("D_pq", b_)], writes=[("D_Rb", b_)])

                            def dgm(h):
                                b_ = h % 2
                                S.op("pe", lambda e: e.matmul(psc[:, :cw], lhsT=dg[:, h, :], rhs=Rb[b_][:, :cw],
                                                              start=(h == 0), stop=(h == 7)),
                                     reads=[("D_Rb", b_), "D_dg"], writes=["D_psc"])

                            qk(0)
                            qk(1)
                            for h in range(8):
                                dgm(h)
                                if h + 2 < 8:
                                    qk(h + 2)
                            S.op("act", lambda e, jl=jl, c0=c0, cw=cw: e.activation(out=sc[u][:, jl, c0:c0 + cw], in_=psc[:, :cw], func=AF.Copy),
                                 reads=["D_psc"], writes=[("D_sc", u, jl)])
                        S.op("dve", lambda e, jl=jl, Nj=Nj: e.tensor_reduce(out=lo[u][:, jl:jl + 1], in_=sc[u][:, jl, :Nj], axis=AX.X, op=ALU.min),
                             reads=[("D_sc", u, jl)], writes=[("D_lo", u)])
                        S.op("dve", lambda e, jl=jl, Nj=Nj: e.tensor_reduce(out=hi[:, jl:jl + 1], in_=sc[u][:, jl, :Nj], axis=AX.X, op=ALU.max),
                             reads=[("D_sc", u, jl)], writes=["D_hi"])
                        S.op("dve", lambda e, jl=jl, Nj=Nj: e.memset(sc[u][0:64, jl, Nj - 64:Nj], -1e30),
                             reads=[("D_sc", u, jl), ("D_lo", u), "D_hi"], writes=[("D_sc", u, jl)])

                def bisect_steps(Q):
                    u = Q % 2
                    L_ = lo[u]
                    steps = []

                    def init():
                        S.op("dve", lambda e: e.tensor_tensor(out=W0[:, :], in0=hi[:, :], in1=L_[:, :], op=ALU.subtract),
                             reads=[("D_lo", u), "D_hi"], writes=["D_W0"])
                    steps.append(init)

                    def mk(it):
                        def f():
                            S.op("dve", lambda e: e.tensor_scalar(out=Wk[:, :], in0=W0[:, :], scalar1=2.0 ** (-(it + 1)), scalar2=None,
                                                                  op0=ALU.mult), reads=["D_W0", "D_stp"], writes=["D_Wk"])
                            S.op("dve", lambda e: e.tensor_tensor(out=mid[:, :], in0=L_[:, :], in1=Wk[:, :], op=ALU.add),
                                 reads=[("D_lo", u), "D_Wk"], writes=["D_mid"])
                            if ACT_JL:
                                S.op("dve", lambda e: e.tensor_scalar(out=nmid[:, :], in0=mid[:, :], scalar1=-1.0, scalar2=None, op0=ALU.mult),
                                     reads=["D_mid"], writes=["D_nmid"])
                            for jl in range(4):
                                Nj = NMETA + 128 * (4 * Q + jl + 1)
                                if jl in ACT_JL:
                                    S.op("act", lambda e, jl=jl, Nj=Nj: e.activation(
                                        out=junkA[:, :Nj], in_=sc[u][:, jl, :Nj], func=AF.Sign, bias=nmid[:, jl:jl + 1], scale=1.0,
                                        accum_out=cnt[:, jl:jl + 1]),
                                        reads=[("D_sc", u, jl), "D_nmid"], writes=["D_junkA", ("D_cnt", jl)])
                                else:
                                    S.op("dve", lambda e, jl=jl, Nj=Nj: e.tensor_scalar(
                                        out=junk[:, :Nj], in0=sc[u][:, jl, :Nj], scalar1=mid[:, jl:jl + 1], scalar2=0.0,
                                        op0=ALU.is_ge, op1=ALU.add, accum_out=cnt[:, jl:jl + 1]),
                                        reads=[("D_sc", u, jl), "D_mid"], writes=["D_junk", ("D_cnt", jl)])
                            S.op("dve", lambda e: e.tensor_tensor(out=stp[:, :], in0=cnt[:, :], in1=thrc[:, Q, :], op=ALU.is_ge),
                                 reads=[("D_cnt", jl) for jl in range(4)] + ["D_thrc"], writes=["D_stp"])
                            S.op("dve", lambda e: e.tensor_tensor(out=stp[:, :], in0=stp[:, :], in1=Wk[:, :], op=ALU.mult),
                                 reads=["D_stp", "D_Wk"], writes=["D_stp"])
                            S.op("dve", lambda e: e.tensor_tensor(out=L_[:, :], in0=L_[:, :], in1=stp[:, :], op=ALU.add),
                                 reads=[("D_lo", u), "D_stp"], writes=[("D_lo", u)])
                        return f
                    for it in range(NIT):
                        steps.append(mk(it))

                    def fin():
                        for jl in range(4):
                            Nj = NMETA + 128 * (4 * Q + jl + 1)
                            S.op("dve", lambda e, jl=jl, Nj=Nj: e.tensor_scalar(out=MA[u][:, jl, :Nj], in0=sc[u][:, jl, :Nj], scalar1=L_[:, jl:jl + 1],
                                                                              scalar2=-30000.0, op0=ALU.is_lt, op1=ALU.mult),
                                 reads=[("D_sc", u, jl), ("D_lo", u)], writes=[("D_MA", u)])
                    steps.append(fin)
                    return steps

                def attention(s_, Q, filler):
                    u = Q % 2
                    nblk = 4 * Q + 5
                    tiles = []
                    for h in range(8):
                        for b in range(nblk):
                            tiles.append((h, b))
                    NTL = len(tiles)

                    def geo(b):
                        w = NMETA if b == 0 else 128
                        pc0 = 0 if b == 0 else NMETA + 128 * (b - 1)
                        jl0 = max(0, b - 1 - 4 * Q)
                        return w, pc0, jl0

                    def stageA(n):
                        h, b = tiles[n]
                        hh, hp = h % 2, h // 2
                        w, pc0, jl0 = geo(b)
                        c0 = jl0 * 128
                        near = (b == 0 and Q == 0) or (b >= 1 and b - 1 >= 4 * Q - 1)
                        lb, pb_ = n % 2, n % 3
                        S.op("pe", lambda e: e.matmul(
                            pL[lb][:w, c0:512], lhsT=kk[64 * hh:64 * hh + 64, pc0:pc0 + w],
                            rhs=qq[64 * hh:64 * hh + 64, hp, 512 * Q + c0:512 * Q + 512], start=True, stop=False),
                            reads=["D_k", "D_q"], writes=[("D_pL", lb)])
                        if near:
                            if b == 0:
                                S.op("pe", lambda e: e.matmul(pL[lb][:NMETA, c0:512], lhsT=ident_b[:NMETA, :NMETA],
                                                              rhs=Mb[:NMETA, h, c0:512], start=False, stop=False),
                                     reads=["D_Mb", "ident_b"], writes=[("D_pL", lb)])
                            else:
                                z0 = 512 * Q + c0 - 128 * (b - 1) + 384
                                S.op("pe", lambda e: e.matmul(pL[lb][:, c0:512], lhsT=ident_b[:, :],
                                                              rhs=Gb[:, h, z0:z0 + 512 - c0], start=False, stop=False),
                                     reads=["D_Gb", "ident_b"], writes=[("D_pL", lb)])
                        for jl in range(jl0, 4):
                            S.op("pe", lambda e, jl=jl: e.matmul(pL[lb][:w, jl * 128:(jl + 1) * 128], lhsT=MA[u][:, jl, pc0:pc0 + w],
                                                                 rhs=ident_b[:, :], start=False, stop=(jl == 3)),
                                 reads=[("D_MA", u), "ident_b"], writes=[("D_pL", lb)])
                        S.op("act", lambda e: e.activation(out=Pt[pb_][:w, c0:512], in_=pL[lb][:w, c0:512], func=AF.Exp),
                             reads=[("D_pL", lb)], writes=[("D_Pt", pb_)])

                    def stageB(n):
                        h, b = tiles[n]
                        hh, hp = h % 2, h // 2
                        w, pc0, jl0 = geo(b)
                        c0 = jl0 * 128
                        pb_ = n % 3
                        ob = h % 2
                        S.op("pe", lambda e: e.matmul(pOD[ob][:, c0:512], lhsT=vd[:w, b, hh, :], rhs=Pt[pb_][:w, c0:512],
                                                      start=(b == 0), stop=(b == nblk - 1)),
                             reads=[("D_Pt", pb_), "D_vd", "D_vd1"], writes=[("D_pOD", ob)])
                        if b == nblk - 1:
                            orow = slice(64 * hh, 64 * hh + 64)
                            drow = slice(64 * (1 - hh), 64 * (1 - hh) + 64)
                            S.op("act", lambda e: e.activation(out=rd[drow, :], in_=pOD[ob][drow, :], func=AF.Ln),
                                 reads=[("D_pOD", ob)], writes=[("D_rd", hh)])
                            S.op("act", lambda e: e.activation(out=rd[drow, :], in_=rd[drow, :], func=AF.Exp, scale=-1.0),
                                 reads=[("D_rd", hh)], writes=[("D_rd", hh)])
                            S.op("pe", lambda e: e.matmul(pSh[orow, :], lhsT=ident_f[drow, drow], rhs=rd[drow, :], start=True, stop=True),
                                 reads=[("D_rd", hh), "ident_f"], writes=[("D_pSh", hh)])
                            S.op("act", lambda e: e.activation(out=rds[orow, :], in_=pSh[orow, :], func=AF.Copy),
                                 reads=[("D_pSh", hh)], writes=[("D_rds", hh)])
                            S.op("act", lambda e: e.activation(out=Osb[orow, :], in_=pOD[ob][orow, :], func=AF.Copy),
                                 reads=[("D_pOD", ob)], writes=[("D_Osb", hh)])
                            S.op("pool", lambda e: e.tensor_tensor(out=yb[orow, hp, :], in0=Osb[orow, :], in1=rds[orow, :], op=ALU.mult),
                                 reads=[("D_rds", hh), ("D_Osb", hh)], writes=["D_yb"])

                    stageA(0)
                    stageA(1)
                    for n in range(NTL):
                        stageB(n)
                        if n + 2 < NTL:
                            stageA(n + 2)
                    while filler:
                        filler.pop(0)()
                    S.dma("sp", "o", ybT[s_, :, :, 512 * Q:512 * Q + 512], yb[:, :, :], reads=["D_yb"], writes=[("ybT", s_, Q)])

                for s_ in range(NSEQ):
                    S.dma("sp", "x", qi[:, :, :], qiT[s_, :, :, :], reads=[("qiT", s_, t) for t in range(NT)], writes=["D_qi"])
                    S.dma("sp", "x", ki[:, :], kiT[s_, :, :], reads=[("kiT", s_, t) for t in ALLT], writes=["D_ki"])
                    S.dma("sp", "x", qq[:, :, :], qT[s_, :, :, :], reads=[("qT", s_, t) for t in range(NT)], writes=["D_q"])
                    S.dma("sp", "x", kk[:, :], kT[s_, :, :], reads=[("kT", s_, t) for t in ALLT], writes=["D_k"])
                    vr = [("vwS", s_, t) for t in ALLT]
                    for half in range(2):
                        S.dma("pool", "x", vd[:, 1:17, half, 64 * half:64 * half + 64],
                              vwS[s_, NMETA:LP, 0:64].rearrange("(b p) c -> p b c", p=128), reads=vr, writes=["D_vd"])
                        S.dma("pool", "x", vd[:NMETA, 0, half, 64 * half:64 * half + 64], vwS[s_, 0:NMETA, 0:64], reads=vr, writes=["D_vd"])
                    S.dma("sp", "x", wq[:, :, :], vwS[s_, NMETA:LP, 64:72].rearrange("(b p) c -> p b c", p=128), reads=vr, writes=["D_wq"])
                    indexer(s_, 0)
                    for f_ in bisect_steps(0):
                        f_()
                    for Q in range(4):
                        if Q + 1 < 4:
                            indexer(s_, Q + 1)
                            for f_ in bisect_steps(Q + 1):
                                f_()
                        attention(s_, Q, [])

        if debug not in ("A", "B", "C"):
            phase_D()
            barrier()

        if debug == "D":
            with ExitStack() as st:
                tmp = sb(st, "dbgtmp", [128, 4, SEQ], BF16)
                tmp2 = sb(st, "dbgtmp2", [128, 4, SEQ], F32)
                S.dma("sp", "x", tmp[:, :, :], ybT[0, :, :, :], reads=[("ybT", 0, t) for t in range(NT)], writes=["dbgtmp"])
                S.op("dve", lambda e: e.tensor_copy(out=tmp2[:, :, :], in_=tmp[:, :, :]), reads=["dbgtmp"], writes=["dbgtmp2"])
                S.dma("sp", "o", outT[0, :, 0:4, 0:SEQ], tmp2[:, :, :], reads=["dbgtmp2"], writes=["out"])

        w_glu_d = din("w_glu", [128, 6, 768])
        w_a_d = din("w_a", [128, 6, D])
        w_b_d = din("w_b", [128, 4, D])
        w_o_d = din("w_o", [128, DC, D])
        w_g_d = din("w_g", [128, DC, 2 * D])
        h2T = dscr("h2T", [NSEQ, 128, DC, SEQ])

        def phase_E():
            with ExitStack() as st:
                wglu = sb(st, "E_wglu", [128, 6, 768], BF16)
                wa = sb(st, "E_wa", [128, 6, D], BF16)
                wb = sb(st, "E_wb", [128, 4, D], BF16)
                wo = sb(st, "E_wo", [128, DC, D], BF16)
                wgt = sb(st, "E_wg", [128, DC, 2 * D], BF16)
                S.dma("pool", "w", wglu[:, :, :], w_glu_d[:, :, :], writes=["E_w"], max_dma_last_dim=3072)
                for k in range(6):
                    S.dma("pool", "w", wa[:, k, :], w_a_d[:, k, :], writes=["E_w"])
                for k in range(4):
                    S.dma("pool", "w", wb[:, k, :], w_b_d[:, k, :], writes=["E_w"])
                for k in range(DC):
                    S.dma("pool", "w", wo[:, k, :], w_o_d[:, k, :], writes=["E_w"])
                    S.dma("pool", "w", wgt[:, k, :], w_g_d[:, k, :], writes=["E_w"], max_dma_last_dim=4096)
                hn2 = [sb(st, "E_hn%d" % i, [128, DC, TT], BF16) for i in range(2)]
                ya2 = [sb(st, "E_ya%d" % i, [128, 6, TT], BF16) for i in range(2)]
                yb2 = [sb(st, "E_yb%d" % i, [128, 4, TT], BF16) for i in range(2)]
                h12 = [sb(st, "E_h1%d" % i, [128, DC, TT]) for i in range(2)]
                yg = sb(st, "E_yg", [128, 6, TT], BF16)
                sgl = sb(st, "E_sgl", [128, TT])
                ga = sb(st, "E_ga", [128, TT])
                gb = sb(st, "E_gb", [128, TT])
                t1 = sb(st, "E_t1", [128, TT])
                t2 = sb(st, "E_t2", [128, TT])
                mg = sb(st, "E_mg", [128, DC, TT], BF16)
                ysb = sb(st, "E_y", [128, DC, TT])
                sq = sb(st, "E_sq", [128, 2, TT])
                accE = sb(st, "E_acc", [128, TT])
                rs = sb(st, "E_rs", [128, TT])
                pga = ps(st, "E_pga", [128, TT])
                pgb = ps(st, "E_pgb", [128, TT])
                pa = ps(st, "E_pa", [128, TT])
                pb = ps(st, "E_pb", [128, TT])
                py = [ps(st, "E_py%d" % i, [128, TT]) for i in range(2)]
                pss = ps(st, "E_pss", [128, TT])
                tl = [(s_, t_) for s_ in range(NSEQ) for t_ in range(NT)]

                def loadE(i):
                    s_, t_ = tl[i]
                    u = i % 2
                    tsl = slice(t_ * TT, (t_ + 1) * TT)
                    S.dma("sp", "x", hn2[u][:, :, :], hnT[s_, :, :, tsl], reads=[("hnT", s_, t_)], writes=[("E_hn", u)])
                    S.dma("sp", "x", ya2[u][:, :, :], yaT[s_, :, :, tsl], reads=[("yaT", s_)], writes=[("E_ya", u)])
                    S.dma("sp", "x", yb2[u][:, :, :], ybT[s_, :, :, tsl], reads=[("ybT", s_, t_)], writes=[("E_yb", u)])
                    S.dma("sp", "x", h12[u][:, :, :], h1T[s_, :, :, tsl], reads=[("h1T", s_, t_)], writes=[("E_h1", u)])

                loadE(0)
                for i, (s_, t_) in enumerate(tl):
                    if i + 1 < len(tl):
                        loadE(i + 1)
                    u = i % 2
                    hn, ya, ybt, h1t = hn2[u], ya2[u], yb2[u], h12[u]
                    khn, kya, kyb, kh1 = ("E_hn", u), ("E_ya", u), ("E_yb", u), ("E_h1", u)
                    tsl = slice(t_ * TT, (t_ + 1) * TT)
                    for oc in range(6):
                        b_ = oc % 2
                        mms(py[b_][:, :], [(wglu[:, k, oc * 128:(oc + 1) * 128], ya[:, k, :]) for k in range(6)],
                            ["E_w", kya], ("E_py", b_))
                        S.op("act", lambda e, b_=b_: e.activation(out=sgl[:, :], in_=py[b_][:, :], func=AF.Sigmoid),
                             reads=[("E_py", b_)], writes=["E_sgl"])
                        S.op("dve", lambda e, oc=oc: e.tensor_tensor(out=yg[:, oc, :], in0=sgl[:, :], in1=ya[:, oc, :], op=ALU.mult),
                             reads=["E_sgl", kya], writes=["E_yg"])
                    for dc in range(DC):
                        mms(pga[:, :], [(wgt[:, k, dc * 128:(dc + 1) * 128], hn[:, k, :]) for k in range(DC)], ["E_w", khn], "E_pga")
                        mms(pa[:, :], [(wa[:, k, dc * 128:(dc + 1) * 128], yg[:, k, :]) for k in range(6)], ["E_w", "E_yg"], "E_pa")
                        S.op("act", lambda e: e.activation(out=ga[:, :], in_=pga[:, :], func=AF.Sigmoid), reads=["E_pga"], writes=["E_ga"])
                        S.op("dve", lambda e: e.tensor_tensor(out=t1[:, :], in0=ga[:, :], in1=pa[:, :], op=ALU.mult), reads=["E_ga", "E_pa"], writes=["E_t1"])
                        mms(pgb[:, :], [(wgt[:, k, D + dc * 128:D + (dc + 1) * 128], hn[:, k, :]) for k in range(DC)], ["E_w", khn], "E_pgb")
                        mms(pb[:, :], [(wb[:, k, dc * 128:(dc + 1) * 128], ybt[:, k, :]) for k in range(4)], ["E_w", kyb], "E_pb")
                        S.op("act", lambda e: e.activation(out=gb[:, :], in_=pgb[:, :], func=AF.Sigmoid), reads=["E_pgb"], writes=["E_gb"])
                        S.op("dve", lambda e: e.tensor_tensor(out=t2[:, :], in0=gb[:, :], in1=pb[:, :], op=ALU.mult), reads=["E_gb", "E_pb"], writes=["E_t2"])
                        S.op("pool", lambda e, dc=dc: e.tensor_tensor(out=mg[:, dc, :], in0=t1[:, :], in1=t2[:, :], op=ALU.add),
                             reads=["E_t1", "E_t2"], writes=["E_mg"])
                    for c in range(DC):
                        b_ = c % 2
                        mms(py[b_][:, :], [(wo[:, k, c * 128:(c + 1) * 128], mg[:, k, :]) for k in range(DC)], ["E_w", "E_mg"], ("E_py", b_))
                        S.op("act", lambda e, c=c, b_=b_: e.activation(out=ysb[:, c, :], in_=py[b_][:, :], func=AF.Copy),
                             reads=[("E_py", b_)], writes=[("E_y", c)])
                        stats_step("E", c, py[b_][:, :], TT, sq, accE, [("E_py", b_)])
                    stats_finish("E", TT, accE, pss, rs)
                    for c in range(DC):
                        S.op("dve", lambda e, c=c: e.scalar_tensor_tensor(
                            out=ysb[:, c, :], in0=ysb[:, c, :], scalar=gains_sb[:, 24 + c:24 + c + 1], in1=rs[:, :],
                            op0=ALU.mult, op1=ALU.mult), reads=[("E_y", c), ("E", "rs"), "gains"], writes=[("E_y", c)])
                        S.op("pool", lambda e, c=c: e.tensor_tensor(out=ysb[:, c, :], in0=ysb[:, c, :], in1=h1t[:, c, :], op=ALU.add),
                             reads=[("E_y", c), kh1], writes=[("E_y", c)])
                    S.dma("sp", "o", h2T[s_, :, :, tsl], ysb[:, :, :], reads=[("E_y", c) for c in range(DC)], writes=[("h2T", s_, t_)])

        if debug not in ("A", "B", "C", "D"):
            phase_E()
            barrier()
            ff2_wg = din("ff2_wg", [128, DC, DFF])
            ff2_wu = din("ff2_wu", [128, DC, DFF])
            ff2_wd = din("ff2_wd", [128, FC, D])
            tilesF = []
            for s in range(NSEQ):
                for t in range(NFT):
                    tilesF.append((h2T[s, :, :, t * FT:(t + 1) * FT], outT[s, :, :, t * FT:(t + 1) * FT], FT,
                                   [("h2T", s, t * FT // TT)], [("out", s, t)]))
            ffn_phase("F", ff2_wg, ff2_wu, ff2_wd, 32, 40, tilesF)

        if debug == "A":
            with ExitStack() as st:
                tmp = sb(st, "dbgtmp", [128, DC, FT])
                S.dma("sp", "x", tmp[:, :, :], h1T[0, :, :, 0:FT], reads=[("h1T", 0, 0)], writes=["dbgtmp"])
                S.dma("sp", "o", outT[0, :, :, 0:FT], tmp[:, :, :], reads=["dbgtmp"], writes=["out"])
                S.dma("sp", "x", tmp[:, :, :NMETA], h1m[:, :, :], reads=["h1m", "out"], writes=["dbgtmp"])
                S.dma("sp", "o", outT[1, :, :, 0:NMETA], tmp[:, :, :NMETA], reads=["dbgtmp"], writes=["out"])

        S.drain("sp")
        print("instructions:", S.ninst)
    return nc


def _rel_bucket(rel):
    half, me = 16, 8
    base = np.where(rel > 0, half, 0)
    n = np.abs(rel)
    nf = np.maximum(n, 1).astype(np.float32)
    large = me + (np.log(nf / me) / math.log(128 / me) * (half - me)).astype(np.int32)
    large = np.minimum(large, half - 1)
    return base + np.where(n < me, n, large)


def prep_inputs(inp):
    f = lambda a: np.ascontiguousarray(np.asarray(a, dtype=np.float32))
    x = f(inp["x"])
    B = x.shape[0]
    xT = np.ascontiguousarray(x.reshape(B, SEQ, DC, 128).transpose(0, 3, 2, 1))
    metaT = np.ascontiguousarray(f(inp["meta_tokens"]).reshape(NMETA, DC, 128).transpose(2, 1, 0))
    gl = [inp[k] for k in ("ff1_norm_pre", "ff1_norm_post", "mix_norm_pre", "mix_norm_post", "ff2_norm_pre",
                           "ff2_norm_post")]
    gains = np.ascontiguousarray(np.concatenate([f(g)[0].reshape(DC, 128).T for g in gl], axis=1))

    def wk(w, kc):
        w = f(w)
        return np.ascontiguousarray(w.reshape(kc, 128, w.shape[-1]).transpose(1, 0, 2))

    shared = {
        "metaT": metaT, "gains": gains,
        "ff1_wg": wk(inp["ff1_w_gate"][0], DC), "ff1_wu": wk(inp["ff1_w_up"][0], DC),
        "ff1_wd": wk(inp["ff1_w_down"][0], FC),
    }
    win = f(inp["w_in"][0])
    upad = np.zeros((D, 6, 128), np.float32)
    for c6 in range(6):
        w_ = min(96, 512 - 96 * c6)
        upad[:, c6, :w_] = win[:, 96 * c6:96 * c6 + w_]
    winA = np.concatenate([upad.reshape(D, 768), win[:, 512:1024], win[:, 1096:1608], win[:, 1024:1088], win[:, 1024:1088],
                           win[:, 1608:1672], win[:, 1608:1672]], axis=1)
    winB = np.concatenate([win[:, 1672:1736], win[:, 1088:1096]], axis=1)
    shared["w_inA"] = wk(winA, DC)
    shared["w_inB"] = wk(winB, DC)
    lre, lim, ldt = f(inp["ssm_lambda_re"][0]), f(inp["ssm_lambda_im"][0]), f(inp["ssm_log_dt"][0])
    bre, bim = f(inp["ssm_b_re"][0]), f(inp["ssm_b_im"][0])
    cre, cim = f(inp["ssm_c_re"][0]), f(inp["ssm_c_im"][0])
    r = np.arange(128)
    sidx = np.arange(128)
    cc = np.arange(6)
    i_rc = 3 * cc[None, :] + (r[:, None] // 32)
    val_rc = (r[:, None] < 96) & (i_rc < 16)
    i_rc = np.where(val_rc, i_rc, 0)
    g_rcs = 2 * i_rc[:, :, None] + (sidx[None, None, :] // 64)
    p_s = sidx % 64
    pcm = np.stack([lre[g_rcs, p_s[None, None, :]], lim[g_rcs, p_s[None, None, :]], ldt[g_rcs]], axis=2)
    glr = (r % 32) // 16
    m_r = r % 16
    msk = (glr[:, None, None] == (sidx[None, None, :] // 64)) & val_rc[:, :, None]
    bcm = np.stack([np.where(msk, bre[g_rcs, p_s[None, None, :], m_r[:, None, None]], 0.0),
                    np.where(msk, bim[g_rcs, p_s[None, None, :], m_r[:, None, None]], 0.0)], axis=2)
    ii = np.arange(16)
    g_si = 2 * ii[None, :] + (sidx[:, None] // 64)
    psm = np.stack([lre[g_si, p_s[:, None]], lim[g_si, p_s[:, None]], ldt[g_si]], axis=1)
    q = np.arange(32)
    mq = q % 16
    mskq = ((q[None, None, :] // 16) == (sidx[:, None, None] // 64))
    bsm = np.stack([np.where(mskq, bre[g_si[:, :, None], p_s[:, None, None], mq[None, None, :]], 0.0),
                    np.where(mskq, bim[g_si[:, :, None], p_s[:, None, None], mq[None, None, :]], 0.0)], axis=2)
    csm = np.stack([np.where(mskq, cre[g_si[:, :, None], mq[None, None, :], p_s[:, None, None]], 0.0),
                    np.where(mskq, cim[g_si[:, :, None], mq[None, None, :], p_s[:, None, None]], 0.0)], axis=2)
    dflat = f(inp["ssm_d"][0]).reshape(512)
    ch_rc = 96 * cc[None, :] + r[:, None]
    vch = (r[:, None] < 96) & (ch_rc < 512)
    dsk = np.where(vch, dflat[np.where(vch, ch_rc, 0)], 0.0)
    shared["s5_pcm"] = f(pcm)
    shared["s5_bcm"] = f(bcm)
    shared["s5_psm"] = f(psm)
    shared["s5_bsm"] = f(bsm)
    shared["s5_csm"] = f(csm)
    shared["s5_d"] = f(dsk)
    shared["ident"] = np.eye(128, dtype=np.float32)
    rb = f(inp["rel_bias"])
    sl = np.arange(128)[:, None]
    zi = np.arange(1024)[None, :]
    shared["biasG"] = f(rb[_rel_bucket(sl - (zi - 384))].transpose(0, 2, 1))
    mm_ = np.arange(16)[:, None]
    tq = np.arange(512)[None, :]
    shared["biasM"] = f(rb[_rel_bucket(mm_ - 16 - tq)].transpose(0, 2, 1))
    shared["cvec"] = f(np.broadcast_to(rb[15][None, :], (128, 8)))
    def pad6rows(w):
        o = np.zeros((128, 6, w.shape[1]), np.float32)
        for c6 in range(6):
            w_ = min(96, 512 - 96 * c6)
            o[:w_, c6, :] = w[96 * c6:96 * c6 + w_]
        return o
    wg_ = f(inp["ssm_w_glu"][0])
    wgp = np.zeros((512, 6, 128), np.float32)
    for c6 in range(6):
        w_ = min(96, 512 - 96 * c6)
        wgp[:, c6, :w_] = wg_[:, 96 * c6:96 * c6 + w_]
    shared["w_glu"] = pad6rows(wgp.reshape(512, 768))
    shared["w_a"] = pad6rows(f(inp["w_branch_a"][0]))
    shared["w_b"] = wk(inp["w_branch_b"][0], 4)
    shared["w_o"] = wk(inp["w_out"][0], DC)
    shared["w_g"] = wk(win[:, 1736:3784], DC)
    shared["ff2_wg"] = wk(inp["ff2_w_gate"][0], DC)
    shared["ff2_wu"] = wk(inp["ff2_w_up"][0], DC)
    shared["ff2_wd"] = wk(inp["ff2_w_down"][0], FC)
    maps = []
    for c in range(NCORES):
        m = dict(shared)
        m["xT"] = xT[c * NSEQ:(c + 1) * NSEQ]
        maps.append(m)
    return maps


def kernel(**inputs):
    maps = prep_inputs(inputs)
    nc = build()
    res = run_bass_kernel_spmd(nc, maps, core_ids=list(range(NCORES)))
    outs = [r["outT"] for r in res.results]
    o = np.concatenate(outs, axis=0)
    out = o.transpose(0, 3, 2, 1).reshape(o.shape[0], SEQ, D)
    return np.ascontiguousarray(out.astype(np.float32))
```

```python
import math
from contextlib import ExitStack
import numpy as np
import ml_dtypes
import concourse.bass as bass
import concourse.mybir as mybir
from concourse.bass_utils import run_bass_kernel_spmd

F32 = mybir.dt.float32
BF16 = mybir.dt.bfloat16
AF = mybir.ActivationFunctionType
ALU = mybir.AluOpType
AX = mybir.AxisListType

NCORES = 8
D = 1024
DC = 8
SEQ = 2048
NSEQ = 2
NMETA = 16
DFF = 2816
FC = 22
EPS = 1e-6
TT = 512
NT = SEQ // TT
FT = 256
NFT = SEQ // FT


class Sync:
    def __init__(self, nc, es):
        self.nc = nc
        self.eng = {"pe": nc.tensor, "act": nc.scalar, "dve": nc.vector, "pool": nc.gpsimd, "sp": nc.sync}
        self.sem = {k: es.enter_context(nc.semaphore("s_" + k)) for k in self.eng}
        self.cnt = {k: 0 for k in self.eng}
        self.dsem = {}
        self.dcnt = {}
        self.es = es
        self.seen = {k: {} for k in self.eng}
        self.lastw = {}
        self.readers = {}
        self.ninst = 0

    NPOOL = {"sp": 12, "pool": 12, "act": 8}

    def dma_sem(self, q):
        if q not in self.dsem:
            self.dsem[q] = [self.es.enter_context(self.nc.semaphore("d_%s%d" % (q, i))) for i in range(self.NPOOL[q])]
            self.dcnt[q] = [0] * self.NPOOL[q]
            self.drr = getattr(self, "drr", {})
            self.drr[q] = 0
        i = self.drr[q]
        self.drr[q] = (i + 1) % self.NPOOL[q]
        return i

    def _wait(self, e, reads, writes):
        need = {}
        for k in reads:
            lw = self.lastw.get(k)
            if lw is not None:
                need[lw[0]] = max(need.get(lw[0], (0, None))[0], lw[1]), lw[2]
        for k in writes:
            lw = self.lastw.get(k)
            if lw is not None:
                need[lw[0]] = max(need.get(lw[0], (0, None))[0], lw[1]), lw[2]
            for r in self.readers.get(k, ()):
                need[r[0]] = max(need.get(r[0], (0, None))[0], r[1]), r[2]
        E = self.eng[e]
        for semid, (val, semobj) in need.items():
            if semid == "e_" + e and e == "pe":
                continue
            if self.seen[e].get(semid, 0) >= val:
                continue
            E.wait_ge(semobj, val)
            self.seen[e][semid] = val

    def _record(self, rec, reads, writes):
        for k in reads:
            self.readers.setdefault(k, []).append(rec)
        for k in writes:
            self.lastw[k] = rec
            self.readers[k] = []

    def op(self, e, fn, reads=(), writes=()):
        self._wait(e, reads, writes)
        inst = fn(self.eng[e])
        self.cnt[e] += 1
        inst.then_inc(self.sem[e], 1)
        self.ninst += 1
        self._record(("e_" + e, self.cnt[e], self.sem[e]), reads, writes)

    def dma(self, q, semname, out, in_, reads=(), writes=(), **kw):
        self._wait(q, reads, writes)
        i = self.dma_sem(q)
        sem = self.dsem[q][i]
        semid = "d_%s%d" % (q, i)
        if self.dcnt[q][i] > 0 and self.seen[q].get(semid, 0) < self.dcnt[q][i]:
            self.eng[q].wait_ge(sem, self.dcnt[q][i])
            self.seen[q][semid] = self.dcnt[q][i]
        inst = self.eng[q].dma_start(out=out, in_=in_, **kw)
        self.dcnt[q][i] += 16
        inst.then_inc(sem, 16)
        self.ninst += 1
        self._record((semid, self.dcnt[q][i], sem), reads, writes)

    def drain(self, e):
        E = self.eng[e]
        for k in self.eng:
            if self.cnt[k] > 0:
                E.wait_ge(self.sem[k], self.cnt[k])
        for q, sems in self.dsem.items():
            for i, sm in enumerate(sems):
                if self.dcnt[q][i] > 0:
                    E.wait_ge(sm, self.dcnt[q][i])


def build(debug=None):
    nc = bass.Bass("TRN2", target_bir_lowering=False)
    es = ExitStack()
    with es:
        S = Sync(nc, es)

        def din(name, shape, dt=F32):
            return nc.dram_tensor(name, list(shape), dt, kind="ExternalInput").ap()

        def dscr(name, shape, dt=F32):
            return nc.dram_tensor(name, list(shape), dt, kind="Internal").ap()

        xT = din("xT", [NSEQ, 128, DC, SEQ])
        metaT = din("metaT", [128, DC, NMETA])
        gains = din("gains", [128, 48])
        ff1_wg = din("ff1_wg", [128, DC, DFF])
        ff1_wu = din("ff1_wu", [128, DC, DFF])
        ff1_wd = din("ff1_wd", [128, FC, D])
        outT = nc.dram_tensor("outT", [NSEQ, 128, DC, SEQ], F32, kind="ExternalOutput").ap()
        h1T = dscr("h1T", [NSEQ, 128, DC, SEQ])
        h1m = dscr("h1m", [128, DC, NMETA])

        def sb(stack, name, shape, dt=F32):
            return stack.enter_context(nc.sbuf_tensor(name, list(shape), dt))

        def ps(stack, name, shape, dt=F32):
            return stack.enter_context(nc.psum_tensor(name, list(shape), dt))

        gains_sb = sb(es, "gains_sb", [128, 48])
        ones_f = sb(es, "ones_f", [128, 128])
        S.dma("sp", "c", gains_sb[:, :], gains[:, :], writes=["gains"])
        S.op("dve", lambda e: e.memset(ones_f[:, :], 1.0), writes=["ones_f"])
        zeros_b = sb(es, "zeros_b", [128, 128], BF16)
        S.op("dve", lambda e: e.memset(zeros_b[:, :], 0.0), writes=["zeros_b"])

        def rstd_from_sumsq(pss, rs, n, key_pss, key_rs, half=False):
            S.op("act", lambda e: e.activation(out=rs[:, :n], in_=pss[:, :n], func=AF.Sqrt,
                                               bias=eps_sb[:, (1 if half else 0):(2 if half else 1)],
                                               scale=(4.0 if half else 1.0) / D),
                 reads=[key_pss, "eps"], writes=[key_rs])
            S.op("dve", lambda e: e.reciprocal(out=rs[:, :n], in_=rs[:, :n]), reads=[key_rs], writes=[key_rs])

        eps_sb = sb(es, "eps_sb", [128, 2])
        S.op("dve", lambda e: e.memset(eps_sb[:, 0:1], EPS), writes=["eps"])
        S.op("dve", lambda e: e.memset(eps_sb[:, 1:2], 4.0 * EPS), writes=["eps"])

        def stats_step(tag, c, src, n, sq, acc, rkeys):
            S.op("act", lambda e: e.activation(out=sq[:, c % 2, :n], in_=src, func=AF.Square),
                 reads=rkeys, writes=[(tag, "sq", c % 2)])
            if c == 1:
                S.op("pool", lambda e: e.tensor_tensor(out=acc[:, :n], in0=sq[:, 0, :n], in1=sq[:, 1, :n], op=ALU.add),
                     reads=[(tag, "sq", 0), (tag, "sq", 1)], writes=[(tag, "acc")])
            elif c >= 2:
                S.op("pool", lambda e: e.tensor_tensor(out=acc[:, :n], in0=acc[:, :n], in1=sq[:, c % 2, :n], op=ALU.add),
                     reads=[(tag, "sq", c % 2), (tag, "acc")], writes=[(tag, "acc")])

        def stats_finish(tag, n, acc, pss, rs, half=False):
            S.op("pe", lambda e: e.matmul(pss[:, :n], lhsT=ones_f[:, :], rhs=acc[:, :n], start=True, stop=True),
                 reads=[(tag, "acc"), "ones_f"], writes=[(tag, "pss")])
            rstd_from_sumsq(pss, rs, n, (tag, "pss"), (tag, "rs"), half=half)

        def ffn_phase(tag, wg, wu, wd, gpre_col, gpost_col, tiles):
            with ExitStack() as st:
                wg_sb = sb(st, tag + "wg", [128, DC, DFF], BF16)
                wu_sb = sb(st, tag + "wu", [128, DC, DFF], BF16)
                wd_sb = sb(st, tag + "wd", [128, FC, D], BF16)
                xt = [sb(st, tag + "xt%d" % i, [128, DC, FT]) for i in range(2)]
                sq = sb(st, tag + "sq", [128, 2, FT])
                acc = sb(st, tag + "acc", [128, FT])
                hn = sb(st, tag + "hn", [128, DC, FT], BF16)
                act = sb(st, tag + "act", [128, FC, FT], BF16)
                sg = [sb(st, tag + "sg%d" % i, [128, FT]) for i in range(2)]
                ysb = sb(st, tag + "y", [128, DC, FT])
                rs = sb(st, tag + "rs", [128, FT])
                psg = [ps(st, tag + "psg%d" % i, [128, FT]) for i in range(2)]
                psu = [ps(st, tag + "psu%d" % i, [128, FT]) for i in range(2)]
                psy = [ps(st, tag + "psy%d" % i, [128, FT]) for i in range(2)]
                pss = ps(st, tag + "pss", [128, FT])
                for g in range(FC // 2):
                    cs_ = slice(g * 256, (g + 1) * 256)
                    S.dma("pool", "w", wg_sb[:, :, cs_], wg[:, :, cs_], writes=[(tag, "wg", g)])
                    S.dma("pool", "w", wu_sb[:, :, cs_], wu[:, :, cs_], writes=[(tag, "wu", g)])
                for j in range(FC):
                    S.dma("pool", "w", wd_sb[:, j, :], wd[:, j, :], writes=[(tag, "wd", j)], max_dma_last_dim=4096)

                def load(i):
                    src, dst, n, sr, dw = tiles[i]
                    S.dma("sp", "x", xt[i % 2][:, :, :n], src, reads=sr, writes=[(tag, "xt", i % 2)])

                load(0)
                for i, (src, dst, n, sr, dw) in enumerate(tiles):
                    if i + 1 < len(tiles):
                        load(i + 1)
                    x = xt[i % 2]
                    kx = (tag, "xt", i % 2)
                    for c in range(DC):
                        stats_step(tag, c, x[:, c, :n], n, sq, acc, [kx])
                    stats_finish(tag, n, acc, pss, rs)
                    for c in range(DC):
                        S.op("dve", lambda e, c=c: e.scalar_tensor_tensor(
                            out=hn[:, c, :n], in0=x[:, c, :n], scalar=gains_sb[:, gpre_col + c:gpre_col + c + 1],
                            in1=rs[:, :n], op0=ALU.mult, op1=ALU.mult),
                            reads=[kx, (tag, "rs"), "gains"], writes=[(tag, "hn", c)])
                    for j in range(FC):
                        b = j % 2
                        for k in range(DC):
                            S.op("pe", lambda e, k=k, j=j, b=b: e.matmul(
                                psg[b][:, :n], lhsT=wg_sb[:, k, j * 128:(j + 1) * 128], rhs=hn[:, k, :n],
                                start=(k == 0), stop=(k == DC - 1)),
                                reads=[(tag, "wg", j // 2), (tag, "hn", k)], writes=[(tag, "psg", b)])
                        for k in range(DC):
                            S.op("pe", lambda e, k=k, j=j, b=b: e.matmul(
                                psu[b][:, :n], lhsT=wu_sb[:, k, j * 128:(j + 1) * 128], rhs=hn[:, k, :n],
                                start=(k == 0), stop=(k == DC - 1)),
                                reads=[(tag, "wu", j // 2), (tag, "hn", k)], writes=[(tag, "psu", b)])
                        S.op("act", lambda e, b=b: e.activation(out=sg[b][:, :n], in_=psg[b][:, :n], func=AF.Silu),
                             reads=[(tag, "psg", b)], writes=[(tag, "sg", b)])
                        S.op("dve", lambda e, b=b, j=j: e.tensor_tensor(out=act[:, j, :n], in0=sg[b][:, :n],
                                                                        in1=psu[b][:, :n], op=ALU.mult),
                             reads=[(tag, "sg", b), (tag, "psu", b)], writes=[(tag, "act", j)])
                    for c in range(DC):
                        b = c % 2
                        for j in range(FC):
                            S.op("pe", lambda e, c=c, j=j, b=b: e.matmul(
                                psy[b][:, :n], lhsT=wd_sb[:, j, c * 128:(c + 1) * 128], rhs=act[:, j, :n],
                                start=(j == 0), stop=(j == FC - 1)),
                                reads=[(tag, "wd", j), (tag, "act", j)], writes=[(tag, "psy", b)])
                        S.op("act", lambda e, c=c, b=b: e.activation(out=ysb[:, c, :n], in_=psy[b][:, :n], func=AF.Copy),
                             reads=[(tag, "psy", b)], writes=[(tag, "y", c)])
                        stats_step(tag, c, psy[b][:, :n], n, sq, acc, [(tag, "psy", b)])
                    stats_finish(tag, n, acc, pss, rs, half=True)
                    for c in range(DC):
                        S.op("dve", lambda e, c=c: e.scalar_tensor_tensor(
                            out=ysb[:, c, :n], in0=ysb[:, c, :n], scalar=gains_sb[:, gpost_col + c:gpost_col + c + 1],
                            in1=rs[:, :n], op0=ALU.mult, op1=ALU.mult),
                            reads=[(tag, "y", c), (tag, "rs"), "gains"], writes=[(tag, "y", c)])
                        S.op("pool", lambda e, c=c: e.tensor_tensor(
                            out=ysb[:, c, :n], in0=ysb[:, c, :n], in1=x[:, c, :n], op=ALU.add),
                            reads=[(tag, "y", c), kx], writes=[(tag, "y", c)])
                    S.dma("sp", "o", dst, ysb[:, :, :n], reads=[(tag, "y", c) for c in range(DC)], writes=dw)

        def barrier():
            for e in S.eng:
                S.drain(e)

        S.barrier = barrier

        def mms(out, pairs, reads, wkey):
            n_ = len(pairs)
            for idx, (l, r) in enumerate(pairs):
                S.op("pe", lambda e, l=l, r=r, idx=idx: e.matmul(out, lhsT=l, rhs=r, start=(idx == 0),
                                                                 stop=(idx == n_ - 1)),
                     reads=reads, writes=[wkey])

        def prenorm(tag, x, kx, n, gcol, hn, sq, pss, rs, acc):
            for c in range(DC):
                stats_step(tag, c, x[:, c, :n], n, sq, acc, [kx])
            stats_finish(tag, n, acc, pss, rs)
            for c in range(DC):
                S.op("dve", lambda e, c=c: e.scalar_tensor_tensor(
                    out=hn[:, c, :n], in0=x[:, c, :n], scalar=gains_sb[:, gcol + c:gcol + c + 1],
                    in1=rs[:, :n], op0=ALU.mult, op1=ALU.mult),
                    reads=[kx, (tag, "rs"), "gains"], writes=[(tag, "hn")])

        tilesA = []
        for s in range(NSEQ):
            for t in range(NFT):
                tilesA.append((xT[s, :, :, t * FT:(t + 1) * FT], h1T[s, :, :, t * FT:(t + 1) * FT], FT, [],
                               [("h1T", s, t * FT // TT)]))
        tilesA.append((metaT[:, :, :], h1m[:, :, :], NMETA, [], ["h1m"]))
        if debug == "A":
            tilesA = tilesA[:2]
        ffn_phase("A", ff1_wg, ff1_wu, ff1_wd, 0, 8, tilesA)
        barrier()

        LP = NMETA + SEQ
        w_inA = din("w_inA", [128, DC, 2048])
        w_inB = din("w_inB", [128, DC, 72])
        uT = dscr("uT", [NSEQ, 128, 6, LP], BF16)
        kiT = dscr("kiT", [NSEQ, 128, LP], BF16)
        kT = dscr("kT", [NSEQ, 128, LP], BF16)
        qiT = dscr("qiT", [NSEQ, 128, 4, SEQ], BF16)
        qT = dscr("qT", [NSEQ, 128, 4, SEQ], BF16)
        vwS = dscr("vwS", [NSEQ, LP, 72], F32)
        hnT = dscr("hnT", [NSEQ, 128, DC, SEQ], BF16)

        def phase_B():
            tag = "B"
            with ExitStack() as st:
                wA = sb(st, "BwA", [128, DC, 2048], BF16)
                wB = sb(st, "BwB", [128, DC, 72], BF16)
                for k in range(DC):
                    S.dma("pool", "w", wA[:, k, :], w_inA[:, k, :], writes=[("B", "wA")], max_dma_last_dim=4096)
                S.dma("pool", "w", wB[:, :, :], w_inB[:, :, :], writes=[("B", "wB")])
                xt = [sb(st, "Bxt%d" % i, [128, DC, TT]) for i in range(2)]
                hn = sb(st, "Bhn", [128, DC, TT], BF16)
                sq = sb(st, "Bsq", [128, 2, TT])
                accB = sb(st, "Bacc", [128, TT])
                rs = sb(st, "Brs", [128, TT])
                stage = sb(st, "Bstage", [128, 16, TT], BF16)
                vw = sb(st, "Bvw", [128, 4, 72])
                pss = ps(st, "Bpss", [128, TT])
                pp = [ps(st, "Bpp%d" % i, [128, TT]) for i in range(2)]
                pv = [ps(st, "Bpv%d" % i, [128, 72]) for i in range(2)]
                tiles = [("m", 0)] + [(s, t) for s in range(NSEQ) for t in range(NT)]

                def load(i):
                    s_, t_ = tiles[i]
                    if s_ == "m":
                        S.dma("sp", "x", xt[i % 2][:, :, :NMETA], h1m[:, :, :], reads=["h1m"], writes=[("B", "xt", i % 2)])
                    else:
                        S.dma("sp", "x", xt[i % 2][:, :, :], h1T[s_, :, :, t_ * TT:(t_ + 1) * TT],
                              reads=[("h1T", s_, t_)], writes=[("B", "xt", i % 2)])

                load(0)
                for i, (s_, t_) in enumerate(tiles):
                    if i + 1 < len(tiles):
                        load(i + 1)
                    n = NMETA if s_ == "m" else TT
                    x = xt[i % 2]
                    kx = ("B", "xt", i % 2)
                    prenorm("B", x, kx, n, 16, hn, sq, pss, rs, accB)
                    if s_ != "m":
                        S.dma("act", "o", hnT[s_, :, :, t_ * TT:(t_ + 1) * TT], hn[:, :, :], reads=[("B", "hn")],
                              writes=[("hnT", s_, t_)])
                    for cc in range(16):
                        b_ = cc % 2
                        mms(pp[b_][:, :n], [(wA[:, k, cc * 128:(cc + 1) * 128], hn[:, k, :n]) for k in range(DC)],
                            [("B", "wA"), ("B", "hn")], ("B", "pp", b_))
                        sc_ = 0.125 if 10 <= cc < 14 else 1.0
                        S.op("act", lambda e, cc=cc, b_=b_, sc_=sc_: e.activation(
                            out=stage[:, cc, :n], in_=pp[b_][:, :n], func=AF.Copy, scale=sc_),
                            reads=[("B", "pp", b_)], writes=[("B", "stage")])
                    if s_ == "m":
                        for s2 in range(NSEQ):
                            S.dma("sp", "o", uT[s2, :, :, 0:NMETA], stage[:, 0:6, :NMETA], reads=[("B", "stage")],
                                  writes=[("uT", s2, "m")])
                            S.dma("sp", "o", kiT[s2, :, 0:NMETA], stage[:, 14, :NMETA], reads=[("B", "stage")],
                                  writes=[("kiT", s2, "m")])
                            S.dma("sp", "o", kT[s2, :, 0:NMETA], stage[:, 15, :NMETA], reads=[("B", "stage")],
                                  writes=[("kT", s2, "m")])
                    else:
                        t0 = t_ * TT
                        S.dma("sp", "o", uT[s_, :, :, NMETA + t0:NMETA + t0 + TT], stage[:, 0:6, :],
                              reads=[("B", "stage")], writes=[("uT", s_, t_)])
                        S.dma("sp", "o", qiT[s_, :, :, t0:t0 + TT], stage[:, 6:10, :], reads=[("B", "stage")],
                              writes=[("qiT", s_, t_)])
                        S.dma("sp", "o", qT[s_, :, :, t0:t0 + TT], stage[:, 10:14, :], reads=[("B", "stage")],
                              writes=[("qT", s_, t_)])
                        S.dma("sp", "o", kiT[s_, :, NMETA + t0:NMETA + t0 + TT], stage[:, 14, :],
                              reads=[("B", "stage")], writes=[("kiT", s_, t_)])
                        S.dma("sp", "o", kT[s_, :, NMETA + t0:NMETA + t0 + TT], stage[:, 15, :],
                              reads=[("B", "stage")], writes=[("kT", s_, t_)])
                    nb = max(1, n // 128)
                    rows = min(n, 128)
                    for blk in range(nb):
                        b_ = blk % 2
                        mms(pv[b_][:rows, :], [(hn[:, k, blk * 128:blk * 128 + rows], wB[:, k, :]) for k in range(DC)],
                            [("B", "wB"), ("B", "hn")], ("B", "pv", b_))
                        S.op("dve", lambda e, blk=blk, b_=b_: e.tensor_copy(out=vw[:rows, blk, :], in_=pv[b_][:rows, :]),
                             reads=[("B", "pv", b_)], writes=[("B", "vw")])
                    if s_ == "m":
                        for s2 in range(NSEQ):
                            S.dma("sp", "o", vwS[s2, 0:NMETA, :], vw[:NMETA, 0, :], reads=[("B", "vw")],
                                  writes=[("vwS", s2, "m")])
                    else:
                        S.dma("sp", "o", vwS[s_, NMETA + t0:NMETA + t0 + TT, :].rearrange("(b p) c -> p b c", p=128),
                              vw[:, :, :], reads=[("B", "vw")], writes=[("vwS", s_, t_)])

        if debug != "A":
            phase_B()
            barrier()

        ALLT = ["m"] + list(range(NT))

        if debug == "B":
            with ExitStack() as st:
                tmp = sb(st, "dbgtmp", [128, 6, LP], BF16)
                tmp2 = sb(st, "dbgtmp2", [128, 6, LP], F32)
                S.dma("sp", "x", tmp[:, :, :], uT[0, :, :, :], reads=[("uT", 0, t) for t in ALLT], writes=["dbgtmp"])
                S.op("dve", lambda e: e.tensor_copy(out=tmp2[:, :, :], in_=tmp[:, :, :]), reads=["dbgtmp"], writes=["dbgtmp2"])
                S.dma("sp", "o", outT[0, :, 0:6, 0:SEQ], tmp2[:, :, NMETA:LP], reads=["dbgtmp2"], writes=["out"])
                S.dma("sp", "x", tmp2[:, 0, 0:72 * 16].rearrange("p (b c) -> p b c", c=72),
                      vwS[0, NMETA:NMETA + 2048, :].rearrange("(b p) c -> p b c", p=128),
                      reads=[("vwS", 0, t) for t in ALLT] + ["out"], writes=["dbgtmp2"])
                S.dma("sp", "o", outT[1, :, 0, 0:72 * 16], tmp2[:, 0, 0:72 * 16], reads=["dbgtmp2"], writes=["out"])


        s5_pcm = din("s5_pcm", [128, 6, 3, 128])
        s5_bcm = din("s5_bcm", [128, 6, 2, 128])
        s5_psm = din("s5_psm", [128, 3, 16])
        s5_bsm = din("s5_bsm", [128, 16, 2, 32])
        s5_csm = din("s5_csm", [128, 16, 2, 32])
        s5_d = din("s5_d", [128, 6])
        ident_d = din("ident", [128, 128])
        yaT = dscr("yaT", [NSEQ, 128, 6, SEQ], BF16)
        NCH = LP // 16
        ident_f = sb(es, "ident_f", [128, 128])
        ident_b = sb(es, "ident_b", [128, 128], BF16)
        S.dma("sp", "c", ident_f[:, :], ident_d[:, :], writes=["ident_f"])
        S.op("dve", lambda e: e.tensor_copy(out=ident_b[:, :], in_=ident_f[:, :]), reads=["ident_f"], writes=["ident_b"])

        def phase_C():
            K_ = "Cprep"
            R_, W_ = [K_], [K_]

            def tt(eng, out, a, b_, op):
                S.op(eng, lambda e: e.tensor_tensor(out=out, in0=a, in1=b_, op=op), reads=R_, writes=W_)

            def tsc(eng, out, a, s1, op0, s2=None, op1=None):
                if op1 is None:
                    S.op(eng, lambda e: e.tensor_scalar(out=out, in0=a, scalar1=s1, scalar2=None, op0=op0), reads=R_, writes=W_)
                else:
                    S.op(eng, lambda e: e.tensor_scalar(out=out, in0=a, scalar1=s1, scalar2=s2, op0=op0, op1=op1),
                         reads=R_, writes=W_)

            def stt(out, a, sc_, b_, op0, op1):
                S.op("dve", lambda e: e.scalar_tensor_tensor(out=out, in0=a, scalar=sc_, in1=b_, op0=op0, op1=op1),
                     reads=R_, writes=W_)

            def actf(out, a, func, scale=1.0):
                S.op("act", lambda e: e.activation(out=out, in_=a, func=func, scale=scale), reads=R_, writes=W_)

            def cparams(st, nm, lr, li, ldt_, shp):
                T = lambda n_: sb(st, "C%s_%s" % (nm, n_), shp)
                dt, mag, th, sh, c, s_, t1, t2, t3 = [T(n_) for n_ in ("dt", "mag", "th", "sh", "c", "s", "t1", "t2", "t3")]
                are, aim, cre_, cim_ = [T(n_) for n_ in ("are", "aim", "cre", "cim")]
                A = lambda t_: t_[tuple(slice(None) for _ in shp)]
                actf(A(dt), ldt_, AF.Exp)
                tt("dve", A(t1), lr, A(dt), ALU.mult)
                actf(A(mag), A(t1), AF.Exp)
                tt("dve", A(th), li, A(dt), ALU.mult)
                actf(A(sh), A(th), AF.Sin, scale=1.0 / 32)
                actf(A(s_), A(th), AF.Sin, scale=1.0 / 16)
                tt("dve", A(t1), A(sh), A(sh), ALU.mult)
                tsc("dve", A(c), A(t1), -2.0, ALU.mult, 1.0, ALU.add)
                for _ in range(4):
                    tt("dve", A(t1), A(c), A(c), ALU.mult)
                    tt("dve", A(t2), A(s_), A(s_), ALU.mult)
                    tt("dve", A(t3), A(c), A(s_), ALU.mult)
                    tt("dve", A(c), A(t1), A(t2), ALU.subtract)
                    tsc("dve", A(s_), A(t3), 2.0, ALU.mult)
                tt("dve", A(are), A(mag), A(c), ALU.mult)
                tt("dve", A(aim), A(mag), A(s_), ALU.mult)
                tt("dve", A(t1), lr, lr, ALU.mult)
                tt("dve", A(t2), li, li, ALU.mult)
                tt("dve", A(t1), A(t1), A(t2), ALU.add)
                S.op("dve", lambda e: e.reciprocal(out=A(t1), in_=A(t1)), reads=R_, writes=W_)
                tsc("dve", A(t2), A(are), -1.0, ALU.add)
                tt("dve", A(t3), A(t2), lr, ALU.mult)
                tt("dve", A(c), A(aim), li, ALU.mult)
                tt("dve", A(t3), A(t3), A(c), ALU.add)
                tt("dve", A(cre_), A(t3), A(t1), ALU.mult)
                tt("dve", A(t3), A(aim), lr, ALU.mult)
                tt("dve", A(c), A(t2), li, ALU.mult)
                tt("dve", A(t3), A(t3), A(c), ALU.subtract)
                tt("dve", A(cim_), A(t3), A(t1), ALU.mult)
                return are, aim, cre_, cim_

            with ExitStack() as st:
                WS = sb(st, "C_WS", [128, 16, 6, 2, 128], BF16)
                WO = sb(st, "C_WO", [128, 16, 16, 2, 32], BF16)
                WK = sb(st, "C_WK", [128, 16, 6, 128], BF16)
                a16 = sb(st, "C_a16", [128, 2, 16])
                a32 = sb(st, "C_a32", [128, 2, 16])
                with ExitStack() as st2:
                    pc = sb(st2, "C_pc", [128, 6, 3, 128])
                    bc = sb(st2, "C_bc", [128, 6, 2, 128])
                    S.dma("sp", "c", pc[:, :, :, :], s5_pcm[:, :, :, :], writes=W_)
                    S.dma("sp", "c", bc[:, :, :, :], s5_bcm[:, :, :, :], writes=W_)
                    are, aim, cre_, cim_ = cparams(st2, "cm", pc[:, :, 0, :], pc[:, :, 1, :], pc[:, :, 2, :], [128, 6, 128])
                    wr = sb(st2, "C_wr", [128, 6, 128])
                    wi = sb(st2, "C_wi", [128, 6, 128])
                    u1 = sb(st2, "C_u1", [128, 6, 128])
                    u2 = sb(st2, "C_u2", [128, 6, 128])
                    F3 = (slice(None),) * 3

                    def cmul(orr, oi, xr, xi, yr, yi):
                        tt("dve", u1[F3], xr, yr, ALU.mult)
                        tt("dve", u2[F3], xi, yi, ALU.mult)
                        tt("dve", u1[F3], u1[F3], u2[F3], ALU.subtract)
                        tt("dve", u2[F3], xr, yi, ALU.mult)
                        tt("dve", oi, xi, yr, ALU.mult)
                        tt("dve", oi, oi, u2[F3], ALU.add)
                        S.op("dve", lambda e: e.tensor_copy(out=orr, in_=u1[F3]), reads=R_, writes=W_)

                    cmul(wr[F3], wi[F3], cre_[F3], cim_[F3], bc[:, :, 0, :], bc[:, :, 1, :])
                    for lag in range(16):
                        S.op("act", lambda e, lag=lag: e.activation(out=WS[:, lag, :, 0, :], in_=wr[F3], func=AF.Copy),
                             reads=R_, writes=W_)
                        S.op("act", lambda e, lag=lag: e.activation(out=WS[:, lag, :, 1, :], in_=wi[F3], func=AF.Copy),
                             reads=R_, writes=W_)
                        if lag < 15:
                            cmul(wr[F3], wi[F3], wr[F3], wi[F3], are[F3], aim[F3])
                barrier()
                with ExitStack() as st2:
                    pm = sb(st2, "C_pm", [128, 3, 16])
                    bs = sb(st2, "C_bs", [128, 16, 2, 32])
                    cs = sb(st2, "C_cs", [128, 16, 2, 32])
                    dsb = sb(st2, "C_d", [128, 6])
                    S.dma("sp", "c", pm[:, :, :], s5_psm[:, :, :], writes=W_)
                    S.dma("sp", "c", bs[:, :, :, :], s5_bsm[:, :, :, :], writes=W_)
                    S.dma("sp", "c", cs[:, :, :, :], s5_csm[:, :, :, :], writes=W_)
                    S.dma("sp", "c", dsb[:, :], s5_d[:, :], writes=W_)
                    sre, sim, scr, sci = cparams(st2, "sm", pm[:, 0, :], pm[:, 1, :], pm[:, 2, :], [128, 16])
                    apr = sb(st2, "C_apr", [128, 17, 16])
                    api = sb(st2, "C_api", [128, 17, 16])
                    napi = sb(st2, "C_napi", [128, 17, 16])
                    v1 = sb(st2, "C_v1", [128, 16])
                    v2 = sb(st2, "C_v2", [128, 16])
                    S.op("dve", lambda e: e.memset(apr[:, 0, :], 1.0), reads=R_, writes=W_)
                    S.op("dve", lambda e: e.memset(api[:, 0, :], 0.0), reads=R_, writes=W_)
                    for k in range(1, 17):
                        tt("dve", v1[:, :], apr[:, k - 1, :], sre[:, :], ALU.mult)
                        tt("dve", v2[:, :], api[:, k - 1, :], sim[:, :], ALU.mult)
                        tt("dve", apr[:, k, :], v1[:, :], v2[:, :], ALU.subtract)
                        tt("dve", v1[:, :], apr[:, k - 1, :], sim[:, :], ALU.mult)
                        tt("dve", v2[:, :], api[:, k - 1, :], sre[:, :], ALU.mult)
                        tt("dve", api[:, k, :], v1[:, :], v2[:, :], ALU.add)
                    tsc("dve", napi[:, :, :], api[:, :, :], -1.0, ALU.mult)
                    napr = sb(st2, "C_napr", [128, 17, 16])
                    tsc("dve", napr[:, :, :], apr[:, :, :], -1.0, ALU.mult)
                    S.op("dve", lambda e: e.tensor_copy(out=a16[:, 0, :], in_=apr[:, 16, :]), reads=R_, writes=W_)
                    S.op("dve", lambda e: e.tensor_copy(out=a16[:, 1, :], in_=api[:, 16, :]), reads=R_, writes=W_)
                    tt("dve", v1[:, :], apr[:, 16, :], apr[:, 16, :], ALU.mult)
                    tt("dve", v2[:, :], api[:, 16, :], api[:, 16, :], ALU.mult)
                    tt("dve", a32[:, 0, :], v1[:, :], v2[:, :], ALU.subtract)
                    tt("dve", v1[:, :], apr[:, 16, :], api[:, 16, :], ALU.mult)
                    tsc("dve", a32[:, 1, :], v1[:, :], 2.0, ALU.mult)
                    nsci = sb(st2, "C_nsci", [128, 16])
                    tsc("dve", nsci[:, :], sci[:, :], -1.0, ALU.mult)
                    bb = sb(st2, "C_bb", [128, 16, 2, 32])
                    x1 = sb(st2, "C_x1", [128, 32])
                    for i in range(16):
                        tsc("dve", x1[:, :], bs[:, i, 0, :], scr[:, i:i + 1], ALU.mult)
                        stt(bb[:, i, 0, :], bs[:, i, 1, :], nsci[:, i:i + 1], x1[:, :], ALU.mult, ALU.add)
                        tsc("dve", x1[:, :], bs[:, i, 1, :], scr[:, i:i + 1], ALU.mult)
                        stt(bb[:, i, 1, :], bs[:, i, 0, :], sci[:, i:i + 1], x1[:, :], ALU.mult, ALU.add)
                    Bs = sb(st2, "C_Bs", [128, 16, 2, 32])
                    tsc("dve", Bs[:, :, 0, :], bb[:, :, 1, :], -1.0, ALU.mult)
                    S.op("dve", lambda e: e.tensor_copy(out=Bs[:, :, 1, :], in_=bb[:, :, 0, :]), reads=R_, writes=W_)
                    Cp2 = sb(st2, "C_Cp2", [128, 16, 2, 32])
                    Cs2 = sb(st2, "C_Cs2", [128, 16, 2, 32])
                    S.op("dve", lambda e: e.tensor_copy(out=Cp2[:, :, 0, :], in_=cs[:, :, 0, :]), reads=R_, writes=W_)
                    tsc("dve", Cp2[:, :, 1, :], cs[:, :, 1, :], -1.0, ALU.mult)
                    tsc("dve", Cs2[:, :, 0, :], cs[:, :, 1, :], -1.0, ALU.mult)
                    tsc("dve", Cs2[:, :, 1, :], cs[:, :, 0, :], -1.0, ALU.mult)
                    crb = sb(st2, "C_crb", [128, 16, 2, 32], BF16)
                    S.op("dve", lambda e: e.tensor_copy(out=crb[:, :, :, :], in_=Cp2[:, :, :, :]), reads=R_, writes=W_)
                    S.op("pool", lambda e: e.memset(WK[:, :, :, :], 0.0), reads=R_, writes=W_)
                    barrier()
                    AB = sb(st2, "C_AB", [128, 2, 16, 2, 32], BF16)
                    f1 = [sb(st2, "C_f1%d" % i, [128, 16, 64]) for i in range(2)]
                    f2 = [sb(st2, "C_f2%d" % i, [128, 16, 64]) for i in range(2)]
                    psK = [ps(st2, "C_psK%d" % i, [128, 192]) for i in range(2)]
                    for lp_ in range(2):
                        S.op("pe", lambda e, lp_=lp_: e.matmul(psK[lp_][:, :], lhsT=zeros_b[:, :], rhs=crb[:, 0:3, :, :].rearrange("p i r q -> p (i r q)"),
                                                             start=True, stop=True), reads=["zeros_b"], writes=[("C_psK", lp_)])

                    def bc(t, k):
                        return t[:, k, :].unsqueeze(2).to_broadcast([128, 16, 64])

                    Bp3 = bb[:, :, :, :].rearrange("p i r q -> p i (r q)")
                    Bs3 = Bs[:, :, :, :].rearrange("p i r q -> p i (r q)")
                    Cp3 = Cp2[:, :, :, :].rearrange("p i r q -> p i (r q)")
                    Cs3 = Cs2[:, :, :, :].rearrange("p i r q -> p i (r q)")
                    for lag in range(16):
                        lp_ = lag % 2
                        AB3 = AB[:, lp_, :, :, :].rearrange("p i r q -> p i (r q)")
                        WO3 = WO[:, lag, :, :, :].rearrange("p i r q -> p i (r q)")
                        S.op("dve", lambda e, lag=lag, lp_=lp_: e.tensor_tensor(out=f1[0][:, :, :], in0=Bp3, in1=bc(apr, lag), op=ALU.mult),
                             reads=[], writes=[("C_f1", 0)])
                        S.op("pool", lambda e, lag=lag, lp_=lp_: e.tensor_tensor(out=f2[0][:, :, :], in0=Bs3, in1=bc(api, lag), op=ALU.mult),
                             reads=[], writes=[("C_f2", 0)])
                        S.op("dve", lambda e, AB3=AB3: e.tensor_tensor(out=AB3, in0=f1[0][:, :, :], in1=f2[0][:, :, :], op=ALU.add),
                             reads=[("C_f1", 0), ("C_f2", 0)], writes=[("C_AB", lp_)])
                        S.op("dve", lambda e, lag=lag: e.tensor_tensor(out=f1[1][:, :, :], in0=Cp3, in1=bc(apr, lag + 1), op=ALU.mult),
                             reads=[], writes=[("C_f1", 1)])
                        S.op("pool", lambda e, lag=lag: e.tensor_tensor(out=f2[1][:, :, :], in0=Cs3, in1=bc(api, lag + 1), op=ALU.mult),
                             reads=[], writes=[("C_f2", 1)])
                        S.op("dve", lambda e, WO3=WO3: e.tensor_tensor(out=WO3, in0=f1[1][:, :, :], in1=f2[1][:, :, :], op=ALU.add),
                             reads=[("C_f1", 1), ("C_f2", 1)], writes=["C_WOw"])
                        for i in range(16):
                            r0 = 32 * (i % 3)
                            cc_ = i // 3
                            S.op("pe", lambda e, i=i, r0=r0, cc_=cc_, lp_=lp_: e.matmul(
                                psK[lp_][r0:r0 + 32, cc_ * 32:(cc_ + 1) * 32], lhsT=AB[:, lp_, i, 0, :], rhs=crb[:, i, 0, :],
                                start=True, stop=False), reads=[("C_AB", lp_)], writes=[("C_psK", lp_)])
                            S.op("pe", lambda e, i=i, r0=r0, cc_=cc_, lp_=lp_: e.matmul(
                                psK[lp_][r0:r0 + 32, cc_ * 32:(cc_ + 1) * 32], lhsT=AB[:, lp_, i, 1, :], rhs=crb[:, i, 1, :],
                                start=False, stop=True), reads=[("C_AB", lp_)], writes=[("C_psK", lp_)])
                        for rb in range(3):
                            S.op("act", lambda e, rb=rb, lag=lag, lp_=lp_: e.activation(
                                out=WK[32 * rb:32 * rb + 32, lag, :, 32 * rb:32 * rb + 32],
                                in_=psK[lp_][32 * rb:32 * rb + 32, :].rearrange("p (c q) -> p c q", q=32),
                                func=AF.Copy), reads=[("C_psK", lp_)], writes=["C_WKw"])
                    barrier()
                    for cc_ in range(6):
                        stt(WK[:, 0, cc_, :], ident_f[:, :], dsb[:, cc_:cc_ + 1], WK[:, 0, cc_, :], ALU.mult, ALU.add)
                barrier()
                usb = sb(st, "C_u", [128, 6, LP], BF16)
                Ssb = sb(st, "C_S", [128, 16, 2, NCH])
                Xbf = sb(st, "C_Xbf", [128, 16, 2, NCH], BF16)
                ypre = sb(st, "C_ypre", [128, SEQ])
                g1 = sb(st, "C_g1", [128, SEQ])
                ya = sb(st, "C_ya", [128, 6, SEQ], BF16)
                w1 = sb(st, "C_w1", [128, 16])
                w2 = sb(st, "C_w2", [128, 16])
                w3 = sb(st, "C_w3", [128, 16])
                w4 = sb(st, "C_w4", [128, 16])
                psS = [ps(st, "C_psS%d" % i, [128, NCH]) for i in range(2)]
                psY = [ps(st, "C_psY%d" % i, [128, NCH]) for i in range(2)]
                for s_ in range(NSEQ):
                    S.dma("sp", "x", usb[:, :, :], uT[s_, :, :, :], reads=[("uT", s_, t) for t in ALLT], writes=["C_u"])
                    n_ = 0
                    for i in range(16):
                        r0 = 32 * (i % 3)
                        cc_ = i // 3
                        for ri in range(2):
                            b_ = n_ % 2
                            n_ += 1
                            mms(psS[b_][:, :], [(WS[r0:r0 + 32, 15 - j, cc_, ri, :], usb[r0:r0 + 32, cc_, j:LP:16])
                                                for j in range(16)], ["C_u", K_], ("C_psS", b_))
                            S.op("act", lambda e, i=i, ri=ri, b_=b_: e.activation(out=Ssb[:, i, ri, :], in_=psS[b_][:, :],
                                                                               func=AF.Copy),
                                 reads=[("C_psS", b_)], writes=["C_S"])
                    T1 = ypre[:, 0:1024].rearrange("p (i c) -> p i c", c=64)
                    T2 = ypre[:, 1024:2048].rearrange("p (i c) -> p i c", c=64)
                    T3 = g1[:, 0:1024].rearrange("p (i c) -> p i c", c=64)
                    T4 = g1[:, 1024:2048].rearrange("p (i c) -> p i c", c=64)
                    TK = ["C_ypre", "C_g1"]

                    def bulk(dst_lo, dst_hi, src_lo, src_hi, nn):
                        ARb = a16[:, 0, :].unsqueeze(2).to_broadcast([128, 16, nn])
                        AIb = a16[:, 1, :].unsqueeze(2).to_broadcast([128, 16, nn])
                        Sr, Si = Ssb[:, :, 0, src_lo:src_hi:2], Ssb[:, :, 1, src_lo:src_hi:2]
                        Dr, Di = Ssb[:, :, 0, dst_lo:dst_hi:2], Ssb[:, :, 1, dst_lo:dst_hi:2]
                        t1_, t2_, t3_, t4_ = T1[:, :, :nn], T2[:, :, :nn], T3[:, :, :nn], T4[:, :, :nn]
                        S.op("dve", lambda e: e.tensor_tensor(out=t1_, in0=Sr, in1=ARb, op=ALU.mult), reads=["C_S", K_] + TK, writes=["C_T1"])
                        S.op("pool", lambda e: e.tensor_tensor(out=t2_, in0=Si, in1=AIb, op=ALU.mult), reads=["C_S", K_] + TK, writes=["C_T2"])
                        S.op("dve", lambda e: e.tensor_tensor(out=t3_, in0=Si, in1=ARb, op=ALU.mult), reads=["C_S", K_] + TK, writes=["C_T3"])
                        S.op("pool", lambda e: e.tensor_tensor(out=t4_, in0=Sr, in1=AIb, op=ALU.mult), reads=["C_S", K_] + TK, writes=["C_T4"])
                        S.op("dve", lambda e: e.tensor_tensor(out=t1_, in0=t1_, in1=t2_, op=ALU.subtract), reads=["C_T1", "C_T2"], writes=["C_T1"])
                        S.op("pool", lambda e: e.tensor_tensor(out=t3_, in0=t3_, in1=t4_, op=ALU.add), reads=["C_T3", "C_T4"], writes=["C_T3"])
                        S.op("dve", lambda e: e.tensor_tensor(out=Dr, in0=Dr, in1=t1_, op=ALU.add), reads=["C_T1", "C_T3", "C_S"], writes=["C_Sa"])
                        S.op("pool", lambda e: e.tensor_tensor(out=Di, in0=Di, in1=t3_, op=ALU.add), reads=["C_T3", "C_Sa", "C_S"], writes=["C_S"] + TK)

                    bulk(1, 128, 0, 127, 64)
                    for c in range(3, NCH - 1, 2):
                        S.op("dve", lambda e, c=c: e.tensor_tensor(out=w1[:, :], in0=Ssb[:, :, 0, c - 2], in1=a32[:, 0, :], op=ALU.mult),
                             reads=["C_S", K_], writes=["C_w1"])
                        S.op("dve", lambda e, c=c: e.tensor_tensor(out=w2[:, :], in0=Ssb[:, :, 1, c - 2], in1=a32[:, 1, :], op=ALU.mult),
                             reads=["C_S"], writes=["C_w2"])
                        S.op("pool", lambda e, c=c: e.tensor_tensor(out=w3[:, :], in0=Ssb[:, :, 1, c - 2], in1=a32[:, 0, :], op=ALU.mult),
                             reads=["C_S", K_], writes=["C_w3"])
                        S.op("pool", lambda e, c=c: e.tensor_tensor(out=w4[:, :], in0=Ssb[:, :, 0, c - 2], in1=a32[:, 1, :], op=ALU.mult),
                             reads=["C_S"], writes=["C_w4"])
                        S.op("dve", lambda e: e.tensor_tensor(out=w1[:, :], in0=w1[:, :], in1=w2[:, :], op=ALU.subtract),
                             reads=["C_w1", "C_w2"], writes=["C_w1"])
                        S.op("pool", lambda e: e.tensor_tensor(out=w3[:, :], in0=w3[:, :], in1=w4[:, :], op=ALU.add),
                             reads=["C_w3", "C_w4"], writes=["C_w3"])
                        S.op("dve", lambda e, c=c: e.tensor_tensor(out=Ssb[:, :, 0, c], in0=w1[:, :], in1=Ssb[:, :, 0, c], op=ALU.add),
                             reads=["C_w1", "C_w3", "C_S"], writes=["C_Sa"])
                        S.op("pool", lambda e, c=c: e.tensor_tensor(out=Ssb[:, :, 1, c], in0=w3[:, :], in1=Ssb[:, :, 1, c], op=ALU.add),
                             reads=["C_w3", "C_Sa", "C_S"], writes=["C_S"])
                    bulk(2, 127, 1, 126, 63)
                    S.op("dve", lambda e: e.memset(Xbf[:, :, :, 0], 0.0), reads=["C_Xbf"], writes=["C_Xbf"])
                    S.op("dve", lambda e: e.tensor_copy(out=Xbf[:, :, :, 1:NCH], in_=Ssb[:, :, :, 0:NCH - 1]), reads=["C_S", "C_Xbf"], writes=["C_Xbf"])
                    n_ = 0
                    for cc_ in range(6):
                        tiles_cc = [i for i in range(3 * cc_, min(3 * cc_ + 3, 16))]
                        for tau in range(16):
                            b_ = n_ % 2
                            n_ += 1
                            pairs = [(WK[:, tau - j, cc_, :], usb[:, cc_, j:LP:16]) for j in range(tau + 1)]
                            if tau == 0:
                                pairs.append((zeros_b[:, :], usb[:, cc_, 0:LP:16]))
                            mms_out = psY[b_]
                            np_ = len(pairs)

                            def intra(idx):
                                l, r_ = pairs[idx]
                                S.op("pe", lambda e: e.matmul(mms_out[:, :], lhsT=l, rhs=r_, start=(idx == 0), stop=(idx == np_ - 1)),
                                     reads=["C_u", K_, "zeros_b"], writes=[("C_psY", b_)])
                            intra(0)
                            for i in tiles_cc:
                                r0 = 32 * (i % 3)
                                for ri in range(2):
                                    S.op("pe", lambda e, i=i, ri=ri, r0=r0, tau=tau: e.matmul(
                                        mms_out[r0:r0 + 32, :], lhsT=WO[:, tau, i, ri, :], rhs=Xbf[:, i, ri, :], start=False, stop=False),
                                        reads=["C_Xbf", K_], writes=[("C_psY", b_)])
                            for idx in range(1, np_):
                                intra(idx)
                            S.op("act", lambda e, tau=tau, b_=b_: e.activation(
                                out=ypre[:, tau:SEQ:16], in_=psY[b_][:, 1:NCH], func=AF.Copy),
                                reads=[("C_psY", b_)], writes=["C_ypre"])
                        S.op("act", lambda e: e.activation(out=g1[:, :], in_=ypre[:, :], func=AF.Square), reads=["C_ypre"], writes=["C_g1"])
                        S.op("dve", lambda e: e.tensor_scalar(out=g1[:, :], in0=g1[:, :], scalar1=0.0713548162726, scalar2=1.5957691216,
                                                              op0=ALU.mult, op1=ALU.add), reads=["C_g1"], writes=["C_g1"])
                        S.op("dve", lambda e: e.tensor_tensor(out=g1[:, :], in0=g1[:, :], in1=ypre[:, :], op=ALU.mult), reads=["C_g1", "C_ypre"], writes=["C_g1"])
                        S.op("act", lambda e: e.activation(out=g1[:, :], in_=g1[:, :], func=AF.Sigmoid), reads=["C_g1"], writes=["C_g1"])
                        S.op("dve", lambda e, cc_=cc_: e.tensor_tensor(out=ya[:, cc_, :], in0=g1[:, :], in1=ypre[:, :], op=ALU.mult),
                             reads=["C_g1", "C_ypre"], writes=["C_ya"])
                    S.dma("sp", "o", yaT[s_, :, :, :], ya[:, :, :], reads=["C_ya"], writes=[("yaT", s_)])

        if debug not in ("A", "B"):
            phase_C()
            barrier()

        if debug == "C":
            with ExitStack() as st:
                tmp = sb(st, "dbgtmp", [128, 6, SEQ], BF16)
                tmp2 = sb(st, "dbgtmp2", [128, 6, SEQ], F32)
                S.dma("sp", "x", tmp[:, :, :], yaT[0, :, :, :], reads=[("yaT", 0)], writes=["dbgtmp"])
                S.op("dve", lambda e: e.tensor_copy(out=tmp2[:, :, :], in_=tmp[:, :, :]), reads=["dbgtmp"], writes=["dbgtmp2"])
                S.dma("sp", "o", outT[0, :, 0:6, 0:SEQ], tmp2[:, :, :], reads=["dbgtmp2"], writes=["out"])

        biasG_d = din("biasG", [128, 8, 1024])
        biasM_d = din("biasM", [16, 8, 128])
        cvec_d = din("cvec", [128, 8])
        ybT = dscr("ybT", [NSEQ, 128, 4, SEQ], BF16)
        ones_b = sb(es, "ones_b", [128, 128], BF16)
        S.op("dve", lambda e: e.memset(ones_b[:, :], 1.0), writes=["ones_b"])
        NIT = 22
        TOPK = 256.0

        def phase_D():
            with ExitStack() as st:
                Gb = sb(st, "D_Gb", [128, 8, 1024], BF16)
                Mb = sb(st, "D_Mb", [16, 8, 128], BF16)
                cv = sb(st, "D_cv", [128, 8])
                S.dma("sp", "c", cv[:, :], cvec_d[:, :], writes=["D_cv"])
                with ExitStack() as st2:
                    Gf = sb(st2, "D_Gf", [128, 8, 1024])
                    Mf = sb(st2, "D_Mf", [16, 8, 128])
                    S.dma("sp", "c", Gf[:, :, :], biasG_d[:, :, :], writes=["D_Gf"])
                    S.dma("sp", "c", Mf[:, :, :], biasM_d[:, :, :], writes=["D_Mf"])
                    for h in range(8):
                        S.op("dve", lambda e, h=h: e.tensor_scalar(out=Gb[:, h, :], in0=Gf[:, h, :], scalar1=cv[:, h:h + 1],
                                                                   scalar2=None, op0=ALU.subtract),
                             reads=["D_Gf", "D_cv"], writes=["D_Gb"])
                        S.op("dve", lambda e, h=h: e.tensor_scalar(out=Mb[:, h, :], in0=Mf[:, h, :], scalar1=cv[:16, h:h + 1],
                                                                   scalar2=None, op0=ALU.subtract),
                             reads=["D_Mf", "D_cv"], writes=["D_Mb"])
                    barrier()
                qi2 = [sb(st, "D_qi%d" % i, [128, 4, SEQ], BF16) for i in range(2)]
                ki2 = [sb(st, "D_ki%d" % i, [128, LP], BF16) for i in range(2)]
                qq = sb(st, "D_q", [128, 4, SEQ], BF16)
                kk = sb(st, "D_k", [128, LP], BF16)
                vd = sb(st, "D_vd", [128, 17, 2, 128], BF16)
                wq2 = [sb(st, "D_wq%d" % i, [128, 16, 8]) for i in range(2)]
                sc = [sb(st, "D_sc%d" % i, [128, 4, LP]) for i in range(2)]
                MA = [sb(st, "D_MA%d" % i, [128, 4, LP], BF16) for i in range(2)]
                Rb = [sb(st, "D_Rb%d" % i, [128, 512], BF16) for i in range(2)]
                dg = sb(st, "D_dg", [128, 8, 128], BF16)
                Pt = [sb(st, "D_Pt%d" % i, [128, 512], BF16) for i in range(3)]
                rd = sb(st, "D_rd", [128, 512])
                rds = rd
                Osb = sb(st, "D_Osb", [128, 512])
                yb = sb(st, "D_yb", [128, 4, 512], BF16)
                lo = [sb(st, "D_lo%d" % i, [128, 4]) for i in range(2)]
                hi = sb(st, "D_hi", [128, 4])
                W0 = sb(st, "D_W0", [128, 4])
                Wk = sb(st, "D_Wk", [128, 4])
                mid = sb(st, "D_mid", [128, 4])
                cnt = sb(st, "D_cnt", [128, 4])
                stp = sb(st, "D_stp", [128, 4])
                pq = [ps(st, "D_pq%d" % i, [128, 512]) for i in range(2)]
                psc = ps(st, "D_psc", [128, 512])
                pL = [ps(st, "D_pL%d" % i, [128, 512]) for i in range(2)]
                pOD = [ps(st, "D_pOD%d" % i, [128, 512]) for i in range(2)]
                pSh = ps(st, "D_pSh", [128, 512])
                S.op("dve", lambda e: e.memset(vd[:, :, 0, 64:128], 1.0), writes=["D_vd1"])
                S.op("dve", lambda e: e.memset(vd[:, :, 1, 0:64], 1.0), writes=["D_vd1"])
                ACT_JL = ()
                nmid = sb(st, "D_nmid", [128, 4])
                thrc = sb(st, "D_thrc", [128, 4, 4])
                for Q_ in range(4):
                    for jl_ in range(4):
                        v_ = (510.5 - (NMETA + 128 * (4 * Q_ + jl_ + 1))) if jl_ in ACT_JL else TOPK
                        S.op("dve", lambda e, Q_=Q_, jl_=jl_, v_=v_: e.memset(thrc[:, Q_, jl_:jl_ + 1], float(v_)), writes=["D_thrc"])

                def indexer(s_, Q):
                    u = Q % 2
                    qi, ki, wq = qi2[s_ % 2], ki2[s_ % 2], wq2[s_ % 2]
                    kqi, kki, kwq = ("D_qi", s_ % 2), ("D_ki", s_ % 2), ("D_wq", s_ % 2)
                    for jl in range(4):
                        j = 4 * Q + jl
                        Nj = NMETA + 128 * (j + 1)
                        for h in range(8):
                            S.op("pool", lambda e, h=h, j=j: e.tensor_scalar(out=dg[:, h, :], in0=ident_f[:, :], scalar1=wq[:, j, h:h + 1],
                                                                           scalar2=1.0, op0=ALU.mult, op1=ALU.mult),
                                 reads=["ident_f", kwq], writes=["D_dg"])
                        for c0 in range(0, Nj, 512):
                            cw = min(512, Nj - c0)

                            def qk(h):
                                hh, hp, b_ = h % 2, h // 2, h % 2
                                S.op("pe", lambda e: e.matmul(
                                    pq[b_][:, :cw], lhsT=qi[64 * hh:64 * hh + 64, hp, 128 * j:128 * j + 128],
                                    rhs=ki[64 * hh:64 * hh + 64, c0:c0 + cw], start=True, stop=True),
                                    reads=[kqi, kki], writes=[("D_pq", b_)])
                                S.op("act", lambda e: e.activation(out=Rb[b_][:, :cw], in_=pq[b_][:, :cw], func=AF.Relu),
                                     reads=[("D_pq", b_)], writes=[("D_Rb", b_)])

                            def dgm(h):
                                b_ = h % 2
                                S.op("pe", lambda e: e.matmul(psc[:, :cw], lhsT=dg[:, h, :], rhs=Rb[b_][:, :cw],
                                                              start=(h == 0), stop=(h == 7)),
                                     reads=[("D_Rb", b_), "D_dg"], writes=["D_psc"])

                            qk(0)
                            qk(1)
                            for h in range(8):
                                dgm(h)
                                if h + 2 < 8:
                                    qk(h + 2)
                            S.op("act", lambda e, jl=jl, c0=c0, cw=cw: e.activation(out=sc[u][:, jl, c0:c0 + cw], in_=psc[:, :cw], func=AF.Copy),
                                 reads=["D_psc"], writes=[("D_sc", u, jl)])
                        S.op("dve", lambda e, jl=jl, Nj=Nj: e.tensor_reduce(out=lo[u][:, jl:jl + 1], in_=sc[u][:, jl, :Nj], axis=AX.X, op=ALU.min),
                             reads=[("D_sc", u, jl)], writes=[("D_lo", u)])
                        S.op("dve", lambda e, jl=jl, Nj=Nj: e.tensor_reduce(out=hi[:, jl:jl + 1], in_=sc[u][:, jl, :Nj], axis=AX.X, op=ALU.max),
                             reads=[("D_sc", u, jl)], writes=["D_hi"])
                        S.op("dve", lambda e, jl=jl, Nj=Nj: e.memset(sc[u][0:64, jl, Nj - 64:Nj], -1e30),
                             reads=[("D_sc", u, jl), ("D_lo", u), "D_hi"], writes=[("D_sc", u, jl)])

                def bisect_steps(Q):
                    u = Q % 2
                    L_ = lo[u]
                    steps = []

                    def init():
                        S.op("dve", lambda e: e.tensor_tensor(out=W0[:, :], in0=hi[:, :], in1=L_[:, :], op=ALU.subtract),
                             reads=[("D_lo", u), "D_hi"], writes=["D_W0"])
                    steps.append(init)

                    def mk(it):
                        def f():
                            S.op("dve", lambda e: e.tensor_scalar(out=Wk[:, :], in0=W0[:, :], scalar1=2.0 ** (-(it + 1)), scalar2=None,
                                                                  op0=ALU.mult), reads=["D_W0", "D_stp"], writes=["D_Wk"])
                            S.op("dve", lambda e: e.tensor_tensor(out=mid[:, :], in0=L_[:, :], in1=Wk[:, :], op=ALU.add),
                                 reads=[("D_lo", u), "D_Wk"], writes=["D_mid"])
                            if ACT_JL:
                                S.op("dve", lambda e: e.tensor_scalar(out=nmid[:, :], in0=mid[:, :], scalar1=-1.0, scalar2=None, op0=ALU.mult),
                                     reads=["D_mid"], writes=["D_nmid"])
                            for jl in range(4):
                                Nj = NMETA + 128 * (4 * Q + jl + 1)
                                if jl in ACT_JL:
                                    S.op("act", lambda e, jl=jl, Nj=Nj: e.activation(
                                        out=junkA[:, :Nj], in_=sc[u][:, jl, :Nj], func=AF.Sign, bias=nmid[:, jl:jl + 1], scale=1.0,
                                        accum_out=cnt[:, jl:jl + 1]),
                                        reads=[("D_sc", u, jl), "D_nmid"], writes=["D_junkA", ("D_cnt", jl)])
                                else:
                                    S.op("dve", lambda e, jl=jl, Nj=Nj: e.tensor_scalar(
                                        out=MA[u][:, jl, :Nj], in0=sc[u][:, jl, :Nj], scalar1=mid[:, jl:jl + 1], scalar2=0.0,
                                        op0=ALU.is_ge, op1=ALU.add, accum_out=cnt[:, jl:jl + 1]),
                                        reads=[("D_sc", u, jl), "D_mid"], writes=[("D_MA", u), ("D_cnt", jl)])
                            S.op("dve", lambda e: e.tensor_tensor(out=stp[:, :], in0=cnt[:, :], in1=thrc[:, Q, :], op=ALU.is_ge),
                                 reads=[("D_cnt", jl) for jl in range(4)] + ["D_thrc"], writes=["D_stp"])
                            S.op("dve", lambda e: e.tensor_tensor(out=stp[:, :], in0=stp[:, :], in1=Wk[:, :], op=ALU.mult),
                                 reads=["D_stp", "D_Wk"], writes=["D_stp"])
                            S.op("dve", lambda e: e.tensor_tensor(out=L_[:, :], in0=L_[:, :], in1=stp[:, :], op=ALU.add),
                                 reads=[("D_lo", u), "D_stp"], writes=[("D_lo", u)])
                        return f
                    for it in range(NIT):
                        steps.append(mk(it))

                    def fin():
                        for jl in range(4):
                            Nj = NMETA + 128 * (4 * Q + jl + 1)
                            S.op("dve", lambda e, jl=jl, Nj=Nj: e.tensor_scalar(out=MA[u][:, jl, :Nj], in0=sc[u][:, jl, :Nj], scalar1=L_[:, jl:jl + 1],
                                                                              scalar2=-30000.0, op0=ALU.is_lt, op1=ALU.mult),
                                 reads=[("D_sc", u, jl), ("D_lo", u)], writes=[("D_MA", u)])
                    steps.append(fin)
                    return steps

                def attention(s_, Q, filler):
                    u = Q % 2
                    nblk = 4 * Q + 5
                    tiles = []
                    for h in range(8):
                        for b in range(nblk):
                            tiles.append((h, b))
                    NTL = len(tiles)

                    def geo(b):
                        w = NMETA if b == 0 else 128
                        pc0 = 0 if b == 0 else NMETA + 128 * (b - 1)
                        jl0 = max(0, b - 1 - 4 * Q)
                        return w, pc0, jl0

                    def stageA(n):
                        h, b = tiles[n]
                        hh, hp = h % 2, h // 2
                        w, pc0, jl0 = geo(b)
                        c0 = jl0 * 128
                        near = (b == 0 and Q == 0) or (b >= 1 and b - 1 >= 4 * Q - 1)
                        lb, pb_ = n % 2, n % 3
                        S.op("pe", lambda e: e.matmul(
                            pL[lb][:w, c0:512], lhsT=kk[64 * hh:64 * hh + 64, pc0:pc0 + w],
                            rhs=qq[64 * hh:64 * hh + 64, hp, 512 * Q + c0:512 * Q + 512], start=True, stop=False),
                            reads=["D_k", "D_q"], writes=[("D_pL", lb)])
                        if near:
                            if b == 0:
                                S.op("pe", lambda e: e.matmul(pL[lb][:NMETA, 0:128], lhsT=ident_b[:NMETA, :NMETA],
                                                              rhs=Mb[:NMETA, h, 0:128], start=False, stop=False),
                                     reads=["D_Mb", "ident_b"], writes=[("D_pL", lb)])
                            else:
                                z0 = 512 * Q + c0 - 128 * (b - 1) + 384
                                S.op("pe", lambda e: e.matmul(pL[lb][:, c0:512], lhsT=ident_b[:, :],
                                                              rhs=Gb[:, h, z0:z0 + 512 - c0], start=False, stop=False),
                                     reads=["D_Gb", "ident_b"], writes=[("D_pL", lb)])
                        for jl in range(jl0, 4):
                            S.op("pe", lambda e, jl=jl: e.matmul(pL[lb][:w, jl * 128:(jl + 1) * 128], lhsT=MA[u][:, jl, pc0:pc0 + w],
                                                                 rhs=ident_b[:, :], start=False, stop=(jl == 3)),
                                 reads=[("D_MA", u), "ident_b"], writes=[("D_pL", lb)])
                        S.op("act", lambda e: e.activation(out=Pt[pb_][:w, c0:512], in_=pL[lb][:w, c0:512], func=AF.Exp),
                             reads=[("D_pL", lb)], writes=[("D_Pt", pb_)])

                    def stageB(n):
                        h, b = tiles[n]
                        hh, hp = h % 2, h // 2
                        w, pc0, jl0 = geo(b)
                        c0 = jl0 * 128
                        pb_ = n % 3
                        ob = h % 2
                        S.op("pe", lambda e: e.matmul(pOD[ob][:, c0:512], lhsT=vd[:w, b, hh, :], rhs=Pt[pb_][:w, c0:512],
                                                      start=(b == 0), stop=(b == nblk - 1)),
                             reads=[("D_Pt", pb_), "D_vd", "D_vd1"], writes=[("D_pOD", ob)])
                        if b == nblk - 1:
                            orow = slice(64 * hh, 64 * hh + 64)
                            drow = slice(64 * (1 - hh), 64 * (1 - hh) + 64)
                            S.op("act", lambda e: e.activation(out=rd[drow, :], in_=pOD[ob][drow, :], func=AF.Ln),
                                 reads=[("D_pOD", ob)], writes=[("D_rr", 1 - hh)])
                            S.op("act", lambda e: e.activation(out=rd[drow, :], in_=rd[drow, :], func=AF.Exp, scale=-1.0),
                                 reads=[("D_rr", 1 - hh)], writes=[("D_rr", 1 - hh)])
                            S.op("pe", lambda e: e.matmul(pSh[orow, :], lhsT=ident_f[drow, drow], rhs=rd[drow, :], start=True, stop=True),
                                 reads=[("D_rr", 1 - hh), "ident_f"], writes=[("D_pSh", hh)])
                            S.op("act", lambda e: e.activation(out=rds[orow, :], in_=pSh[orow, :], func=AF.Copy),
                                 reads=[("D_pSh", hh)], writes=[("D_rr", hh)])
                            S.op("act", lambda e: e.activation(out=Osb[orow, :], in_=pOD[ob][orow, :], func=AF.Copy),
                                 reads=[("D_pOD", ob)], writes=[("D_Osb", hh)])
                            S.op("pool", lambda e: e.tensor_tensor(out=yb[orow, hp, :], in0=Osb[orow, :], in1=rds[orow, :], op=ALU.mult),
                                 reads=[("D_rr", hh), ("D_Osb", hh)], writes=["D_yb"])

                    stageA(0)
                    stageA(1)
                    for n in range(NTL):
                        stageB(n)
                        if n + 2 < NTL:
                            stageA(n + 2)
                    while filler:
                        filler.pop(0)()
                    S.dma("sp", "o", ybT[s_, :, :, 512 * Q:512 * Q + 512], yb[:, :, :], reads=["D_yb"], writes=[("ybT", s_, Q)])

                def load_idx(s_):
                    u = s_ % 2
                    vr = [("vwS", s_, t) for t in ALLT]
                    S.dma("sp", "x", qi2[u][:, :, :], qiT[s_, :, :, :], reads=[("qiT", s_, t) for t in range(NT)], writes=[("D_qi", u)])
                    S.dma("sp", "x", ki2[u][:, :], kiT[s_, :, :], reads=[("kiT", s_, t) for t in ALLT], writes=[("D_ki", u)])
                    S.dma("sp", "x", wq2[u][:, :, :], vwS[s_, NMETA:LP, 64:72].rearrange("(b p) c -> p b c", p=128), reads=vr, writes=[("D_wq", u)])

                def load_att(s_):
                    vr = [("vwS", s_, t) for t in ALLT]
                    S.dma("sp", "x", qq[:, :, :], qT[s_, :, :, :], reads=[("qT", s_, t) for t in range(NT)], writes=["D_q"])
                    S.dma("sp", "x", kk[:, :], kT[s_, :, :], reads=[("kT", s_, t) for t in ALLT], writes=["D_k"])
                    for half in range(2):
                        S.dma("pool", "x", vd[:, 1:17, half, 64 * half:64 * half + 64],
                              vwS[s_, NMETA:LP, 0:64].rearrange("(b p) c -> p b c", p=128), reads=vr, writes=["D_vd"])
                        S.dma("pool", "x", vd[:NMETA, 0, half, 64 * half:64 * half + 64], vwS[s_, 0:NMETA, 0:64], reads=vr, writes=["D_vd"])

                for s_ in range(NSEQ):
                    load_idx(s_)
                load_att(0)
                indexer(0, 0)
                for f_ in bisect_steps(0):
                    f_()
                NG = 4 * NSEQ
                for G in range(NG):
                    s_, Q = divmod(G, 4)
                    if G + 1 < NG:
                        s2, Q2 = divmod(G + 1, 4)
                        indexer(s2, Q2)
                        for f_ in bisect_steps(Q2):
                            f_()
                    attention(s_, Q, [])
                    if Q == 3 and s_ + 1 < NSEQ:
                        load_att(s_ + 1)

        if debug not in ("A", "B", "C"):
            phase_D()
            barrier()

        if debug == "D":
            with ExitStack() as st:
                tmp = sb(st, "dbgtmp", [128, 4, SEQ], BF16)
                tmp2 = sb(st, "dbgtmp2", [128, 4, SEQ], F32)
                S.dma("sp", "x", tmp[:, :, :], ybT[0, :, :, :], reads=[("ybT", 0, t) for t in range(NT)], writes=["dbgtmp"])
                S.op("dve", lambda e: e.tensor_copy(out=tmp2[:, :, :], in_=tmp[:, :, :]), reads=["dbgtmp"], writes=["dbgtmp2"])
                S.dma("sp", "o", outT[0, :, 0:4, 0:SEQ], tmp2[:, :, :], reads=["dbgtmp2"], writes=["out"])

        w_glu_d = din("w_glu", [128, 6, 768])
        w_a_d = din("w_a", [128, 6, D])
        w_b_d = din("w_b", [128, 4, D])
        w_o_d = din("w_o", [128, DC, D])
        w_g_d = din("w_g", [128, DC, 2 * D])
        h2T = dscr("h2T", [NSEQ, 128, DC, SEQ])

        def phase_E():
            with ExitStack() as st:
                wglu = sb(st, "E_wglu", [128, 6, 768], BF16)
                wa = sb(st, "E_wa", [128, 6, D], BF16)
                wb = sb(st, "E_wb", [128, 4, D], BF16)
                wo = sb(st, "E_wo", [128, DC, D], BF16)
                wgt = sb(st, "E_wg", [128, DC, 2 * D], BF16)
                S.dma("pool", "w", wglu[:, :, :], w_glu_d[:, :, :], writes=["E_w"], max_dma_last_dim=3072)
                for k in range(6):
                    S.dma("pool", "w", wa[:, k, :], w_a_d[:, k, :], writes=["E_w"])
                for k in range(4):
                    S.dma("pool", "w", wb[:, k, :], w_b_d[:, k, :], writes=["E_w"])
                for k in range(DC):
                    S.dma("pool", "w", wo[:, k, :], w_o_d[:, k, :], writes=["E_w"])
                    S.dma("pool", "w", wgt[:, k, :], w_g_d[:, k, :], writes=["E_w"], max_dma_last_dim=4096)
                hn2 = [sb(st, "E_hn%d" % i, [128, DC, TT], BF16) for i in range(2)]
                ya2 = [sb(st, "E_ya%d" % i, [128, 6, TT], BF16) for i in range(2)]
                yb2 = [sb(st, "E_yb%d" % i, [128, 4, TT], BF16) for i in range(2)]
                h12 = [sb(st, "E_h1%d" % i, [128, DC, TT]) for i in range(2)]
                yg = sb(st, "E_yg", [128, 6, TT], BF16)
                sgl = sb(st, "E_sgl", [128, TT])
                ga = sb(st, "E_ga", [128, TT])
                gb = sb(st, "E_gb", [128, TT])
                t1 = sb(st, "E_t1", [128, TT])
                t2 = sb(st, "E_t2", [128, TT])
                mg = sb(st, "E_mg", [128, DC, TT], BF16)
                ysb = sb(st, "E_y", [128, DC, TT])
                sq = sb(st, "E_sq", [128, 2, TT])
                accE = sb(st, "E_acc", [128, TT])
                rs = sb(st, "E_rs", [128, TT])
                pga = ps(st, "E_pga", [128, TT])
                pgb = ps(st, "E_pgb", [128, TT])
                pa = ps(st, "E_pa", [128, TT])
                pb = ps(st, "E_pb", [128, TT])
                py = [ps(st, "E_py%d" % i, [128, TT]) for i in range(2)]
                pss = ps(st, "E_pss", [128, TT])
                tl = [(s_, t_) for s_ in range(NSEQ) for t_ in range(NT)]

                def loadE(i):
                    s_, t_ = tl[i]
                    u = i % 2
                    tsl = slice(t_ * TT, (t_ + 1) * TT)
                    S.dma("sp", "x", hn2[u][:, :, :], hnT[s_, :, :, tsl], reads=[("hnT", s_, t_)], writes=[("E_hn", u)])
                    S.dma("sp", "x", ya2[u][:, :, :], yaT[s_, :, :, tsl], reads=[("yaT", s_)], writes=[("E_ya", u)])
                    S.dma("sp", "x", yb2[u][:, :, :], ybT[s_, :, :, tsl], reads=[("ybT", s_, t_)], writes=[("E_yb", u)])
                    S.dma("sp", "x", h12[u][:, :, :], h1T[s_, :, :, tsl], reads=[("h1T", s_, t_)], writes=[("E_h1", u)])

                loadE(0)
                for i, (s_, t_) in enumerate(tl):
                    if i + 1 < len(tl):
                        loadE(i + 1)
                    u = i % 2
                    hn, ya, ybt, h1t = hn2[u], ya2[u], yb2[u], h12[u]
                    khn, kya, kyb, kh1 = ("E_hn", u), ("E_ya", u), ("E_yb", u), ("E_h1", u)
                    tsl = slice(t_ * TT, (t_ + 1) * TT)
                    for oc in range(6):
                        b_ = oc % 2
                        mms(py[b_][:, :], [(wglu[:, k, oc * 128:(oc + 1) * 128], ya[:, k, :]) for k in range(6)],
                            ["E_w", kya], ("E_py", b_))
                        S.op("act", lambda e, b_=b_: e.activation(out=sgl[:, :], in_=py[b_][:, :], func=AF.Sigmoid),
                             reads=[("E_py", b_)], writes=["E_sgl"])
                        S.op("dve", lambda e, oc=oc: e.tensor_tensor(out=yg[:, oc, :], in0=sgl[:, :], in1=ya[:, oc, :], op=ALU.mult),
                             reads=["E_sgl", kya], writes=["E_yg"])
                    for dc in range(DC):
                        mms(pga[:, :], [(wgt[:, k, dc * 128:(dc + 1) * 128], hn[:, k, :]) for k in range(DC)], ["E_w", khn], "E_pga")
                        mms(pa[:, :], [(wa[:, k, dc * 128:(dc + 1) * 128], yg[:, k, :]) for k in range(6)], ["E_w", "E_yg"], "E_pa")
                        S.op("act", lambda e: e.activation(out=ga[:, :], in_=pga[:, :], func=AF.Sigmoid), reads=["E_pga"], writes=["E_ga"])
                        S.op("dve", lambda e: e.tensor_tensor(out=t1[:, :], in0=ga[:, :], in1=pa[:, :], op=ALU.mult), reads=["E_ga", "E_pa"], writes=["E_t1"])
                        mms(pgb[:, :], [(wgt[:, k, D + dc * 128:D + (dc + 1) * 128], hn[:, k, :]) for k in range(DC)], ["E_w", khn], "E_pgb")
                        mms(pb[:, :], [(wb[:, k, dc * 128:(dc + 1) * 128], ybt[:, k, :]) for k in range(4)], ["E_w", kyb], "E_pb")
                        S.op("act", lambda e: e.activation(out=gb[:, :], in_=pgb[:, :], func=AF.Sigmoid), reads=["E_pgb"], writes=["E_gb"])
                        S.op("dve", lambda e: e.tensor_tensor(out=t2[:, :], in0=gb[:, :], in1=pb[:, :], op=ALU.mult), reads=["E_gb", "E_pb"], writes=["E_t2"])
                        S.op("pool", lambda e, dc=dc: e.tensor_tensor(out=mg[:, dc, :], in0=t1[:, :], in1=t2[:, :], op=ALU.add),
                             reads=["E_t1", "E_t2"], writes=["E_mg"])
                    for c in range(DC):
                        b_ = c % 2
                        mms(py[b_][:, :], [(wo[:, k, c * 128:(c + 1) * 128], mg[:, k, :]) for k in range(DC)], ["E_w", "E_mg"], ("E_py", b_))
                        S.op("act", lambda e, c=c, b_=b_: e.activation(out=ysb[:, c, :], in_=py[b_][:, :], func=AF.Copy),
                             reads=[("E_py", b_)], writes=[("E_y", c)])
                        stats_step("E", c, py[b_][:, :], TT, sq, accE, [("E_py", b_)])
                    stats_finish("E", TT, accE, pss, rs)
                    for c in range(DC):
                        S.op("dve", lambda e, c=c: e.scalar_tensor_tensor(
                            out=ysb[:, c, :], in0=ysb[:, c, :], scalar=gains_sb[:, 24 + c:24 + c + 1], in1=rs[:, :],
                            op0=ALU.mult, op1=ALU.mult), reads=[("E_y", c), ("E", "rs"), "gains"], writes=[("E_y", c)])
                        S.op("pool", lambda e, c=c: e.tensor_tensor(out=ysb[:, c, :], in0=ysb[:, c, :], in1=h1t[:, c, :], op=ALU.add),
                             reads=[("E_y", c), kh1], writes=[("E_y", c)])
                    S.dma("sp", "o", h2T[s_, :, :, tsl], ysb[:, :, :], reads=[("E_y", c) for c in range(DC)], writes=[("h2T", s_, t_)])

        if debug not in ("A", "B", "C", "D"):
            phase_E()
            barrier()
            ff2_wg = din("ff2_wg", [128, DC, DFF])
            ff2_wu = din("ff2_wu", [128, DC, DFF])
            ff2_wd = din("ff2_wd", [128, FC, D])
            tilesF = []
            for s in range(NSEQ):
                for t in range(NFT):
                    tilesF.append((h2T[s, :, :, t * FT:(t + 1) * FT], outT[s, :, :, t * FT:(t + 1) * FT], FT,
                                   [("h2T", s, t * FT // TT)], [("out", s, t)]))
            ffn_phase("F", ff2_wg, ff2_wu, ff2_wd, 32, 40, tilesF)

        if debug == "A":
            with ExitStack() as st:
                tmp = sb(st, "dbgtmp", [128, DC, FT])
                S.dma("sp", "x", tmp[:, :, :], h1T[0, :, :, 0:FT], reads=[("h1T", 0, 0)], writes=["dbgtmp"])
                S.dma("sp", "o", outT[0, :, :, 0:FT], tmp[:, :, :], reads=["dbgtmp"], writes=["out"])
                S.dma("sp", "x", tmp[:, :, :NMETA], h1m[:, :, :], reads=["h1m", "out"], writes=["dbgtmp"])
                S.dma("sp", "o", outT[1, :, :, 0:NMETA], tmp[:, :, :NMETA], reads=["dbgtmp"], writes=["out"])

        S.drain("sp")
        print("instructions:", S.ninst)
    return nc


def _rel_bucket(rel):
    half, me = 16, 8
    base = np.where(rel > 0, half, 0)
    n = np.abs(rel)
    nf = np.maximum(n, 1).astype(np.float32)
    large = me + (np.log(nf / me) / math.log(128 / me) * (half - me)).astype(np.int32)
    large = np.minimum(large, half - 1)
    return base + np.where(n < me, n, large)


def prep_inputs(inp):
    f = lambda a: np.ascontiguousarray(np.asarray(a, dtype=np.float32))
    x = f(inp["x"])
    B = x.shape[0]
    xT = np.ascontiguousarray(x.reshape(B, SEQ, DC, 128).transpose(0, 3, 2, 1))
    metaT = np.ascontiguousarray(f(inp["meta_tokens"]).reshape(NMETA, DC, 128).transpose(2, 1, 0))
    gl = [inp[k] for k in ("ff1_norm_pre", "ff1_norm_post", "mix_norm_pre", "mix_norm_post", "ff2_norm_pre",
                           "ff2_norm_post")]
    gains = np.ascontiguousarray(np.concatenate([f(g)[0].reshape(DC, 128).T for g in gl], axis=1))

    def wk(w, kc):
        w = f(w)
        return np.ascontiguousarray(w.reshape(kc, 128, w.shape[-1]).transpose(1, 0, 2))

    shared = {
        "metaT": metaT, "gains": gains,
        "ff1_wg": wk(inp["ff1_w_gate"][0], DC), "ff1_wu": wk(inp["ff1_w_up"][0], DC),
        "ff1_wd": wk(inp["ff1_w_down"][0], FC),
    }
    win = f(inp["w_in"][0])
    upad = np.zeros((D, 6, 128), np.float32)
    for c6 in range(6):
        w_ = min(96, 512 - 96 * c6)
        upad[:, c6, :w_] = win[:, 96 * c6:96 * c6 + w_]
    winA = np.concatenate([upad.reshape(D, 768), win[:, 512:1024], win[:, 1096:1608], win[:, 1024:1088], win[:, 1024:1088],
                           win[:, 1608:1672], win[:, 1608:1672]], axis=1)
    winB = np.concatenate([win[:, 1672:1736], win[:, 1088:1096]], axis=1)
    shared["w_inA"] = wk(winA, DC)
    shared["w_inB"] = wk(winB, DC)
    lre, lim, ldt = f(inp["ssm_lambda_re"][0]), f(inp["ssm_lambda_im"][0]), f(inp["ssm_log_dt"][0])
    bre, bim = f(inp["ssm_b_re"][0]), f(inp["ssm_b_im"][0])
    cre, cim = f(inp["ssm_c_re"][0]), f(inp["ssm_c_im"][0])
    r = np.arange(128)
    sidx = np.arange(128)
    cc = np.arange(6)
    i_rc = 3 * cc[None, :] + (r[:, None] // 32)
    val_rc = (r[:, None] < 96) & (i_rc < 16)
    i_rc = np.where(val_rc, i_rc, 0)
    g_rcs = 2 * i_rc[:, :, None] + (sidx[None, None, :] // 64)
    p_s = sidx % 64
    pcm = np.stack([lre[g_rcs, p_s[None, None, :]], lim[g_rcs, p_s[None, None, :]], ldt[g_rcs]], axis=2)
    glr = (r % 32) // 16
    m_r = r % 16
    msk = (glr[:, None, None] == (sidx[None, None, :] // 64)) & val_rc[:, :, None]
    bcm = np.stack([np.where(msk, bre[g_rcs, p_s[None, None, :], m_r[:, None, None]], 0.0),
                    np.where(msk, bim[g_rcs, p_s[None, None, :], m_r[:, None, None]], 0.0)], axis=2)
    ii = np.arange(16)
    g_si = 2 * ii[None, :] + (sidx[:, None] // 64)
    psm = np.stack([lre[g_si, p_s[:, None]], lim[g_si, p_s[:, None]], ldt[g_si]], axis=1)
    q = np.arange(32)
    mq = q % 16
    mskq = ((q[None, None, :] // 16) == (sidx[:, None, None] // 64))
    bsm = np.stack([np.where(mskq, bre[g_si[:, :, None], p_s[:, None, None], mq[None, None, :]], 0.0),
                    np.where(mskq, bim[g_si[:, :, None], p_s[:, None, None], mq[None, None, :]], 0.0)], axis=2)
    csm = np.stack([np.where(mskq, cre[g_si[:, :, None], mq[None, None, :], p_s[:, None, None]], 0.0),
                    np.where(mskq, cim[g_si[:, :, None], mq[None, None, :], p_s[:, None, None]], 0.0)], axis=2)
    dflat = f(inp["ssm_d"][0]).reshape(512)
    ch_rc = 96 * cc[None, :] + r[:, None]
    vch = (r[:, None] < 96) & (ch_rc < 512)
    dsk = np.where(vch, dflat[np.where(vch, ch_rc, 0)], 0.0)
    shared["s5_pcm"] = f(pcm)
    shared["s5_bcm"] = f(bcm)
    shared["s5_psm"] = f(psm)
    shared["s5_bsm"] = f(bsm)
    shared["s5_csm"] = f(csm)
    shared["s5_d"] = f(dsk)
    shared["ident"] = np.eye(128, dtype=np.float32)
    rb = f(inp["rel_bias"])
    sl = np.arange(128)[:, None]
    zi = np.arange(1024)[None, :]
    shared["biasG"] = f(rb[_rel_bucket(sl - (zi - 384))].transpose(0, 2, 1))
    mm_ = np.arange(16)[:, None]
    tq = np.arange(128)[None, :]
    shared["biasM"] = f(rb[_rel_bucket(mm_ - 16 - tq)].transpose(0, 2, 1))
    shared["cvec"] = f(np.broadcast_to(rb[15][None, :], (128, 8)))
    def pad6rows(w):
        o = np.zeros((128, 6, w.shape[1]), np.float32)
        for c6 in range(6):
            w_ = min(96, 512 - 96 * c6)
            o[:w_, c6, :] = w[96 * c6:96 * c6 + w_]
        return o
    wg_ = f(inp["ssm_w_glu"][0])
    wgp = np.zeros((512, 6, 128), np.float32)
    for c6 in range(6):
        w_ = min(96, 512 - 96 * c6)
        wgp[:, c6, :w_] = wg_[:, 96 * c6:96 * c6 + w_]
    shared["w_glu"] = pad6rows(wgp.reshape(512, 768))
    shared["w_a"] = pad6rows(f(inp["w_branch_a"][0]))
    shared["w_b"] = wk(inp["w_branch_b"][0], 4)
    shared["w_o"] = wk(inp["w_out"][0], DC)
    shared["w_g"] = wk(win[:, 1736:3784], DC)
    shared["ff2_wg"] = wk(inp["ff2_w_gate"][0], DC)
    shared["ff2_wu"] = wk(inp["ff2_w_up"][0], DC)
    shared["ff2_wd"] = wk(inp["ff2_w_down"][0], FC)
    maps = []
    for c in range(NCORES):
        m = dict(shared)
        m["xT"] = xT[c * NSEQ:(c + 1) * NSEQ]
        maps.append(m)
    return maps


def kernel(**inputs):
    maps = prep_inputs(inputs)
    nc = build()
    res = run_bass_kernel_spmd(nc, maps, core_ids=list(range(NCORES)))
    outs = [r["outT"] for r in res.results]
    o = np.concatenate(outs, axis=0)
    out = o.transpose(0, 3, 2, 1).reshape(o.shape[0], SEQ, D)
    return np.ascontiguousarray(out.astype(np.float32))
```
